# Optimizing a Trainium2 kernel written in Bass

```python
import math
import jax
import jax.numpy as jnp
from jax import lax
import numpy as np

D_MODEL = 1024
BATCH = 16
SEQ = 4096
DEPTH = 2

GRID_W = 64
CTX_LEN = 256
HEAD_DIM = 64
ROPE_BASE = 10000.0
RMS_EPS = 1e-6
QBLOCK = 128
NEG_INF = -1e30
N_MOD = 6

NA_HEADS = 8
NA_KR = 8
NA_KC = 16
SWA_HEADS = 8
SWA_KV_HEADS = 2
SWA_GROUP = SWA_HEADS // SWA_KV_HEADS
SWA_WINDOW = 128
MLA_HEADS = 8
MLA_Q_RANK = 256
MLA_KV_RANK = 128
MLA_NOPE = 64
MLA_ROPE = 32
MLA_V = 64
DIFF_HEADS = 4
DIFF_QK = 64
DIFF_V = 2 * DIFF_QK
PEER_HEADS = 8
PEER_NKEYS = 128
PEER_EXPERTS = PEER_NKEYS * PEER_NKEYS
PEER_DQ = 256
PEER_TOPK = 16
PEER_TBLOCK = 128

AB_SPLITS = (NA_HEADS * HEAD_DIM,) * 3 + (SWA_HEADS * HEAD_DIM, SWA_KV_HEADS * HEAD_DIM, SWA_KV_HEADS * HEAD_DIM)
AB_IN = sum(AB_SPLITS)
CD_SPLITS = (MLA_Q_RANK, MLA_KV_RANK, MLA_ROPE, DIFF_HEADS * 2 * DIFF_QK, DIFF_HEADS * 2 * DIFF_QK, DIFF_HEADS * DIFF_V)
CD_IN = sum(CD_SPLITS)
MIX_WIDTH = NA_HEADS * HEAD_DIM + SWA_HEADS * HEAD_DIM

kernel_name = 'hybrid_prefix_na_swa_mla_diff_peer'


def rmsnorm(x, g):
    xf = x.astype(jnp.float32)
    y = xf * lax.rsqrt(jnp.mean(jnp.square(xf), axis=-1, keepdims=True) + RMS_EPS)
    return (y * g.astype(jnp.float32)).astype(x.dtype)


def split_cols(z, sizes):
    out, o = [], 0
    for s in sizes:
        out.append(z[..., o:o + s])
        o += s
    return out


def split_heads(t, n):
    return t.reshape(t.shape[:-1] + (n, t.shape[-1] // n))


def rope1d(x, pos):
    half = x.shape[-1] // 2
    freq = ROPE_BASE ** (-jnp.arange(half, dtype=jnp.float32) / half)
    ang = pos.astype(jnp.float32)[:, None] * freq[None, :]
    bshape = (pos.shape[0],) + (1,) * (x.ndim - 3) + (half,)
    cos = jnp.cos(ang).reshape(bshape).astype(x.dtype)
    sin = jnp.sin(ang).reshape(bshape).astype(x.dtype)
    x1, x2 = x[..., :half], x[..., half:]
    return jnp.concatenate([x1 * cos - x2 * sin, x1 * sin + x2 * cos], axis=-1)


def axial_rope(x, row, col):
    h = x.shape[-1] // 2
    return jnp.concatenate([rope1d(x[..., :h], row), rope1d(x[..., h:], col)], axis=-1)


def map_blocks(fn, q):
    B, T = q.shape[:2]
    nb = T // QBLOCK
    qb = jnp.moveaxis(q.reshape((B, nb, QBLOCK) + q.shape[2:]), 1, 0)
    out = lax.map(lambda a: fn(a[0], a[1]), (jnp.arange(nb, dtype=jnp.int32), qb))
    out = jnp.moveaxis(out, 0, 1)
    return out.reshape((B, T) + out.shape[3:])


def global_attention(q, k, v, scale, sink=None):
    if sink is not None:
        sink_l = sink.astype(jnp.float32).reshape(1, k.shape[2], q.shape[3], 1, 1)

    def blk(i, qb):
        s = jnp.einsum('bqkgd,bskd->bkgqs', qb, k).astype(jnp.float32) * scale
        if sink is not None:
            s = jnp.concatenate([s, jnp.broadcast_to(sink_l, s.shape[:-1] + (1,))], axis=-1)
        p = jax.nn.softmax(s, axis=-1)
        if sink is not None:
            p = p[..., :-1]
        return jnp.einsum('bkgqs,bskd->bqkgd', p.astype(v.dtype), v)

    return map_blocks(blk, q)


def neighbourhood_attention(q, k, v, kc, vc, rpb):
    B, S, H, d = q.shape
    rows = S // GRID_W
    kr = min(NA_KR, rows)
    qg = q.reshape(B, rows, GRID_W, H, d)
    kg = k.reshape(B, rows, GRID_W, H, d)
    vg = v.reshape(B, rows, GRID_W, H, d)
    cols = jnp.arange(GRID_W, dtype=jnp.int32)
    col_start = jnp.clip(cols - NA_KC // 2, 0, GRID_W - NA_KC)
    col_valid = (cols[None, :] >= col_start[:, None]) & (cols[None, :] < col_start[:, None] + NA_KC)
    col_idx = jnp.clip(cols[None, :] - cols[:, None] + NA_KC - 1, 0, 2 * NA_KC - 2)
    rpb_cols = rpb[:, :, col_idx]
    scale = d ** -0.5
    nlat = kr * GRID_W

    def row_fn(r):
        r0 = jnp.clip(r - kr // 2, 0, rows - kr)
        qr = lax.dynamic_index_in_dim(qg, r, axis=1, keepdims=False)
        kw = lax.dynamic_slice_in_dim(kg, r0, kr, axis=1)
        vw = lax.dynamic_slice_in_dim(vg, r0, kr, axis=1)
        bias = jnp.take(rpb_cols, r0 + jnp.arange(kr, dtype=jnp.int32) - r + NA_KR - 1, axis=1)
        bias = jnp.transpose(bias, (0, 2, 1, 3)).astype(jnp.float32)
        s_lat = jnp.einsum('bqhd,brkhd->bhqrk', qr, kw).astype(jnp.float32) * scale + bias
        s_lat = jnp.where(col_valid[:, None, :], s_lat, NEG_INF).reshape(B, H, GRID_W, nlat)
        s_ctx = jnp.einsum('bqhd,bchd->bhqc', qr, kc).astype(jnp.float32) * scale
        p = jax.nn.softmax(jnp.concatenate([s_lat, s_ctx], axis=-1), axis=-1).astype(v.dtype)
        p_lat = p[..., :nlat].reshape(B, H, GRID_W, kr, GRID_W)
        return (jnp.einsum('bhqrk,brkhd->bqhd', p_lat, vw)
                + jnp.einsum('bhqc,bchd->bqhd', p[..., nlat:], vc))

    out = lax.map(row_fn, jnp.arange(rows, dtype=jnp.int32))
    return jnp.moveaxis(out, 0, 1).reshape(B, S, H, d)


def window_attention(q, k, v, kc, vc, sink):
    S = q.shape[1]
    kb = QBLOCK + 2 * SWA_WINDOW
    pad = ((0, 0), (SWA_WINDOW, SWA_WINDOW), (0, 0), (0, 0))
    kp = jnp.pad(k, pad)
    vp = jnp.pad(v, pad)
    scale = q.shape[-1] ** -0.5
    nc = kc.shape[1]
    sink_l = sink.astype(jnp.float32).reshape(1, SWA_KV_HEADS, SWA_GROUP, 1, 1)

    def blk(i, qb):
        start = i * QBLOCK
        kw = lax.dynamic_slice_in_dim(kp, start, kb, axis=1)
        vw = lax.dynamic_slice_in_dim(vp, start, kb, axis=1)
        qpos = start + jnp.arange(QBLOCK, dtype=jnp.int32)
        kpos = start - SWA_WINDOW + jnp.arange(kb, dtype=jnp.int32)
        valid = ((kpos[None, :] >= 0) & (kpos[None, :] < S)
                 & (jnp.abs(kpos[None, :] - qpos[:, None]) <= SWA_WINDOW))
        s_lat = jnp.einsum('bqkgd,bskd->bkgqs', qb, kw).astype(jnp.float32) * scale
        s_lat = jnp.where(valid, s_lat, NEG_INF)
        s_ctx = jnp.einsum('bqkgd,bckd->bkgqc', qb, kc).astype(jnp.float32) * scale
        s_sink = jnp.broadcast_to(sink_l, s_ctx.shape[:-1] + (1,))
        p = jax.nn.softmax(jnp.concatenate([s_lat, s_ctx, s_sink], axis=-1), axis=-1).astype(v.dtype)
        return (jnp.einsum('bkgqs,bskd->bqkgd', p[..., :kb], vw)
                + jnp.einsum('bkgqc,bckd->bqkgd', p[..., kb:kb + nc], vc))

    return map_blocks(blk, q)


def mixer_ab(h, hc, w_in, rpb, sink, w_out, pos, need_ctx):
    B, S, _ = h.shape

    def project(z, rope_pos):
        T = z.shape[1]
        aq, ak, av, bq, bk, bv = split_cols(z, AB_SPLITS)
        aq, ak, av = split_heads(aq, NA_HEADS), split_heads(ak, NA_HEADS), split_heads(av, NA_HEADS)
        bq, bk, bv = split_heads(bq, SWA_HEADS), split_heads(bk, SWA_KV_HEADS), split_heads(bv, SWA_KV_HEADS)
        if rope_pos is not None:
            bq = axial_rope(bq, *rope_pos)
            bk = axial_rope(bk, *rope_pos)
        bq = bq.reshape(B, T, SWA_KV_HEADS, SWA_GROUP, HEAD_DIM)
        return aq, ak, av, bq, bk, bv

    aq, ak, av, bq, bk, bv = project(h @ w_in, pos)
    caq, cak, cav, cbq, cbk, cbv = project(hc @ w_in, None)
    ya = neighbourhood_attention(aq, ak, av, cak, cav, rpb)
    yb = window_attention(bq, bk, bv, cbk, cbv, sink)
    y = jnp.concatenate([ya.reshape(B, S, -1), yb.reshape(B, S, -1)], axis=-1) @ w_out
    yc = None
    if need_ctx:
        C = hc.shape[1]
        yca = global_attention(caq[:, :, :, None], cak, cav, HEAD_DIM ** -0.5)
        ycb = global_attention(cbq, cbk, cbv, HEAD_DIM ** -0.5, sink)
        yc = jnp.concatenate([yca.reshape(B, C, -1), ycb.reshape(B, C, -1)], axis=-1) @ w_out
    return y, yc


def mixer_cd(h, hc, w_in, q_norm_g, w_uq, kv_norm_g, w_ukv, lam_vecs, subln_g, w_out, pos, lam_init, need_ctx):
    B = h.shape[0]
    lv = lam_vecs.astype(jnp.float32)
    lam = jnp.exp(jnp.sum(lv[0] * lv[1])) - jnp.exp(jnp.sum(lv[2] * lv[3])) + lam_init

    def project(z, rope_pos):
        cq, ckv, kr, dq, dk, dv = split_cols(z, CD_SPLITS)
        q = split_heads(rmsnorm(cq, q_norm_g) @ w_uq, MLA_HEADS)
        kv = split_heads(rmsnorm(ckv, kv_norm_g) @ w_ukv, MLA_HEADS)
        q_nope, q_rope = q[..., :MLA_NOPE], q[..., MLA_NOPE:]
        k_nope, v = kv[..., :MLA_NOPE], kv[..., MLA_NOPE:]
        k_rope = kr[:, :, None, :]
        dq = dq.reshape(dq.shape[:2] + (DIFF_HEADS, 2, DIFF_QK))
        dk = dk.reshape(dk.shape[:2] + (DIFF_HEADS, 2, DIFF_QK))
        dv = split_heads(dv, DIFF_HEADS)
        if rope_pos is not None:
            q_rope = axial_rope(q_rope, *rope_pos)
            k_rope = axial_rope(k_rope, *rope_pos)
            dq = axial_rope(dq, *rope_pos)
            dk = axial_rope(dk, *rope_pos)
        q_mla = jnp.concatenate([q_nope, q_rope], axis=-1)
        k_mla = jnp.concatenate([k_nope, jnp.broadcast_to(k_rope, k_nope.shape[:-1] + (MLA_ROPE,))], axis=-1)
        return q_mla, k_mla, v, dq, dk, dv

    def attend(q_mla, dq, k_mla, v_mla, dk, dv):
        T = q_mla.shape[1]
        y_c = global_attention(q_mla[:, :, :, None], k_mla, v_mla, (MLA_NOPE + MLA_ROPE) ** -0.5)[:, :, :, 0]
        o1 = global_attention(dq[:, :, :, 0:1], dk[:, :, :, 0], dv, DIFF_QK ** -0.5)[:, :, :, 0]
        o2 = global_attention(dq[:, :, :, 1:2], dk[:, :, :, 1], dv, DIFF_QK ** -0.5)[:, :, :, 0]
        o = rmsnorm(o1 - lam.astype(o1.dtype) * o2, subln_g) * (1.0 - lam_init)
        return jnp.concatenate([y_c.reshape(B, T, -1), o.reshape(B, T, -1)], axis=-1) @ w_out

    q_l, k_l, v_l, dq_l, dk_l, dv_l = project(h @ w_in, pos)
    q_c, k_c, v_c, dq_c, dk_c, dv_c = project(hc @ w_in, None)
    y = attend(q_l, dq_l,
               jnp.concatenate([k_l, k_c], axis=1), jnp.concatenate([v_l, v_c], axis=1),
               jnp.concatenate([dk_l, dk_c], axis=1), jnp.concatenate([dv_l, dv_c], axis=1))
    yc = attend(q_c, dq_c, k_c, v_c, dk_c, dv_c) if need_ctx else None
    return y, yc


def peer_ffn(hx, wq, sub_keys, u, v):
    n, d = hx.shape
    hb = hx.reshape(n // PEER_TBLOCK, PEER_TBLOCK, d)

    def blk(xb):
        q = (xb @ wq).reshape(PEER_TBLOCK, PEER_HEADS, 2, PEER_DQ // 2)
        s = jnp.einsum('thpd,pnd->thpn', q, sub_keys).astype(jnp.float32)
        sv, si = lax.top_k(s, PEER_TOPK)
        cand = (sv[:, :, 0, :, None] + sv[:, :, 1, None, :]).reshape(PEER_TBLOCK, PEER_HEADS, PEER_TOPK * PEER_TOPK)
        cidx = (si[:, :, 0, :, None] * PEER_NKEYS + si[:, :, 1, None, :]).reshape(PEER_TBLOCK, PEER_HEADS, PEER_TOPK * PEER_TOPK)
        fv, fp = lax.top_k(cand, PEER_TOPK)
        eidx = jnp.take_along_axis(cidx, fp, axis=-1)
        g = jax.nn.softmax(fv, axis=-1)
        ue = jnp.take(u, eidx, axis=0)
        ve = jnp.take(v, eidx, axis=0)
        a = jnp.einsum('thkd,td->thk', ue, xb).astype(jnp.float32)
        wgt = (jax.nn.gelu(a, approximate=False) * g).astype(xb.dtype)
        return jnp.einsum('thk,thkd->td', wgt, ve)

    return lax.map(blk, hb).reshape(n, d)


def diff_lambda_init(layer_idx):
    return 0.8 - 0.6 * math.exp(-0.3 * layer_idx)


def setup_inputs(seed: int = 0) -> dict:
    key = jax.random.key(seed)
    ks = iter(jax.random.split(key, 32))
    n_even = (DEPTH + 1) // 2
    n_odd = DEPTH // 2

    def normal(shape, scale):
        return jax.random.normal(next(ks), shape, jnp.float32) * scale

    def gain(shape):
        return 1.0 + 0.02 * jax.random.normal(next(ks), shape, jnp.float32)

    D = D_MODEL
    return {
        'x': normal((BATCH, SEQ, D), 1.0),
        'c': normal((BATCH, D), 1.0),
        'ctx': normal((BATCH, CTX_LEN, D), 1.0),
        'c_ctx': normal((D,), 1.0),
        'ada_w': normal((DEPTH, D, N_MOD * D), 0.5 * D ** -0.5),
        'ada_b': normal((DEPTH, N_MOD * D), 0.02),
        'norm1_g': gain((DEPTH, D)),
        'norm2_g': gain((DEPTH, D)),
        'w_out': normal((DEPTH, MIX_WIDTH, D), MIX_WIDTH ** -0.5),
        'peer_wq': normal((DEPTH, D, PEER_HEADS * PEER_DQ), D ** -0.5),
        'peer_keys': normal((DEPTH, 2, PEER_NKEYS, PEER_DQ // 2), (PEER_DQ // 2) ** -0.5),
        'peer_u': normal((DEPTH, PEER_EXPERTS, D), D ** -0.5),
        'peer_v': normal((DEPTH, PEER_EXPERTS, D), PEER_HEADS ** -0.5),
        'ab_w_in': normal((n_even, D, AB_IN), D ** -0.5),
        'na_rpb': normal((n_even, NA_HEADS, 2 * NA_KR - 1, 2 * NA_KC - 1), 0.2),
        'swa_sink': normal((n_even, SWA_HEADS), 0.5),
        'cd_w_in': normal((n_odd, D, CD_IN), D ** -0.5),
        'mla_q_norm_g': gain((n_odd, MLA_Q_RANK)),
        'mla_w_uq': normal((n_odd, MLA_Q_RANK, MLA_HEADS * (MLA_NOPE + MLA_ROPE)), MLA_Q_RANK ** -0.5),
        'mla_kv_norm_g': gain((n_odd, MLA_KV_RANK)),
        'mla_w_ukv': normal((n_odd, MLA_KV_RANK, MLA_HEADS * (MLA_NOPE + MLA_V)), MLA_KV_RANK ** -0.5),
        'diff_lambda': normal((n_odd, 4, DIFF_QK), 0.1),
        'diff_subln_g': gain((n_odd, DIFF_V)),
        'final_norm_g': gain((D,)),
    }


def reference(x, c, ctx, c_ctx, ada_w, ada_b, norm1_g, norm2_g, w_out, peer_wq, peer_keys, peer_u, peer_v,
              ab_w_in, na_rpb, swa_sink, cd_w_in, mla_q_norm_g, mla_w_uq, mla_kv_norm_g, mla_w_ukv,
              diff_lambda, diff_subln_g, final_norm_g):
    B, S, D = x.shape
    C = ctx.shape[1]
    t = jnp.arange(S, dtype=jnp.int32)
    pos = (t // GRID_W, t % GRID_W)
    for l in range(DEPTH):
        last = l == DEPTH - 1
        m = jax.nn.silu(c) @ ada_w[l] + ada_b[l]
        mc = jax.nn.silu(c_ctx) @ ada_w[l] + ada_b[l]
        sh1, sc1, g1, sh2, sc2, g2 = split_cols(m[:, None, :], (D,) * N_MOD)
        csh1, csc1, cg1, csh2, csc2, cg2 = split_cols(mc, (D,) * N_MOD)
        h = rmsnorm(x, norm1_g[l]) * (1 + sc1) + sh1
        hc = rmsnorm(ctx, norm1_g[l]) * (1 + csc1) + csh1
        j = l // 2
        if l % 2 == 0:
            y, yc = mixer_ab(h, hc, ab_w_in[j], na_rpb[j], swa_sink[j], w_out[l], pos, not last)
        else:
            y, yc = mixer_cd(h, hc, cd_w_in[j], mla_q_norm_g[j], mla_w_uq[j], mla_kv_norm_g[j], mla_w_ukv[j],
                             diff_lambda[j], diff_subln_g[j], w_out[l], pos, diff_lambda_init(l), not last)
        x = x + g1 * y
        h2 = rmsnorm(x, norm2_g[l]) * (1 + sc2) + sh2
        if last:
            f = peer_ffn(h2.reshape(B * S, D), peer_wq[l], peer_keys[l], peer_u[l], peer_v[l])
            x = x + g2 * f.reshape(B, S, D)
        else:
            ctx = ctx + cg1 * yc
            h2c = rmsnorm(ctx, norm2_g[l]) * (1 + csc2) + csh2
            f = peer_ffn(jnp.concatenate([h2.reshape(B * S, D), h2c.reshape(B * C, D)], axis=0),
                         peer_wq[l], peer_keys[l], peer_u[l], peer_v[l])
            x = x + g2 * f[:B * S].reshape(B, S, D)
            ctx = ctx + cg2 * f[B * S:].reshape(B, C, D)
    return rmsnorm(x, final_norm_g)
```

```python
import math
import numpy as np
from contextlib import ExitStack
import concourse.bass as bass
import concourse.mybir as mybir
from concourse.bass_utils import run_bass_kernel_spmd

F32 = mybir.dt.float32
BF16 = mybir.dt.bfloat16
U32 = mybir.dt.uint32
AF = mybir.ActivationFunctionType
ALU = mybir.AluOpType
AX = mybir.AxisListType

NB = 2
D = 1024
SL = 4096
CT = 256
SA = 4352
NT = 34
EPS = 1e-6
NEG = -30000.0
SEM_ROT = 30000
N_CORES = 8


class Sched:
    ENGS = ('pe', 'act', 'dve', 'pool', 'sp')

    def __init__(self, nc, stack, n_lanes=28, same_engine_sync=True):
        self.nc = nc
        self.stack = stack
        self.prog = {e: [] for e in self.ENGS}
        self.cnt = {e: 0 for e in self.ENGS}
        self.esems = {e: [] for e in self.ENGS}
        self.seen = {e: {} for e in self.ENGS}
        self.res = {}
        self.lanes = []
        for i in range(n_lanes):
            s = stack.enter_context(nc.semaphore(f"lane{i}"))
            self.lanes.append([s, 0])
        self.lane_rr = 0
        self.same_engine_sync = same_engine_sync

    def _esem(self, e, n):
        k = (n - 1) // SEM_ROT
        while len(self.esems[e]) <= k:
            s = self.stack.enter_context(self.nc.semaphore(f"es_{e}_{len(self.esems[e])}"))
            self.esems[e].append(s)
        return self.esems[e][k], n - k * SEM_ROT

    def _deps(self, reads, writes):
        deps = []
        for r in reads:
            st = self.res.get(r)
            if st is not None and st['w'] is not None:
                deps.append(st['w'])
        for w in writes:
            st = self.res.get(w)
            if st is not None:
                if st['w'] is not None:
                    deps.append(st['w'])
                deps.extend(st['r'].values())
        return deps

    def _commit(self, ev, reads, writes):
        for r in reads:
            st = self.res.setdefault(r, {'w': None, 'r': {}})
            k = id(ev[1])
            if k not in st['r'] or st['r'][k][2] < ev[2]:
                st['r'][k] = ev
        for w in writes:
            self.res[w] = {'w': ev, 'r': {}}

    def _add_waits(self, e, deps):
        best = {}
        for (eng_src, sem, val) in deps:
            if eng_src == e and (e == 'pe' or not self.same_engine_sync):
                continue
            key = id(sem)
            if key not in best or best[key][1] < val:
                best[key] = (sem, val)
        for key, (sem, val) in best.items():
            if self.seen[e].get(key, 0) >= val:
                continue
            self.seen[e][key] = val
            self.prog[e].append(('wait', sem, val))

    def op(self, e, fn, reads=(), writes=()):
        deps = self._deps(reads, writes)
        self._add_waits(e, deps)
        self.cnt[e] += 1
        sem, val = self._esem(e, self.cnt[e])
        self.prog[e].append(('op', fn, sem, 1))
        ev = (e, sem, val)
        self._commit(ev, reads, writes)
        return ev

    def dma(self, q, out, in_, reads=(), writes=(), **kw):
        deps = self._deps(reads, writes)
        lane = self.lanes[self.lane_rr]
        self.lane_rr = (self.lane_rr + 1) % len(self.lanes)
        if lane[1] > 0:
            deps.append(('dma', lane[0], 16 * lane[1]))
        self._add_waits(q, deps)
        lane[1] += 1
        sem = lane[0]

        def fn(eng, out=out, in_=in_, kw=kw):
            return eng.dma_start(out=out, in_=in_, **kw)
        self.prog[q].append(('op', fn, sem, 16))
        ev = ('dma', sem, 16 * lane[1])
        self._commit(ev, reads, writes)
        return ev

    def barrier(self):
        evs = []
        for e in self.ENGS:
            if self.cnt[e] > 0:
                sem, val = self._esem(e, self.cnt[e])
                evs.append((e + '_b', sem, val))
        for lane in self.lanes:
            if lane[1] > 0:
                evs.append(('dma', lane[0], 16 * lane[1]))
        for e in self.ENGS:
            self._add_waits(e, evs)
        self.res = {}

    def emit(self):
        nc = self.nc
        engobj = {'pe': 'tensor', 'act': 'scalar', 'dve': 'vector', 'pool': 'gpsimd', 'sp': 'sync'}
        with nc.Block() as block:
            for e in self.ENGS:
                items = self.prog[e]
                if not items:
                    continue

                def body(eng, items=items):
                    for it in items:
                        if it[0] == 'wait':
                            eng.wait_ge(it[1], it[2])
                        else:
                            ins = it[1](eng)
                            ins.then_inc(it[2], it[3])
                getattr(block, engobj[e])(body)


class Arena:
    def __init__(self, nc, limit=228000):
        self.nc = nc
        self.off = 17408
        self.n = 0
        self.limit = limit

    def alloc(self, shape, dtype, name='t'):
        isz = 4 if dtype in (F32, U32) else 2
        nbytes = int(np.prod(shape[1:])) * isz
        nbytes = (nbytes + 63) // 64 * 64
        self.n += 1
        t = self.nc.alloc_sbuf_tensor_at(f"{name}_{self.n}", list(shape), dtype, offset=self.off)
        self.off += nbytes
        assert self.off <= self.limit, (name, self.off)
        return t

    def mark(self):
        return self.off

    def release(self, m):
        self.off = m


class Builder:
    def __init__(self, stop_after=None, skip_l0=False, sub=None):
        self.stop_after = stop_after
        self.skip_l0 = skip_l0
        self.sub = sub
        self.nc = nc = bass.Bass("TRN2", target_bir_lowering=False)
        self.stack = ExitStack()
        self.S = Sched(nc, self.stack)
        self.A = Arena(nc)
        di = lambda n, sh, dt=F32: nc.dram_tensor(n, list(sh), dt, kind="ExternalInput").ap()
        self.x = di("x", [NB, SL, D])
        self.ctx = di("ctx", [NB, CT, D])
        self.cc = di("cc", [3, D])
        self.ada_w = di("ada_w", [2, D, 6 * D])
        self.ada_b = di("ada_b", [2, 6 * D])
        self.norm1_g = di("norm1_g", [2, D])
        self.norm2_g = di("norm2_g", [2, D])
        self.w_out = di("w_out", [2, D, D])
        self.peer_wq = di("peer_wq", [2, D, 2048])
        self.peer_keys = di("peer_keys", [2, 2, 128, 128])
        self.peer_u = di("peer_u", [2, 16384, D])
        self.peer_v = di("peer_v", [2, 16384, D])
        self.ab_w = di("ab_w", [D, 2304])
        self.rpbT = di("rpbT", [128, 7680])
        self.maskI = di("maskI", [128, 7680])
        self.maskE = di("maskE", [128, 7680])
        self.swaL = di("swaL", [128, 128])
        self.swaU = di("swaU", [128, 128])
        self.sink = di("sink", [1, 8])
        self.cd_w = di("cd_w", [D, 1952])
        self.qn_g = di("qn_g", [256, 1])
        self.w_uq = di("w_uq", [256, 768])
        self.kvn_g = di("kvn_g", [128, 1])
        self.w_ukv = di("w_ukv", [128, 1024])
        self.dlam = di("dlam", [1, 256])
        self.subln = di("subln", [1, 128])
        self.fin_g = di("fin_g", [1, D])
        self.ropeC = di("ropeC", [128, SA])
        self.ropeS = di("ropeS", [128, SA])
        self.ropeCm = di("ropeCm", [96, SA])
        self.ropeSm = di("ropeSm", [96, SA])
        self.iota_in = di("iota_in", [128, 128])
        self.out = nc.dram_tensor("out", [NB, SL, D], F32, kind="ExternalOutput").ap()
        if stop_after is not None:
            self.dbg = nc.dram_tensor("dbg", [NB, SA, D], F32, kind="ExternalOutput").ap()
        ds = lambda n, sh, dt: nc.dram_tensor(n, list(sh), dt).ap()
        self.xs = ds("xs", [NB, SA, D], F32)
        self.mod = ds("mod", [2, 3, 6 * D], F32)
        self.QT = ds("QT", [NB, 12, 128, SA], BF16)
        self.KT = ds("KT", [NB, 12, 128, SA], BF16)
        self.V = ds("V", [NB, SA, 1040], BF16)
        self.Y = ds("Y", [NB, SA, D], BF16)
        self.UTs = ds("UTs", [128, 128, D], BF16)
        self.Vs = ds("Vs", [128, 128, D], BF16)
        self.WQs = ds("WQs", [4, 128, 8 * 512], BF16)
        self.PS = nc.alloc_psum_tensor("psall", [128, 8, 512], F32)

    def act(self, out, in_, func, r, w, **kw):
        self.S.op('act', lambda e: e.activation(out=out, in_=in_, func=func, **kw), r, w)

    def mm(self, out, lhsT, rhs, start, stop, r, w):
        self.S.op('pe', lambda e: e.matmul(out, lhsT=lhsT, rhs=rhs, start=start, stop=stop), r, w)

    def tr(self, out, in_, r, w, f32=False):
        idn = self.identf if f32 else self.ident
        self.S.op('pe', lambda e: e.transpose(out=out, in_=in_, identity=idn[:]), r, w)

    def tt(self, eng, out, in0, in1, op, r, w):
        self.S.op(eng, lambda e: e.tensor_tensor(out=out, in0=in0, in1=in1, op=op), r, w)

    def ts(self, eng, out, in0, s1, s2, op0, op1, r, w):
        if op1 is None:
            self.S.op(eng, lambda e: e.tensor_scalar(out=out, in0=in0, scalar1=s1, scalar2=None, op0=op0), r, w)
        else:
            self.S.op(eng, lambda e: e.tensor_scalar(out=out, in0=in0, scalar1=s1, scalar2=s2, op0=op0, op1=op1), r, w)

    def stt(self, eng, out, in0, scalar, in1, op0, op1, r, w):
        self.S.op(eng, lambda e: e.scalar_tensor_tensor(out=out, in0=in0, scalar=scalar, in1=in1, op0=op0, op1=op1), r, w)

    def cp(self, eng, out, in_, r, w):
        if eng == 'act':
            self.S.op('act', lambda e: e.copy(out=out, in_=in_), r, w)
        else:
            self.S.op(eng, lambda e: e.tensor_copy(out=out, in_=in_), r, w)

    def ps(self, bank, n=512):
        return self.PS[:, bank, 0:n]

    def psbf(self, bank):
        return self.PS[:, bank, :].bitcast(BF16)

    def src(self, layer, b, tg):
        if layer == 0:
            if tg < 32:
                return self.x[b, tg * 128:(tg + 1) * 128, :]
            return self.ctx[b, (tg - 32) * 128:(tg - 31) * 128, :]
        return self.xs[b, tg * 128:(tg + 1) * 128, :]

    def setup_consts(self):
        S, A = self.S, self.A
        self.ident = A.alloc([128, 128], BF16, 'ident')
        self.identf = A.alloc([128, 128], F32, 'identf')
        self.iota = A.alloc([128, 128], F32, 'iota')
        self.epsT = A.alloc([128, 1], F32, 'eps')
        self.sinkexp = A.alloc([128, 8], F32, 'sinkexp')
        self.mL = A.alloc([128, 128], BF16, 'mL')
        self.mU = A.alloc([128, 128], BF16, 'mU')
        identf, ident = self.identf, self.ident
        S.op('pool', lambda e: e.memset(identf[:], 0.0), [], ['identf'])
        S.op('pool', lambda e: e.affine_select(out=identf[:], in_=identf[:], pattern=[[-1, 128]],
                                               compare_op=ALU.not_equal, fill=1.0, base=0, channel_multiplier=1),
             ['identf'], ['identf'])
        self.cp('dve', ident[:], identf[:], ['identf'], ['ident'])
        S.op('pool', lambda e: e.memset(self.epsT[:], EPS), [], ['eps'])
        S.dma('sp', self.iota[:], self.iota_in, [], ['iota'])
        S.dma('sp', self.sinkexp[:], self.sink[0:1, :].partition_broadcast(128)[:, 0, :], [], ['sinkexp'])
        self.act(self.sinkexp[:], self.sinkexp[:], AF.Exp, ['sinkexp'], ['sinkexp'])
        mk = A.mark()
        t0 = A.alloc([128, 128], F32, 'mstg')
        for (m_in, m_sb, key) in ((self.swaL, self.mL, 'mL'), (self.swaU, self.mU, 'mU')):
            S.dma('sp', t0[:, 0:128], m_in, ['t0'], ['t0'])
            self.cp('dve', m_sb[:], t0[:, 0:128], ['t0'], [key])
        S.barrier()
        A.release(mk)

    def phase_mod(self, l):
        S, A = self.S, self.A
        mk = A.mark()
        ccT = A.alloc([128, 8, 3], F32, 'ccT')
        for kc in range(8):
            S.dma('sp', ccT[:, kc, :], self.cc[:, kc * 128:(kc + 1) * 128].rearrange("r p -> p r"), [], ['ccT'],
                  allow_slow_non_contiguous=True)
        self.act(ccT[:], ccT[:], AF.Silu, ['ccT'], ['ccT'])
        modsb = A.alloc([3, 6 * D], F32, 'modsb')
        adab = A.alloc([3, 6 * D], F32, 'adab')
        S.dma('sp', adab[:], self.ada_b[l:l + 1, :].partition_broadcast(3)[:, 0, :], [], ['adab'])
        wb = [A.alloc([128, 8, 512], F32, 'modw') for _ in range(2)]
        for n in range(12):
            w = wb[n % 2]
            S.dma('sp', w[:], self.ada_w[l, :, n * 512:(n + 1) * 512].rearrange("(kc p) n -> p kc n", p=128),
                  [], [('modw', n % 2)])
            for kc in range(8):
                self.mm(self.PS[0:3, n % 2, :], ccT[:, kc, :], w[:, kc, :], kc == 0, kc == 7,
                        ['ccT', ('modw', n % 2)], [('ps', n % 2)])
            self.tt('dve', modsb[:, n * 512:(n + 1) * 512], self.PS[0:3, n % 2, :], adab[:, n * 512:(n + 1) * 512],
                    ALU.add, [('ps', n % 2), 'adab'], ['modsb'])
        S.dma('sp', self.mod[l], modsb[:], ['modsb'], ['mod'])
        S.barrier()
        A.release(mk)

    def load_row_bc(self, dst, src_row, key):
        self.S.dma('sp', dst, src_row.partition_broadcast(128)[:, 0, :], [], [key])

    def load_norm_mod(self, l, row, which, tag):
        A = self.A
        G = A.alloc([128, D], F32, 'G' + tag)
        SH = A.alloc([128, D], F32, 'SH' + tag)
        tmp = A.alloc([128, D], F32, 'ng' + tag)
        sh_off = 0 if which == 1 else 3
        ng = self.norm1_g if which == 1 else self.norm2_g
        self.load_row_bc(G[:], self.mod[l, row:row + 1, (sh_off + 1) * D:(sh_off + 2) * D], 'G' + tag)
        self.load_row_bc(SH[:], self.mod[l, row:row + 1, sh_off * D:(sh_off + 1) * D], 'SH' + tag)
        self.load_row_bc(tmp[:], ng[l:l + 1, :], 'ng' + tag)
        self.stt('dve', G[:], G[:], 1.0, tmp[:], ALU.add, ALU.mult, ['G' + tag, 'ng' + tag], ['G' + tag])
        return G, SH

    def load_gate(self, l, row, which, tag):
        A = self.A
        g = A.alloc([128, D], F32, 'gate' + tag)
        off = 2 if which == 1 else 5
        self.load_row_bc(g[:], self.mod[l, row:row + 1, off * D:(off + 1) * D], 'gate' + tag)
        return g

    def alloc_norm_bufs(self):
        A = self.A
        nb = {}
        nb['xt'] = [A.alloc([128, D], F32, 'xt') for _ in range(2)]
        nb['tmp'] = A.alloc([128, D], F32, 'ntmp')
        nb['junk'] = A.alloc([128, D], BF16, 'junk')
        nb['hb'] = [A.alloc([128, D], BF16, 'hb') for _ in range(2)]
        nb['ss'] = A.alloc([128, 4], F32, 'ss')
        nb['i'] = 0
        return nb

    def norm_tile(self, nb, src_ap, G, SH, gk, hT_dst, hT_key, psbank, xt_keep=False):
        S = self.S
        p = nb['i'] % 2
        nb['i'] += 1
        xt, hb, ss = nb['xt'][p], nb['hb'][p], nb['ss']
        S.dma('sp', xt[:], src_ap, [], [('xt', p)])
        self.act(nb['junk'][:], xt[:], AF.Square, [('xt', p)], ['junk', ('ss', p)], accum_out=ss[:, p:p + 1])
        self.act(ss[:, 2 + p:3 + p], ss[:, p:p + 1], AF.Sqrt, [('ss', p), 'eps'], [('rs', p)], scale=1.0 / D,
                 bias=self.epsT[:, 0:1])
        S.op('dve', lambda e: e.reciprocal(out=ss[:, 2 + p:3 + p], in_=ss[:, 2 + p:3 + p]), [('rs', p)], [('rs', p)])
        self.stt('dve', nb['tmp'][:], xt[:], ss[:, 2 + p:3 + p], G[:], ALU.mult, ALU.mult,
                 [('xt', p), ('rs', p)] + gk, ['ntmp'])
        self.tt('pool', hb[:], nb['tmp'][:], SH[:], ALU.add, ['ntmp'] + gk, [('hb', p)])
        pst = self.psbf(psbank)
        for kc in range(8):
            self.tr(pst[:, kc * 128:(kc + 1) * 128], hb[:, kc * 128:(kc + 1) * 128], [('hb', p), 'ident'],
                    [('ps', psbank)])
        self.cp('act', hT_dst, pst.rearrange("p (k t) -> p k t", k=8), [('ps', psbank)], [hT_key])
        return p

    def load_w_bf16(self, dst, src, ncols, key, piece=512):
        S, A = self.S, self.A
        mk = A.mark()
        stg = [A.alloc([128, 8, piece], F32, 'wstg') for _ in range(2)]
        i = 0
        for c0 in range(0, ncols, piece):
            n = min(piece, ncols - c0)
            s = stg[i % 2]
            S.dma('sp', s[:, :, 0:n], src[:, c0:c0 + n].rearrange("(kc p) n -> p kc n", p=128), [], [('wstg', i % 2)])
            self.cp('dve' if i % 2 == 0 else 'act', dst[:, :, c0:c0 + n], s[:, :, 0:n], [('wstg', i % 2)], [key])
            i += 1
        return mk

    def make_rot(self, W, src0, dst0, nblk, half, key):
        for kc in range(8):
            s = W[:, kc, src0:src0 + nblk * 2 * half].rearrange("p (b t h) -> p b t h", t=2, h=half)
            d = W[:, kc, dst0:dst0 + nblk * 2 * half].rearrange("p (b t h) -> p b t h", t=2, h=half)
            self.S.op('act', lambda e, s=s, d=d: e.mul(out=d[:, :, 0, :], in_=s[:, :, 1, :], mul=-1.0), [key], [key])
            self.cp('pool', d[:, :, 1, :], s[:, :, 0, :], [key], [key])

    def attn_setup(self):
        A = self.A
        self.E = [A.alloc([128, 512], BF16, 'E') for _ in range(3)]
        self.ei = 0
        self.sti = 0
        self.acci = 0
        self.den = A.alloc([128, 8], F32, 'den')

    def attn_unit(self, qT, nq, ktiles, dv, scale, qkeys, out_fn, sink_col=None):
        S = self.S
        nqs = nq // 128
        aset = self.acci % 2
        self.acci += 1
        accs = []
        for qs in range(nqs):
            bank = 4 + 2 * aset + qs // 2
            sub = qs % 2
            accs.append((self.PS[:, bank, sub * 256:sub * 256 + dv + 1], ('ps', bank, sub)))
        nk = len(ktiles)
        for ki, (kT, v, bias, kkeys) in enumerate(ktiles):
            sb = self.sti % 2
            self.sti += 1
            st = self.PS[:, sb, 0:nq]
            self.mm(st, kT, qT, True, bias is None, qkeys + kkeys, [('ps', sb)])
            if bias is not None:
                self.mm(st, self.ident[:], bias[0], False, True, ['ident'] + bias[1], [('ps', sb)])
            eb = self.ei % 3
            self.ei += 1
            E = self.E[eb]
            self.act(E[:, 0:nq], st, AF.Exp, [('ps', sb)], [('E', eb)], scale=scale)
            for qs in range(nqs):
                self.mm(accs[qs][0], E[:, qs * 128:(qs + 1) * 128], v, ki == 0, ki == nk - 1,
                        [('E', eb)] + kkeys, [accs[qs][1]])
        for qs in range(nqs):
            acc, akey = accs[qs]
            dcol = self.den[:, (aset * 4 + qs):(aset * 4 + qs) + 1]
            dkey = ('den', aset * 4 + qs)
            if sink_col is not None:
                self.tt('dve', dcol, acc[:, dv:dv + 1], self.sinkexp[:, sink_col:sink_col + 1], ALU.add,
                        [akey, 'sinkexp'], [dkey])
                S.op('dve', lambda e, dcol=dcol: e.reciprocal(out=dcol, in_=dcol), [dkey], [dkey])
            else:
                S.op('dve', lambda e, dcol=dcol, acc=acc: e.reciprocal(out=dcol, in_=acc[:, dv:dv + 1]), [akey], [dkey])
            out_fn(qs, acc, dcol, [akey, dkey])

    def phase_proj0(self, l=0):
        S, A = self.S, self.A
        mk = A.mark()
        wA = A.alloc([128, 8, 3328], BF16, 'wA')
        m2 = self.load_w_bf16(wA, self.ab_w, 2304, 'wA', piece=576)
        S.barrier()
        A.release(m2)
        self.make_rot(wA, 1536, 2304, 16, 16, 'wA')
        for kc in range(8):
            for r in range(4):
                self.cp('dve', wA[:, kc, 2816 + r * 64:2880 + r * 64], wA[:, kc, 2048 + (r // 2) * 64:2112 + (r // 2) * 64],
                        ['wA'], ['wA'])
        self.make_rot(wA, 2816, 3072, 8, 16, 'wA')
        rC = A.alloc([128, SA], F32, 'rC')
        rS = A.alloc([128, SA], F32, 'rS')
        S.dma('sp', rC[:], self.ropeC, [], ['rC'])
        S.dma('sp', rS[:], self.ropeS, [], ['rS'])
        fm = ([('Q', i, 128 * i, None) for i in range(4)] + [('K', i, 512 + 128 * i, None) for i in range(4)]
              + [('Q', 4 + i, 1536 + 128 * i, 2304 + 128 * i) for i in range(4)]
              + [('K', 4 + i, 2816 + 128 * i, 3072 + 128 * i) for i in range(2)])
        tmv = [(1024, 512, 0, 8, 64), (2176, 128, 8, 2, 64)]
        for b in range(NB):
            self.proj_batch(l, b, wA, fm, tmv, 10, rC, rS, None)
        S.barrier()
        A.release(mk)

    def proj_batch(self, l, b, wA, fm, tmv, nvh, rC, rS, extra):
        S, A = self.S, self.A
        mk = A.mark()
        Gb, SHb = self.load_norm_mod(l, b, 1, 'b')
        Gc, SHc = self.load_norm_mod(l, 2, 1, 'c')
        nb = self.alloc_norm_bufs()
        hTs = [A.alloc([128, 8, 512], BF16, 'hT') for _ in range(2)]
        stg = [A.alloc([128, 512], BF16, 'stg') for _ in range(3)]
        t1 = A.alloc([128, 512], F32, 't1')
        t2 = A.alloc([128, 512], F32, 't2')
        vdv = tmv[0][4]
        vw = sum(nh * (dvv + 1) for (_, _, _, nh, dvv) in tmv)
        vst = [A.alloc([128, vw], BF16, 'vst') for _ in range(2)]
        for v in vst:
            S.op('pool', lambda e, v=v: e.memset(v[:], 1.0), [], [('vst', 0), ('vst', 1)])
        si = 0
        pi = 0
        for ci in range(9):
            ntile = 4 if ci < 8 else 2
            n = ntile * 128
            t0 = ci * 512
            hT = hTs[ci % 2]
            hkey = ('hT', ci % 2)
            G, SH, gk = (Gb, SHb, ['Gb', 'SHb']) if ci < 8 else (Gc, SHc, ['Gc', 'SHc'])
            for ti in range(ntile):
                tg = ci * 4 + ti
                self.norm_tile(nb, self.src(l, b, tg), G, SH, gk, hT[:, :, ti * 128:(ti + 1) * 128], hkey, 6)
            for (dst, idx, col0, rot0) in fm:
                pa = pi % 2
                pi += 1
                for kc in range(8):
                    self.mm(self.PS[:, pa, 0:n], wA[:, kc, col0:col0 + 128], hT[:, kc, 0:n], kc == 0, kc == 7,
                            ['wA', hkey], [('ps', pa)])
                sg = stg[si % 3]
                skey = ('stg', si % 3)
                si += 1
                if rot0 is None:
                    self.cp('act', sg[:, 0:n], self.PS[:, pa, 0:n], [('ps', pa)], [skey])
                else:
                    for kc in range(8):
                        self.mm(self.PS[:, 2 + pa, 0:n], wA[:, kc, rot0:rot0 + 128], hT[:, kc, 0:n], kc == 0, kc == 7,
                                ['wA', hkey], [('ps', 2 + pa)])
                    self.tt('dve', t1[:, 0:n], self.PS[:, pa, 0:n], rC[:, t0:t0 + n], ALU.mult, [('ps', pa), 'rC'], ['t1'])
                    self.tt('dve', t2[:, 0:n], self.PS[:, 2 + pa, 0:n], rS[:, t0:t0 + n], ALU.mult, [('ps', 2 + pa), 'rS'], ['t2'])
                    self.tt('pool', sg[:, 0:n], t1[:, 0:n], t2[:, 0:n], ALU.add, ['t1', 't2'], [skey])
                dram = self.QT if dst == 'Q' else self.KT
                S.dma('pool', dram[b, idx, :, t0:t0 + n], sg[:, 0:n], [skey], [(dst, b, idx)])
            if extra is not None:
                extra(b, ci, t0, n, hT, hkey)
            for ti in range(ntile):
                tg = ci * 4 + ti
                vs = vst[tg % 2]
                vkey = ('vst', tg % 2)
                co = 0
                for gi, (col0, ncols, h0, nh, dvv) in enumerate(tmv):
                    bank = 4 + gi
                    for kc in range(8):
                        self.mm(self.PS[:, bank, 0:ncols], hT[:, kc, ti * 128:(ti + 1) * 128], wA[:, kc, col0:col0 + ncols],
                                kc == 0, kc == 7, ['wA', hkey], [('ps', bank)])
                    ov = vs[:, co:co + nh * (dvv + 1)].rearrange("p (h d) -> p h d", d=dvv + 1)[:, :, 0:dvv]
                    self.cp('act', ov, self.PS[:, bank, 0:ncols].rearrange("p (h d) -> p h d", d=dvv), [('ps', bank)], [vkey])
                    co += nh * (dvv + 1)
                S.dma('pool', self.V[b, tg * 128:(tg + 1) * 128, 0:vw], vs[:], [vkey], [('V', b)])
        S.barrier()
        A.release(mk)

    def phase_attn0(self):
        S, A = self.S, self.A
        mk = A.mark()
        self.TabI = A.alloc([128, 7680], BF16, 'TabI')
        self.TabE = A.alloc([128, 7680], BF16, 'TabE')
        mk2 = A.mark()
        t0 = A.alloc([128, 7680], F32, 'tabstg0')
        t1 = A.alloc([128, 7680], F32, 'tabstg1')
        S.dma('sp', t0[:], self.rpbT, [], ['t0'])
        for (msk, Tab, key) in ((self.maskI, self.TabI, 'TabI'), (self.maskE, self.TabE, 'TabE')):
            S.dma('sp', t1[:], msk, [], ['t1'])
            self.tt('dve', t1[:], t1[:], t0[:], ALU.add, ['t0', 't1'], ['t1'])
            self.ts('dve', Tab[:], t1[:], 8.0, None, ALU.mult, None, ['t1'], [key])
        S.barrier()
        A.release(mk2)
        self.attn_setup()
        Qs = [A.alloc([128, SA], BF16, 'Qs') for _ in range(2)]
        Ks = [A.alloc([128, SA], BF16, 'Ks') for _ in range(2)]
        Vsl = [A.alloc([128, NT, 130], BF16, 'Vsl') for _ in range(2)]
        ystg = [A.alloc([128, 128], BF16, 'ystg') for _ in range(2)]
        gi = 0
        yi = 0
        for b in range(NB):
            for grp in range(8):
                p = gi % 2
                gi += 1
                na = grp < 4
                c = grp if na else grp - 4
                qidx = grp
                kidx = c if na else 4 + c // 2
                nvc = 130 if na else 65
                vcol0 = (2 * c) * 65 if na else (8 + c // 2) * 65
                S.dma('sp', Qs[p][:], self.QT[b, qidx], [('Q', b, qidx)], [('Qs', p)])
                S.dma('sp', Ks[p][:], self.KT[b, kidx], [('K', b, kidx)], [('Ks', p)])
                S.dma('sp', Vsl[p][:, :, 0:nvc],
                      self.V[b, :, vcol0:vcol0 + nvc].rearrange("(t p) c -> p t c", p=128), [('V', b)], [('Vs', p)])
                for m in range(NT):
                    ys = ystg[yi % 2]
                    ykey = ('ystg', yi % 2)
                    yi += 1
                    for hh in range(2):
                        h = 2 * c + hh
                        pb = hh * 64
                        qT = Qs[p][pb:pb + 64, m * 128:(m + 1) * 128]
                        kts = []

                        def ktile(kt, bias):
                            vo = hh * 65 if na else 0
                            return (Ks[p][pb:pb + 64, kt * 128:(kt + 1) * 128], Vsl[p][:, kt, vo:vo + 65], bias,
                                    [('Ks', p), ('Vs', p)])
                        if m < 32:
                            if na:
                                if m < 2:
                                    lat, Tab, tk = range(0, 4), self.TabE, 'TabE'
                                elif m >= 30:
                                    lat, Tab, tk = range(28, 32), self.TabE, 'TabE'
                                else:
                                    lat, Tab, tk = range(m - 2, m + 3), self.TabI, 'TabI'
                                for kt in lat:
                                    j = kt - m
                                    p0 = 7 - 2 * j
                                    bias = (Tab[:, h * 960 + p0 * 64:h * 960 + p0 * 64 + 128], [tk])
                                    kts.append(ktile(kt, bias))
                            else:
                                for kt in range(max(0, m - 1), min(31, m + 1) + 1):
                                    j = kt - m
                                    bias = None if j == 0 else ((self.mL[:], ['mL']) if j < 0 else (self.mU[:], ['mU']))
                                    kts.append(ktile(kt, bias))
                        kts.append(ktile(32, None))
                        kts.append(ktile(33, None))

                        def out_fn(qs, acc, rden, keys, ys=ys, ykey=ykey, hh=hh):
                            self.ts('dve', ys[:, hh * 64:(hh + 1) * 64], acc[:, 0:64], rden, None, ALU.mult, None,
                                    keys, [ykey])
                        self.attn_unit(qT, 128, kts, 64, 0.125, [('Qs', p)], out_fn, sink_col=None if na else h)
                    S.dma('pool', self.Y[b, m * 128:(m + 1) * 128, grp * 128:(grp + 1) * 128], ys[:], [ykey], [('Y', b)])
        S.barrier()
        A.release(mk)

    def phase_out(self, l, ntiles):
        S, A = self.S, self.A
        mk = A.mark()
        wO = A.alloc([128, 8, D], BF16, 'wO')
        m2 = self.load_w_bf16(wO, self.w_out[l], D, 'wO')
        S.barrier()
        A.release(m2)
        gc = self.load_gate(l, 2, 1, 'c')
        ysb = [A.alloc([128, D], BF16, 'ysb') for _ in range(2)]
        yT = [A.alloc([128, 8, 128], BF16, 'yT') for _ in range(2)]
        xt = [A.alloc([128, D], F32, 'xo') for _ in range(2)]
        tmp = [A.alloc([128, D], F32, 'otmp') for _ in range(2)]
        i = 0
        for b in range(NB):
            m3 = A.mark()
            gb = self.load_gate(l, b, 1, 'b')
            for m in range(ntiles):
                p = i % 2
                i += 1
                g, gk = (gb, 'gateb') if m < 32 else (gc, 'gatec')
                S.dma('sp', ysb[p][:], self.Y[b, m * 128:(m + 1) * 128, :], [('Y', b)], [('ysb', p)])
                S.dma('sp', xt[p][:], self.src(l, b, m), [('xs', b, m)], [('xo', p)])
                pst = self.psbf(6 + p)
                for kc in range(8):
                    self.tr(pst[:, kc * 128:(kc + 1) * 128], ysb[p][:, kc * 128:(kc + 1) * 128], [('ysb', p), 'ident'],
                            [('ps', 6 + p)])
                self.cp('act', yT[p][:], pst.rearrange("p (k t) -> p k t", k=8), [('ps', 6 + p)], [('yT', p)])
                for half in range(2):
                    bank = 2 * p + half
                    for kc in range(8):
                        self.mm(self.PS[:, bank, :], yT[p][:, kc, :], wO[:, kc, half * 512:(half + 1) * 512], kc == 0, kc == 7,
                                [('yT', p), 'wO'], [('ps', bank)])
                    self.tt('dve', tmp[p][:, half * 512:(half + 1) * 512], self.PS[:, bank, :],
                            g[:, half * 512:(half + 1) * 512], ALU.mult, [('ps', bank), gk], [('otmp', p, half)])
                self.tt('pool', tmp[p][:], tmp[p][:], xt[p][:], ALU.add, [('otmp', p, 0), ('otmp', p, 1), ('xo', p)],
                        [('otmp', p, 0), ('otmp', p, 1)])
                S.dma('pool', self.xs[b, m * 128:(m + 1) * 128, :], tmp[p][:], [('otmp', p, 0), ('otmp', p, 1)],
                      [('xs', b, m)])
            A.release(m3)
        S.barrier()
        A.release(mk)

    def build(self):
        S = self.S
        self.setup_consts()
        if self.skip_l0:
            for b in range(NB):
                for r0 in range(0, SL, 512):
                    S.dma('sp', self.xs[b, r0:r0 + 512, :], self.x[b, r0:r0 + 512, :], [], [('xsi', b, r0)])
                S.dma('sp', self.xs[b, SL:SA, :], self.ctx[b], [], [('xsi', b, SL)])
            S.barrier()
            self.phase_mod(1)
            self.phase_proj1()
            return self.finish_dbg()
        self.phase_mod(0)
        self.phase_proj0()
        self.phase_attn0()
        self.phase_out(0, NT)
        if self.stop_after == 'attn0':
            return self.finish_dbg()
        self.phase_peer(0, NT)
        if self.stop_after == 'peer0':
            return self.finish_dbg()
        self.phase_mod(1)
        self.phase_proj1()
        if self.stop_after == 'proj1':
            return self.finish_dbg()
        self.phase_attn1()
        self.phase_out(1, 32)
        if self.stop_after == 'attn1':
            return self.finish_dbg()
        self.phase_peer(1, 32)
        S.barrier()
        S.emit()
        return self.nc

    def finish_dbg(self):
        S = self.S
        for b in range(NB):
            for r0 in range(0, SA, 544):
                S.dma('sp', self.dbg[b, r0:r0 + 544, :], self.xs[b, r0:r0 + 544, :], [], [('dbg', b, r0)])
        S.barrier()
        S.emit()
        return self.nc


def _consts():
    f = np.float32
    t = np.arange(SL)
    row, col = (t // 64).astype(f), (t % 64).astype(f)

    def table(dim_half, npart_rep):
        freq = (10000.0 ** (-np.arange(dim_half, dtype=f) / dim_half)).astype(f)
        ang_r = row[None, :] * freq[:, None]
        ang_c = col[None, :] * freq[:, None]
        C = np.concatenate([np.cos(ang_r), np.cos(ang_r), np.cos(ang_c), np.cos(ang_c)], 0)
        Sn = np.concatenate([np.sin(ang_r), np.sin(ang_r), np.sin(ang_c), np.sin(ang_c)], 0)
        C = np.concatenate([C, np.ones((C.shape[0], CT), f)], 1)
        Sn = np.concatenate([Sn, np.zeros((Sn.shape[0], CT), f)], 1)
        return C.astype(f), Sn.astype(f)
    C64, S64 = table(16, 2)
    ropeC = np.concatenate([C64, C64], 0)
    ropeS = np.concatenate([S64, S64], 0)
    C32, S32 = table(8, 1)
    ropeCm = np.concatenate([np.ones((64, SA), f), C32], 0)
    ropeSm = np.concatenate([np.zeros((64, SA), f), S32], 0)
    cq = np.arange(64)
    col_start = np.clip(cq - 8, 0, 48)
    ck = np.arange(64)
    valid = (ck[:, None] >= col_start[None, :]) & (ck[:, None] < col_start[None, :] + 16)
    maskI = np.full((2, 64, 8, 15, 64), NEG, f)
    maskE = np.full((2, 64, 8, 15, 64), NEG, f)
    for kr in range(2):
        for p in range(15):
            dr = (7 - p) if kr == 0 else (8 - p)
            if dr < -7 or dr > 7:
                continue
            mE = np.where(valid, 0.0, NEG).astype(f)
            maskE[kr, :, :, p, :] = mE[:, None, :]
            if -4 <= dr <= 3:
                maskI[kr, :, :, p, :] = mE[:, None, :]
    swaL = np.where(np.arange(128)[:, None] >= np.arange(128)[None, :], 0.0, NEG).astype(f)
    swaU = np.where(np.arange(128)[:, None] <= np.arange(128)[None, :], 0.0, NEG).astype(f)
    iota = np.tile(np.arange(128, dtype=f)[None, :], (128, 1))
    return dict(ropeC=ropeC, ropeS=ropeS, ropeCm=ropeCm, ropeSm=ropeSm, maskI=maskI.reshape(128, 7680),
                maskE=maskE.reshape(128, 7680), swaL=swaL, swaU=swaU, iota_in=iota)


def _rpb_layout(rpb):
    ck = np.arange(64)[:, None]
    cq = np.arange(64)[None, :]
    cidx = np.clip(ck - cq + 15, 0, 30)
    out = np.zeros((2, 64, 8, 15, 64), np.float32)
    for kr in range(2):
        for p in range(15):
            dr = (7 - p) if kr == 0 else (8 - p)
            if dr < -7 or dr > 7:
                continue
            out[kr, :, :, p, :] = np.transpose(rpb[:, dr + 7, :][:, cidx], (1, 0, 2))
    return np.ascontiguousarray(out.reshape(128, 7680))


_CONSTS = None


def make_in_maps(inputs, cores):
    global _CONSTS
    if _CONSTS is None:
        _CONSTS = _consts()
    f = np.float32
    g = {k: np.ascontiguousarray(np.asarray(v, dtype=f)) for k, v in inputs.items()}
    shared = dict(
        ada_w=g['ada_w'], ada_b=g['ada_b'], norm1_g=g['norm1_g'], norm2_g=g['norm2_g'], w_out=g['w_out'],
        peer_wq=g['peer_wq'], peer_keys=g['peer_keys'], peer_u=g['peer_u'], peer_v=g['peer_v'],
        ab_w=g['ab_w_in'][0], rpbT=_rpb_layout(g['na_rpb'][0]), sink=g['swa_sink'][0:1],
        cd_w=g['cd_w_in'][0], qn_g=g['mla_q_norm_g'][0].reshape(256, 1), w_uq=g['mla_w_uq'][0],
        kvn_g=g['mla_kv_norm_g'][0].reshape(128, 1), w_ukv=g['mla_w_ukv'][0], dlam=g['diff_lambda'][0].reshape(1, 256),
        subln=g['diff_subln_g'][0].reshape(1, 128), fin_g=g['final_norm_g'].reshape(1, D), **_CONSTS)
    maps = []
    for ci in cores:
        b0 = ci * NB
        m = dict(shared)
        m['x'] = g['x'][b0:b0 + NB]
        m['ctx'] = g['ctx'][b0:b0 + NB]
        m['cc'] = np.ascontiguousarray(np.concatenate([g['c'][b0:b0 + NB], g['c_ctx'][None, :]], 0))
        maps.append(m)
    return maps


def kernel(**inputs):
    nc = Builder().build()
    maps = make_in_maps(inputs, list(range(N_CORES)))
    res = run_bass_kernel_spmd(nc, maps, core_ids=list(range(N_CORES)))
    return np.concatenate([r["out"] for r in res.results], axis=0).astype(np.float32)


def _peer_methods():
    def topk16(self, src, vals, idx, wk, rk, wkey):
        S = self.S
        S.op('dve', lambda e: e.max(out=vals[:, 0:8], in_=src), rk, [wkey + ('v',)])
        S.op('dve', lambda e: e.max_index(out=idx[:, 0:8], in_max=vals[:, 0:8], in_values=src), rk + [wkey + ('v',)], [wkey + ('i',)])
        S.op('dve', lambda e: e.match_replace(out=wk, in_to_replace=vals[:, 0:8], in_values=src, imm_value=-1e30),
             rk + [wkey + ('v',)], ['tk_wk'])
        S.op('dve', lambda e: e.max(out=vals[:, 8:16], in_=wk), ['tk_wk'], [wkey + ('v',)])
        S.op('dve', lambda e: e.max_index(out=idx[:, 8:16], in_max=vals[:, 8:16], in_values=wk), ['tk_wk', wkey + ('v',)],
             [wkey + ('i',)])

    def phase_peer_prep(self, l):
        S, A = self.S, self.A
        mk = A.mark()
        ustg = [A.alloc([128, D], F32, 'ustg') for _ in range(2)]
        vstg = [A.alloc([128, D], F32, 'vstg') for _ in range(2)]
        ubf = [A.alloc([128, D], BF16, 'ubf') for _ in range(2)]
        vbf = [A.alloc([128, D], BF16, 'vbf') for _ in range(2)]
        utb = [A.alloc([128, 8, 128], BF16, 'utb') for _ in range(2)]
        U = self.peer_u[l].rearrange("(i j) d -> j i d", j=128)
        Vv = self.peer_v[l].rearrange("(i j) d -> j i d", j=128)
        for j in range(128):
            p = j % 2
            S.dma('sp', ustg[p][:], U[j], [], [('ustg', p)])
            S.dma('sp', vstg[p][:], Vv[j], [], [('vstg', p)])
            self.cp('dve', ubf[p][:], ustg[p][:], [('ustg', p)], [('ubf', p)])
            pst = self.psbf(6 + p)
            for kc in range(8):
                self.tr(pst[:, kc * 128:(kc + 1) * 128], ubf[p][:, kc * 128:(kc + 1) * 128], [('ubf', p), 'ident'], [('ps', 6 + p)])
            self.cp('act', utb[p][:], pst.rearrange("p (k t) -> p k t", k=8), [('ps', 6 + p)], [('utb', p)])
            S.dma('pool', self.UTs[j], utb[p][:].rearrange("p k t -> p (k t)"), [('utb', p)], [('UTs', j)])
            self.cp('pool', vbf[p][:], vstg[p][:], [('vstg', p)], [('vbf', p)])
            S.dma('pool', self.Vs[j], vbf[p][:], [('vbf', p)], [('Vs', j)])
        wst = [A.alloc([128, 8, 512], F32, 'wqst') for _ in range(2)]
        wbf = [A.alloc([128, 8, 512], BF16, 'wqbf') for _ in range(2)]
        for c in range(4):
            p = c % 2
            S.dma('sp', wst[p][:], self.peer_wq[l, :, c * 512:(c + 1) * 512].rearrange("(kc p) n -> p kc n", p=128), [], [('wqst', p)])
            self.cp('dve', wbf[p][:], wst[p][:], [('wqst', p)], [('wqbf', p)])
            S.dma('pool', self.WQs[c], wbf[p][:].rearrange("p k t -> p (k t)"), [('wqbf', p)], [('WQs', c)])
        S.barrier()
        A.release(mk)

    def phase_peer(self, l, ntiles):
        S, A = self.S, self.A
        self.phase_peer_prep(l)
        mk = A.mark()
        last = (l == 1)
        kT = A.alloc([128, 2, 128], BF16, 'kT')
        kst = A.alloc([128, 2, 128], F32, 'kst')
        kbf = A.alloc([128, 2, 128], BF16, 'kbf')
        for p in range(2):
            S.dma('sp', kst[:, p, :], self.peer_keys[l, p], [], ['kst'])
        self.cp('dve', kbf[:], kst[:], ['kst'], ['kbf'])
        pst = self.psbf(2)
        for p in range(2):
            self.tr(pst[:, p * 128:(p + 1) * 128], kbf[:, p, :], ['kbf', 'ident'], [('ps', 2)])
        self.cp('act', kT[:], pst[:, 0:256].rearrange("p (k t) -> p k t", k=2), [('ps', 2)], ['kT'])
        G2 = A.alloc([128, D], F32, 'G2')
        SH2 = A.alloc([128, D], F32, 'SH2')
        g2 = A.alloc([128, D], F32, 'g2')
        ngt = A.alloc([128, D], F32, 'ng2')
        if last:
            fing = A.alloc([128, D], F32, 'fing')
            self.load_row_bc(fing[:], self.fin_g[0:1, :], 'fing')
        nb = self.alloc_norm_bufs()
        h2T = A.alloc([128, 8, 256], BF16, 'h2T')
        qT = A.alloc([128, 16, 256], BF16, 'qT')
        wqb = [A.alloc([128, 8, 512], BF16, 'wqb') for _ in range(2)]
        s_sb = A.alloc([128, 2048], F32, 's_sb')
        cand = A.alloc([128, 2048], F32, 'cand')
        sv = A.alloc([128, 256], F32, 'sv')
        si = A.alloc([128, 256], U32, 'si')
        sif = A.alloc([128, 256], F32, 'sif')
        wk = A.alloc([128, 256], F32, 'wk')
        fv = A.alloc([128, 128], F32, 'fv')
        fp = A.alloc([128, 128], U32, 'fp')
        fpu = A.alloc([128, 128], U32, 'fpu')
        Aa = A.alloc([128, 128], F32, 'Aa')
        Bb = A.alloc([128, 128], F32, 'Bb')
        ijg = A.alloc([128, 3, 128], F32, 'ijg')
        ijgT = A.alloc([128, 3, 128], F32, 'ijgT')
        gz = A.alloc([128, 16], F32, 'gz')
        RT = 32
        R1 = A.alloc([128, RT, 128], BF16, 'R1')
        R2 = A.alloc([128, RT, 128], BF16, 'R2')
        Gs = A.alloc([128, 128, 256], BF16, 'Gs')
        utl = [A.alloc([128, 8, 128], BF16, 'utl') for _ in range(3)]
        vl = [A.alloc([128, D], BF16, 'vl') for _ in range(3)]
        ga = [A.alloc([128, 256], F32, 'ga') for _ in range(2)]
        wT = [A.alloc([128, 256], BF16, 'wT') for _ in range(3)]
        etmp = A.alloc([128, D], F32, 'etmp')
        iota16 = self.iota[:, 0:16]
        nchunks = ntiles // 2
        gskeys = [('Gs', a, c) for a in range(2) for c in range(128 // RT)]
        ji = 0
        for b in range(NB):
            cur_row = None
            for ch in range(nchunks):
                row = b if ch < 16 else 2
                if row != cur_row:
                    cur_row = row
                    sh_off = 3
                    self.load_row_bc(G2[:], self.mod[l, row:row + 1, 4 * D:5 * D], 'G2')
                    self.load_row_bc(SH2[:], self.mod[l, row:row + 1, 3 * D:4 * D], 'SH2')
                    self.load_row_bc(ngt[:], self.norm2_g[l:l + 1, :], 'ng2')
                    self.stt('dve', G2[:], G2[:], 1.0, ngt[:], ALU.add, ALU.mult, ['G2', 'ng2'], ['G2'])
                    self.load_row_bc(g2[:], self.mod[l, row:row + 1, 5 * D:6 * D], 'g2')
                xpar = []
                for ti in range(2):
                    tg = ch * 2 + ti
                    xpar.append(self.norm_tile(nb, self.xs[b, tg * 128:(tg + 1) * 128, :], G2, SH2, ['G2', 'SH2'],
                                               h2T[:, :, ti * 128:(ti + 1) * 128], 'h2T', 2))
                for c in range(4):
                    wb = wqb[c % 2]
                    S.dma('sp', wb[:].rearrange("p k t -> p (k t)"), self.WQs[c], [('WQs', c)], [('wqb', c % 2)])
                    for q in range(4):
                        hp = c * 4 + q
                        bank = 2 + hp % 2
                        for kc in range(8):
                            self.mm(self.PS[:, bank, 0:256], wb[:, kc, q * 128:(q + 1) * 128], h2T[:, kc, :], kc == 0, kc == 7,
                                    [('wqb', c % 2), 'h2T'], [('ps', bank)])
                        self.cp('act', qT[:, hp, :], self.PS[:, bank, 0:256], [('ps', bank)], ['qT'])
                for ti in range(2):
                    for g4 in range(4):
                        bank = 2 + g4 % 2
                        for q in range(4):
                            hp = g4 * 4 + q
                            self.mm(self.PS[:, bank, q * 128:(q + 1) * 128], qT[:, hp, ti * 128:(ti + 1) * 128], kT[:, hp % 2, :],
                                    True, True, ['qT', 'kT'], [('ps', bank)])
                        self.cp('act', s_sb[:, g4 * 512:(g4 + 1) * 512], self.PS[:, bank, :], [('ps', bank)], [('s_sb', g4)])
                    for hp in range(16):
                        self.topk16(s_sb[:, hp * 128:(hp + 1) * 128], sv[:, hp * 16:(hp + 1) * 16], si[:, hp * 16:(hp + 1) * 16],
                                    wk[:, 0:128], [('s_sb', hp // 4)], ('sv', hp))
                    svk = [('sv', hp, 'v') for hp in range(16)]
                    sik = [('sv', hp, 'i') for hp in range(16)]
                    self.cp('dve', sif[:], si[:], sik, ['sif'])
                    sv4 = sv[:].rearrange("p (h t k) -> p h t k", h=8, t=2)
                    sif4 = sif[:].rearrange("p (h t k) -> p h t k", h=8, t=2)
                    cand4 = cand[:].rearrange("p (h a b) -> p h a b", h=8, a=16)
                    self.tt('dve', cand4, sv4[:, :, 0, :].unsqueeze(3).to_broadcast([128, 8, 16, 16]),
                            sv4[:, :, 1, :].unsqueeze(2).to_broadcast([128, 8, 16, 16]), ALU.add, svk, ['cand'])
                    for h in range(8):
                        self.topk16(cand[:, h * 256:(h + 1) * 256], fv[:, h * 16:(h + 1) * 16], fp[:, h * 16:(h + 1) * 16],
                                    wk[:, 0:256], ['cand'], ('fv', h))
                    fvk = [('fv', h, 'v') for h in range(8)]
                    fpk = [('fv', h, 'i') for h in range(8)]
                    S.op('dve', lambda e: e.tensor_single_scalar(out=fpu[:], in_=fp[:], scalar=4, op=ALU.logical_shift_right), fpk, ['fpu'])
                    self.cp('dve', Aa[:], fpu[:], ['fpu'], ['Aa'])
                    S.op('dve', lambda e: e.tensor_single_scalar(out=fpu[:], in_=fp[:], scalar=15, op=ALU.bitwise_and), fpk + ['fpu'], ['fpu'])
                    self.cp('dve', Bb[:], fpu[:], ['fpu'], ['Bb'])
                    io4 = iota16.unsqueeze(1).unsqueeze(1).to_broadcast([128, 8, 16, 16])
                    for (sel, t_, slot, skey) in ((Aa, 0, 0, 'Aa'), (Bb, 1, 1, 'Bb')):
                        sel4 = sel[:].rearrange("p (h k) -> p h k", h=8).unsqueeze(3).to_broadcast([128, 8, 16, 16])
                        self.tt('dve', cand4, sel4, io4, ALU.is_equal, [skey, 'iota', 'cand'], ['cand'])
                        self.tt('dve', cand4, cand4, sif4[:, :, t_, :].unsqueeze(2).to_broadcast([128, 8, 16, 16]), ALU.mult,
                                ['cand', 'sif'], ['cand'])
                        S.op('dve', lambda e, slot=slot: e.tensor_reduce(
                            out=ijg[:, slot, :].rearrange("p (h k) -> p h k", h=8), in_=cand4, axis=AX.X, op=ALU.add),
                            ['cand'], [('ijg', slot)])
                    fv3 = fv[:].rearrange("p (h k) -> p h k", h=8)
                    g3 = ijg[:, 2, :].rearrange("p (h k) -> p h k", h=8)
                    self.tt('dve', g3, fv3, fv3[:, :, 0:1].to_broadcast([128, 8, 16]), ALU.subtract, fvk, [('ijg', 2)])
                    self.act(ijg[:, 2, :], ijg[:, 2, :], AF.Exp, [('ijg', 2)], [('ijg', 2)])
                    S.op('dve', lambda e: e.tensor_reduce(out=gz[:, 0:8], in_=g3, axis=AX.X, op=ALU.add), [('ijg', 2)], ['gz'])
                    S.op('dve', lambda e: e.reciprocal(out=gz[:, 0:8], in_=gz[:, 0:8]), ['gz'], ['gz'])
                    self.tt('dve', g3, g3, gz[:, 0:8].unsqueeze(2).to_broadcast([128, 8, 16]), ALU.mult, [('ijg', 2), 'gz'], [('ijg', 2)])
                    for s3 in range(3):
                        self.tr(self.PS[:, 3, s3 * 128:(s3 + 1) * 128], ijg[:, s3, :], [('ijg', s3), 'identf'], [('ps', 3)], f32=True)
                    self.cp('act', ijgT[:], self.PS[:, 3, 0:384].rearrange("p (s t) -> p s t", s=3), [('ps', 3)], ['ijgT'])
                    for rq in range(128 // RT):
                        tq = rq * RT
                        iob = self.iota[:].unsqueeze(1).to_broadcast([128, RT, 128])
                        self.tt('dve', R1[:], iob, ijgT[:, 0, tq:tq + RT].unsqueeze(2).to_broadcast([128, RT, 128]), ALU.is_equal,
                                ['iota', 'ijgT'], ['R1'])
                        self.tt('pool', R1[:], R1[:], ijgT[:, 2, tq:tq + RT].unsqueeze(2).to_broadcast([128, RT, 128]), ALU.mult,
                                ['R1', 'ijgT'], ['R1'])
                        self.tt('dve', R2[:], iob, ijgT[:, 1, tq:tq + RT].unsqueeze(2).to_broadcast([128, RT, 128]), ALU.is_equal,
                                ['iota', 'ijgT'], ['R2'])
                        for t4 in range(RT // 4):
                            bank = t4 % 2
                            for t_ in range(4):
                                t = t4 * 4 + t_
                                self.mm(self.PS[:, bank, t_ * 128:(t_ + 1) * 128], R1[:, t, :], R2[:, t, :], True, True, ['R1', 'R2'],
                                        [('ps', bank, 0), ('ps', bank, 1)])
                            tok0 = ti * 128 + tq + t4 * 4
                            self.cp('act' if t4 % 2 == 0 else 'dve', Gs[:, :, tok0:tok0 + 4],
                                    self.PS[:, bank, :].rearrange("p (t j) -> p j t", t=4), [('ps', bank, 0), ('ps', bank, 1)],
                                    [('Gs', ti, rq)])
                for j in range(128):
                    u = ji % 3
                    S.dma('sp', utl[u][:].rearrange("p k t -> p (k t)"), self.UTs[j], [('UTs', j)], [('utl', u)])
                    S.dma('sp', vl[u][:], self.Vs[j], [('Vs', j)], [('vl', u)])
                    slot = ji % 4
                    bank, half = slot // 2, slot % 2
                    pa = self.PS[:, bank, half * 256:(half + 1) * 256]
                    for kc in range(8):
                        self.mm(pa, utl[u][:, kc, :], h2T[:, kc, :], kc == 0, kc == 7, [('utl', u), 'h2T'], [('ps', bank, half)])
                    gaa = ga[ji % 2]
                    self.act(gaa[:], pa, AF.Gelu, [('ps', bank, half)], [('ga', ji % 2)])
                    w = wT[u]
                    self.tt('dve', w[:], gaa[:], Gs[:, j, :], ALU.mult, [('ga', ji % 2)] + gskeys, [('wT', u)])
                    for ti in range(2):
                        for hf in range(2):
                            self.mm(self.PS[:, 4 + 2 * ti + hf, :], w[:, ti * 128:(ti + 1) * 128], vl[u][:, hf * 512:(hf + 1) * 512],
                                    j == 0, j == 127, [('wT', u), ('vl', u)], [('ps', 4 + 2 * ti + hf)])
                    ji += 1
                for ti in range(2):
                    tg = ch * 2 + ti
                    xt = nb['xt'][xpar[ti]]
                    xk = ('xt', xpar[ti])
                    for hf in range(2):
                        self.tt('dve', etmp[:, hf * 512:(hf + 1) * 512], self.PS[:, 4 + 2 * ti + hf, :], g2[:, hf * 512:(hf + 1) * 512],
                                ALU.mult, [('ps', 4 + 2 * ti + hf), 'g2'], [('etmp', hf)])
                    self.tt('pool', etmp[:], etmp[:], xt[:], ALU.add, [('etmp', 0), ('etmp', 1), xk], [('etmp', 0), ('etmp', 1)])
                    ek = [('etmp', 0), ('etmp', 1)]
                    if not last:
                        S.dma('pool', self.xs[b, tg * 128:(tg + 1) * 128, :], etmp[:], ek, [('xs', b, tg)])
                    else:
                        ss = nb['ss']
                        self.act(nb['junk'][:], etmp[:], AF.Square, ek, ['junk', 'fss'], accum_out=ss[:, 0:1])
                        self.act(ss[:, 2:3], ss[:, 0:1], AF.Sqrt, ['fss', 'eps'], ['frs'], scale=1.0 / D, bias=self.epsT[:, 0:1])
                        S.op('dve', lambda e, ss=ss: e.reciprocal(out=ss[:, 2:3], in_=ss[:, 2:3]), ['frs'], ['frs'])
                        self.stt('dve', etmp[:], etmp[:], ss[:, 2:3], fing[:], ALU.mult, ALU.mult, ek + ['frs', 'fing'], ek)
                        S.dma('pool', self.out[b, tg * 128:(tg + 1) * 128, :], etmp[:], ek, [('out', b, tg)])
        S.barrier()
        A.release(mk)

    Builder.topk16 = topk16
    Builder.phase_peer_prep = phase_peer_prep
    Builder.phase_peer = phase_peer


_peer_methods()


LAM_INIT1 = 0.8 - 0.6 * math.exp(-0.3 * 1)


def _layer1_methods():
    def phase_proj1(self, l=1):
        S, A = self.S, self.A
        mk = A.mark()
        wA = A.alloc([128, 8, 3072], BF16, 'wA')
        m2 = self.load_w_bf16(wA, self.cd_w, 1952, 'wA', piece=488)
        S.barrier()
        A.release(m2)
        self.make_rot(wA, 416, 1952, 16, 16, 'wA')
        self.make_rot(wA, 928, 2464, 16, 16, 'wA')
        self.make_rot(wA, 384, 2976, 2, 8, 'wA')
        Wg = A.alloc([128, 2, 768], BF16, 'Wg')
        Wgr = A.alloc([128, 2, 768], BF16, 'Wgr')
        Wkv = A.alloc([128, 1024], BF16, 'Wkv')
        onesf = A.alloc([128, 128], BF16, 'onesf')
        S.op('pool', lambda e: e.memset(onesf[:], 1.0), [], ['onesf'])
        m3 = A.mark()
        wst = A.alloc([128, 1024], F32, 'w1st')
        gq = A.alloc([128, 2], F32, 'gq')
        gkv = A.alloc([128, 1], F32, 'gkv')
        for c in range(2):
            S.dma('sp', gq[:, c:c + 1], self.qn_g[c * 128:(c + 1) * 128, :], [], ['gq'])
        S.dma('sp', gkv[:], self.kvn_g, [], ['gkv'])
        for c in range(2):
            S.dma('sp', wst[:, 0:768], self.w_uq[c * 128:(c + 1) * 128, :], ['w1st'], ['w1st'])
            self.ts('dve', Wg[:, c, :], wst[:, 0:768], gq[:, c:c + 1], None, ALU.mult, None, ['w1st', 'gq'], ['Wg'])
        S.op('pool', lambda e: e.memset(Wgr[:], 0.0), [], ['Wgr'])
        for c in range(2):
            s = Wg[:, c, :].rearrange("p (h f) -> p h f", f=96)[:, :, 64:96].rearrange("p h (b t e) -> p h b t e", b=2, t=2)
            d = Wgr[:, c, :].rearrange("p (h f) -> p h f", f=96)[:, :, 64:96].rearrange("p h (b t e) -> p h b t e", b=2, t=2)
            for bb in range(2):
                S.op('act', lambda e, s=s, d=d, bb=bb: e.mul(out=d[:, :, bb, 0, :], in_=s[:, :, bb, 1, :], mul=-1.0), ['Wg', 'Wgr'], ['Wgr'])
                self.cp('pool', d[:, :, bb, 1, :], s[:, :, bb, 0, :], ['Wg', 'Wgr'], ['Wgr'])
        S.dma('sp', wst[:], self.w_ukv, ['w1st'], ['w1st'])
        self.ts('dve', Wkv[:], wst[:], gkv[:, 0:1], None, ALU.mult, None, ['w1st', 'gkv'], ['Wkv'])
        S.barrier()
        A.release(m3)
        if self.sub == 'w':
            return
        for b in range(NB):
            self.proj_batch1(l, b, wA, Wg, Wgr, Wkv, onesf)
            if self.sub is not None:
                break
        S.barrier()
        A.release(mk)

    def proj_batch1(self, l, b, wA, Wg, Wgr, Wkv, onesf):
        S, A = self.S, self.A
        mk = A.mark()
        Gb, SHb = self.load_norm_mod(l, b, 1, 'b')
        Gc, SHc = self.load_norm_mod(l, 2, 1, 'c')
        nb = self.alloc_norm_bufs()
        hTs = [A.alloc([128, 8, 512], BF16, 'hT') for _ in range(2)]
        stg = [A.alloc([128, 512], BF16, 'stg') for _ in range(3)]
        t1 = A.alloc([128, 512], F32, 't1')
        t2 = A.alloc([128, 512], F32, 't2')
        rC = A.alloc([128, 512], F32, 'rC')
        rS = A.alloc([128, 512], F32, 'rS')
        rCm = A.alloc([128, 512], F32, 'rCm')
        rSm = A.alloc([128, 512], F32, 'rSm')
        rCk = A.alloc([128, 512], F32, 'rCk')
        rSk = A.alloc([128, 512], F32, 'rSk')
        cqb = A.alloc([128, 2, 512], BF16, 'cqb')
        sq = A.alloc([128, 512], BF16, 'sq')
        rq = A.alloc([128, 512], F32, 'rq')
        rkv = A.alloc([128, 512], F32, 'rkv')
        ckvf = A.alloc([128, 512], F32, 'ckvf')
        ckvn = A.alloc([128, 512], BF16, 'ckvn')
        krs = A.alloc([128, 512], BF16, 'krs')
        VW = 1036
        vst = [A.alloc([128, VW], BF16, 'vst') for _ in range(2)]
        for v in vst:
            S.op('pool', lambda e, v=v: e.memset(v[:], 1.0), [], [('vst', 0), ('vst', 1)])
        si = 0
        pi = 0
        for ci in range(9):
            ntile = 4 if ci < 8 else 2
            n = ntile * 128
            t0 = ci * 512
            hT = hTs[ci % 2]
            hkey = ('hT', ci % 2)
            G, SH, gk = (Gb, SHb, ['Gb', 'SHb']) if ci < 8 else (Gc, SHc, ['Gc', 'SHc'])
            for ti in range(ntile):
                tg = ci * 4 + ti
                self.norm_tile(nb, self.src(l, b, tg), G, SH, gk, hT[:, :, ti * 128:(ti + 1) * 128], hkey, 6)
            S.dma('sp', rC[:, 0:n], self.ropeC[:, t0:t0 + n], [], ['rC'])
            S.dma('sp', rS[:, 0:n], self.ropeS[:, t0:t0 + n], [], ['rS'])
            S.dma('sp', rCm[0:96, 0:n], self.ropeCm[:, t0:t0 + n], [], ['rCm'])
            S.dma('sp', rSm[0:96, 0:n], self.ropeSm[:, t0:t0 + n], [], ['rSm'])
            S.dma('sp', rCk[0:32, 0:n], self.ropeCm[64:96, t0:t0 + n], [], ['rCk'])
            S.dma('sp', rSk[0:32, 0:n], self.ropeSm[64:96, t0:t0 + n], [], ['rSk'])

            def proj(bank, col0, m, nn=n, hT=hT, hkey=hkey):
                for kc in range(8):
                    self.mm(self.PS[0:m, bank, 0:nn], wA[:, kc, col0:col0 + m], hT[:, kc, 0:nn], kc == 0, kc == 7,
                            ['wA', hkey], [('ps', bank)])
            if self.sub == 'c0':
                break
            for c in range(2):
                proj(c, 128 * c, 128)
                self.cp('dve', cqb[:, c, 0:n], self.PS[:, c, 0:n], [('ps', c)], [('cqb', c)])
                if self.sub == 'c0a':
                    continue
                self.cp('dve', ckvf[:, 0:n], self.PS[:, c, 0:n], [('ps', c)], ['ckvf'])
                self.act(sq[:, 0:n], ckvf[:, 0:n], AF.Square, ['ckvf'], ['sq'])
                self.mm(self.PS[:, 7, 0:n], onesf[:], sq[:, 0:n], c == 0, c == 1, ['onesf', 'sq'], [('ps', 7)])
            if self.sub != 'c0a':
                self.cp('dve', rq[:, 0:n], self.PS[:, 7, 0:n], [('ps', 7)], ['rq'])
                self.act(rq[:, 0:n], rq[:, 0:n], AF.Sqrt, ['rq', 'eps'], ['rq'], scale=1.0 / 256, bias=self.epsT[:, 0:1])
                S.op('dve', lambda e, n=n: e.reciprocal(out=rq[:, 0:n], in_=rq[:, 0:n]), ['rq'], ['rq'])
            if self.sub == 'c0a':
                break
            if self.sub == 'c0b':
                break
            proj(2, 256, 128)
            self.cp('dve', ckvf[:, 0:n], self.PS[:, 2, 0:n], [('ps', 2)], ['ckvf'])
            self.act(sq[:, 0:n], ckvf[:, 0:n], AF.Square, ['ckvf'], ['sq'])
            self.mm(self.PS[:, 7, 0:n], onesf[:], sq[:, 0:n], True, True, ['onesf', 'sq'], [('ps', 7)])
            self.cp('dve', rkv[:, 0:n], self.PS[:, 7, 0:n], [('ps', 7)], ['rkv'])
            self.act(rkv[:, 0:n], rkv[:, 0:n], AF.Sqrt, ['rkv', 'eps'], ['rkv'], scale=1.0 / 128, bias=self.epsT[:, 0:1])
            S.op('dve', lambda e, n=n: e.reciprocal(out=rkv[:, 0:n], in_=rkv[:, 0:n]), ['rkv'], ['rkv'])
            self.tt('dve', ckvn[:, 0:n], ckvf[:, 0:n], rkv[:, 0:n], ALU.mult, ['ckvf', 'rkv'], ['ckvn'])
            if self.sub == 'c1':
                break
            proj(0, 384, 32)
            proj(2, 2976, 32)
            self.tt('dve', t1[0:32, 0:n], self.PS[0:32, 0, 0:n], rCk[0:32, 0:n], ALU.mult, [('ps', 0), 'rCk'], ['t1'])
            self.tt('dve', t2[0:32, 0:n], self.PS[0:32, 2, 0:n], rSk[0:32, 0:n], ALU.mult, [('ps', 2), 'rSk'], ['t2'])
            self.tt('pool', krs[0:32, 0:n], t1[0:32, 0:n], t2[0:32, 0:n], ALU.add, ['t1', 't2'], ['krs'])
            for h in range(8):
                S.dma('pool', self.KT[b, h, 64:96, t0:t0 + n], krs[0:32, 0:n], ['krs'], [('K', b, h, 1)])
            if self.sub == 'c2':
                break
            for h in range(8):
                pa = pi % 2
                pi += 1
                for c in range(2):
                    self.mm(self.PS[0:96, pa, 0:n], Wg[:, c, h * 96:(h + 1) * 96], cqb[:, c, 0:n], c == 0, c == 1,
                            ['Wg', ('cqb', 0), ('cqb', 1)], [('ps', pa)])
                for c in range(2):
                    self.mm(self.PS[0:96, 2 + pa, 0:n], Wgr[:, c, h * 96:(h + 1) * 96], cqb[:, c, 0:n], c == 0, c == 1,
                            ['Wgr', ('cqb', 0), ('cqb', 1)], [('ps', 2 + pa)])
                sg = stg[si % 3]
                skey = ('stg', si % 3)
                si += 1
                self.tt('dve', t1[0:96, 0:n], self.PS[0:96, pa, 0:n], rCm[0:96, 0:n], ALU.mult, [('ps', pa), 'rCm'], ['t1'])
                self.tt('dve', t2[0:96, 0:n], self.PS[0:96, 2 + pa, 0:n], rSm[0:96, 0:n], ALU.mult, [('ps', 2 + pa), 'rSm'], ['t2'])
                self.tt('pool', t1[0:96, 0:n], t1[0:96, 0:n], t2[0:96, 0:n], ALU.add, ['t1', 't2'], ['t1'])
                self.tt('pool', sg[0:96, 0:n], t1[0:96, 0:n], rq[0:96, 0:n], ALU.mult, ['t1', 'rq'], [skey])
                S.dma('pool', self.QT[b, h, 0:96, t0:t0 + n], sg[0:96, 0:n], [skey], [('Q', b, h)])
                pa = pi % 2
                pi += 1
                self.mm(self.PS[0:64, pa, 0:n], Wkv[:, h * 128:h * 128 + 64], ckvn[:, 0:n], True, True, ['Wkv', 'ckvn'], [('ps', pa)])
                sg = stg[si % 3]
                skey = ('stg', si % 3)
                si += 1
                self.cp('act', sg[0:64, 0:n], self.PS[0:64, pa, 0:n], [('ps', pa)], [skey])
                S.dma('pool', self.KT[b, h, 0:64, t0:t0 + n], sg[0:64, 0:n], [skey], [('K', b, h, 0)])
            if self.sub == 'c3':
                break
            for (dst, idx, col0, rot0) in ([('Q', 8 + d, 416 + 128 * d, 1952 + 128 * d) for d in range(4)]
                                           + [('K', 8 + d, 928 + 128 * d, 2464 + 128 * d) for d in range(4)]):
                pa = pi % 2
                pi += 1
                proj(pa, col0, 128)
                proj(2 + pa, rot0, 128)
                sg = stg[si % 3]
                skey = ('stg', si % 3)
                si += 1
                self.tt('dve', t1[:, 0:n], self.PS[:, pa, 0:n], rC[:, 0:n], ALU.mult, [('ps', pa), 'rC'], ['t1'])
                self.tt('dve', t2[:, 0:n], self.PS[:, 2 + pa, 0:n], rS[:, 0:n], ALU.mult, [('ps', 2 + pa), 'rS'], ['t2'])
                self.tt('pool', sg[:, 0:n], t1[:, 0:n], t2[:, 0:n], ALU.add, ['t1', 't2'], [skey])
                dram = self.QT if dst == 'Q' else self.KT
                S.dma('pool', dram[b, idx, :, t0:t0 + n], sg[:, 0:n], [skey], [(dst, b, idx)])
            if self.sub == 'c4':
                break
            for ti in range(ntile):
                tg = ci * 4 + ti
                vs = vst[tg % 2]
                vkey = ('vst', tg % 2)
                self.mm(self.PS[:, 4, :], ckvn[:, ti * 128:(ti + 1) * 128],
                        Wkv[:].rearrange("p (h f) -> p h f", f=128)[:, :, 64:128], True, True, ['ckvn', 'Wkv'], [('ps', 4)])
                self.cp('act', vs[:, 0:520].rearrange("p (h d) -> p h d", d=65)[:, :, 0:64],
                        self.PS[:, 4, :].rearrange("p (h d) -> p h d", d=64), [('ps', 4)], [vkey])
                for kc in range(8):
                    self.mm(self.PS[:, 5, :], hT[:, kc, ti * 128:(ti + 1) * 128], wA[:, kc, 1440:1952], kc == 0, kc == 7,
                            ['wA', hkey], [('ps', 5)])
                self.cp('act', vs[:, 520:1036].rearrange("p (h d) -> p h d", d=129)[:, :, 0:128],
                        self.PS[:, 5, :].rearrange("p (h d) -> p h d", d=128), [('ps', 5)], [vkey])
                S.dma('pool', self.V[b, tg * 128:(tg + 1) * 128, 0:VW], vs[:], [vkey], [('V', b)])
            if self.sub == 'c5':
                break
        S.barrier()
        A.release(mk)

    def phase_attn1(self):
        S, A = self.S, self.A
        mk = A.mark()
        self.attn_setup()
        lam = A.alloc([128, 8], F32, 'lam')
        dl = A.alloc([128, 256], F32, 'dl')
        self.load_row_bc(dl[:], self.dlam[0:1, :], 'dl')
        dl4 = dl[:].rearrange("p (a d) -> p a d", a=4)
        self.tt('dve', dl4[:, 0, :], dl4[:, 0, :], dl4[:, 1, :], ALU.mult, ['dl'], ['dl'])
        self.tt('dve', dl4[:, 2, :], dl4[:, 2, :], dl4[:, 3, :], ALU.mult, ['dl'], ['dl'])
        S.op('dve', lambda e: e.tensor_reduce(out=lam[:, 0:1], in_=dl4[:, 0, :], axis=AX.X, op=ALU.add), ['dl'], ['lam'])
        S.op('dve', lambda e: e.tensor_reduce(out=lam[:, 1:2], in_=dl4[:, 2, :], axis=AX.X, op=ALU.add), ['dl', 'lam'], ['lam'])
        self.act(lam[:, 0:2], lam[:, 0:2], AF.Exp, ['lam'], ['lam'])
        self.tt('dve', lam[:, 2:3], lam[:, 1:2], lam[:, 0:1], ALU.subtract, ['lam'], ['lam'])
        self.ts('dve', lam[:, 3:4], lam[:, 2:3], -LAM_INIT1, None, ALU.add, None, ['lam'], ['lam'])
        subg = A.alloc([128, 128], F32, 'subg')
        self.load_row_bc(subg[:], self.subln[0:1, :], 'subg')
        self.ts('dve', subg[:], subg[:], 1.0 - LAM_INIT1, None, ALU.mult, None, ['subg'], ['subg'])
        Qs = [A.alloc([128, SL], BF16, 'Qs') for _ in range(2)]
        Ks = [A.alloc([128, SA], BF16, 'Ks') for _ in range(2)]
        Vsl = [A.alloc([128, NT, 129], BF16, 'Vsl') for _ in range(2)]
        ystg = [A.alloc([128, 128], BF16, 'ystg') for _ in range(4)]
        o1 = [A.alloc([128, 128], F32, 'o1') for _ in range(4)]
        o2 = [A.alloc([128, 128], F32, 'o2') for _ in range(2)]
        oj = A.alloc([128, 128], BF16, 'oj')
        oss = A.alloc([128, 8], F32, 'oss')
        gi = 0
        yi = 0
        for b in range(NB):
            for grp in range(12):
                p = gi % 2
                gi += 1
                mla = grp < 8
                rows = 96 if mla else 128
                dv = 64 if mla else 128
                vcol0 = grp * 65 if mla else 520 + (grp - 8) * 129
                S.dma('sp', Qs[p][0:rows, :], self.QT[b, grp, 0:rows, 0:SL], [('Q', b, grp)], [('Qs', p)])
                S.dma('sp', Ks[p][0:rows, :], self.KT[b, grp, 0:rows, :],
                      [('K', b, grp), ('K', b, grp, 0), ('K', b, grp, 1)], [('Ks', p)])
                S.dma('sp', Vsl[p][:, :, 0:dv + 1],
                      self.V[b, :, vcol0:vcol0 + dv + 1].rearrange("(t p) c -> p t c", p=128), [('V', b)], [('Vs', p)])
                for qc in range(8):
                    if mla:
                        kts = [(Ks[p][0:96, kt * 128:(kt + 1) * 128], Vsl[p][:, kt, 0:65], None, [('Ks', p), ('Vs', p)])
                               for kt in range(NT)]

                        def out_fn(qs, acc, rden, keys, qc=qc, grp=grp, b=b):
                            nonlocal yi
                            ys = ystg[yi % 4]
                            ykey = ('ystg', yi % 4)
                            yi += 1
                            self.ts('dve', ys[:, 0:64], acc[:, 0:64], rden, None, ALU.mult, None, keys, [ykey])
                            tg = qc * 4 + qs
                            S.dma('pool', self.Y[b, tg * 128:(tg + 1) * 128, grp * 64:(grp + 1) * 64], ys[:, 0:64], [ykey], [('Y', b)])
                        self.attn_unit(Qs[p][0:96, qc * 512:(qc + 1) * 512], 512, kts, 64, 96 ** -0.5, [('Qs', p)], out_fn)
                    else:
                        d = grp - 8
                        for w in range(2):
                            kts = [(Ks[p][w * 64:(w + 1) * 64, kt * 128:(kt + 1) * 128], Vsl[p][:, kt, 0:129], None,
                                    [('Ks', p), ('Vs', p)]) for kt in range(NT)]

                            def out_fn(qs, acc, rden, keys, qc=qc, d=d, b=b, w=w):
                                nonlocal yi
                                if w == 0:
                                    self.ts('dve', o1[qs][:], acc[:, 0:128], rden, None, ALU.mult, None, keys, [('o1', qs)])
                                    return
                                oo = o2[qs % 2]
                                ok = ('o2', qs % 2)
                                self.ts('dve', oo[:], acc[:, 0:128], rden, None, ALU.mult, None, keys, [ok])
                                self.stt('dve', oo[:], oo[:], lam[:, 3:4], o1[qs][:], ALU.mult, ALU.add, [ok, ('o1', qs), 'lam'], [ok])
                                sk = ('oss', qs % 2)
                                c0 = qs % 2
                                self.act(oj[:], oo[:], AF.Square, [ok], ['oj', sk], accum_out=oss[:, c0:c0 + 1])
                                self.act(oss[:, 2 + c0:3 + c0], oss[:, c0:c0 + 1], AF.Sqrt, [sk, 'eps'], [sk], scale=1.0 / 128,
                                         bias=self.epsT[:, 0:1])
                                S.op('dve', lambda e, c0=c0: e.reciprocal(out=oss[:, 2 + c0:3 + c0], in_=oss[:, 2 + c0:3 + c0]), [sk], [sk])
                                ys = ystg[yi % 4]
                                ykey = ('ystg', yi % 4)
                                yi += 1
                                self.stt('dve', ys[:], oo[:], oss[:, 2 + c0:3 + c0], subg[:], ALU.mult, ALU.mult, [ok, sk, 'subg'], [ykey])
                                tg = qc * 4 + qs
                                S.dma('pool', self.Y[b, tg * 128:(tg + 1) * 128, 512 + d * 128:512 + (d + 1) * 128], ys[:], [ykey],
                                      [('Y', b)])
                            self.attn_unit(Qs[p][w * 64:(w + 1) * 64, qc * 512:(qc + 1) * 512], 512, kts, 128, 0.125, [('Qs', p)], out_fn)
        S.barrier()
        A.release(mk)

    Builder.phase_proj1 = phase_proj1
    Builder.proj_batch1 = proj_batch1
    Builder.phase_attn1 = phase_attn1


_layer1_methods()
```

```python
import math
import numpy as np
from contextlib import ExitStack
import concourse.bass as bass
import concourse.mybir as mybir
from concourse.bass_utils import run_bass_kernel_spmd

F32 = mybir.dt.float32
BF16 = mybir.dt.bfloat16
U32 = mybir.dt.uint32
AF = mybir.ActivationFunctionType
ALU = mybir.AluOpType
AX = mybir.AxisListType

NB = 2
D = 1024
SL = 4096
CT = 256
SA = 4352
NT = 34
EPS = 1e-6
NEG = -30000.0
SEM_ROT = 30000
N_CORES = 8


class Sched:
    ENGS = ('pe', 'act', 'dve', 'pool', 'sp')

    def __init__(self, nc, stack, n_lanes=28, same_engine_sync=True):
        self.nc = nc
        self.stack = stack
        self.prog = {e: [] for e in self.ENGS}
        self.cnt = {e: 0 for e in self.ENGS}
        self.esems = {e: [] for e in self.ENGS}
        self.seen = {e: {} for e in self.ENGS}
        self.res = {}
        self.lanes = []
        for i in range(n_lanes):
            s = stack.enter_context(nc.semaphore(f"lane{i}"))
            self.lanes.append([s, 0])
        self.lane_rr = 0
        self.same_engine_sync = same_engine_sync

    def _esem(self, e, n):
        k = (n - 1) // SEM_ROT
        while len(self.esems[e]) <= k:
            s = self.stack.enter_context(self.nc.semaphore(f"es_{e}_{len(self.esems[e])}"))
            self.esems[e].append(s)
        return self.esems[e][k], n - k * SEM_ROT

    def _deps(self, reads, writes):
        deps = []
        for r in reads:
            st = self.res.get(r)
            if st is not None and st['w'] is not None:
                deps.append(st['w'])
        for w in writes:
            st = self.res.get(w)
            if st is not None:
                if st['w'] is not None:
                    deps.append(st['w'])
                deps.extend(st['r'].values())
        return deps

    def _commit(self, ev, reads, writes):
        for r in reads:
            st = self.res.setdefault(r, {'w': None, 'r': {}})
            k = id(ev[1])
            if k not in st['r'] or st['r'][k][2] < ev[2]:
                st['r'][k] = ev
        for w in writes:
            self.res[w] = {'w': ev, 'r': {}}

    def _add_waits(self, e, deps):
        best = {}
        for (eng_src, sem, val) in deps:
            if eng_src == e and (e == 'pe' or not self.same_engine_sync):
                continue
            key = id(sem)
            if key not in best or best[key][1] < val:
                best[key] = (sem, val)
        for key, (sem, val) in best.items():
            if self.seen[e].get(key, 0) >= val:
                continue
            self.seen[e][key] = val
            self.prog[e].append(('wait', sem, val))

    def op(self, e, fn, reads=(), writes=()):
        deps = self._deps(reads, writes)
        self._add_waits(e, deps)
        self.cnt[e] += 1
        sem, val = self._esem(e, self.cnt[e])
        self.prog[e].append(('op', fn, sem, 1))
        ev = (e, sem, val)
        self._commit(ev, reads, writes)
        return ev

    def dma(self, q, out, in_, reads=(), writes=(), **kw):
        deps = self._deps(reads, writes)
        lane = self.lanes[self.lane_rr]
        self.lane_rr = (self.lane_rr + 1) % len(self.lanes)
        if lane[1] > 0:
            deps.append(('dma', lane[0], 16 * lane[1]))
        self._add_waits(q, deps)
        lane[1] += 1
        sem = lane[0]

        def fn(eng, out=out, in_=in_, kw=kw):
            return eng.dma_start(out=out, in_=in_, **kw)
        self.prog[q].append(('op', fn, sem, 16))
        ev = ('dma', sem, 16 * lane[1])
        self._commit(ev, reads, writes)
        return ev

    def barrier(self):
        evs = []
        for e in self.ENGS:
            if self.cnt[e] > 0:
                sem, val = self._esem(e, self.cnt[e])
                evs.append((e + '_b', sem, val))
        for lane in self.lanes:
            if lane[1] > 0:
                evs.append(('dma', lane[0], 16 * lane[1]))
        for e in self.ENGS:
            self._add_waits(e, evs)
        self.res = {}

    def emit(self):
        nc = self.nc
        engobj = {'pe': 'tensor', 'act': 'scalar', 'dve': 'vector', 'pool': 'gpsimd', 'sp': 'sync'}
        with nc.Block() as block:
            for e in self.ENGS:
                items = self.prog[e]
                if not items:
                    continue

                def body(eng, items=items):
                    for it in items:
                        if it[0] == 'wait':
                            eng.wait_ge(it[1], it[2])
                        else:
                            ins = it[1](eng)
                            ins.then_inc(it[2], it[3])
                getattr(block, engobj[e])(body)


class Arena:
    def __init__(self, nc, limit=229300):
        self.nc = nc
        self.off = 17408
        self.n = 0
        self.limit = limit

    def alloc(self, shape, dtype, name='t'):
        isz = 4 if dtype in (F32, U32) else 2
        nbytes = int(np.prod(shape[1:])) * isz
        nbytes = (nbytes + 63) // 64 * 64
        self.n += 1
        t = self.nc.alloc_sbuf_tensor_at(f"{name}_{self.n}", list(shape), dtype, offset=self.off)
        self.off += nbytes
        assert self.off <= self.limit, (name, self.off)
        return t

    def mark(self):
        return self.off

    def release(self, m):
        self.off = m


class Builder:
    def __init__(self, stop_after=None, skip_l0=False, sub=None):
        self.stop_after = stop_after
        self.skip_l0 = skip_l0
        self.sub = sub
        self.nc = nc = bass.Bass("TRN2", target_bir_lowering=False)
        self.stack = ExitStack()
        self.S = Sched(nc, self.stack)
        self.A = Arena(nc)
        di = lambda n, sh, dt=F32: nc.dram_tensor(n, list(sh), dt, kind="ExternalInput").ap()
        self.x = di("x", [NB, SL, D])
        self.ctx = di("ctx", [NB, CT, D])
        self.cc = di("cc", [3, D])
        self.ada_w = di("ada_w", [2, D, 6 * D])
        self.ada_b = di("ada_b", [2, 6 * D])
        self.norm1_g = di("norm1_g", [2, D])
        self.norm2_g = di("norm2_g", [2, D])
        self.w_out = di("w_out", [2, D, D])
        self.peer_wq = di("peer_wq", [2, D, 2048])
        self.peer_keys = di("peer_keys", [2, 2, 128, 128])
        self.peer_u = di("peer_u", [2, 16384, D])
        self.peer_v = di("peer_v", [2, 16384, D])
        self.ab_w = di("ab_w", [D, 2304])
        self.rpbT = di("rpbT", [128, 7680])
        self.maskI = di("maskI", [128, 7680])
        self.maskE = di("maskE", [128, 7680])
        self.swaL = di("swaL", [128, 128])
        self.swaU = di("swaU", [128, 128])
        self.sink = di("sink", [1, 8])
        self.cd_w = di("cd_w", [D, 1952])
        self.qn_g = di("qn_g", [256, 1])
        self.w_uq = di("w_uq", [256, 768])
        self.kvn_g = di("kvn_g", [128, 1])
        self.w_ukv = di("w_ukv", [128, 1024])
        self.dlam = di("dlam", [1, 256])
        self.subln = di("subln", [1, 128])
        self.fin_g = di("fin_g", [1, D])
        self.ropeC = di("ropeC", [128, SA])
        self.ropeS = di("ropeS", [128, SA])
        self.ropeCm = di("ropeCm", [96, SA])
        self.ropeSm = di("ropeSm", [96, SA])
        self.iota_in = di("iota_in", [128, 128])
        self.out = nc.dram_tensor("out", [NB, SL, D], F32, kind="ExternalOutput").ap()
        if stop_after is not None:
            self.dbg = nc.dram_tensor("dbg", [NB, SA, D], F32, kind="ExternalOutput").ap()
        ds = lambda n, sh, dt: nc.dram_tensor(n, list(sh), dt).ap()
        self.xs = ds("xs", [NB, SA, D], F32)
        self.mod = ds("mod", [2, 3, 6 * D], F32)
        self.QT = ds("QT", [NB, 12, 128, SA], BF16)
        self.KT = ds("KT", [NB, 12, 128, SA], BF16)
        self.V = ds("V", [NB, SA, 1040], BF16)
        self.Y = ds("Y", [NB, SA, D], BF16)
        self.UTs = ds("UTs", [128, 128, D], BF16)
        self.Vs = ds("Vs", [128, 128, D], BF16)
        self.WQs = ds("WQs", [4, 128, 8 * 512], BF16)
        self.PS = nc.alloc_psum_tensor("psall", [128, 8, 512], F32)

    def act(self, out, in_, func, r, w, **kw):
        self.S.op('act', lambda e: e.activation(out=out, in_=in_, func=func, **kw), r, w)

    def mm(self, out, lhsT, rhs, start, stop, r, w):
        self.S.op('pe', lambda e: e.matmul(out, lhsT=lhsT, rhs=rhs, start=start, stop=stop), r, w)

    def tr(self, out, in_, r, w, f32=False):
        idn = self.identf if f32 else self.ident
        self.S.op('pe', lambda e: e.transpose(out=out, in_=in_, identity=idn[:]), r, w)

    def tt(self, eng, out, in0, in1, op, r, w):
        self.S.op(eng, lambda e: e.tensor_tensor(out=out, in0=in0, in1=in1, op=op), r, w)

    def ts(self, eng, out, in0, s1, s2, op0, op1, r, w):
        if op1 is None:
            self.S.op(eng, lambda e: e.tensor_scalar(out=out, in0=in0, scalar1=s1, scalar2=None, op0=op0), r, w)
        else:
            self.S.op(eng, lambda e: e.tensor_scalar(out=out, in0=in0, scalar1=s1, scalar2=s2, op0=op0, op1=op1), r, w)

    def stt(self, eng, out, in0, scalar, in1, op0, op1, r, w):
        self.S.op(eng, lambda e: e.scalar_tensor_tensor(out=out, in0=in0, scalar=scalar, in1=in1, op0=op0, op1=op1), r, w)

    def cp(self, eng, out, in_, r, w):
        if eng == 'act':
            self.S.op('act', lambda e: e.copy(out=out, in_=in_), r, w)
        else:
            self.S.op(eng, lambda e: e.tensor_copy(out=out, in_=in_), r, w)

    def ps(self, bank, n=512):
        return self.PS[:, bank, 0:n]

    def psbf(self, bank):
        return self.PS[:, bank, :].bitcast(BF16)

    def src(self, layer, b, tg):
        if layer == 0:
            if tg < 32:
                return self.x[b, tg * 128:(tg + 1) * 128, :]
            return self.ctx[b, (tg - 32) * 128:(tg - 31) * 128, :]
        return self.xs[b, tg * 128:(tg + 1) * 128, :]

    def setup_consts(self):
        S, A = self.S, self.A
        self.ident = A.alloc([128, 128], BF16, 'ident')
        self.identf = A.alloc([128, 128], F32, 'identf')
        self.iota = A.alloc([128, 128], F32, 'iota')
        self.epsT = A.alloc([128, 1], F32, 'eps')
        self.sinkexp = A.alloc([128, 8], F32, 'sinkexp')
        self.mL = A.alloc([128, 128], BF16, 'mL')
        self.mU = A.alloc([128, 128], BF16, 'mU')
        identf, ident = self.identf, self.ident
        S.op('pool', lambda e: e.memset(identf[:], 0.0), [], ['identf'])
        S.op('pool', lambda e: e.affine_select(out=identf[:], in_=identf[:], pattern=[[-1, 128]],
                                               compare_op=ALU.not_equal, fill=1.0, base=0, channel_multiplier=1),
             ['identf'], ['identf'])
        self.cp('dve', ident[:], identf[:], ['identf'], ['ident'])
        S.op('pool', lambda e: e.memset(self.epsT[:], EPS), [], ['eps'])
        S.dma('sp', self.iota[:], self.iota_in, [], ['iota'])
        S.dma('sp', self.sinkexp[:], self.sink[0:1, :].partition_broadcast(128)[:, 0, :], [], ['sinkexp'])
        self.act(self.sinkexp[:], self.sinkexp[:], AF.Exp, ['sinkexp'], ['sinkexp'])
        mk = A.mark()
        t0 = A.alloc([128, 128], F32, 'mstg')
        for (m_in, m_sb, key) in ((self.swaL, self.mL, 'mL'), (self.swaU, self.mU, 'mU')):
            S.dma('sp', t0[:, 0:128], m_in, ['t0'], ['t0'])
            self.cp('dve', m_sb[:], t0[:, 0:128], ['t0'], [key])
        S.barrier()
        A.release(mk)

    def phase_mod(self, l):
        S, A = self.S, self.A
        mk = A.mark()
        ccT = A.alloc([128, 8, 3], F32, 'ccT')
        for kc in range(8):
            S.dma('sp', ccT[:, kc, :], self.cc[:, kc * 128:(kc + 1) * 128].rearrange("r p -> p r"), [], ['ccT'],
                  allow_slow_non_contiguous=True)
        self.act(ccT[:], ccT[:], AF.Silu, ['ccT'], ['ccT'])
        modsb = A.alloc([3, 6 * D], F32, 'modsb')
        adab = A.alloc([3, 6 * D], F32, 'adab')
        S.dma('sp', adab[:], self.ada_b[l:l + 1, :].partition_broadcast(3)[:, 0, :], [], ['adab'])
        wb = [A.alloc([128, 8, 512], F32, 'modw') for _ in range(2)]
        for n in range(12):
            w = wb[n % 2]
            S.dma('sp', w[:], self.ada_w[l, :, n * 512:(n + 1) * 512].rearrange("(kc p) n -> p kc n", p=128),
                  [], [('modw', n % 2)])
            for kc in range(8):
                self.mm(self.PS[0:3, n % 2, :], ccT[:, kc, :], w[:, kc, :], kc == 0, kc == 7,
                        ['ccT', ('modw', n % 2)], [('ps', n % 2)])
            self.tt('dve', modsb[:, n * 512:(n + 1) * 512], self.PS[0:3, n % 2, :], adab[:, n * 512:(n + 1) * 512],
                    ALU.add, [('ps', n % 2), 'adab'], ['modsb'])
        S.dma('sp', self.mod[l], modsb[:], ['modsb'], ['mod'])
        S.barrier()
        A.release(mk)

    def load_row_bc(self, dst, src_row, key):
        self.S.dma('sp', dst, src_row.partition_broadcast(128)[:, 0, :], [], [key])

    def load_norm_mod(self, l, row, which, tag):
        A = self.A
        G = A.alloc([128, D], F32, 'G' + tag)
        SH = A.alloc([128, D], F32, 'SH' + tag)
        tmp = A.alloc([128, D], F32, 'ng' + tag)
        sh_off = 0 if which == 1 else 3
        ng = self.norm1_g if which == 1 else self.norm2_g
        self.load_row_bc(G[:], self.mod[l, row:row + 1, (sh_off + 1) * D:(sh_off + 2) * D], 'G' + tag)
        self.load_row_bc(SH[:], self.mod[l, row:row + 1, sh_off * D:(sh_off + 1) * D], 'SH' + tag)
        self.load_row_bc(tmp[:], ng[l:l + 1, :], 'ng' + tag)
        self.stt('dve', G[:], G[:], 1.0, tmp[:], ALU.add, ALU.mult, ['G' + tag, 'ng' + tag], ['G' + tag])
        return G, SH

    def load_gate(self, l, row, which, tag):
        A = self.A
        g = A.alloc([128, D], F32, 'gate' + tag)
        off = 2 if which == 1 else 5
        self.load_row_bc(g[:], self.mod[l, row:row + 1, off * D:(off + 1) * D], 'gate' + tag)
        return g

    def alloc_norm_bufs(self, nxt=2):
        A = self.A
        nb = {}
        nb['xt'] = [A.alloc([128, D], F32, 'xt') for _ in range(nxt)]
        nb['tmp'] = A.alloc([128, D], F32, 'ntmp')
        nb['junk'] = nb['tmp'][:].bitcast(BF16)[:, 0:D]
        nb['hb'] = [A.alloc([128, D], BF16, 'hb') for _ in range(2)]
        nb['ss'] = A.alloc([128, 4], F32, 'ss')
        nb['i'] = 0
        return nb

    def norm_tile(self, nb, src_ap, G, SH, gk, hT_dst, hT_key, psbank, xt_keep=False):
        S = self.S
        px = nb['i'] % len(nb['xt'])
        p = nb['i'] % 2
        nb['i'] += 1
        xt, hb, ss = nb['xt'][px], nb['hb'][p], nb['ss']
        S.dma('sp', xt[:], src_ap, [], [('xt', px)])
        self.act(nb['junk'], xt[:], AF.Square, [('xt', px)], ['ntmp', ('ss', p)], accum_out=ss[:, p:p + 1])
        self.act(ss[:, 2 + p:3 + p], ss[:, p:p + 1], AF.Sqrt, [('ss', p), 'eps'], [('rs', p)], scale=1.0 / D,
                 bias=self.epsT[:, 0:1])
        S.op('dve', lambda e: e.reciprocal(out=ss[:, 2 + p:3 + p], in_=ss[:, 2 + p:3 + p]), [('rs', p)], [('rs', p)])
        self.stt('dve', nb['tmp'][:], xt[:], ss[:, 2 + p:3 + p], G[:], ALU.mult, ALU.mult,
                 [('xt', px), ('rs', p)] + gk, ['ntmp'])
        self.tt('pool', hb[:], nb['tmp'][:], SH[:], ALU.add, ['ntmp'] + gk, [('hb', p)])
        pst = self.psbf(psbank)
        for kc in range(8):
            self.tr(pst[:, kc * 128:(kc + 1) * 128], hb[:, kc * 128:(kc + 1) * 128], [('hb', p), 'ident'],
                    [('ps', psbank)])
        self.cp('act', hT_dst, pst.rearrange("p (k t) -> p k t", k=8), [('ps', psbank)], [hT_key])
        return px

    def load_w_bf16(self, dst, src, ncols, key, piece=512):
        S, A = self.S, self.A
        mk = A.mark()
        stg = [A.alloc([128, 8, piece], F32, 'wstg') for _ in range(2)]
        i = 0
        for c0 in range(0, ncols, piece):
            n = min(piece, ncols - c0)
            s = stg[i % 2]
            S.dma('sp', s[:, :, 0:n], src[:, c0:c0 + n].rearrange("(kc p) n -> p kc n", p=128), [], [('wstg', i % 2)])
            self.cp('dve' if i % 2 == 0 else 'act', dst[:, :, c0:c0 + n], s[:, :, 0:n], [('wstg', i % 2)], [key])
            i += 1
        return mk

    def make_rot(self, W, src0, dst0, nblk, half, key):
        for kc in range(8):
            s = W[:, kc, src0:src0 + nblk * 2 * half].rearrange("p (b t h) -> p b t h", t=2, h=half)
            d = W[:, kc, dst0:dst0 + nblk * 2 * half].rearrange("p (b t h) -> p b t h", t=2, h=half)
            self.S.op('act', lambda e, s=s, d=d: e.mul(out=d[:, :, 0, :], in_=s[:, :, 1, :], mul=-1.0), [key], [key])
            self.cp('pool', d[:, :, 1, :], s[:, :, 0, :], [key], [key])

    def attn_setup(self):
        A = self.A
        self.E = [A.alloc([128, 512], BF16, 'E') for _ in range(4)]
        self.ei = 0
        self.sti = 0
        self.acci = 0
        self.den = A.alloc([128, 8], F32, 'den')

    def attn_unit(self, qT, nq, ktiles, dv, scale, qkeys, out_fn, sink_col=None):
        S = self.S
        nqs = nq // 128
        aset = self.acci % 2
        self.acci += 1
        accs = []
        for qs in range(nqs):
            bank = 4 + 2 * aset + qs // 2
            sub = qs % 2
            accs.append((self.PS[:, bank, sub * 256:sub * 256 + dv + 1], ('ps', bank, sub)))
        nk = len(ktiles)
        LA = 2
        live = {}

        def qk(ki):
            kT, v, bias, kkeys = ktiles[ki]
            sb = self.sti % 4
            self.sti += 1
            st = self.PS[:, sb, 0:nq]
            self.mm(st, kT, qT, True, bias is None, qkeys + kkeys, [('ps', sb)])
            if bias is not None:
                self.mm(st, self.ident[:], bias[0], False, True, ['ident'] + bias[1], [('ps', sb)])
            eb = self.ei % 4
            self.ei += 1
            E = self.E[eb]
            self.act(E[:, 0:nq], st, AF.Exp, [('ps', sb)], [('E', eb)], scale=scale)
            live[ki] = (E, eb)

        def pv(ki):
            kT, v, bias, kkeys = ktiles[ki]
            E, eb = live.pop(ki)
            for qs in range(nqs):
                self.mm(accs[qs][0], E[:, qs * 128:(qs + 1) * 128], v, ki == 0, ki == nk - 1,
                        [('E', eb)] + kkeys, [accs[qs][1]])
        for step in range(nk + LA):
            if step < nk:
                qk(step)
            if step >= LA:
                pv(step - LA)
        for qs in range(nqs):
            acc, akey = accs[qs]
            dcol = self.den[:, (aset * 4 + qs):(aset * 4 + qs) + 1]
            dkey = ('den', aset * 4 + qs)
            if sink_col is not None:
                self.tt('dve', dcol, acc[:, dv:dv + 1], self.sinkexp[:, sink_col:sink_col + 1], ALU.add,
                        [akey, 'sinkexp'], [dkey])
                S.op('dve', lambda e, dcol=dcol: e.reciprocal(out=dcol, in_=dcol), [dkey], [dkey])
            else:
                S.op('dve', lambda e, dcol=dcol, acc=acc: e.reciprocal(out=dcol, in_=acc[:, dv:dv + 1]), [akey], [dkey])
            out_fn(qs, acc, dcol, [akey, dkey])

    def phase_proj0(self, l=0):
        S, A = self.S, self.A
        mk = A.mark()
        wA = A.alloc([128, 8, 3328], BF16, 'wA')
        m2 = self.load_w_bf16(wA, self.ab_w, 2304, 'wA', piece=576)
        S.barrier()
        A.release(m2)
        self.make_rot(wA, 1536, 2304, 16, 16, 'wA')
        for kc in range(8):
            for r in range(4):
                self.cp('dve', wA[:, kc, 2816 + r * 64:2880 + r * 64], wA[:, kc, 2048 + (r // 2) * 64:2112 + (r // 2) * 64],
                        ['wA'], ['wA'])
        self.make_rot(wA, 2816, 3072, 8, 16, 'wA')
        rC = A.alloc([128, SA], F32, 'rC')
        rS = A.alloc([128, SA], F32, 'rS')
        S.dma('sp', rC[:], self.ropeC, [], ['rC'])
        S.dma('sp', rS[:], self.ropeS, [], ['rS'])
        fm = ([('Q', i, 128 * i, None) for i in range(4)] + [('K', i, 512 + 128 * i, None) for i in range(4)]
              + [('Q', 4 + i, 1536 + 128 * i, 2304 + 128 * i) for i in range(4)]
              + [('K', 4 + i, 2816 + 128 * i, 3072 + 128 * i) for i in range(2)])
        tmv = [(1024, 512, 0, 8, 64), (2176, 128, 8, 2, 64)]
        for b in range(NB):
            self.proj_batch(l, b, wA, fm, tmv, 10, rC, rS, None)
        S.barrier()
        A.release(mk)

    def proj_batch(self, l, b, wA, fm, tmv, nvh, rC, rS, extra):
        S, A = self.S, self.A
        mk = A.mark()
        Gb, SHb = self.load_norm_mod(l, b, 1, 'b')
        Gc, SHc = self.load_norm_mod(l, 2, 1, 'c')
        nb = self.alloc_norm_bufs()
        hTs = [A.alloc([128, 8, 512], BF16, 'hT') for _ in range(2)]
        stg = [A.alloc([128, 512], BF16, 'stg') for _ in range(3)]
        t1 = A.alloc([128, 512], F32, 't1')
        t2 = A.alloc([128, 512], F32, 't2')
        vdv = tmv[0][4]
        vw = sum(nh * (dvv + 1) for (_, _, _, nh, dvv) in tmv)
        vst = [A.alloc([128, vw], BF16, 'vst') for _ in range(2)]
        for v in vst:
            S.op('pool', lambda e, v=v: e.memset(v[:], 1.0), [], [('vst', 0), ('vst', 1)])
        si = 0
        pi = 0
        for ci in range(9):
            ntile = 4 if ci < 8 else 2
            n = ntile * 128
            t0 = ci * 512
            hT = hTs[ci % 2]
            hkey = ('hT', ci % 2)
            G, SH, gk = (Gb, SHb, ['Gb', 'SHb']) if ci < 8 else (Gc, SHc, ['Gc', 'SHc'])
            for ti in range(ntile):
                tg = ci * 4 + ti
                self.norm_tile(nb, self.src(l, b, tg), G, SH, gk, hT[:, :, ti * 128:(ti + 1) * 128], hkey, 6)
            for (dst, idx, col0, rot0) in fm:
                pa = pi % 2
                pi += 1
                for kc in range(8):
                    self.mm(self.PS[:, pa, 0:n], wA[:, kc, col0:col0 + 128], hT[:, kc, 0:n], kc == 0, kc == 7,
                            ['wA', hkey], [('ps', pa)])
                sg = stg[si % 3]
                skey = ('stg', si % 3)
                si += 1
                if rot0 is None:
                    self.cp('act', sg[:, 0:n], self.PS[:, pa, 0:n], [('ps', pa)], [skey])
                else:
                    for kc in range(8):
                        self.mm(self.PS[:, 2 + pa, 0:n], wA[:, kc, rot0:rot0 + 128], hT[:, kc, 0:n], kc == 0, kc == 7,
                                ['wA', hkey], [('ps', 2 + pa)])
                    self.tt('dve', t1[:, 0:n], self.PS[:, pa, 0:n], rC[:, t0:t0 + n], ALU.mult, [('ps', pa), 'rC'], ['t1'])
                    self.tt('dve', t2[:, 0:n], self.PS[:, 2 + pa, 0:n], rS[:, t0:t0 + n], ALU.mult, [('ps', 2 + pa), 'rS'], ['t2'])
                    self.tt('pool', sg[:, 0:n], t1[:, 0:n], t2[:, 0:n], ALU.add, ['t1', 't2'], [skey])
                dram = self.QT if dst == 'Q' else self.KT
                S.dma('pool', dram[b, idx, :, t0:t0 + n], sg[:, 0:n], [skey], [(dst, b, idx)])
            if extra is not None:
                extra(b, ci, t0, n, hT, hkey)
            for ti in range(ntile):
                tg = ci * 4 + ti
                vs = vst[tg % 2]
                vkey = ('vst', tg % 2)
                co = 0
                for gi, (col0, ncols, h0, nh, dvv) in enumerate(tmv):
                    bank = 4 + gi
                    for kc in range(8):
                        self.mm(self.PS[:, bank, 0:ncols], hT[:, kc, ti * 128:(ti + 1) * 128], wA[:, kc, col0:col0 + ncols],
                                kc == 0, kc == 7, ['wA', hkey], [('ps', bank)])
                    ov = vs[:, co:co + nh * (dvv + 1)].rearrange("p (h d) -> p h d", d=dvv + 1)[:, :, 0:dvv]
                    self.cp('act', ov, self.PS[:, bank, 0:ncols].rearrange("p (h d) -> p h d", d=dvv), [('ps', bank)], [vkey])
                    co += nh * (dvv + 1)
                S.dma('pool', self.V[b, tg * 128:(tg + 1) * 128, 0:vw], vs[:], [vkey], [('V', b)])
        S.barrier()
        A.release(mk)

    def phase_attn0(self):
        S, A = self.S, self.A
        mk = A.mark()
        self.TabI = A.alloc([128, 7680], BF16, 'TabI')
        self.TabE = A.alloc([128, 7680], BF16, 'TabE')
        mk2 = A.mark()
        t0 = A.alloc([128, 7680], F32, 'tabstg0')
        t1 = A.alloc([128, 7680], F32, 'tabstg1')
        S.dma('sp', t0[:], self.rpbT, [], ['t0'])
        for (msk, Tab, key) in ((self.maskI, self.TabI, 'TabI'), (self.maskE, self.TabE, 'TabE')):
            S.dma('sp', t1[:], msk, [], ['t1'])
            self.tt('dve', t1[:], t1[:], t0[:], ALU.add, ['t0', 't1'], ['t1'])
            self.ts('dve', Tab[:], t1[:], 8.0, None, ALU.mult, None, ['t1'], [key])
        S.barrier()
        A.release(mk2)
        self.attn_setup()
        Qs = [A.alloc([128, SA], BF16, 'Qs') for _ in range(2)]
        Ks = [A.alloc([128, SA], BF16, 'Ks') for _ in range(2)]
        Vsl = [A.alloc([128, NT, 130], BF16, 'Vsl') for _ in range(2)]
        ystg = [A.alloc([128, 128], BF16, 'ystg') for _ in range(2)]
        gi = 0
        yi = 0
        for b in range(NB):
            for grp in range(8):
                p = gi % 2
                gi += 1
                na = grp < 4
                c = grp if na else grp - 4
                qidx = grp
                kidx = c if na else 4 + c // 2
                nvc = 130 if na else 65
                vcol0 = (2 * c) * 65 if na else (8 + c // 2) * 65
                S.dma('sp', Qs[p][:], self.QT[b, qidx], [('Q', b, qidx)], [('Qs', p)])
                S.dma('sp', Ks[p][:], self.KT[b, kidx], [('K', b, kidx)], [('Ks', p)])
                S.dma('sp', Vsl[p][:, :, 0:nvc],
                      self.V[b, :, vcol0:vcol0 + nvc].rearrange("(t p) c -> p t c", p=128), [('V', b)], [('Vs', p)])
                for m in range(NT):
                    ys = ystg[yi % 2]
                    ykey = ('ystg', yi % 2)
                    yi += 1
                    for hh in range(2):
                        h = 2 * c + hh
                        pb = hh * 64
                        qT = Qs[p][pb:pb + 64, m * 128:(m + 1) * 128]
                        kts = []

                        def ktile(kt, bias):
                            vo = hh * 65 if na else 0
                            return (Ks[p][pb:pb + 64, kt * 128:(kt + 1) * 128], Vsl[p][:, kt, vo:vo + 65], bias,
                                    [('Ks', p), ('Vs', p)])
                        if m < 32:
                            if na:
                                if m < 2:
                                    lat, Tab, tk = range(0, 4), self.TabE, 'TabE'
                                elif m >= 30:
                                    lat, Tab, tk = range(28, 32), self.TabE, 'TabE'
                                else:
                                    lat, Tab, tk = range(m - 2, m + 3), self.TabI, 'TabI'
                                for kt in lat:
                                    j = kt - m
                                    p0 = 7 - 2 * j
                                    bias = (Tab[:, h * 960 + p0 * 64:h * 960 + p0 * 64 + 128], [tk])
                                    kts.append(ktile(kt, bias))
                            else:
                                for kt in range(max(0, m - 1), min(31, m + 1) + 1):
                                    j = kt - m
                                    bias = None if j == 0 else ((self.mL[:], ['mL']) if j < 0 else (self.mU[:], ['mU']))
                                    kts.append(ktile(kt, bias))
                        kts.append(ktile(32, None))
                        kts.append(ktile(33, None))

                        def out_fn(qs, acc, rden, keys, ys=ys, ykey=ykey, hh=hh):
                            self.ts('dve', ys[:, hh * 64:(hh + 1) * 64], acc[:, 0:64], rden, None, ALU.mult, None,
                                    keys, [ykey])
                        self.attn_unit(qT, 128, kts, 64, 0.125, [('Qs', p)], out_fn, sink_col=None if na else h)
                    S.dma('pool', self.Y[b, m * 128:(m + 1) * 128, grp * 128:(grp + 1) * 128], ys[:], [ykey], [('Y', b)])
        S.barrier()
        A.release(mk)

    def phase_out(self, l, ntiles):
        S, A = self.S, self.A
        mk = A.mark()
        wO = A.alloc([128, 8, D], BF16, 'wO')
        m2 = self.load_w_bf16(wO, self.w_out[l], D, 'wO')
        S.barrier()
        A.release(m2)
        gc = self.load_gate(l, 2, 1, 'c')
        ysb = [A.alloc([128, D], BF16, 'ysb') for _ in range(2)]
        yT = [A.alloc([128, 8, 128], BF16, 'yT') for _ in range(2)]
        xt = [A.alloc([128, D], F32, 'xo') for _ in range(2)]
        tmp = [A.alloc([128, D], F32, 'otmp') for _ in range(2)]
        i = 0
        for b in range(NB):
            m3 = A.mark()
            gb = self.load_gate(l, b, 1, 'b')
            for m in range(ntiles):
                p = i % 2
                i += 1
                g, gk = (gb, 'gateb') if m < 32 else (gc, 'gatec')
                S.dma('sp', ysb[p][:], self.Y[b, m * 128:(m + 1) * 128, :], [('Y', b)], [('ysb', p)])
                S.dma('sp', xt[p][:], self.src(l, b, m), [('xs', b, m)], [('xo', p)])
                pst = self.psbf(6 + p)
                for kc in range(8):
                    self.tr(pst[:, kc * 128:(kc + 1) * 128], ysb[p][:, kc * 128:(kc + 1) * 128], [('ysb', p), 'ident'],
                            [('ps', 6 + p)])
                self.cp('act', yT[p][:], pst.rearrange("p (k t) -> p k t", k=8), [('ps', 6 + p)], [('yT', p)])
                for half in range(2):
                    bank = 2 * p + half
                    for kc in range(8):
                        self.mm(self.PS[:, bank, :], yT[p][:, kc, :], wO[:, kc, half * 512:(half + 1) * 512], kc == 0, kc == 7,
                                [('yT', p), 'wO'], [('ps', bank)])
                    self.tt('dve', tmp[p][:, half * 512:(half + 1) * 512], self.PS[:, bank, :],
                            g[:, half * 512:(half + 1) * 512], ALU.mult, [('ps', bank), gk], [('otmp', p, half)])
                self.tt('pool', tmp[p][:], tmp[p][:], xt[p][:], ALU.add, [('otmp', p, 0), ('otmp', p, 1), ('xo', p)],
                        [('otmp', p, 0), ('otmp', p, 1)])
                S.dma('pool', self.xs[b, m * 128:(m + 1) * 128, :], tmp[p][:], [('otmp', p, 0), ('otmp', p, 1)],
                      [('xs', b, m)])
            A.release(m3)
        S.barrier()
        A.release(mk)

    def build(self):
        S = self.S
        self.setup_consts()
        if self.skip_l0:
            for b in range(NB):
                for r0 in range(0, SL, 512):
                    S.dma('sp', self.xs[b, r0:r0 + 512, :], self.x[b, r0:r0 + 512, :], [], [('xsi', b, r0)])
                S.dma('sp', self.xs[b, SL:SA, :], self.ctx[b], [], [('xsi', b, SL)])
            S.barrier()
            self.phase_mod(1)
            self.phase_proj1()
            return self.finish_dbg()
        self.phase_mod(0)
        self.phase_proj0()
        self.phase_attn0()
        self.phase_out(0, NT)
        if self.stop_after == 'attn0':
            return self.finish_dbg()
        self.phase_peer(0, NT)
        if self.stop_after == 'peer0':
            return self.finish_dbg()
        self.phase_mod(1)
        self.phase_proj1()
        if self.stop_after == 'proj1':
            return self.finish_dbg()
        self.phase_attn1()
        self.phase_out(1, 32)
        if self.stop_after == 'attn1':
            return self.finish_dbg()
        self.phase_peer(1, 32)
        S.barrier()
        S.emit()
        return self.nc

    def finish_dbg(self):
        S = self.S
        for b in range(NB):
            for r0 in range(0, SA, 544):
                S.dma('sp', self.dbg[b, r0:r0 + 544, :], self.xs[b, r0:r0 + 544, :], [], [('dbg', b, r0)])
        S.barrier()
        S.emit()
        return self.nc


def _consts():
    f = np.float32
    t = np.arange(SL)
    row, col = (t // 64).astype(f), (t % 64).astype(f)

    def table(dim_half, npart_rep):
        freq = (10000.0 ** (-np.arange(dim_half, dtype=f) / dim_half)).astype(f)
        ang_r = row[None, :] * freq[:, None]
        ang_c = col[None, :] * freq[:, None]
        C = np.concatenate([np.cos(ang_r), np.cos(ang_r), np.cos(ang_c), np.cos(ang_c)], 0)
        Sn = np.concatenate([np.sin(ang_r), np.sin(ang_r), np.sin(ang_c), np.sin(ang_c)], 0)
        C = np.concatenate([C, np.ones((C.shape[0], CT), f)], 1)
        Sn = np.concatenate([Sn, np.zeros((Sn.shape[0], CT), f)], 1)
        return C.astype(f), Sn.astype(f)
    C64, S64 = table(16, 2)
    ropeC = np.concatenate([C64, C64], 0)
    ropeS = np.concatenate([S64, S64], 0)
    C32, S32 = table(8, 1)
    ropeCm = np.concatenate([np.ones((64, SA), f), C32], 0)
    ropeSm = np.concatenate([np.zeros((64, SA), f), S32], 0)
    cq = np.arange(64)
    col_start = np.clip(cq - 8, 0, 48)
    ck = np.arange(64)
    valid = (ck[:, None] >= col_start[None, :]) & (ck[:, None] < col_start[None, :] + 16)
    maskI = np.full((2, 64, 8, 15, 64), NEG, f)
    maskE = np.full((2, 64, 8, 15, 64), NEG, f)
    for kr in range(2):
        for p in range(15):
            dr = (7 - p) if kr == 0 else (8 - p)
            if dr < -7 or dr > 7:
                continue
            mE = np.where(valid, 0.0, NEG).astype(f)
            maskE[kr, :, :, p, :] = mE[:, None, :]
            if -4 <= dr <= 3:
                maskI[kr, :, :, p, :] = mE[:, None, :]
    swaL = np.where(np.arange(128)[:, None] >= np.arange(128)[None, :], 0.0, NEG).astype(f)
    swaU = np.where(np.arange(128)[:, None] <= np.arange(128)[None, :], 0.0, NEG).astype(f)
    iota = np.tile(np.arange(128, dtype=f)[None, :], (128, 1))
    return dict(ropeC=ropeC, ropeS=ropeS, ropeCm=ropeCm, ropeSm=ropeSm, maskI=maskI.reshape(128, 7680),
                maskE=maskE.reshape(128, 7680), swaL=swaL, swaU=swaU, iota_in=iota)


def _rpb_layout(rpb):
    ck = np.arange(64)[:, None]
    cq = np.arange(64)[None, :]
    cidx = np.clip(ck - cq + 15, 0, 30)
    out = np.zeros((2, 64, 8, 15, 64), np.float32)
    for kr in range(2):
        for p in range(15):
            dr = (7 - p) if kr == 0 else (8 - p)
            if dr < -7 or dr > 7:
                continue
            out[kr, :, :, p, :] = np.transpose(rpb[:, dr + 7, :][:, cidx], (1, 0, 2))
    return np.ascontiguousarray(out.reshape(128, 7680))


_CONSTS = None


def make_in_maps(inputs, cores):
    global _CONSTS
    if _CONSTS is None:
        _CONSTS = _consts()
    f = np.float32
    g = {k: np.ascontiguousarray(np.asarray(v, dtype=f)) for k, v in inputs.items()}
    shared = dict(
        ada_w=g['ada_w'], ada_b=g['ada_b'], norm1_g=g['norm1_g'], norm2_g=g['norm2_g'], w_out=g['w_out'],
        peer_wq=g['peer_wq'], peer_keys=g['peer_keys'], peer_u=g['peer_u'], peer_v=g['peer_v'],
        ab_w=g['ab_w_in'][0], rpbT=_rpb_layout(g['na_rpb'][0]), sink=g['swa_sink'][0:1],
        cd_w=g['cd_w_in'][0], qn_g=g['mla_q_norm_g'][0].reshape(256, 1), w_uq=g['mla_w_uq'][0],
        kvn_g=g['mla_kv_norm_g'][0].reshape(128, 1), w_ukv=g['mla_w_ukv'][0], dlam=g['diff_lambda'][0].reshape(1, 256),
        subln=g['diff_subln_g'][0].reshape(1, 128), fin_g=g['final_norm_g'].reshape(1, D), **_CONSTS)
    maps = []
    for ci in cores:
        b0 = ci * NB
        m = dict(shared)
        m['x'] = g['x'][b0:b0 + NB]
        m['ctx'] = g['ctx'][b0:b0 + NB]
        m['cc'] = np.ascontiguousarray(np.concatenate([g['c'][b0:b0 + NB], g['c_ctx'][None, :]], 0))
        maps.append(m)
    return maps


def kernel(**inputs):
    nc = Builder().build()
    maps = make_in_maps(inputs, list(range(N_CORES)))
    res = run_bass_kernel_spmd(nc, maps, core_ids=list(range(N_CORES)))
    return np.concatenate([r["out"] for r in res.results], axis=0).astype(np.float32)


def _peer_methods():
    def topk16_multi(self, items):
        S = self.S
        for (src, vals, idx, wk, rk, wkey) in items:
            S.op('dve', lambda e, src=src, vals=vals: e.max(out=vals[:, 0:8], in_=src), rk, [wkey + ('v',)])
            yield
        for (src, vals, idx, wk, rk, wkey) in items:
            S.op('dve', lambda e, src=src, vals=vals, idx=idx: e.max_index(out=idx[:, 0:8], in_max=vals[:, 0:8], in_values=src),
                 rk + [wkey + ('v',)], [wkey + ('i',)])
            yield
        for (src, vals, idx, wk, rk, wkey) in items:
            S.op('dve', lambda e, src=src, vals=vals, wk=wk: e.match_replace(out=wk, in_to_replace=vals[:, 0:8], in_values=src,
                                                                             imm_value=-1e30),
                 rk + [wkey + ('v',)], [wkey + ('w',)])
            yield
        for (src, vals, idx, wk, rk, wkey) in items:
            S.op('dve', lambda e, vals=vals, wk=wk: e.max(out=vals[:, 8:16], in_=wk), [wkey + ('w',)], [wkey + ('v2',)])
            yield
        for (src, vals, idx, wk, rk, wkey) in items:
            S.op('dve', lambda e, vals=vals, idx=idx, wk=wk: e.max_index(out=idx[:, 8:16], in_max=vals[:, 8:16], in_values=wk),
                 [wkey + ('w',), wkey + ('v2',)], [wkey + ('i2',)])
            yield

    def phase_peer_prep(self, l):
        S, A = self.S, self.A
        mk = A.mark()
        ustg = [A.alloc([128, D], F32, 'ustg') for _ in range(2)]
        vstg = [A.alloc([128, D], F32, 'vstg') for _ in range(2)]
        ubf = [A.alloc([128, D], BF16, 'ubf') for _ in range(2)]
        vbf = [A.alloc([128, D], BF16, 'vbf') for _ in range(2)]
        utb = [A.alloc([128, 8, 128], BF16, 'utb') for _ in range(2)]
        U = self.peer_u[l].rearrange("(i j) d -> j i d", j=128)
        Vv = self.peer_v[l].rearrange("(i j) d -> j i d", j=128)
        for j in range(128):
            p = j % 2
            S.dma('sp', ustg[p][:], U[j], [], [('ustg', p)])
            S.dma('sp', vstg[p][:], Vv[j], [], [('vstg', p)])
            self.cp('dve', ubf[p][:], ustg[p][:], [('ustg', p)], [('ubf', p)])
            pst = self.psbf(6 + p)
            for kc in range(8):
                self.tr(pst[:, kc * 128:(kc + 1) * 128], ubf[p][:, kc * 128:(kc + 1) * 128], [('ubf', p), 'ident'], [('ps', 6 + p)])
            self.cp('act', utb[p][:], pst.rearrange("p (k t) -> p k t", k=8), [('ps', 6 + p)], [('utb', p)])
            S.dma('pool', self.UTs[j], utb[p][:].rearrange("p k t -> p (k t)"), [('utb', p)], [('UTs', j)])
            self.cp('pool', vbf[p][:], vstg[p][:], [('vstg', p)], [('vbf', p)])
            S.dma('pool', self.Vs[j], vbf[p][:], [('vbf', p)], [('Vs', j)])
        wst = [A.alloc([128, 8, 512], F32, 'wqst') for _ in range(2)]
        wbf = [A.alloc([128, 8, 512], BF16, 'wqbf') for _ in range(2)]
        for c in range(4):
            p = c % 2
            S.dma('sp', wst[p][:], self.peer_wq[l, :, c * 512:(c + 1) * 512].rearrange("(kc p) n -> p kc n", p=128), [], [('wqst', p)])
            self.cp('dve', wbf[p][:], wst[p][:], [('wqst', p)], [('wqbf', p)])
            S.dma('pool', self.WQs[c], wbf[p][:].rearrange("p k t -> p (k t)"), [('wqbf', p)], [('WQs', c)])
        S.barrier()
        A.release(mk)

    def phase_peer(self, l, ntiles):
        S, A = self.S, self.A
        self.phase_peer_prep(l)
        mk = A.mark()
        last = (l == 1)
        kT = A.alloc([128, 2, 128], BF16, 'kT')
        mk0 = A.mark()
        kst = A.alloc([128, 2, 128], F32, 'kst')
        kbf = A.alloc([128, 2, 128], BF16, 'kbf')
        for p in range(2):
            S.dma('sp', kst[:, p, :], self.peer_keys[l, p], [], ['kst'])
        self.cp('dve', kbf[:], kst[:], ['kst'], ['kbf'])
        pst = self.psbf(2)
        for p in range(2):
            self.tr(pst[:, p * 128:(p + 1) * 128], kbf[:, p, :], ['kbf', 'ident'], [('ps', 2)])
        self.cp('act', kT[:], pst[:, 0:256].rearrange("p (k t) -> p k t", k=2), [('ps', 2)], ['kT'])
        S.barrier()
        A.release(mk0)
        G2 = A.alloc([128, D], F32, 'G2')
        SH2 = A.alloc([128, D], F32, 'SH2')
        g2 = A.alloc([128, D], F32, 'g2')
        fss = A.alloc([128, 4], F32, 'fss')
        if last:
            fing = A.alloc([128, D], F32, 'fing')
            self.load_row_bc(fing[:], self.fin_g[0:1, :], 'fing')
        nb = self.alloc_norm_bufs(nxt=4)
        h2Ts = [A.alloc([128, 8, 256], BF16, 'h2T') for _ in range(2)]
        qT = A.alloc([128, 16, 256], BF16, 'qT')
        s_sb = A.alloc([128, 2048], F32, 's_sb')
        wqb = s_sb[:].bitcast(BF16).rearrange("p (k t) -> p k t", k=8)
        cand = A.alloc([128, 2048], F32, 'cand')
        ngt = cand[:, 0:D]
        sv = A.alloc([128, 256], F32, 'sv')
        si = A.alloc([128, 256], U32, 'si')
        sif = A.alloc([128, 256], F32, 'sif')
        wk = A.alloc([128, 2048], F32, 'wk')
        fv = A.alloc([128, 128], F32, 'fv')
        fp = A.alloc([128, 128], U32, 'fp')
        fpu = A.alloc([128, 128], U32, 'fpu')
        Aa = A.alloc([128, 128], F32, 'Aa')
        Bb = A.alloc([128, 128], F32, 'Bb')
        ijg = A.alloc([128, 3, 128], F32, 'ijg')
        ijgTs = [[A.alloc([128, 3, 128], F32, 'ijgT') for _ in range(2)] for _ in range(2)]
        gz = A.alloc([128, 16], F32, 'gz')
        RT = 16
        R1 = A.alloc([128, RT, 128], BF16, 'R1')
        R2 = A.alloc([128, RT, 128], BF16, 'R2')
        Gs = A.alloc([128, 128, 256], BF16, 'Gs')
        utl = [A.alloc([128, 8, 128], BF16, 'utl') for _ in range(6)]
        vl = [A.alloc([128, D], BF16, 'vl') for _ in range(6)]
        ga = [A.alloc([128, 512], F32, 'ga') for _ in range(3)]
        wT = [A.alloc([128, 512], BF16, 'wT') for _ in range(3)]
        etmp = A.alloc([128, D], F32, 'etmp')
        iota16 = self.iota[:, 0:16]
        nchunks = ntiles // 2
        gskeys = [('Gs', a, c) for a in range(2) for c in range(128 // RT)]
        items = [(b, ch) for b in range(NB) for ch in range(nchunks)]
        state = {'row': None, 'grow': None, 'ji': 0}
        xpars = {}

        def partA(seq):
            b, ch = items[seq]
            par = seq % 2
            h2T = h2Ts[par]
            hkey = ('h2T', par)
            row = b if ch < 16 else 2
            if row != state['row']:
                state['row'] = row
                self.load_row_bc(G2[:], self.mod[l, row:row + 1, 4 * D:5 * D], 'G2')
                self.load_row_bc(SH2[:], self.mod[l, row:row + 1, 3 * D:4 * D], 'SH2')
                self.load_row_bc(ngt, self.norm2_g[l:l + 1, :], 'cand')
                self.stt('dve', G2[:], G2[:], 1.0, ngt, ALU.add, ALU.mult, ['G2', 'cand'], ['G2'])
                yield
            xp = []
            for ti in range(2):
                tg = ch * 2 + ti
                xp.append(self.norm_tile(nb, self.xs[b, tg * 128:(tg + 1) * 128, :], G2, SH2, ['G2', 'SH2'],
                                         h2T[:, :, ti * 128:(ti + 1) * 128], hkey, 3))
                yield
            xpars[seq] = xp
            for c in range(4):
                S.dma('sp', wqb.rearrange("p k t -> p (k t)"), self.WQs[c], [('WQs', c)], ['swq'])
                for q in range(4):
                    hp = c * 4 + q
                    bank = 3
                    for kc in range(8):
                        self.mm(self.PS[:, bank, 0:256], wqb[:, kc, q * 128:(q + 1) * 128], h2T[:, kc, :], kc == 0, kc == 7,
                                ['swq', hkey], [('ps', bank)])
                        if kc % 2 == 1:
                            yield
                    self.cp('act', qT[:, hp, :], self.PS[:, bank, 0:256], [('ps', bank)], ['qT'])
                    yield
            for ti in range(2):
                for g4 in range(4):
                    bank = 3
                    for q in range(4):
                        hp = g4 * 4 + q
                        self.mm(self.PS[:, bank, q * 128:(q + 1) * 128], qT[:, hp, ti * 128:(ti + 1) * 128], kT[:, hp % 2, :],
                                True, True, ['qT', 'kT'], [('ps', bank)])
                    self.cp('act', s_sb[:, g4 * 512:(g4 + 1) * 512], self.PS[:, bank, :], [('ps', bank)], ['swq'])
                    yield
                tkA = [(s_sb[:, hp * 128:(hp + 1) * 128], sv[:, hp * 16:(hp + 1) * 16], si[:, hp * 16:(hp + 1) * 16],
                        wk[:, hp * 128:(hp + 1) * 128], ['swq'], ('sv', hp)) for hp in range(16)]
                for _ in self.topk16_multi(tkA):
                    yield
                svk = [('sv', hp, x) for hp in range(16) for x in ('v', 'v2')]
                sik = [('sv', hp, x) for hp in range(16) for x in ('i', 'i2')]
                self.cp('dve', sif[:], si[:], sik, ['sif'])
                sv4 = sv[:].rearrange("p (h t k) -> p h t k", h=8, t=2)
                sif4 = sif[:].rearrange("p (h t k) -> p h t k", h=8, t=2)
                cand4 = cand[:].rearrange("p (h a b) -> p h a b", h=8, a=16)
                self.tt('dve', cand4, sv4[:, :, 0, :].unsqueeze(3).to_broadcast([128, 8, 16, 16]),
                        sv4[:, :, 1, :].unsqueeze(2).to_broadcast([128, 8, 16, 16]), ALU.add, svk, ['cand'])
                yield
                tkB = [(cand[:, h * 256:(h + 1) * 256], fv[:, h * 16:(h + 1) * 16], fp[:, h * 16:(h + 1) * 16],
                        wk[:, h * 256:(h + 1) * 256], ['cand'], ('fv', h)) for h in range(8)]
                for _ in self.topk16_multi(tkB):
                    yield
                fvk = [('fv', h, x) for h in range(8) for x in ('v', 'v2')]
                fpk = [('fv', h, x) for h in range(8) for x in ('i', 'i2')]
                S.op('dve', lambda e: e.tensor_single_scalar(out=fpu[:], in_=fp[:], scalar=4, op=ALU.logical_shift_right), fpk, ['fpu'])
                self.cp('dve', Aa[:], fpu[:], ['fpu'], ['Aa'])
                S.op('dve', lambda e: e.tensor_single_scalar(out=fpu[:], in_=fp[:], scalar=15, op=ALU.bitwise_and), fpk + ['fpu'], ['fpu'])
                self.cp('dve', Bb[:], fpu[:], ['fpu'], ['Bb'])
                yield
                fv3 = fv[:].rearrange("p (h k) -> p h k", h=8)
                g3 = ijg[:, 2, :].rearrange("p (h k) -> p h k", h=8)
                self.tt('dve', g3, fv3, fv3[:, :, 0:1].to_broadcast([128, 8, 16]), ALU.subtract, fvk, [('ijg', 2)])
                self.act(ijg[:, 2, :], ijg[:, 2, :], AF.Exp, [('ijg', 2)], [('ijg', 2)])
                io4 = iota16.unsqueeze(1).unsqueeze(1).to_broadcast([128, 8, 16, 16])
                for (sel, t_, slot, skey) in ((Aa, 0, 0, 'Aa'), (Bb, 1, 1, 'Bb')):
                    sel4 = sel[:].rearrange("p (h k) -> p h k", h=8).unsqueeze(3).to_broadcast([128, 8, 16, 16])
                    self.tt('dve', cand4, sel4, io4, ALU.is_equal, [skey, 'iota', 'cand'], ['cand'])
                    self.tt('dve', cand4, cand4, sif4[:, :, t_, :].unsqueeze(2).to_broadcast([128, 8, 16, 16]), ALU.mult,
                            ['cand', 'sif'], ['cand'])
                    S.op('dve', lambda e, slot=slot: e.tensor_reduce(
                        out=ijg[:, slot, :].rearrange("p (h k) -> p h k", h=8), in_=cand4, axis=AX.X, op=ALU.add),
                        ['cand'], [('ijg', slot)])
                    yield
                S.op('dve', lambda e: e.tensor_reduce(out=gz[:, 0:8], in_=g3, axis=AX.X, op=ALU.add), [('ijg', 2)], ['gz'])
                S.op('dve', lambda e: e.reciprocal(out=gz[:, 0:8], in_=gz[:, 0:8]), ['gz'], ['gz'])
                self.tt('dve', g3, g3, gz[:, 0:8].unsqueeze(2).to_broadcast([128, 8, 16]), ALU.mult, [('ijg', 2), 'gz'], [('ijg', 2)])
                for s3 in range(3):
                    self.tr(self.PS[:, 3, s3 * 128:(s3 + 1) * 128], ijg[:, s3, :], [('ijg', s3), 'identf'], [('ps', 3)], f32=True)
                self.cp('act', ijgTs[par][ti][:], self.PS[:, 3, 0:384].rearrange("p (s t) -> p s t", s=3), [('ps', 3)],
                        [('ijgT', par, ti)])
                yield

        def partB(seq):
            par = seq % 2
            bi = 0
            for ti in range(2):
                ijgT = ijgTs[par][ti]
                ik = ('ijgT', par, ti)
                for rq in range(128 // RT):
                    tq = rq * RT
                    iob = self.iota[:].unsqueeze(1).to_broadcast([128, RT, 128])
                    self.tt('dve', R1[:], iob, ijgT[:, 0, tq:tq + RT].unsqueeze(2).to_broadcast([128, RT, 128]), ALU.is_equal,
                            ['iota', ik], ['R1'])
                    self.tt('pool', R1[:], R1[:], ijgT[:, 2, tq:tq + RT].unsqueeze(2).to_broadcast([128, RT, 128]), ALU.mult,
                            ['R1', ik], ['R1'])
                    self.tt('dve', R2[:], iob, ijgT[:, 1, tq:tq + RT].unsqueeze(2).to_broadcast([128, RT, 128]), ALU.is_equal,
                            ['iota', ik], ['R2'])
                    for t4 in range(RT // 4):
                        bank = bi % 4
                        bi += 1
                        for t_ in range(4):
                            t = t4 * 4 + t_
                            self.mm(self.PS[:, bank, t_ * 128:(t_ + 1) * 128], R1[:, t, :], R2[:, t, :], True, True, ['R1', 'R2'],
                                    [('ps', bank, 0), ('ps', bank, 1), ('ps', bank)])
                        tok0 = ti * 128 + tq + t4 * 4
                        self.cp('act' if t4 % 2 == 0 else 'dve', Gs[:, :, tok0:tok0 + 4],
                                self.PS[:, bank, :].rearrange("p (t j) -> p j t", t=4),
                                [('ps', bank, 0), ('ps', bank, 1), ('ps', bank)], [('Gs', ti, rq)])

        def jloop(seq, gen):
            par = seq % 2
            h2T = h2Ts[par]
            hkey = ('h2T', par)
            LA = 2
            base = state['ji']

            def a_pair(pp):
                pi_ = base + pp
                u = pi_ % 3
                bank = pi_ % 3
                for jj in (2 * pp, 2 * pp + 1):
                    b6 = (2 * pi_ + jj % 2) % 6
                    S.dma('sp', utl[b6][:].rearrange("p k t -> p (k t)"), self.UTs[jj], [('UTs', jj)], [('utl', b6)])
                    S.dma('sp', vl[b6][:], self.Vs[jj], [('Vs', jj)], [('vl', b6)])
                    pa = self.PS[:, bank, (jj % 2) * 256:(jj % 2 + 1) * 256]
                    for kc in range(8):
                        self.mm(pa, utl[b6][:, kc, :], h2T[:, kc, :], kc == 0, kc == 7, [('utl', b6), hkey], [('ps', bank, 0)])
                self.act(ga[u][:], self.PS[:, bank, :], AF.Gelu, [('ps', bank, 0)], [('ga', u)])
                self.tt('dve', wT[u][:], ga[u][:], Gs[:, 2 * pp:2 * pp + 2, :].rearrange("p j t -> p (j t)"), ALU.mult,
                        [('ga', u)] + gskeys, [('wT', u)])

            def f_pair(pp):
                pi_ = base + pp
                u = pi_ % 3
                for jj in (2 * pp, 2 * pp + 1):
                    b6 = (2 * pi_ + jj % 2) % 6
                    for ti in range(2):
                        for hf in range(2):
                            self.mm(self.PS[:, 4 + 2 * ti + hf, :], wT[u][:, (jj % 2) * 256 + ti * 128:(jj % 2) * 256 + (ti + 1) * 128],
                                    vl[b6][:, hf * 512:(hf + 1) * 512], jj == 0, jj == 127, [('wT', u), ('vl', b6)],
                                    [('ps', 4 + 2 * ti + hf)])
            for step in range(64 + LA):
                if step < 64:
                    a_pair(step)
                if step >= LA:
                    f_pair(step - LA)
                if gen is not None:
                    for _ in range(12):
                        if next(gen, 'done') == 'done':
                            gen = None
                            break
            state['ji'] += 64
            if gen is not None:
                for _ in gen:
                    pass

        def epilogue(seq):
            b, ch = items[seq]
            row = b if ch < 16 else 2
            if row != state['grow']:
                state['grow'] = row
                self.load_row_bc(g2[:], self.mod[l, row:row + 1, 5 * D:6 * D], 'g2')
            for ti in range(2):
                tg = ch * 2 + ti
                px = xpars[seq][ti]
                xt = nb['xt'][px]
                xk = ('xt', px)
                for hf in range(2):
                    self.tt('dve', etmp[:, hf * 512:(hf + 1) * 512], self.PS[:, 4 + 2 * ti + hf, :], g2[:, hf * 512:(hf + 1) * 512],
                            ALU.mult, [('ps', 4 + 2 * ti + hf), 'g2'], [('etmp', hf)])
                self.tt('pool', etmp[:], etmp[:], xt[:], ALU.add, [('etmp', 0), ('etmp', 1), xk], [('etmp', 0), ('etmp', 1)])
                ek = [('etmp', 0), ('etmp', 1)]
                if not last:
                    S.dma('pool', self.xs[b, tg * 128:(tg + 1) * 128, :], etmp[:], ek, [('xs', b, tg)])
                else:
                    self.act(nb['junk'], etmp[:], AF.Square, ek, ['ntmp', 'fss'], accum_out=fss[:, 0:1])
                    self.act(fss[:, 2:3], fss[:, 0:1], AF.Sqrt, ['fss', 'eps'], ['frs'], scale=1.0 / D, bias=self.epsT[:, 0:1])
                    S.op('dve', lambda e: e.reciprocal(out=fss[:, 2:3], in_=fss[:, 2:3]), ['frs'], ['frs'])
                    self.stt('dve', etmp[:], etmp[:], fss[:, 2:3], fing[:], ALU.mult, ALU.mult, ek + ['frs', 'fing'], ek)
                    S.dma('pool', self.out[b, tg * 128:(tg + 1) * 128, :], etmp[:], ek, [('out', b, tg)])

        for _ in partA(0):
            pass
        partB(0)
        for seq in range(len(items)):
            gen = partA(seq + 1) if seq + 1 < len(items) else None
            jloop(seq, gen)
            epilogue(seq)
            if seq + 1 < len(items):
                partB(seq + 1)
        S.barrier()
        A.release(mk)

    Builder.topk16_multi = topk16_multi
    Builder.phase_peer_prep = phase_peer_prep
    Builder.phase_peer = phase_peer


_peer_methods()


LAM_INIT1 = 0.8 - 0.6 * math.exp(-0.3 * 1)


def _layer1_methods():
    def phase_proj1(self, l=1):
        S, A = self.S, self.A
        mk = A.mark()
        wA = A.alloc([128, 8, 3072], BF16, 'wA')
        m2 = self.load_w_bf16(wA, self.cd_w, 1952, 'wA', piece=488)
        S.barrier()
        A.release(m2)
        self.make_rot(wA, 416, 1952, 16, 16, 'wA')
        self.make_rot(wA, 928, 2464, 16, 16, 'wA')
        self.make_rot(wA, 384, 2976, 2, 8, 'wA')
        Wg = A.alloc([128, 2, 768], BF16, 'Wg')
        Wgr = A.alloc([128, 2, 768], BF16, 'Wgr')
        Wkv = A.alloc([128, 1024], BF16, 'Wkv')
        onesf = A.alloc([128, 128], BF16, 'onesf')
        S.op('pool', lambda e: e.memset(onesf[:], 1.0), [], ['onesf'])
        m3 = A.mark()
        wst = A.alloc([128, 1024], F32, 'w1st')
        gq = A.alloc([128, 2], F32, 'gq')
        gkv = A.alloc([128, 1], F32, 'gkv')
        for c in range(2):
            S.dma('sp', gq[:, c:c + 1], self.qn_g[c * 128:(c + 1) * 128, :], [], ['gq'])
        S.dma('sp', gkv[:], self.kvn_g, [], ['gkv'])
        for c in range(2):
            S.dma('sp', wst[:, 0:768], self.w_uq[c * 128:(c + 1) * 128, :], ['w1st'], ['w1st'])
            self.ts('dve', Wg[:, c, :], wst[:, 0:768], gq[:, c:c + 1], None, ALU.mult, None, ['w1st', 'gq'], ['Wg'])
        S.op('pool', lambda e: e.memset(Wgr[:], 0.0), [], ['Wgr'])
        for c in range(2):
            s = Wg[:, c, :].rearrange("p (h f) -> p h f", f=96)[:, :, 64:96].rearrange("p h (b t e) -> p h b t e", b=2, t=2)
            d = Wgr[:, c, :].rearrange("p (h f) -> p h f", f=96)[:, :, 64:96].rearrange("p h (b t e) -> p h b t e", b=2, t=2)
            for bb in range(2):
                S.op('act', lambda e, s=s, d=d, bb=bb: e.mul(out=d[:, :, bb, 0, :], in_=s[:, :, bb, 1, :], mul=-1.0), ['Wg', 'Wgr'], ['Wgr'])
                self.cp('pool', d[:, :, bb, 1, :], s[:, :, bb, 0, :], ['Wg', 'Wgr'], ['Wgr'])
        S.dma('sp', wst[:], self.w_ukv, ['w1st'], ['w1st'])
        self.ts('dve', Wkv[:], wst[:], gkv[:, 0:1], None, ALU.mult, None, ['w1st', 'gkv'], ['Wkv'])
        S.barrier()
        A.release(m3)
        if self.sub == 'w':
            return
        for b in range(NB):
            self.proj_batch1(l, b, wA, Wg, Wgr, Wkv, onesf)
            if self.sub is not None:
                break
        S.barrier()
        A.release(mk)

    def proj_batch1(self, l, b, wA, Wg, Wgr, Wkv, onesf):
        S, A = self.S, self.A
        mk = A.mark()
        Gb, SHb = self.load_norm_mod(l, b, 1, 'b')
        Gc, SHc = self.load_norm_mod(l, 2, 1, 'c')
        nb = self.alloc_norm_bufs()
        hTs = [A.alloc([128, 8, 512], BF16, 'hT') for _ in range(2)]
        stg = [A.alloc([128, 512], BF16, 'stg') for _ in range(3)]
        t1 = A.alloc([128, 512], F32, 't1')
        t2 = A.alloc([128, 512], F32, 't2')
        rC = A.alloc([128, 512], F32, 'rC')
        rS = A.alloc([128, 512], F32, 'rS')
        rCm = A.alloc([128, 512], F32, 'rCm')
        rSm = A.alloc([128, 512], F32, 'rSm')
        rCk = A.alloc([128, 512], F32, 'rCk')
        rSk = A.alloc([128, 512], F32, 'rSk')
        cqb = A.alloc([128, 2, 512], BF16, 'cqb')
        sq = A.alloc([128, 512], BF16, 'sq')
        rq = A.alloc([128, 512], F32, 'rq')
        rkv = A.alloc([128, 512], F32, 'rkv')
        ckvf = A.alloc([128, 512], F32, 'ckvf')
        ckvn = A.alloc([128, 512], BF16, 'ckvn')
        krs = A.alloc([128, 512], BF16, 'krs')
        VW = 1036
        vst = [A.alloc([128, VW], BF16, 'vst') for _ in range(2)]
        for v in vst:
            S.op('pool', lambda e, v=v: e.memset(v[:], 1.0), [], [('vst', 0), ('vst', 1)])
        si = 0
        pi = 0
        for ci in range(9):
            ntile = 4 if ci < 8 else 2
            n = ntile * 128
            t0 = ci * 512
            hT = hTs[ci % 2]
            hkey = ('hT', ci % 2)
            G, SH, gk = (Gb, SHb, ['Gb', 'SHb']) if ci < 8 else (Gc, SHc, ['Gc', 'SHc'])
            for ti in range(ntile):
                tg = ci * 4 + ti
                self.norm_tile(nb, self.src(l, b, tg), G, SH, gk, hT[:, :, ti * 128:(ti + 1) * 128], hkey, 6)
            S.dma('sp', rC[:, 0:n], self.ropeC[:, t0:t0 + n], [], ['rC'])
            S.dma('sp', rS[:, 0:n], self.ropeS[:, t0:t0 + n], [], ['rS'])
            S.dma('sp', rCm[0:96, 0:n], self.ropeCm[:, t0:t0 + n], [], ['rCm'])
            S.dma('sp', rSm[0:96, 0:n], self.ropeSm[:, t0:t0 + n], [], ['rSm'])
            S.dma('sp', rCk[0:32, 0:n], self.ropeCm[64:96, t0:t0 + n], [], ['rCk'])
            S.dma('sp', rSk[0:32, 0:n], self.ropeSm[64:96, t0:t0 + n], [], ['rSk'])

            def proj(bank, col0, m, nn=n, hT=hT, hkey=hkey):
                for kc in range(8):
                    self.mm(self.PS[0:m, bank, 0:nn], wA[:, kc, col0:col0 + m], hT[:, kc, 0:nn], kc == 0, kc == 7,
                            ['wA', hkey], [('ps', bank)])
            if self.sub == 'c0':
                break
            for c in range(2):
                proj(c, 128 * c, 128)
                self.cp('dve', cqb[:, c, 0:n], self.PS[:, c, 0:n], [('ps', c)], [('cqb', c)])
                if self.sub == 'c0a':
                    continue
                self.cp('dve', ckvf[:, 0:n], self.PS[:, c, 0:n], [('ps', c)], ['ckvf'])
                self.act(sq[:, 0:n], ckvf[:, 0:n], AF.Square, ['ckvf'], ['sq'])
                self.mm(self.PS[:, 7, 0:n], onesf[:], sq[:, 0:n], c == 0, c == 1, ['onesf', 'sq'], [('ps', 7)])
            if self.sub != 'c0a':
                self.cp('dve', rq[:, 0:n], self.PS[:, 7, 0:n], [('ps', 7)], ['rq'])
                self.act(rq[:, 0:n], rq[:, 0:n], AF.Sqrt, ['rq', 'eps'], ['rq'], scale=1.0 / 256, bias=self.epsT[:, 0:1])
                S.op('dve', lambda e, n=n: e.reciprocal(out=rq[:, 0:n], in_=rq[:, 0:n]), ['rq'], ['rq'])
            if self.sub == 'c0a':
                break
            if self.sub == 'c0b':
                break
            proj(2, 256, 128)
            self.cp('dve', ckvf[:, 0:n], self.PS[:, 2, 0:n], [('ps', 2)], ['ckvf'])
            self.act(sq[:, 0:n], ckvf[:, 0:n], AF.Square, ['ckvf'], ['sq'])
            self.mm(self.PS[:, 7, 0:n], onesf[:], sq[:, 0:n], True, True, ['onesf', 'sq'], [('ps', 7)])
            self.cp('dve', rkv[:, 0:n], self.PS[:, 7, 0:n], [('ps', 7)], ['rkv'])
            self.act(rkv[:, 0:n], rkv[:, 0:n], AF.Sqrt, ['rkv', 'eps'], ['rkv'], scale=1.0 / 128, bias=self.epsT[:, 0:1])
            S.op('dve', lambda e, n=n: e.reciprocal(out=rkv[:, 0:n], in_=rkv[:, 0:n]), ['rkv'], ['rkv'])
            self.tt('dve', ckvn[:, 0:n], ckvf[:, 0:n], rkv[:, 0:n], ALU.mult, ['ckvf', 'rkv'], ['ckvn'])
            if self.sub == 'c1':
                break
            proj(0, 384, 32)
            proj(2, 2976, 32)
            self.tt('dve', t1[0:32, 0:n], self.PS[0:32, 0, 0:n], rCk[0:32, 0:n], ALU.mult, [('ps', 0), 'rCk'], ['t1'])
            self.tt('dve', t2[0:32, 0:n], self.PS[0:32, 2, 0:n], rSk[0:32, 0:n], ALU.mult, [('ps', 2), 'rSk'], ['t2'])
            self.tt('pool', krs[0:32, 0:n], t1[0:32, 0:n], t2[0:32, 0:n], ALU.add, ['t1', 't2'], ['krs'])
            for h in range(8):
                S.dma('pool', self.KT[b, h, 64:96, t0:t0 + n], krs[0:32, 0:n], ['krs'], [('K', b, h, 1)])
            if self.sub == 'c2':
                break
            for h in range(8):
                pa = pi % 2
                pi += 1
                for c in range(2):
                    self.mm(self.PS[0:96, pa, 0:n], Wg[:, c, h * 96:(h + 1) * 96], cqb[:, c, 0:n], c == 0, c == 1,
                            ['Wg', ('cqb', 0), ('cqb', 1)], [('ps', pa)])
                for c in range(2):
                    self.mm(self.PS[0:96, 2 + pa, 0:n], Wgr[:, c, h * 96:(h + 1) * 96], cqb[:, c, 0:n], c == 0, c == 1,
                            ['Wgr', ('cqb', 0), ('cqb', 1)], [('ps', 2 + pa)])
                sg = stg[si % 3]
                skey = ('stg', si % 3)
                si += 1
                self.tt('dve', t1[0:96, 0:n], self.PS[0:96, pa, 0:n], rCm[0:96, 0:n], ALU.mult, [('ps', pa), 'rCm'], ['t1'])
                self.tt('dve', t2[0:96, 0:n], self.PS[0:96, 2 + pa, 0:n], rSm[0:96, 0:n], ALU.mult, [('ps', 2 + pa), 'rSm'], ['t2'])
                self.tt('pool', t1[0:96, 0:n], t1[0:96, 0:n], t2[0:96, 0:n], ALU.add, ['t1', 't2'], ['t1'])
                self.tt('pool', sg[0:96, 0:n], t1[0:96, 0:n], rq[0:96, 0:n], ALU.mult, ['t1', 'rq'], [skey])
                S.dma('pool', self.QT[b, h, 0:96, t0:t0 + n], sg[0:96, 0:n], [skey], [('Q', b, h)])
                pa = pi % 2
                pi += 1
                self.mm(self.PS[0:64, pa, 0:n], Wkv[:, h * 128:h * 128 + 64], ckvn[:, 0:n], True, True, ['Wkv', 'ckvn'], [('ps', pa)])
                sg = stg[si % 3]
                skey = ('stg', si % 3)
                si += 1
                self.cp('act', sg[0:64, 0:n], self.PS[0:64, pa, 0:n], [('ps', pa)], [skey])
                S.dma('pool', self.KT[b, h, 0:64, t0:t0 + n], sg[0:64, 0:n], [skey], [('K', b, h, 0)])
            if self.sub == 'c3':
                break
            for (dst, idx, col0, rot0) in ([('Q', 8 + d, 416 + 128 * d, 1952 + 128 * d) for d in range(4)]
                                           + [('K', 8 + d, 928 + 128 * d, 2464 + 128 * d) for d in range(4)]):
                pa = pi % 2
                pi += 1
                proj(pa, col0, 128)
                proj(2 + pa, rot0, 128)
                sg = stg[si % 3]
                skey = ('stg', si % 3)
                si += 1
                self.tt('dve', t1[:, 0:n], self.PS[:, pa, 0:n], rC[:, 0:n], ALU.mult, [('ps', pa), 'rC'], ['t1'])
                self.tt('dve', t2[:, 0:n], self.PS[:, 2 + pa, 0:n], rS[:, 0:n], ALU.mult, [('ps', 2 + pa), 'rS'], ['t2'])
                self.tt('pool', sg[:, 0:n], t1[:, 0:n], t2[:, 0:n], ALU.add, ['t1', 't2'], [skey])
                dram = self.QT if dst == 'Q' else self.KT
                S.dma('pool', dram[b, idx, :, t0:t0 + n], sg[:, 0:n], [skey], [(dst, b, idx)])
            if self.sub == 'c4':
                break
            for ti in range(ntile):
                tg = ci * 4 + ti
                vs = vst[tg % 2]
                vkey = ('vst', tg % 2)
                self.mm(self.PS[:, 4, :], ckvn[:, ti * 128:(ti + 1) * 128],
                        Wkv[:].rearrange("p (h f) -> p h f", f=128)[:, :, 64:128], True, True, ['ckvn', 'Wkv'], [('ps', 4)])
                self.cp('act', vs[:, 0:520].rearrange("p (h d) -> p h d", d=65)[:, :, 0:64],
                        self.PS[:, 4, :].rearrange("p (h d) -> p h d", d=64), [('ps', 4)], [vkey])
                for kc in range(8):
                    self.mm(self.PS[:, 5, :], hT[:, kc, ti * 128:(ti + 1) * 128], wA[:, kc, 1440:1952], kc == 0, kc == 7,
                            ['wA', hkey], [('ps', 5)])
                self.cp('act', vs[:, 520:1036].rearrange("p (h d) -> p h d", d=129)[:, :, 0:128],
                        self.PS[:, 5, :].rearrange("p (h d) -> p h d", d=128), [('ps', 5)], [vkey])
                S.dma('pool', self.V[b, tg * 128:(tg + 1) * 128, 0:VW], vs[:], [vkey], [('V', b)])
            if self.sub == 'c5':
                break
        S.barrier()
        A.release(mk)

    def phase_attn1(self):
        S, A = self.S, self.A
        mk = A.mark()
        self.attn_setup()
        lam = A.alloc([128, 8], F32, 'lam')
        dl = A.alloc([128, 256], F32, 'dl')
        self.load_row_bc(dl[:], self.dlam[0:1, :], 'dl')
        dl4 = dl[:].rearrange("p (a d) -> p a d", a=4)
        self.tt('dve', dl4[:, 0, :], dl4[:, 0, :], dl4[:, 1, :], ALU.mult, ['dl'], ['dl'])
        self.tt('dve', dl4[:, 2, :], dl4[:, 2, :], dl4[:, 3, :], ALU.mult, ['dl'], ['dl'])
        S.op('dve', lambda e: e.tensor_reduce(out=lam[:, 0:1], in_=dl4[:, 0, :], axis=AX.X, op=ALU.add), ['dl'], ['lam'])
        S.op('dve', lambda e: e.tensor_reduce(out=lam[:, 1:2], in_=dl4[:, 2, :], axis=AX.X, op=ALU.add), ['dl', 'lam'], ['lam'])
        self.act(lam[:, 0:2], lam[:, 0:2], AF.Exp, ['lam'], ['lam'])
        self.tt('dve', lam[:, 2:3], lam[:, 1:2], lam[:, 0:1], ALU.subtract, ['lam'], ['lam'])
        self.ts('dve', lam[:, 3:4], lam[:, 2:3], -LAM_INIT1, None, ALU.add, None, ['lam'], ['lam'])
        subg = A.alloc([128, 128], F32, 'subg')
        self.load_row_bc(subg[:], self.subln[0:1, :], 'subg')
        self.ts('dve', subg[:], subg[:], 1.0 - LAM_INIT1, None, ALU.mult, None, ['subg'], ['subg'])
        Qs = [A.alloc([128, SL], BF16, 'Qs') for _ in range(2)]
        Ks = [A.alloc([128, SA], BF16, 'Ks') for _ in range(2)]
        Vsl = [A.alloc([128, NT, 129], BF16, 'Vsl') for _ in range(2)]
        ystg = [A.alloc([128, 128], BF16, 'ystg') for _ in range(4)]
        o1 = [A.alloc([128, 128], F32, 'o1') for _ in range(4)]
        o2 = [A.alloc([128, 128], F32, 'o2') for _ in range(2)]
        oj = A.alloc([128, 128], BF16, 'oj')
        oss = A.alloc([128, 8], F32, 'oss')
        gi = 0
        yi = 0
        for b in range(NB):
            for grp in range(12):
                p = gi % 2
                gi += 1
                mla = grp < 8
                rows = 96 if mla else 128
                dv = 64 if mla else 128
                vcol0 = grp * 65 if mla else 520 + (grp - 8) * 129
                S.dma('sp', Qs[p][0:rows, :], self.QT[b, grp, 0:rows, 0:SL], [('Q', b, grp)], [('Qs', p)])
                S.dma('sp', Ks[p][0:rows, :], self.KT[b, grp, 0:rows, :],
                      [('K', b, grp), ('K', b, grp, 0), ('K', b, grp, 1)], [('Ks', p)])
                S.dma('sp', Vsl[p][:, :, 0:dv + 1],
                      self.V[b, :, vcol0:vcol0 + dv + 1].rearrange("(t p) c -> p t c", p=128), [('V', b)], [('Vs', p)])
                for qc in range(8):
                    if mla:
                        kts = [(Ks[p][0:96, kt * 128:(kt + 1) * 128], Vsl[p][:, kt, 0:65], None, [('Ks', p), ('Vs', p)])
                               for kt in range(NT)]

                        def out_fn(qs, acc, rden, keys, qc=qc, grp=grp, b=b):
                            nonlocal yi
                            ys = ystg[yi % 4]
                            ykey = ('ystg', yi % 4)
                            yi += 1
                            self.ts('dve', ys[:, 0:64], acc[:, 0:64], rden, None, ALU.mult, None, keys, [ykey])
                            tg = qc * 4 + qs
                            S.dma('pool', self.Y[b, tg * 128:(tg + 1) * 128, grp * 64:(grp + 1) * 64], ys[:, 0:64], [ykey], [('Y', b)])
                        self.attn_unit(Qs[p][0:96, qc * 512:(qc + 1) * 512], 512, kts, 64, 96 ** -0.5, [('Qs', p)], out_fn)
                    else:
                        d = grp - 8
                        for w in range(2):
                            kts = [(Ks[p][w * 64:(w + 1) * 64, kt * 128:(kt + 1) * 128], Vsl[p][:, kt, 0:129], None,
                                    [('Ks', p), ('Vs', p)]) for kt in range(NT)]

                            def out_fn(qs, acc, rden, keys, qc=qc, d=d, b=b, w=w):
                                nonlocal yi
                                if w == 0:
                                    self.ts('dve', o1[qs][:], acc[:, 0:128], rden, None, ALU.mult, None, keys, [('o1', qs)])
                                    return
                                oo = o2[qs % 2]
                                ok = ('o2', qs % 2)
                                self.ts('dve', oo[:], acc[:, 0:128], rden, None, ALU.mult, None, keys, [ok])
                                self.stt('dve', oo[:], oo[:], lam[:, 3:4], o1[qs][:], ALU.mult, ALU.add, [ok, ('o1', qs), 'lam'], [ok])
                                sk = ('oss', qs % 2)
                                c0 = qs % 2
                                self.act(oj[:], oo[:], AF.Square, [ok], ['oj', sk], accum_out=oss[:, c0:c0 + 1])
                                self.act(oss[:, 2 + c0:3 + c0], oss[:, c0:c0 + 1], AF.Sqrt, [sk, 'eps'], [sk], scale=1.0 / 128,
                                         bias=self.epsT[:, 0:1])
                                S.op('dve', lambda e, c0=c0: e.reciprocal(out=oss[:, 2 + c0:3 + c0], in_=oss[:, 2 + c0:3 + c0]), [sk], [sk])
                                ys = ystg[yi % 4]
                                ykey = ('ystg', yi % 4)
                                yi += 1
                                self.stt('dve', ys[:], oo[:], oss[:, 2 + c0:3 + c0], subg[:], ALU.mult, ALU.mult, [ok, sk, 'subg'], [ykey])
                                tg = qc * 4 + qs
                                S.dma('pool', self.Y[b, tg * 128:(tg + 1) * 128, 512 + d * 128:512 + (d + 1) * 128], ys[:], [ykey],
                                      [('Y', b)])
                            self.attn_unit(Qs[p][w * 64:(w + 1) * 64, qc * 512:(qc + 1) * 512], 512, kts, 128, 0.125, [('Qs', p)], out_fn)
        S.barrier()
        A.release(mk)

    Builder.phase_proj1 = phase_proj1
    Builder.proj_batch1 = proj_batch1
    Builder.phase_attn1 = phase_attn1


_layer1_methods()
```

```python
import math
import numpy as np
from contextlib import ExitStack
import concourse.bass as bass
import concourse.mybir as mybir
from concourse.bass_utils import run_bass_kernel_spmd

F32 = mybir.dt.float32
BF16 = mybir.dt.bfloat16
U32 = mybir.dt.uint32
AF = mybir.ActivationFunctionType
ALU = mybir.AluOpType
AX = mybir.AxisListType

NB = 2
D = 1024
SL = 4096
CT = 256
SA = 4352
NT = 34
EPS = 1e-6
NEG = -30000.0
SEM_ROT = 30000
N_CORES = 8


class Sched:
    ENGS = ('pe', 'act', 'dve', 'pool', 'sp')

    def __init__(self, nc, stack, n_lanes=28, same_engine_sync=True):
        self.nc = nc
        self.stack = stack
        self.prog = {e: [] for e in self.ENGS}
        self.cnt = {e: 0 for e in self.ENGS}
        self.esems = {e: [] for e in self.ENGS}
        self.seen = {e: {} for e in self.ENGS}
        self.res = {}
        self.lanes = []
        for i in range(n_lanes):
            s = stack.enter_context(nc.semaphore(f"lane{i}"))
            self.lanes.append([s, 0])
        self.lane_rr = 0
        self.same_engine_sync = same_engine_sync

    def _esem(self, e, n):
        k = (n - 1) // SEM_ROT
        while len(self.esems[e]) <= k:
            s = self.stack.enter_context(self.nc.semaphore(f"es_{e}_{len(self.esems[e])}"))
            self.esems[e].append(s)
        return self.esems[e][k], n - k * SEM_ROT

    def _deps(self, reads, writes):
        deps = []
        for r in reads:
            st = self.res.get(r)
            if st is not None and st['w'] is not None:
                deps.append(st['w'])
        for w in writes:
            st = self.res.get(w)
            if st is not None:
                if st['w'] is not None:
                    deps.append(st['w'])
                deps.extend(st['r'].values())
        return deps

    def _commit(self, ev, reads, writes):
        for r in reads:
            st = self.res.setdefault(r, {'w': None, 'r': {}})
            k = id(ev[1])
            if k not in st['r'] or st['r'][k][2] < ev[2]:
                st['r'][k] = ev
        for w in writes:
            self.res[w] = {'w': ev, 'r': {}}

    def _add_waits(self, e, deps):
        best = {}
        for (eng_src, sem, val) in deps:
            if eng_src == e and (e == 'pe' or not self.same_engine_sync):
                continue
            key = id(sem)
            if key not in best or best[key][1] < val:
                best[key] = (sem, val)
        for key, (sem, val) in best.items():
            if self.seen[e].get(key, 0) >= val:
                continue
            self.seen[e][key] = val
            self.prog[e].append(('wait', sem, val))

    def op(self, e, fn, reads=(), writes=()):
        deps = self._deps(reads, writes)
        self._add_waits(e, deps)
        self.cnt[e] += 1
        sem, val = self._esem(e, self.cnt[e])
        self.prog[e].append(('op', fn, sem, 1))
        ev = (e, sem, val)
        self._commit(ev, reads, writes)
        return ev

    def dma(self, q, out, in_, reads=(), writes=(), **kw):
        deps = self._deps(reads, writes)
        lane = self.lanes[self.lane_rr]
        self.lane_rr = (self.lane_rr + 1) % len(self.lanes)
        if lane[1] > 0:
            deps.append(('dma', lane[0], 16 * lane[1]))
        self._add_waits(q, deps)
        lane[1] += 1
        sem = lane[0]

        def fn(eng, out=out, in_=in_, kw=kw):
            return eng.dma_start(out=out, in_=in_, **kw)
        self.prog[q].append(('op', fn, sem, 16))
        ev = ('dma', sem, 16 * lane[1])
        self._commit(ev, reads, writes)
        return ev

    def barrier(self):
        evs = []
        for e in self.ENGS:
            if self.cnt[e] > 0:
                sem, val = self._esem(e, self.cnt[e])
                evs.append((e + '_b', sem, val))
        for lane in self.lanes:
            if lane[1] > 0:
                evs.append(('dma', lane[0], 16 * lane[1]))
        for e in self.ENGS:
            self._add_waits(e, evs)
        self.res = {}

    def emit(self):
        nc = self.nc
        engobj = {'pe': 'tensor', 'act': 'scalar', 'dve': 'vector', 'pool': 'gpsimd', 'sp': 'sync'}
        with nc.Block() as block:
            for e in self.ENGS:
                items = self.prog[e]
                if not items:
                    continue

                def body(eng, items=items):
                    for it in items:
                        if it[0] == 'wait':
                            eng.wait_ge(it[1], it[2])
                        else:
                            ins = it[1](eng)
                            ins.then_inc(it[2], it[3])
                getattr(block, engobj[e])(body)


class Arena:
    def __init__(self, nc, limit=229300):
        self.nc = nc
        self.off = 17408
        self.n = 0
        self.limit = limit

    def alloc(self, shape, dtype, name='t'):
        isz = 4 if dtype in (F32, U32) else 2
        nbytes = int(np.prod(shape[1:])) * isz
        nbytes = (nbytes + 63) // 64 * 64
        self.n += 1
        t = self.nc.alloc_sbuf_tensor_at(f"{name}_{self.n}", list(shape), dtype, offset=self.off)
        self.off += nbytes
        assert self.off <= self.limit, (name, self.off)
        return t

    def mark(self):
        return self.off

    def release(self, m):
        self.off = m


class Builder:
    def __init__(self, stop_after=None, skip_l0=False, sub=None):
        self.stop_after = stop_after
        self.skip_l0 = skip_l0
        self.sub = sub
        self.nc = nc = bass.Bass("TRN2", target_bir_lowering=False)
        self.stack = ExitStack()
        self.S = Sched(nc, self.stack)
        self.A = Arena(nc)
        di = lambda n, sh, dt=F32: nc.dram_tensor(n, list(sh), dt, kind="ExternalInput").ap()
        self.x = di("x", [NB, SL, D])
        self.ctx = di("ctx", [NB, CT, D])
        self.cc = di("cc", [3, D])
        self.ada_w = di("ada_w", [2, D, 6 * D])
        self.ada_b = di("ada_b", [2, 6 * D])
        self.norm1_g = di("norm1_g", [2, D])
        self.norm2_g = di("norm2_g", [2, D])
        self.w_out = di("w_out", [2, D, D])
        self.peer_wq = di("peer_wq", [2, D, 2048])
        self.peer_keys = di("peer_keys", [2, 2, 128, 128])
        self.peer_u = di("peer_u", [2, 16384, D])
        self.peer_v = di("peer_v", [2, 16384, D])
        self.ab_w = di("ab_w", [D, 2304])
        self.rpbT = di("rpbT", [128, 7680])
        self.maskI = di("maskI", [128, 7680])
        self.maskE = di("maskE", [128, 7680])
        self.swaL = di("swaL", [128, 128])
        self.swaU = di("swaU", [128, 128])
        self.sink = di("sink", [1, 8])
        self.cd_w = di("cd_w", [D, 1952])
        self.qn_g = di("qn_g", [256, 1])
        self.w_uq = di("w_uq", [256, 768])
        self.kvn_g = di("kvn_g", [128, 1])
        self.w_ukv = di("w_ukv", [128, 1024])
        self.dlam = di("dlam", [1, 256])
        self.subln = di("subln", [1, 128])
        self.fin_g = di("fin_g", [1, D])
        self.ropeC = di("ropeC", [128, SA])
        self.ropeS = di("ropeS", [128, SA])
        self.ropeCm = di("ropeCm", [96, SA])
        self.ropeSm = di("ropeSm", [96, SA])
        self.iota_in = di("iota_in", [128, 128])
        self.out = nc.dram_tensor("out", [NB, SL, D], F32, kind="ExternalOutput").ap()
        if stop_after is not None:
            self.dbg = nc.dram_tensor("dbg", [NB, SA, D], F32, kind="ExternalOutput").ap()
        ds = lambda n, sh, dt: nc.dram_tensor(n, list(sh), dt).ap()
        self.xs = ds("xs", [NB, SA, D], F32)
        self.mod = ds("mod", [2, 3, 6 * D], F32)
        self.QT = ds("QT", [NB, 12, 128, SA], BF16)
        self.KT = ds("KT", [NB, 12, 128, SA], BF16)
        self.V = ds("V", [NB, SA, 1040], BF16)
        self.Y = ds("Y", [NB, SA, D], BF16)
        self.UTs = ds("UTs", [128, 128, D], BF16)
        self.Vs = ds("Vs", [128, 128, D], BF16)
        self.WQs = ds("WQs", [4, 128, 8 * 512], BF16)
        self.PS = nc.alloc_psum_tensor("psall", [128, 8, 512], F32)

    def act(self, out, in_, func, r, w, **kw):
        self.S.op('act', lambda e: e.activation(out=out, in_=in_, func=func, **kw), r, w)

    def mm(self, out, lhsT, rhs, start, stop, r, w):
        self.S.op('pe', lambda e: e.matmul(out, lhsT=lhsT, rhs=rhs, start=start, stop=stop), r, w)

    def tr(self, out, in_, r, w, f32=False):
        idn = self.identf if f32 else self.ident
        self.S.op('pe', lambda e: e.transpose(out=out, in_=in_, identity=idn[:]), r, w)

    def tt(self, eng, out, in0, in1, op, r, w):
        self.S.op(eng, lambda e: e.tensor_tensor(out=out, in0=in0, in1=in1, op=op), r, w)

    def ts(self, eng, out, in0, s1, s2, op0, op1, r, w):
        if op1 is None:
            self.S.op(eng, lambda e: e.tensor_scalar(out=out, in0=in0, scalar1=s1, scalar2=None, op0=op0), r, w)
        else:
            self.S.op(eng, lambda e: e.tensor_scalar(out=out, in0=in0, scalar1=s1, scalar2=s2, op0=op0, op1=op1), r, w)

    def stt(self, eng, out, in0, scalar, in1, op0, op1, r, w):
        self.S.op(eng, lambda e: e.scalar_tensor_tensor(out=out, in0=in0, scalar=scalar, in1=in1, op0=op0, op1=op1), r, w)

    def cp(self, eng, out, in_, r, w):
        if eng == 'act':
            self.S.op('act', lambda e: e.copy(out=out, in_=in_), r, w)
        else:
            self.S.op(eng, lambda e: e.tensor_copy(out=out, in_=in_), r, w)

    def ps(self, bank, n=512):
        return self.PS[:, bank, 0:n]

    def psbf(self, bank):
        return self.PS[:, bank, :].bitcast(BF16)

    def src(self, layer, b, tg):
        if layer == 0:
            if tg < 32:
                return self.x[b, tg * 128:(tg + 1) * 128, :]
            return self.ctx[b, (tg - 32) * 128:(tg - 31) * 128, :]
        return self.xs[b, tg * 128:(tg + 1) * 128, :]

    def setup_consts(self):
        S, A = self.S, self.A
        self.ident = A.alloc([128, 128], BF16, 'ident')
        self.identf = A.alloc([128, 128], F32, 'identf')
        self.iota = A.alloc([128, 128], F32, 'iota')
        self.epsT = A.alloc([128, 1], F32, 'eps')
        self.sinkexp = A.alloc([128, 8], F32, 'sinkexp')
        self.mL = A.alloc([128, 128], BF16, 'mL')
        self.mU = A.alloc([128, 128], BF16, 'mU')
        identf, ident = self.identf, self.ident
        S.op('pool', lambda e: e.memset(identf[:], 0.0), [], ['identf'])
        S.op('pool', lambda e: e.affine_select(out=identf[:], in_=identf[:], pattern=[[-1, 128]],
                                               compare_op=ALU.not_equal, fill=1.0, base=0, channel_multiplier=1),
             ['identf'], ['identf'])
        self.cp('dve', ident[:], identf[:], ['identf'], ['ident'])
        S.op('pool', lambda e: e.memset(self.epsT[:], EPS), [], ['eps'])
        S.dma('sp', self.iota[:], self.iota_in, [], ['iota'])
        S.dma('sp', self.sinkexp[:], self.sink[0:1, :].partition_broadcast(128)[:, 0, :], [], ['sinkexp'])
        self.act(self.sinkexp[:], self.sinkexp[:], AF.Exp, ['sinkexp'], ['sinkexp'])
        mk = A.mark()
        t0 = A.alloc([128, 128], F32, 'mstg')
        for (m_in, m_sb, key) in ((self.swaL, self.mL, 'mL'), (self.swaU, self.mU, 'mU')):
            S.dma('sp', t0[:, 0:128], m_in, ['t0'], ['t0'])
            self.cp('dve', m_sb[:], t0[:, 0:128], ['t0'], [key])
        S.barrier()
        A.release(mk)

    def phase_mod(self, l):
        S, A = self.S, self.A
        mk = A.mark()
        ccT = A.alloc([128, 8, 3], F32, 'ccT')
        for kc in range(8):
            S.dma('sp', ccT[:, kc, :], self.cc[:, kc * 128:(kc + 1) * 128].rearrange("r p -> p r"), [], ['ccT'],
                  allow_slow_non_contiguous=True)
        self.act(ccT[:], ccT[:], AF.Silu, ['ccT'], ['ccT'])
        modsb = A.alloc([3, 6 * D], F32, 'modsb')
        adab = A.alloc([3, 6 * D], F32, 'adab')
        S.dma('sp', adab[:], self.ada_b[l:l + 1, :].partition_broadcast(3)[:, 0, :], [], ['adab'])
        wb = [A.alloc([128, 8, 512], F32, 'modw') for _ in range(2)]
        for n in range(12):
            w = wb[n % 2]
            S.dma('sp', w[:], self.ada_w[l, :, n * 512:(n + 1) * 512].rearrange("(kc p) n -> p kc n", p=128),
                  [], [('modw', n % 2)])
            for kc in range(8):
                self.mm(self.PS[0:3, n % 2, :], ccT[:, kc, :], w[:, kc, :], kc == 0, kc == 7,
                        ['ccT', ('modw', n % 2)], [('ps', n % 2)])
            self.tt('dve', modsb[:, n * 512:(n + 1) * 512], self.PS[0:3, n % 2, :], adab[:, n * 512:(n + 1) * 512],
                    ALU.add, [('ps', n % 2), 'adab'], ['modsb'])
        S.dma('sp', self.mod[l], modsb[:], ['modsb'], ['mod'])
        S.barrier()
        A.release(mk)

    def load_row_bc(self, dst, src_row, key):
        self.S.dma('sp', dst, src_row.partition_broadcast(128)[:, 0, :], [], [key])

    def load_norm_mod(self, l, row, which, tag):
        A = self.A
        G = A.alloc([128, D], F32, 'G' + tag)
        SH = A.alloc([128, D], F32, 'SH' + tag)
        tmp = A.alloc([128, D], F32, 'ng' + tag)
        sh_off = 0 if which == 1 else 3
        ng = self.norm1_g if which == 1 else self.norm2_g
        self.load_row_bc(G[:], self.mod[l, row:row + 1, (sh_off + 1) * D:(sh_off + 2) * D], 'G' + tag)
        self.load_row_bc(SH[:], self.mod[l, row:row + 1, sh_off * D:(sh_off + 1) * D], 'SH' + tag)
        self.load_row_bc(tmp[:], ng[l:l + 1, :], 'ng' + tag)
        self.stt('dve', G[:], G[:], 1.0, tmp[:], ALU.add, ALU.mult, ['G' + tag, 'ng' + tag], ['G' + tag])
        return G, SH

    def load_gate(self, l, row, which, tag):
        A = self.A
        g = A.alloc([128, D], F32, 'gate' + tag)
        off = 2 if which == 1 else 5
        self.load_row_bc(g[:], self.mod[l, row:row + 1, off * D:(off + 1) * D], 'gate' + tag)
        return g

    def alloc_norm_bufs(self, nxt=2):
        A = self.A
        nb = {}
        nb['xt'] = [A.alloc([128, D], F32, 'xt') for _ in range(nxt)]
        nb['tmp'] = A.alloc([128, D], F32, 'ntmp')
        nb['junk'] = nb['tmp'][:].bitcast(BF16)[:, 0:D]
        nb['hb'] = [A.alloc([128, D], BF16, 'hb') for _ in range(2)]
        nb['ss'] = A.alloc([128, 4], F32, 'ss')
        nb['i'] = 0
        return nb

    def norm_tile(self, nb, src_ap, G, SH, gk, hT_dst, hT_key, psbank, xt_keep=False):
        S = self.S
        px = nb['i'] % len(nb['xt'])
        p = nb['i'] % 2
        nb['i'] += 1
        xt, hb, ss = nb['xt'][px], nb['hb'][p], nb['ss']
        S.dma('sp', xt[:], src_ap, [], [('xt', px)])
        self.act(nb['junk'], xt[:], AF.Square, [('xt', px)], ['ntmp', ('ss', p)], accum_out=ss[:, p:p + 1])
        self.act(ss[:, 2 + p:3 + p], ss[:, p:p + 1], AF.Sqrt, [('ss', p), 'eps'], [('rs', p)], scale=1.0 / D,
                 bias=self.epsT[:, 0:1])
        S.op('dve', lambda e: e.reciprocal(out=ss[:, 2 + p:3 + p], in_=ss[:, 2 + p:3 + p]), [('rs', p)], [('rs', p)])
        self.stt('dve', nb['tmp'][:], xt[:], ss[:, 2 + p:3 + p], G[:], ALU.mult, ALU.mult,
                 [('xt', px), ('rs', p)] + gk, ['ntmp'])
        self.tt('pool', hb[:], nb['tmp'][:], SH[:], ALU.add, ['ntmp'] + gk, [('hb', p)])
        pst = self.psbf(psbank)
        for kc in range(8):
            self.tr(pst[:, kc * 128:(kc + 1) * 128], hb[:, kc * 128:(kc + 1) * 128], [('hb', p), 'ident'],
                    [('ps', psbank)])
        self.cp('act', hT_dst, pst.rearrange("p (k t) -> p k t", k=8), [('ps', psbank)], [hT_key])
        return px

    def load_w_bf16(self, dst, src, ncols, key, piece=512):
        S, A = self.S, self.A
        mk = A.mark()
        stg = [A.alloc([128, 8, piece], F32, 'wstg') for _ in range(2)]
        i = 0
        for c0 in range(0, ncols, piece):
            n = min(piece, ncols - c0)
            s = stg[i % 2]
            S.dma('sp', s[:, :, 0:n], src[:, c0:c0 + n].rearrange("(kc p) n -> p kc n", p=128), [], [('wstg', i % 2)])
            self.cp('dve' if i % 2 == 0 else 'act', dst[:, :, c0:c0 + n], s[:, :, 0:n], [('wstg', i % 2)], [key])
            i += 1
        return mk

    def make_rot(self, W, src0, dst0, nblk, half, key):
        for kc in range(8):
            s = W[:, kc, src0:src0 + nblk * 2 * half].rearrange("p (b t h) -> p b t h", t=2, h=half)
            d = W[:, kc, dst0:dst0 + nblk * 2 * half].rearrange("p (b t h) -> p b t h", t=2, h=half)
            self.S.op('act', lambda e, s=s, d=d: e.mul(out=d[:, :, 0, :], in_=s[:, :, 1, :], mul=-1.0), [key], [key])
            self.cp('pool', d[:, :, 1, :], s[:, :, 0, :], [key], [key])

    def attn_setup(self):
        A = self.A
        self.E = [A.alloc([128, 512], BF16, 'E') for _ in range(4)]
        self.ei = 0
        self.sti = 0
        self.acci = 0
        self.den = A.alloc([128, 8], F32, 'den')

    def attn_unit(self, qT, nq, ktiles, dv, scale, qkeys, out_fn, sink_col=None):
        S = self.S
        nqs = nq // 128
        aset = self.acci % 2
        self.acci += 1
        accs = []
        for qs in range(nqs):
            bank = 4 + 2 * aset + qs // 2
            sub = qs % 2
            accs.append((self.PS[:, bank, sub * 256:sub * 256 + dv + 1], ('ps', bank, sub)))
        nk = len(ktiles)
        LA = 2
        live = {}

        def qk(ki):
            kT, v, bias, kkeys = ktiles[ki]
            sb = self.sti % 4
            self.sti += 1
            st = self.PS[:, sb, 0:nq]
            self.mm(st, kT, qT, True, bias is None, qkeys + kkeys, [('ps', sb)])
            if bias is not None:
                self.mm(st, self.ident[:], bias[0], False, True, ['ident'] + bias[1], [('ps', sb)])
            eb = self.ei % 4
            self.ei += 1
            E = self.E[eb]
            self.act(E[:, 0:nq], st, AF.Exp, [('ps', sb)], [('E', eb)], scale=scale)
            live[ki] = (E, eb)

        def pv(ki):
            kT, v, bias, kkeys = ktiles[ki]
            E, eb = live.pop(ki)
            for qs in range(nqs):
                self.mm(accs[qs][0], E[:, qs * 128:(qs + 1) * 128], v, ki == 0, ki == nk - 1,
                        [('E', eb)] + kkeys, [accs[qs][1]])
        for step in range(nk + LA):
            if step < nk:
                qk(step)
            if step >= LA:
                pv(step - LA)
        for qs in range(nqs):
            acc, akey = accs[qs]
            dcol = self.den[:, (aset * 4 + qs):(aset * 4 + qs) + 1]
            dkey = ('den', aset * 4 + qs)
            if sink_col is not None:
                self.tt('dve', dcol, acc[:, dv:dv + 1], self.sinkexp[:, sink_col:sink_col + 1], ALU.add,
                        [akey, 'sinkexp'], [dkey])
                S.op('dve', lambda e, dcol=dcol: e.reciprocal(out=dcol, in_=dcol), [dkey], [dkey])
            else:
                S.op('dve', lambda e, dcol=dcol, acc=acc: e.reciprocal(out=dcol, in_=acc[:, dv:dv + 1]), [akey], [dkey])
            out_fn(qs, acc, dcol, [akey, dkey])

    def phase_proj0(self, l=0):
        S, A = self.S, self.A
        mk = A.mark()
        wA = A.alloc([128, 8, 3328], BF16, 'wA')
        m2 = self.load_w_bf16(wA, self.ab_w, 2304, 'wA', piece=576)
        S.barrier()
        A.release(m2)
        self.make_rot(wA, 1536, 2304, 16, 16, 'wA')
        for kc in range(8):
            for r in range(4):
                self.cp('dve', wA[:, kc, 2816 + r * 64:2880 + r * 64], wA[:, kc, 2048 + (r // 2) * 64:2112 + (r // 2) * 64],
                        ['wA'], ['wA'])
        self.make_rot(wA, 2816, 3072, 8, 16, 'wA')
        rC = A.alloc([128, SA], F32, 'rC')
        rS = A.alloc([128, SA], F32, 'rS')
        S.dma('sp', rC[:], self.ropeC, [], ['rC'])
        S.dma('sp', rS[:], self.ropeS, [], ['rS'])
        fm = ([('Q', i, 128 * i, None) for i in range(4)] + [('K', i, 512 + 128 * i, None) for i in range(4)]
              + [('Q', 4 + i, 1536 + 128 * i, 2304 + 128 * i) for i in range(4)]
              + [('K', 4 + i, 2816 + 128 * i, 3072 + 128 * i) for i in range(2)])
        tmv = [(1024, 512, 0, 8, 64), (2176, 128, 8, 2, 64)]
        for b in range(NB):
            self.proj_batch(l, b, wA, fm, tmv, 10, rC, rS, None)
        S.barrier()
        A.release(mk)

    def proj_batch(self, l, b, wA, fm, tmv, nvh, rC, rS, extra):
        S, A = self.S, self.A
        mk = A.mark()
        Gb, SHb = self.load_norm_mod(l, b, 1, 'b')
        Gc, SHc = self.load_norm_mod(l, 2, 1, 'c')
        nb = self.alloc_norm_bufs()
        hTs = [A.alloc([128, 8, 512], BF16, 'hT') for _ in range(2)]
        stg = [A.alloc([128, 512], BF16, 'stg') for _ in range(3)]
        t1 = A.alloc([128, 512], F32, 't1')
        t2 = A.alloc([128, 512], F32, 't2')
        vdv = tmv[0][4]
        vw = sum(nh * (dvv + 1) for (_, _, _, nh, dvv) in tmv)
        vst = [A.alloc([128, vw], BF16, 'vst') for _ in range(2)]
        for v in vst:
            S.op('pool', lambda e, v=v: e.memset(v[:], 1.0), [], [('vst', 0), ('vst', 1)])
        si = 0
        pi = 0
        for ci in range(9):
            ntile = 4 if ci < 8 else 2
            n = ntile * 128
            t0 = ci * 512
            hT = hTs[ci % 2]
            hkey = ('hT', ci % 2)
            G, SH, gk = (Gb, SHb, ['Gb', 'SHb']) if ci < 8 else (Gc, SHc, ['Gc', 'SHc'])
            for ti in range(ntile):
                tg = ci * 4 + ti
                self.norm_tile(nb, self.src(l, b, tg), G, SH, gk, hT[:, :, ti * 128:(ti + 1) * 128], hkey, 6)
            for (dst, idx, col0, rot0) in fm:
                pa = pi % 2
                pi += 1
                for kc in range(8):
                    self.mm(self.PS[:, pa, 0:n], wA[:, kc, col0:col0 + 128], hT[:, kc, 0:n], kc == 0, kc == 7,
                            ['wA', hkey], [('ps', pa)])
                sg = stg[si % 3]
                skey = ('stg', si % 3)
                si += 1
                if rot0 is None:
                    self.cp('act', sg[:, 0:n], self.PS[:, pa, 0:n], [('ps', pa)], [skey])
                else:
                    for kc in range(8):
                        self.mm(self.PS[:, 2 + pa, 0:n], wA[:, kc, rot0:rot0 + 128], hT[:, kc, 0:n], kc == 0, kc == 7,
                                ['wA', hkey], [('ps', 2 + pa)])
                    self.tt('dve', t1[:, 0:n], self.PS[:, pa, 0:n], rC[:, t0:t0 + n], ALU.mult, [('ps', pa), 'rC'], ['t1'])
                    self.tt('dve', t2[:, 0:n], self.PS[:, 2 + pa, 0:n], rS[:, t0:t0 + n], ALU.mult, [('ps', 2 + pa), 'rS'], ['t2'])
                    self.tt('pool', sg[:, 0:n], t1[:, 0:n], t2[:, 0:n], ALU.add, ['t1', 't2'], [skey])
                dram = self.QT if dst == 'Q' else self.KT
                S.dma('pool', dram[b, idx, :, t0:t0 + n], sg[:, 0:n], [skey], [(dst, b, idx)])
            if extra is not None:
                extra(b, ci, t0, n, hT, hkey)
            for ti in range(ntile):
                tg = ci * 4 + ti
                vs = vst[tg % 2]
                vkey = ('vst', tg % 2)
                co = 0
                for gi, (col0, ncols, h0, nh, dvv) in enumerate(tmv):
                    bank = 4 + gi
                    for kc in range(8):
                        self.mm(self.PS[:, bank, 0:ncols], hT[:, kc, ti * 128:(ti + 1) * 128], wA[:, kc, col0:col0 + ncols],
                                kc == 0, kc == 7, ['wA', hkey], [('ps', bank)])
                    ov = vs[:, co:co + nh * (dvv + 1)].rearrange("p (h d) -> p h d", d=dvv + 1)[:, :, 0:dvv]
                    self.cp('act', ov, self.PS[:, bank, 0:ncols].rearrange("p (h d) -> p h d", d=dvv), [('ps', bank)], [vkey])
                    co += nh * (dvv + 1)
                S.dma('pool', self.V[b, tg * 128:(tg + 1) * 128, 0:vw], vs[:], [vkey], [('V', b)])
        S.barrier()
        A.release(mk)

    def phase_attn0(self):
        S, A = self.S, self.A
        mk = A.mark()
        self.TabI = A.alloc([128, 7680], BF16, 'TabI')
        self.TabE = A.alloc([128, 7680], BF16, 'TabE')
        mk2 = A.mark()
        t0 = A.alloc([128, 7680], F32, 'tabstg0')
        t1 = A.alloc([128, 7680], F32, 'tabstg1')
        S.dma('sp', t0[:], self.rpbT, [], ['t0'])
        for (msk, Tab, key) in ((self.maskI, self.TabI, 'TabI'), (self.maskE, self.TabE, 'TabE')):
            S.dma('sp', t1[:], msk, [], ['t1'])
            self.tt('dve', t1[:], t1[:], t0[:], ALU.add, ['t0', 't1'], ['t1'])
            self.ts('dve', Tab[:], t1[:], 8.0, None, ALU.mult, None, ['t1'], [key])
        S.barrier()
        A.release(mk2)
        self.attn_setup()
        Qs = [A.alloc([128, SA], BF16, 'Qs') for _ in range(2)]
        Ks = [A.alloc([128, SA], BF16, 'Ks') for _ in range(2)]
        Vsl = [A.alloc([128, NT, 130], BF16, 'Vsl') for _ in range(2)]
        ystg = [A.alloc([128, 128], BF16, 'ystg') for _ in range(2)]
        gi = 0
        yi = 0
        for b in range(NB):
            for grp in range(8):
                p = gi % 2
                gi += 1
                na = grp < 4
                c = grp if na else grp - 4
                qidx = grp
                kidx = c if na else 4 + c // 2
                nvc = 130 if na else 65
                vcol0 = (2 * c) * 65 if na else (8 + c // 2) * 65
                S.dma('sp', Qs[p][:], self.QT[b, qidx], [('Q', b, qidx)], [('Qs', p)])
                S.dma('sp', Ks[p][:], self.KT[b, kidx], [('K', b, kidx)], [('Ks', p)])
                S.dma('sp', Vsl[p][:, :, 0:nvc],
                      self.V[b, :, vcol0:vcol0 + nvc].rearrange("(t p) c -> p t c", p=128), [('V', b)], [('Vs', p)])
                for m in range(NT):
                    ys = ystg[yi % 2]
                    ykey = ('ystg', yi % 2)
                    yi += 1
                    for hh in range(2):
                        h = 2 * c + hh
                        pb = hh * 64
                        qT = Qs[p][pb:pb + 64, m * 128:(m + 1) * 128]
                        kts = []

                        def ktile(kt, bias):
                            vo = hh * 65 if na else 0
                            return (Ks[p][pb:pb + 64, kt * 128:(kt + 1) * 128], Vsl[p][:, kt, vo:vo + 65], bias,
                                    [('Ks', p), ('Vs', p)])
                        if m < 32:
                            if na:
                                if m < 2:
                                    lat, Tab, tk = range(0, 4), self.TabE, 'TabE'
                                elif m >= 30:
                                    lat, Tab, tk = range(28, 32), self.TabE, 'TabE'
                                else:
                                    lat, Tab, tk = range(m - 2, m + 3), self.TabI, 'TabI'
                                for kt in lat:
                                    j = kt - m
                                    p0 = 7 - 2 * j
                                    bias = (Tab[:, h * 960 + p0 * 64:h * 960 + p0 * 64 + 128], [tk])
                                    kts.append(ktile(kt, bias))
                            else:
                                for kt in range(max(0, m - 1), min(31, m + 1) + 1):
                                    j = kt - m
                                    bias = None if j == 0 else ((self.mL[:], ['mL']) if j < 0 else (self.mU[:], ['mU']))
                                    kts.append(ktile(kt, bias))
                        kts.append(ktile(32, None))
                        kts.append(ktile(33, None))

                        def out_fn(qs, acc, rden, keys, ys=ys, ykey=ykey, hh=hh):
                            self.ts('dve', ys[:, hh * 64:(hh + 1) * 64], acc[:, 0:64], rden, None, ALU.mult, None,
                                    keys, [ykey])
                        self.attn_unit(qT, 128, kts, 64, 0.125, [('Qs', p)], out_fn, sink_col=None if na else h)
                    S.dma('pool', self.Y[b, m * 128:(m + 1) * 128, grp * 128:(grp + 1) * 128], ys[:], [ykey], [('Y', b)])
        S.barrier()
        A.release(mk)

    def phase_out(self, l, ntiles):
        S, A = self.S, self.A
        mk = A.mark()
        wO = A.alloc([128, 8, D], BF16, 'wO')
        m2 = self.load_w_bf16(wO, self.w_out[l], D, 'wO')
        S.barrier()
        A.release(m2)
        gc = self.load_gate(l, 2, 1, 'c')
        ysb = [A.alloc([128, D], BF16, 'ysb') for _ in range(2)]
        yT = [A.alloc([128, 8, 128], BF16, 'yT') for _ in range(2)]
        xt = [A.alloc([128, D], F32, 'xo') for _ in range(2)]
        tmp = [A.alloc([128, D], F32, 'otmp') for _ in range(2)]
        i = 0
        for b in range(NB):
            m3 = A.mark()
            gb = self.load_gate(l, b, 1, 'b')
            for m in range(ntiles):
                p = i % 2
                i += 1
                g, gk = (gb, 'gateb') if m < 32 else (gc, 'gatec')
                S.dma('sp', ysb[p][:], self.Y[b, m * 128:(m + 1) * 128, :], [('Y', b)], [('ysb', p)])
                S.dma('sp', xt[p][:], self.src(l, b, m), [('xs', b, m)], [('xo', p)])
                pst = self.psbf(6 + p)
                for kc in range(8):
                    self.tr(pst[:, kc * 128:(kc + 1) * 128], ysb[p][:, kc * 128:(kc + 1) * 128], [('ysb', p), 'ident'],
                            [('ps', 6 + p)])
                self.cp('act', yT[p][:], pst.rearrange("p (k t) -> p k t", k=8), [('ps', 6 + p)], [('yT', p)])
                for half in range(2):
                    bank = 2 * p + half
                    for kc in range(8):
                        self.mm(self.PS[:, bank, :], yT[p][:, kc, :], wO[:, kc, half * 512:(half + 1) * 512], kc == 0, kc == 7,
                                [('yT', p), 'wO'], [('ps', bank)])
                    self.tt('dve', tmp[p][:, half * 512:(half + 1) * 512], self.PS[:, bank, :],
                            g[:, half * 512:(half + 1) * 512], ALU.mult, [('ps', bank), gk], [('otmp', p, half)])
                self.tt('pool', tmp[p][:], tmp[p][:], xt[p][:], ALU.add, [('otmp', p, 0), ('otmp', p, 1), ('xo', p)],
                        [('otmp', p, 0), ('otmp', p, 1)])
                S.dma('pool', self.xs[b, m * 128:(m + 1) * 128, :], tmp[p][:], [('otmp', p, 0), ('otmp', p, 1)],
                      [('xs', b, m)])
            A.release(m3)
        S.barrier()
        A.release(mk)

    def build(self):
        S = self.S
        self.setup_consts()
        if self.skip_l0:
            for b in range(NB):
                for r0 in range(0, SL, 512):
                    S.dma('sp', self.xs[b, r0:r0 + 512, :], self.x[b, r0:r0 + 512, :], [], [('xsi', b, r0)])
                S.dma('sp', self.xs[b, SL:SA, :], self.ctx[b], [], [('xsi', b, SL)])
            S.barrier()
            self.phase_mod(1)
            self.phase_proj1()
            return self.finish_dbg()
        self.phase_mod(0)
        self.phase_proj0()
        self.phase_attn0()
        self.phase_out(0, NT)
        if self.stop_after == 'attn0':
            return self.finish_dbg()
        self.phase_peer(0, NT)
        if self.stop_after == 'peer0':
            return self.finish_dbg()
        self.phase_mod(1)
        self.phase_proj1()
        if self.stop_after == 'proj1':
            return self.finish_dbg()
        self.phase_attn1()
        self.phase_out(1, 32)
        if self.stop_after == 'attn1':
            return self.finish_dbg()
        self.phase_peer(1, 32)
        S.barrier()
        S.emit()
        return self.nc

    def finish_dbg(self):
        S = self.S
        for b in range(NB):
            for r0 in range(0, SA, 544):
                S.dma('sp', self.dbg[b, r0:r0 + 544, :], self.xs[b, r0:r0 + 544, :], [], [('dbg', b, r0)])
        S.barrier()
        S.emit()
        return self.nc


def _consts():
    f = np.float32
    t = np.arange(SL)
    row, col = (t // 64).astype(f), (t % 64).astype(f)

    def table(dim_half, npart_rep):
        freq = (10000.0 ** (-np.arange(dim_half, dtype=f) / dim_half)).astype(f)
        ang_r = row[None, :] * freq[:, None]
        ang_c = col[None, :] * freq[:, None]
        C = np.concatenate([np.cos(ang_r), np.cos(ang_r), np.cos(ang_c), np.cos(ang_c)], 0)
        Sn = np.concatenate([np.sin(ang_r), np.sin(ang_r), np.sin(ang_c), np.sin(ang_c)], 0)
        C = np.concatenate([C, np.ones((C.shape[0], CT), f)], 1)
        Sn = np.concatenate([Sn, np.zeros((Sn.shape[0], CT), f)], 1)
        return C.astype(f), Sn.astype(f)
    C64, S64 = table(16, 2)
    ropeC = np.concatenate([C64, C64], 0)
    ropeS = np.concatenate([S64, S64], 0)
    C32, S32 = table(8, 1)
    ropeCm = np.concatenate([np.ones((64, SA), f), C32], 0)
    ropeSm = np.concatenate([np.zeros((64, SA), f), S32], 0)
    cq = np.arange(64)
    col_start = np.clip(cq - 8, 0, 48)
    ck = np.arange(64)
    valid = (ck[:, None] >= col_start[None, :]) & (ck[:, None] < col_start[None, :] + 16)
    maskI = np.full((2, 64, 8, 15, 64), NEG, f)
    maskE = np.full((2, 64, 8, 15, 64), NEG, f)
    for kr in range(2):
        for p in range(15):
            dr = (7 - p) if kr == 0 else (8 - p)
            if dr < -7 or dr > 7:
                continue
            mE = np.where(valid, 0.0, NEG).astype(f)
            maskE[kr, :, :, p, :] = mE[:, None, :]
            if -4 <= dr <= 3:
                maskI[kr, :, :, p, :] = mE[:, None, :]
    swaL = np.where(np.arange(128)[:, None] >= np.arange(128)[None, :], 0.0, NEG).astype(f)
    swaU = np.where(np.arange(128)[:, None] <= np.arange(128)[None, :], 0.0, NEG).astype(f)
    iota = np.tile(np.arange(128, dtype=f)[None, :], (128, 1))
    return dict(ropeC=ropeC, ropeS=ropeS, ropeCm=ropeCm, ropeSm=ropeSm, maskI=maskI.reshape(128, 7680),
                maskE=maskE.reshape(128, 7680), swaL=swaL, swaU=swaU, iota_in=iota)


def _rpb_layout(rpb):
    ck = np.arange(64)[:, None]
    cq = np.arange(64)[None, :]
    cidx = np.clip(ck - cq + 15, 0, 30)
    out = np.zeros((2, 64, 8, 15, 64), np.float32)
    for kr in range(2):
        for p in range(15):
            dr = (7 - p) if kr == 0 else (8 - p)
            if dr < -7 or dr > 7:
                continue
            out[kr, :, :, p, :] = np.transpose(rpb[:, dr + 7, :][:, cidx], (1, 0, 2))
    return np.ascontiguousarray(out.reshape(128, 7680))


_CONSTS = None


def make_in_maps(inputs, cores):
    global _CONSTS
    if _CONSTS is None:
        _CONSTS = _consts()
    f = np.float32
    g = {k: np.ascontiguousarray(np.asarray(v, dtype=f)) for k, v in inputs.items()}
    shared = dict(
        ada_w=g['ada_w'], ada_b=g['ada_b'], norm1_g=g['norm1_g'], norm2_g=g['norm2_g'], w_out=g['w_out'],
        peer_wq=g['peer_wq'], peer_keys=g['peer_keys'], peer_u=g['peer_u'], peer_v=g['peer_v'],
        ab_w=g['ab_w_in'][0], rpbT=_rpb_layout(g['na_rpb'][0]), sink=g['swa_sink'][0:1],
        cd_w=g['cd_w_in'][0], qn_g=g['mla_q_norm_g'][0].reshape(256, 1), w_uq=g['mla_w_uq'][0],
        kvn_g=g['mla_kv_norm_g'][0].reshape(128, 1), w_ukv=g['mla_w_ukv'][0], dlam=g['diff_lambda'][0].reshape(1, 256),
        subln=g['diff_subln_g'][0].reshape(1, 128), fin_g=g['final_norm_g'].reshape(1, D), **_CONSTS)
    maps = []
    for ci in cores:
        b0 = ci * NB
        m = dict(shared)
        m['x'] = g['x'][b0:b0 + NB]
        m['ctx'] = g['ctx'][b0:b0 + NB]
        m['cc'] = np.ascontiguousarray(np.concatenate([g['c'][b0:b0 + NB], g['c_ctx'][None, :]], 0))
        maps.append(m)
    return maps


def kernel(**inputs):
    nc = Builder().build()
    maps = make_in_maps(inputs, list(range(N_CORES)))
    res = run_bass_kernel_spmd(nc, maps, core_ids=list(range(N_CORES)))
    return np.concatenate([r["out"] for r in res.results], axis=0).astype(np.float32)


def _peer_methods():
    def topk16_multi(self, items):
        S = self.S
        for (src, vals, idx, wk, rk, wkey) in items:
            S.op('dve', lambda e, src=src, vals=vals: e.max(out=vals[:, 0:8], in_=src), rk, [wkey + ('v',)])
            yield
        for (src, vals, idx, wk, rk, wkey) in items:
            S.op('dve', lambda e, src=src, vals=vals, idx=idx: e.max_index(out=idx[:, 0:8], in_max=vals[:, 0:8], in_values=src),
                 rk + [wkey + ('v',)], [wkey + ('i',)])
            yield
        for (src, vals, idx, wk, rk, wkey) in items:
            S.op('dve', lambda e, src=src, vals=vals, wk=wk: e.match_replace(out=wk, in_to_replace=vals[:, 0:8], in_values=src,
                                                                             imm_value=-1e30),
                 rk + [wkey + ('v',)], [wkey + ('w',)])
            yield
        for (src, vals, idx, wk, rk, wkey) in items:
            S.op('dve', lambda e, vals=vals, wk=wk: e.max(out=vals[:, 8:16], in_=wk), [wkey + ('w',)], [wkey + ('v2',)])
            yield
        for (src, vals, idx, wk, rk, wkey) in items:
            S.op('dve', lambda e, vals=vals, idx=idx, wk=wk: e.max_index(out=idx[:, 8:16], in_max=vals[:, 8:16], in_values=wk),
                 [wkey + ('w',), wkey + ('v2',)], [wkey + ('i2',)])
            yield

    def phase_peer_prep(self, l):
        S, A = self.S, self.A
        mk = A.mark()
        ustg = [A.alloc([128, D], F32, 'ustg') for _ in range(2)]
        vstg = [A.alloc([128, D], F32, 'vstg') for _ in range(2)]
        ubf = [A.alloc([128, D], BF16, 'ubf') for _ in range(2)]
        vbf = [A.alloc([128, D], BF16, 'vbf') for _ in range(2)]
        utb = [A.alloc([128, 8, 128], BF16, 'utb') for _ in range(2)]
        U = self.peer_u[l].rearrange("(i j) d -> j i d", j=128)
        Vv = self.peer_v[l].rearrange("(i j) d -> j i d", j=128)
        for j in range(128):
            p = j % 2
            S.dma('sp', ustg[p][:], U[j], [], [('ustg', p)])
            S.dma('sp', vstg[p][:], Vv[j], [], [('vstg', p)])
            self.cp('dve', ubf[p][:], ustg[p][:], [('ustg', p)], [('ubf', p)])
            pst = self.psbf(6 + p)
            for kc in range(8):
                self.tr(pst[:, kc * 128:(kc + 1) * 128], ubf[p][:, kc * 128:(kc + 1) * 128], [('ubf', p), 'ident'], [('ps', 6 + p)])
            self.cp('act', utb[p][:], pst.rearrange("p (k t) -> p k t", k=8), [('ps', 6 + p)], [('utb', p)])
            S.dma('pool', self.UTs[j], utb[p][:].rearrange("p k t -> p (k t)"), [('utb', p)], [('UTs', j)])
            self.cp('pool', vbf[p][:], vstg[p][:], [('vstg', p)], [('vbf', p)])
            S.dma('pool', self.Vs[j], vbf[p][:], [('vbf', p)], [('Vs', j)])
        wst = [A.alloc([128, 8, 512], F32, 'wqst') for _ in range(2)]
        wbf = [A.alloc([128, 8, 512], BF16, 'wqbf') for _ in range(2)]
        for c in range(4):
            p = c % 2
            S.dma('sp', wst[p][:], self.peer_wq[l, :, c * 512:(c + 1) * 512].rearrange("(kc p) n -> p kc n", p=128), [], [('wqst', p)])
            self.cp('dve', wbf[p][:], wst[p][:], [('wqst', p)], [('wqbf', p)])
            S.dma('pool', self.WQs[c], wbf[p][:].rearrange("p k t -> p (k t)"), [('wqbf', p)], [('WQs', c)])
        S.barrier()
        A.release(mk)

    def phase_peer(self, l, ntiles):
        S, A = self.S, self.A
        self.phase_peer_prep(l)
        mk = A.mark()
        last = (l == 1)
        kT = A.alloc([128, 2, 128], BF16, 'kT')
        mk0 = A.mark()
        kst = A.alloc([128, 2, 128], F32, 'kst')
        kbf = A.alloc([128, 2, 128], BF16, 'kbf')
        for p in range(2):
            S.dma('sp', kst[:, p, :], self.peer_keys[l, p], [], ['kst'])
        self.cp('dve', kbf[:], kst[:], ['kst'], ['kbf'])
        pst = self.psbf(2)
        for p in range(2):
            self.tr(pst[:, p * 128:(p + 1) * 128], kbf[:, p, :], ['kbf', 'ident'], [('ps', 2)])
        self.cp('act', kT[:], pst[:, 0:256].rearrange("p (k t) -> p k t", k=2), [('ps', 2)], ['kT'])
        S.barrier()
        A.release(mk0)
        G2 = A.alloc([128, D], F32, 'G2')
        SH2 = A.alloc([128, D], F32, 'SH2')
        g2 = A.alloc([128, D], F32, 'g2')
        fss = A.alloc([128, 4], F32, 'fss')
        if last:
            fing = A.alloc([128, D], F32, 'fing')
            self.load_row_bc(fing[:], self.fin_g[0:1, :], 'fing')
        nb = self.alloc_norm_bufs(nxt=4)
        h2Ts = [A.alloc([128, 8, 256], BF16, 'h2T') for _ in range(2)]
        qT = A.alloc([128, 16, 256], BF16, 'qT')
        s_sb = A.alloc([128, 2048], F32, 's_sb')
        wqb = s_sb[:].bitcast(BF16).rearrange("p (k t) -> p k t", k=8)
        cand = A.alloc([128, 2048], F32, 'cand')
        ngt = cand[:, 0:D]
        sv = A.alloc([128, 256], F32, 'sv')
        si = A.alloc([128, 256], U32, 'si')
        sif = A.alloc([128, 256], F32, 'sif')
        wk = A.alloc([128, 2048], F32, 'wk')
        fv = A.alloc([128, 128], F32, 'fv')
        fp = A.alloc([128, 128], U32, 'fp')
        fpu = A.alloc([128, 128], U32, 'fpu')
        Aa = A.alloc([128, 128], F32, 'Aa')
        Bb = A.alloc([128, 128], F32, 'Bb')
        ijg = A.alloc([128, 3, 128], F32, 'ijg')
        ijgTs = [[A.alloc([128, 3, 128], F32, 'ijgT') for _ in range(2)] for _ in range(2)]
        gz = A.alloc([128, 16], F32, 'gz')
        RT = 16
        R1 = A.alloc([128, RT, 128], BF16, 'R1')
        R2 = A.alloc([128, RT, 128], BF16, 'R2')
        Gs = A.alloc([128, 128, 256], BF16, 'Gs')
        utl = [A.alloc([128, 8, 128], BF16, 'utl') for _ in range(6)]
        vl = [A.alloc([128, D], BF16, 'vl') for _ in range(6)]
        ga = [A.alloc([128, 512], F32, 'ga') for _ in range(3)]
        wT = [A.alloc([128, 512], BF16, 'wT') for _ in range(3)]
        etmp = A.alloc([128, D], F32, 'etmp')
        iota16 = self.iota[:, 0:16]
        nchunks = ntiles // 2
        gskeys = [('Gs', a, c) for a in range(2) for c in range(128 // RT)]
        items = [(b, ch) for b in range(NB) for ch in range(nchunks)]
        state = {'row': None, 'grow': None, 'ji': 0}
        xpars = {}

        def partA(seq):
            b, ch = items[seq]
            par = seq % 2
            h2T = h2Ts[par]
            hkey = ('h2T', par)
            row = b if ch < 16 else 2
            if row != state['row']:
                state['row'] = row
                self.load_row_bc(G2[:], self.mod[l, row:row + 1, 4 * D:5 * D], 'G2')
                self.load_row_bc(SH2[:], self.mod[l, row:row + 1, 3 * D:4 * D], 'SH2')
                self.load_row_bc(ngt, self.norm2_g[l:l + 1, :], 'cand')
                self.stt('dve', G2[:], G2[:], 1.0, ngt, ALU.add, ALU.mult, ['G2', 'cand'], ['G2'])
                yield
            xp = []
            for ti in range(2):
                tg = ch * 2 + ti
                xp.append(self.norm_tile(nb, self.xs[b, tg * 128:(tg + 1) * 128, :], G2, SH2, ['G2', 'SH2'],
                                         h2T[:, :, ti * 128:(ti + 1) * 128], hkey, 3))
                yield
            xpars[seq] = xp
            for c in range(4):
                S.dma('sp', wqb.rearrange("p k t -> p (k t)"), self.WQs[c], [('WQs', c)], ['swq'])
                for q in range(4):
                    hp = c * 4 + q
                    bank = 3
                    for kc in range(8):
                        self.mm(self.PS[:, bank, 0:256], wqb[:, kc, q * 128:(q + 1) * 128], h2T[:, kc, :], kc == 0, kc == 7,
                                ['swq', hkey], [('ps', bank)])
                        if kc % 2 == 1:
                            yield
                    self.cp('act', qT[:, hp, :], self.PS[:, bank, 0:256], [('ps', bank)], ['qT'])
                    yield
            for ti in range(2):
                for g4 in range(4):
                    bank = 3
                    for q in range(4):
                        hp = g4 * 4 + q
                        self.mm(self.PS[:, bank, q * 128:(q + 1) * 128], qT[:, hp, ti * 128:(ti + 1) * 128], kT[:, hp % 2, :],
                                True, True, ['qT', 'kT'], [('ps', bank)])
                    self.cp('act', s_sb[:, g4 * 512:(g4 + 1) * 512], self.PS[:, bank, :], [('ps', bank)], ['swq'])
                    yield
                tkA = [(s_sb[:, hp * 128:(hp + 1) * 128], sv[:, hp * 16:(hp + 1) * 16], si[:, hp * 16:(hp + 1) * 16],
                        wk[:, hp * 128:(hp + 1) * 128], ['swq'], ('sv', hp)) for hp in range(16)]
                for _ in self.topk16_multi(tkA):
                    yield
                svk = [('sv', hp, x) for hp in range(16) for x in ('v', 'v2')]
                sik = [('sv', hp, x) for hp in range(16) for x in ('i', 'i2')]
                self.cp('dve', sif[:], si[:], sik, ['sif'])
                sv4 = sv[:].rearrange("p (h t k) -> p h t k", h=8, t=2)
                sif4 = sif[:].rearrange("p (h t k) -> p h t k", h=8, t=2)
                cand4 = cand[:].rearrange("p (h a b) -> p h a b", h=8, a=16)
                self.tt('dve', cand4, sv4[:, :, 0, :].unsqueeze(3).to_broadcast([128, 8, 16, 16]),
                        sv4[:, :, 1, :].unsqueeze(2).to_broadcast([128, 8, 16, 16]), ALU.add, svk, ['cand'])
                yield
                tkB = [(cand[:, h * 256:(h + 1) * 256], fv[:, h * 16:(h + 1) * 16], fp[:, h * 16:(h + 1) * 16],
                        wk[:, h * 256:(h + 1) * 256], ['cand'], ('fv', h)) for h in range(8)]
                for _ in self.topk16_multi(tkB):
                    yield
                fvk = [('fv', h, x) for h in range(8) for x in ('v', 'v2')]
                fpk = [('fv', h, x) for h in range(8) for x in ('i', 'i2')]
                S.op('dve', lambda e: e.tensor_single_scalar(out=fpu[:], in_=fp[:], scalar=4, op=ALU.logical_shift_right), fpk, ['fpu'])
                self.cp('dve', Aa[:], fpu[:], ['fpu'], ['Aa'])
                S.op('dve', lambda e: e.tensor_single_scalar(out=fpu[:], in_=fp[:], scalar=15, op=ALU.bitwise_and), fpk + ['fpu'], ['fpu'])
                self.cp('dve', Bb[:], fpu[:], ['fpu'], ['Bb'])
                yield
                fv3 = fv[:].rearrange("p (h k) -> p h k", h=8)
                g3 = ijg[:, 2, :].rearrange("p (h k) -> p h k", h=8)
                self.tt('dve', g3, fv3, fv3[:, :, 0:1].to_broadcast([128, 8, 16]), ALU.subtract, fvk, [('ijg', 2)])
                self.act(ijg[:, 2, :], ijg[:, 2, :], AF.Exp, [('ijg', 2)], [('ijg', 2)])
                io4 = iota16.unsqueeze(1).unsqueeze(1).to_broadcast([128, 8, 16, 16])
                for (sel, t_, slot, skey) in ((Aa, 0, 0, 'Aa'), (Bb, 1, 1, 'Bb')):
                    sel4 = sel[:].rearrange("p (h k) -> p h k", h=8).unsqueeze(3).to_broadcast([128, 8, 16, 16])
                    self.tt('dve', cand4, sel4, io4, ALU.is_equal, [skey, 'iota', 'cand'], ['cand'])
                    self.tt('dve', cand4, cand4, sif4[:, :, t_, :].unsqueeze(2).to_broadcast([128, 8, 16, 16]), ALU.mult,
                            ['cand', 'sif'], ['cand'])
                    S.op('dve', lambda e, slot=slot: e.tensor_reduce(
                        out=ijg[:, slot, :].rearrange("p (h k) -> p h k", h=8), in_=cand4, axis=AX.X, op=ALU.add),
                        ['cand'], [('ijg', slot)])
                    yield
                S.op('dve', lambda e: e.tensor_reduce(out=gz[:, 0:8], in_=g3, axis=AX.X, op=ALU.add), [('ijg', 2)], ['gz'])
                S.op('dve', lambda e: e.reciprocal(out=gz[:, 0:8], in_=gz[:, 0:8]), ['gz'], ['gz'])
                self.tt('dve', g3, g3, gz[:, 0:8].unsqueeze(2).to_broadcast([128, 8, 16]), ALU.mult, [('ijg', 2), 'gz'], [('ijg', 2)])
                for s3 in range(3):
                    self.tr(self.PS[:, 3, s3 * 128:(s3 + 1) * 128], ijg[:, s3, :], [('ijg', s3), 'identf'], [('ps', 3)], f32=True)
                self.cp('act', ijgTs[par][ti][:], self.PS[:, 3, 0:384].rearrange("p (s t) -> p s t", s=3), [('ps', 3)],
                        [('ijgT', par, ti)])
                yield

        def partB(seq):
            par = seq % 2
            bi = 0
            for ti in range(2):
                ijgT = ijgTs[par][ti]
                ik = ('ijgT', par, ti)
                for rq in range(128 // RT):
                    tq = rq * RT
                    iob = self.iota[:].unsqueeze(1).to_broadcast([128, RT, 128])
                    self.tt('dve', R1[:], iob, ijgT[:, 0, tq:tq + RT].unsqueeze(2).to_broadcast([128, RT, 128]), ALU.is_equal,
                            ['iota', ik], ['R1'])
                    self.tt('dve', R2[:], iob, ijgT[:, 1, tq:tq + RT].unsqueeze(2).to_broadcast([128, RT, 128]), ALU.is_equal,
                            ['iota', ik], ['R2'])
                    self.tt('dve', R1[:], R1[:], ijgT[:, 2, tq:tq + RT].unsqueeze(2).to_broadcast([128, RT, 128]), ALU.mult,
                            ['R1', ik], ['R1'])
                    for t4 in range(RT // 4):
                        bank = bi % 4
                        bi += 1
                        for t_ in range(4):
                            t = t4 * 4 + t_
                            self.mm(self.PS[:, bank, t_ * 128:(t_ + 1) * 128], R1[:, t, :], R2[:, t, :], True, True, ['R1', 'R2'],
                                    [('ps', bank, 0), ('ps', bank, 1), ('ps', bank)])
                        tok0 = ti * 128 + tq + t4 * 4
                        self.cp('act' if t4 % 2 == 0 else 'dve', Gs[:, :, tok0:tok0 + 4],
                                self.PS[:, bank, :].rearrange("p (t j) -> p j t", t=4),
                                [('ps', bank, 0), ('ps', bank, 1), ('ps', bank)], [('Gs', ti, rq)])

        def jloop(seq, gen):
            par = seq % 2
            h2T = h2Ts[par]
            hkey = ('h2T', par)
            LA = 2
            base = state['ji']

            def a_pair(pp):
                pi_ = base + pp
                u = pi_ % 3
                bank = pi_ % 3
                for jj in (2 * pp, 2 * pp + 1):
                    b6 = (2 * pi_ + jj % 2) % 6
                    S.dma('sp', utl[b6][:].rearrange("p k t -> p (k t)"), self.UTs[jj], [('UTs', jj)], [('utl', b6)])
                    S.dma('sp', vl[b6][:], self.Vs[jj], [('Vs', jj)], [('vl', b6)])
                    pa = self.PS[:, bank, (jj % 2) * 256:(jj % 2 + 1) * 256]
                    for kc in range(8):
                        self.mm(pa, utl[b6][:, kc, :], h2T[:, kc, :], kc == 0, kc == 7, [('utl', b6), hkey], [('ps', bank, 0)])
                self.act(ga[u][:], self.PS[:, bank, :], AF.Gelu, [('ps', bank, 0)], [('ga', u)])
                self.tt('dve', wT[u][:], ga[u][:], Gs[:, 2 * pp:2 * pp + 2, :].rearrange("p j t -> p (j t)"), ALU.mult,
                        [('ga', u)] + gskeys, [('wT', u)])

            def f_pair(pp):
                pi_ = base + pp
                u = pi_ % 3
                for jj in (2 * pp, 2 * pp + 1):
                    b6 = (2 * pi_ + jj % 2) % 6
                    for ti in range(2):
                        for hf in range(2):
                            self.mm(self.PS[:, 4 + 2 * ti + hf, :], wT[u][:, (jj % 2) * 256 + ti * 128:(jj % 2) * 256 + (ti + 1) * 128],
                                    vl[b6][:, hf * 512:(hf + 1) * 512], jj == 0, jj == 127, [('wT', u), ('vl', b6)],
                                    [('ps', 4 + 2 * ti + hf)])
            for step in range(64 + LA):
                if step < 64:
                    a_pair(step)
                if step >= LA:
                    f_pair(step - LA)
                if gen is not None:
                    for _ in range(6):
                        if next(gen, 'done') == 'done':
                            gen = None
                            break
            state['ji'] += 64
            if gen is not None:
                for _ in gen:
                    pass

        def epilogue(seq):
            b, ch = items[seq]
            row = b if ch < 16 else 2
            if row != state['grow']:
                state['grow'] = row
                self.load_row_bc(g2[:], self.mod[l, row:row + 1, 5 * D:6 * D], 'g2')
            for ti in range(2):
                tg = ch * 2 + ti
                px = xpars[seq][ti]
                xt = nb['xt'][px]
                xk = ('xt', px)
                for hf in range(2):
                    self.tt('dve', etmp[:, hf * 512:(hf + 1) * 512], self.PS[:, 4 + 2 * ti + hf, :], g2[:, hf * 512:(hf + 1) * 512],
                            ALU.mult, [('ps', 4 + 2 * ti + hf), 'g2'], [('etmp', hf)])
                self.tt('pool', etmp[:], etmp[:], xt[:], ALU.add, [('etmp', 0), ('etmp', 1), xk], [('etmp', 0), ('etmp', 1)])
                ek = [('etmp', 0), ('etmp', 1)]
                if not last:
                    S.dma('pool', self.xs[b, tg * 128:(tg + 1) * 128, :], etmp[:], ek, [('xs', b, tg)])
                else:
                    self.act(nb['junk'], etmp[:], AF.Square, ek, ['ntmp', 'fss'], accum_out=fss[:, 0:1])
                    self.act(fss[:, 2:3], fss[:, 0:1], AF.Sqrt, ['fss', 'eps'], ['frs'], scale=1.0 / D, bias=self.epsT[:, 0:1])
                    S.op('dve', lambda e: e.reciprocal(out=fss[:, 2:3], in_=fss[:, 2:3]), ['frs'], ['frs'])
                    self.stt('dve', etmp[:], etmp[:], fss[:, 2:3], fing[:], ALU.mult, ALU.mult, ek + ['frs', 'fing'], ek)
                    S.dma('pool', self.out[b, tg * 128:(tg + 1) * 128, :], etmp[:], ek, [('out', b, tg)])

        for _ in partA(0):
            pass
        partB(0)
        for seq in range(len(items)):
            gen = partA(seq + 1) if seq + 1 < len(items) else None
            jloop(seq, gen)
            epilogue(seq)
            if seq + 1 < len(items):
                partB(seq + 1)
        S.barrier()
        A.release(mk)

    Builder.topk16_multi = topk16_multi
    Builder.phase_peer_prep = phase_peer_prep
    Builder.phase_peer = phase_peer


_peer_methods()


LAM_INIT1 = 0.8 - 0.6 * math.exp(-0.3 * 1)


def _layer1_methods():
    def phase_proj1(self, l=1):
        S, A = self.S, self.A
        mk = A.mark()
        wA = A.alloc([128, 8, 3072], BF16, 'wA')
        m2 = self.load_w_bf16(wA, self.cd_w, 1952, 'wA', piece=488)
        S.barrier()
        A.release(m2)
        self.make_rot(wA, 416, 1952, 16, 16, 'wA')
        self.make_rot(wA, 928, 2464, 16, 16, 'wA')
        self.make_rot(wA, 384, 2976, 2, 8, 'wA')
        Wg = A.alloc([128, 2, 768], BF16, 'Wg')
        Wgr = A.alloc([128, 2, 768], BF16, 'Wgr')
        Wkv = A.alloc([128, 1024], BF16, 'Wkv')
        onesf = A.alloc([128, 128], BF16, 'onesf')
        S.op('pool', lambda e: e.memset(onesf[:], 1.0), [], ['onesf'])
        m3 = A.mark()
        wst = A.alloc([128, 1024], F32, 'w1st')
        gq = A.alloc([128, 2], F32, 'gq')
        gkv = A.alloc([128, 1], F32, 'gkv')
        for c in range(2):
            S.dma('sp', gq[:, c:c + 1], self.qn_g[c * 128:(c + 1) * 128, :], [], ['gq'])
        S.dma('sp', gkv[:], self.kvn_g, [], ['gkv'])
        for c in range(2):
            S.dma('sp', wst[:, 0:768], self.w_uq[c * 128:(c + 1) * 128, :], ['w1st'], ['w1st'])
            self.ts('dve', Wg[:, c, :], wst[:, 0:768], gq[:, c:c + 1], None, ALU.mult, None, ['w1st', 'gq'], ['Wg'])
        S.op('pool', lambda e: e.memset(Wgr[:], 0.0), [], ['Wgr'])
        for c in range(2):
            s = Wg[:, c, :].rearrange("p (h f) -> p h f", f=96)[:, :, 64:96].rearrange("p h (b t e) -> p h b t e", b=2, t=2)
            d = Wgr[:, c, :].rearrange("p (h f) -> p h f", f=96)[:, :, 64:96].rearrange("p h (b t e) -> p h b t e", b=2, t=2)
            for bb in range(2):
                S.op('act', lambda e, s=s, d=d, bb=bb: e.mul(out=d[:, :, bb, 0, :], in_=s[:, :, bb, 1, :], mul=-1.0), ['Wg', 'Wgr'], ['Wgr'])
                self.cp('pool', d[:, :, bb, 1, :], s[:, :, bb, 0, :], ['Wg', 'Wgr'], ['Wgr'])
        S.dma('sp', wst[:], self.w_ukv, ['w1st'], ['w1st'])
        self.ts('dve', Wkv[:], wst[:], gkv[:, 0:1], None, ALU.mult, None, ['w1st', 'gkv'], ['Wkv'])
        S.barrier()
        A.release(m3)
        if self.sub == 'w':
            return
        for b in range(NB):
            self.proj_batch1(l, b, wA, Wg, Wgr, Wkv, onesf)
            if self.sub is not None:
                break
        S.barrier()
        A.release(mk)

    def proj_batch1(self, l, b, wA, Wg, Wgr, Wkv, onesf):
        S, A = self.S, self.A
        mk = A.mark()
        Gb, SHb = self.load_norm_mod(l, b, 1, 'b')
        Gc, SHc = self.load_norm_mod(l, 2, 1, 'c')
        nb = self.alloc_norm_bufs()
        hTs = [A.alloc([128, 8, 512], BF16, 'hT') for _ in range(2)]
        stg = [A.alloc([128, 512], BF16, 'stg') for _ in range(3)]
        t1 = A.alloc([128, 512], F32, 't1')
        t2 = A.alloc([128, 512], F32, 't2')
        rC = A.alloc([128, 512], F32, 'rC')
        rS = A.alloc([128, 512], F32, 'rS')
        rCm = A.alloc([128, 512], F32, 'rCm')
        rSm = A.alloc([128, 512], F32, 'rSm')
        rCk = A.alloc([128, 512], F32, 'rCk')
        rSk = A.alloc([128, 512], F32, 'rSk')
        cqb = A.alloc([128, 2, 512], BF16, 'cqb')
        sq = A.alloc([128, 512], BF16, 'sq')
        rq = A.alloc([128, 512], F32, 'rq')
        rkv = A.alloc([128, 512], F32, 'rkv')
        ckvf = A.alloc([128, 512], F32, 'ckvf')
        ckvn = A.alloc([128, 512], BF16, 'ckvn')
        krs = A.alloc([128, 512], BF16, 'krs')
        VW = 1036
        vst = [A.alloc([128, VW], BF16, 'vst') for _ in range(2)]
        for v in vst:
            S.op('pool', lambda e, v=v: e.memset(v[:], 1.0), [], [('vst', 0), ('vst', 1)])
        si = 0
        pi = 0
        for ci in range(9):
            ntile = 4 if ci < 8 else 2
            n = ntile * 128
            t0 = ci * 512
            hT = hTs[ci % 2]
            hkey = ('hT', ci % 2)
            G, SH, gk = (Gb, SHb, ['Gb', 'SHb']) if ci < 8 else (Gc, SHc, ['Gc', 'SHc'])
            for ti in range(ntile):
                tg = ci * 4 + ti
                self.norm_tile(nb, self.src(l, b, tg), G, SH, gk, hT[:, :, ti * 128:(ti + 1) * 128], hkey, 6)
            S.dma('sp', rC[:, 0:n], self.ropeC[:, t0:t0 + n], [], ['rC'])
            S.dma('sp', rS[:, 0:n], self.ropeS[:, t0:t0 + n], [], ['rS'])
            S.dma('sp', rCm[0:96, 0:n], self.ropeCm[:, t0:t0 + n], [], ['rCm'])
            S.dma('sp', rSm[0:96, 0:n], self.ropeSm[:, t0:t0 + n], [], ['rSm'])
            S.dma('sp', rCk[0:32, 0:n], self.ropeCm[64:96, t0:t0 + n], [], ['rCk'])
            S.dma('sp', rSk[0:32, 0:n], self.ropeSm[64:96, t0:t0 + n], [], ['rSk'])

            def proj(bank, col0, m, nn=n, hT=hT, hkey=hkey):
                for kc in range(8):
                    self.mm(self.PS[0:m, bank, 0:nn], wA[:, kc, col0:col0 + m], hT[:, kc, 0:nn], kc == 0, kc == 7,
                            ['wA', hkey], [('ps', bank)])
            if self.sub == 'c0':
                break
            for c in range(2):
                proj(c, 128 * c, 128)
                self.cp('dve', cqb[:, c, 0:n], self.PS[:, c, 0:n], [('ps', c)], [('cqb', c)])
                if self.sub == 'c0a':
                    continue
                self.cp('dve', ckvf[:, 0:n], self.PS[:, c, 0:n], [('ps', c)], ['ckvf'])
                self.act(sq[:, 0:n], ckvf[:, 0:n], AF.Square, ['ckvf'], ['sq'])
                self.mm(self.PS[:, 7, 0:n], onesf[:], sq[:, 0:n], c == 0, c == 1, ['onesf', 'sq'], [('ps', 7)])
            if self.sub != 'c0a':
                self.cp('dve', rq[:, 0:n], self.PS[:, 7, 0:n], [('ps', 7)], ['rq'])
                self.act(rq[:, 0:n], rq[:, 0:n], AF.Sqrt, ['rq', 'eps'], ['rq'], scale=1.0 / 256, bias=self.epsT[:, 0:1])
                S.op('dve', lambda e, n=n: e.reciprocal(out=rq[:, 0:n], in_=rq[:, 0:n]), ['rq'], ['rq'])
            if self.sub == 'c0a':
                break
            if self.sub == 'c0b':
                break
            proj(2, 256, 128)
            self.cp('dve', ckvf[:, 0:n], self.PS[:, 2, 0:n], [('ps', 2)], ['ckvf'])
            self.act(sq[:, 0:n], ckvf[:, 0:n], AF.Square, ['ckvf'], ['sq'])
            self.mm(self.PS[:, 7, 0:n], onesf[:], sq[:, 0:n], True, True, ['onesf', 'sq'], [('ps', 7)])
            self.cp('dve', rkv[:, 0:n], self.PS[:, 7, 0:n], [('ps', 7)], ['rkv'])
            self.act(rkv[:, 0:n], rkv[:, 0:n], AF.Sqrt, ['rkv', 'eps'], ['rkv'], scale=1.0 / 128, bias=self.epsT[:, 0:1])
            S.op('dve', lambda e, n=n: e.reciprocal(out=rkv[:, 0:n], in_=rkv[:, 0:n]), ['rkv'], ['rkv'])
            self.tt('dve', ckvn[:, 0:n], ckvf[:, 0:n], rkv[:, 0:n], ALU.mult, ['ckvf', 'rkv'], ['ckvn'])
            if self.sub == 'c1':
                break
            proj(0, 384, 32)
            proj(2, 2976, 32)
            self.tt('dve', t1[0:32, 0:n], self.PS[0:32, 0, 0:n], rCk[0:32, 0:n], ALU.mult, [('ps', 0), 'rCk'], ['t1'])
            self.tt('dve', t2[0:32, 0:n], self.PS[0:32, 2, 0:n], rSk[0:32, 0:n], ALU.mult, [('ps', 2), 'rSk'], ['t2'])
            self.tt('pool', krs[0:32, 0:n], t1[0:32, 0:n], t2[0:32, 0:n], ALU.add, ['t1', 't2'], ['krs'])
            for h in range(8):
                S.dma('pool', self.KT[b, h, 64:96, t0:t0 + n], krs[0:32, 0:n], ['krs'], [('K', b, h, 1)])
            if self.sub == 'c2':
                break
            for h in range(8):
                pa = pi % 2
                pi += 1
                for c in range(2):
                    self.mm(self.PS[0:96, pa, 0:n], Wg[:, c, h * 96:(h + 1) * 96], cqb[:, c, 0:n], c == 0, c == 1,
                            ['Wg', ('cqb', 0), ('cqb', 1)], [('ps', pa)])
                for c in range(2):
                    self.mm(self.PS[0:96, 2 + pa, 0:n], Wgr[:, c, h * 96:(h + 1) * 96], cqb[:, c, 0:n], c == 0, c == 1,
                            ['Wgr', ('cqb', 0), ('cqb', 1)], [('ps', 2 + pa)])
                sg = stg[si % 3]
                skey = ('stg', si % 3)
                si += 1
                self.tt('dve', t1[0:96, 0:n], self.PS[0:96, pa, 0:n], rCm[0:96, 0:n], ALU.mult, [('ps', pa), 'rCm'], ['t1'])
                self.tt('dve', t2[0:96, 0:n], self.PS[0:96, 2 + pa, 0:n], rSm[0:96, 0:n], ALU.mult, [('ps', 2 + pa), 'rSm'], ['t2'])
                self.tt('pool', t1[0:96, 0:n], t1[0:96, 0:n], t2[0:96, 0:n], ALU.add, ['t1', 't2'], ['t1'])
                self.tt('pool', sg[0:96, 0:n], t1[0:96, 0:n], rq[0:96, 0:n], ALU.mult, ['t1', 'rq'], [skey])
                S.dma('pool', self.QT[b, h, 0:96, t0:t0 + n], sg[0:96, 0:n], [skey], [('Q', b, h)])
                pa = pi % 2
                pi += 1
                self.mm(self.PS[0:64, pa, 0:n], Wkv[:, h * 128:h * 128 + 64], ckvn[:, 0:n], True, True, ['Wkv', 'ckvn'], [('ps', pa)])
                sg = stg[si % 3]
                skey = ('stg', si % 3)
                si += 1
                self.cp('act', sg[0:64, 0:n], self.PS[0:64, pa, 0:n], [('ps', pa)], [skey])
                S.dma('pool', self.KT[b, h, 0:64, t0:t0 + n], sg[0:64, 0:n], [skey], [('K', b, h, 0)])
            if self.sub == 'c3':
                break
            for (dst, idx, col0, rot0) in ([('Q', 8 + d, 416 + 128 * d, 1952 + 128 * d) for d in range(4)]
                                           + [('K', 8 + d, 928 + 128 * d, 2464 + 128 * d) for d in range(4)]):
                pa = pi % 2
                pi += 1
                proj(pa, col0, 128)
                proj(2 + pa, rot0, 128)
                sg = stg[si % 3]
                skey = ('stg', si % 3)
                si += 1
                self.tt('dve', t1[:, 0:n], self.PS[:, pa, 0:n], rC[:, 0:n], ALU.mult, [('ps', pa), 'rC'], ['t1'])
                self.tt('dve', t2[:, 0:n], self.PS[:, 2 + pa, 0:n], rS[:, 0:n], ALU.mult, [('ps', 2 + pa), 'rS'], ['t2'])
                self.tt('pool', sg[:, 0:n], t1[:, 0:n], t2[:, 0:n], ALU.add, ['t1', 't2'], [skey])
                dram = self.QT if dst == 'Q' else self.KT
                S.dma('pool', dram[b, idx, :, t0:t0 + n], sg[:, 0:n], [skey], [(dst, b, idx)])
            if self.sub == 'c4':
                break
            for ti in range(ntile):
                tg = ci * 4 + ti
                vs = vst[tg % 2]
                vkey = ('vst', tg % 2)
                self.mm(self.PS[:, 4, :], ckvn[:, ti * 128:(ti + 1) * 128],
                        Wkv[:].rearrange("p (h f) -> p h f", f=128)[:, :, 64:128], True, True, ['ckvn', 'Wkv'], [('ps', 4)])
                self.cp('act', vs[:, 0:520].rearrange("p (h d) -> p h d", d=65)[:, :, 0:64],
                        self.PS[:, 4, :].rearrange("p (h d) -> p h d", d=64), [('ps', 4)], [vkey])
                for kc in range(8):
                    self.mm(self.PS[:, 5, :], hT[:, kc, ti * 128:(ti + 1) * 128], wA[:, kc, 1440:1952], kc == 0, kc == 7,
                            ['wA', hkey], [('ps', 5)])
                self.cp('act', vs[:, 520:1036].rearrange("p (h d) -> p h d", d=129)[:, :, 0:128],
                        self.PS[:, 5, :].rearrange("p (h d) -> p h d", d=128), [('ps', 5)], [vkey])
                S.dma('pool', self.V[b, tg * 128:(tg + 1) * 128, 0:VW], vs[:], [vkey], [('V', b)])
            if self.sub == 'c5':
                break
        S.barrier()
        A.release(mk)

    def phase_attn1(self):
        S, A = self.S, self.A
        mk = A.mark()
        self.attn_setup()
        lam = A.alloc([128, 8], F32, 'lam')
        dl = A.alloc([128, 256], F32, 'dl')
        self.load_row_bc(dl[:], self.dlam[0:1, :], 'dl')
        dl4 = dl[:].rearrange("p (a d) -> p a d", a=4)
        self.tt('dve', dl4[:, 0, :], dl4[:, 0, :], dl4[:, 1, :], ALU.mult, ['dl'], ['dl'])
        self.tt('dve', dl4[:, 2, :], dl4[:, 2, :], dl4[:, 3, :], ALU.mult, ['dl'], ['dl'])
        S.op('dve', lambda e: e.tensor_reduce(out=lam[:, 0:1], in_=dl4[:, 0, :], axis=AX.X, op=ALU.add), ['dl'], ['lam'])
        S.op('dve', lambda e: e.tensor_reduce(out=lam[:, 1:2], in_=dl4[:, 2, :], axis=AX.X, op=ALU.add), ['dl', 'lam'], ['lam'])
        self.act(lam[:, 0:2], lam[:, 0:2], AF.Exp, ['lam'], ['lam'])
        self.tt('dve', lam[:, 2:3], lam[:, 1:2], lam[:, 0:1], ALU.subtract, ['lam'], ['lam'])
        self.ts('dve', lam[:, 3:4], lam[:, 2:3], -LAM_INIT1, None, ALU.add, None, ['lam'], ['lam'])
        subg = A.alloc([128, 128], F32, 'subg')
        self.load_row_bc(subg[:], self.subln[0:1, :], 'subg')
        self.ts('dve', subg[:], subg[:], 1.0 - LAM_INIT1, None, ALU.mult, None, ['subg'], ['subg'])
        Qs = [A.alloc([128, SL], BF16, 'Qs') for _ in range(2)]
        Ks = [A.alloc([128, SA], BF16, 'Ks') for _ in range(2)]
        Vsl = [A.alloc([128, NT, 129], BF16, 'Vsl') for _ in range(2)]
        ystg = [A.alloc([128, 128], BF16, 'ystg') for _ in range(4)]
        o1 = [A.alloc([128, 128], F32, 'o1') for _ in range(4)]
        o2 = [A.alloc([128, 128], F32, 'o2') for _ in range(2)]
        oj = A.alloc([128, 128], BF16, 'oj')
        oss = A.alloc([128, 8], F32, 'oss')
        gi = 0
        yi = 0
        for b in range(NB):
            for grp in range(12):
                p = gi % 2
                gi += 1
                mla = grp < 8
                rows = 96 if mla else 128
                dv = 64 if mla else 128
                vcol0 = grp * 65 if mla else 520 + (grp - 8) * 129
                S.dma('sp', Qs[p][0:rows, :], self.QT[b, grp, 0:rows, 0:SL], [('Q', b, grp)], [('Qs', p)])
                S.dma('sp', Ks[p][0:rows, :], self.KT[b, grp, 0:rows, :],
                      [('K', b, grp), ('K', b, grp, 0), ('K', b, grp, 1)], [('Ks', p)])
                S.dma('sp', Vsl[p][:, :, 0:dv + 1],
                      self.V[b, :, vcol0:vcol0 + dv + 1].rearrange("(t p) c -> p t c", p=128), [('V', b)], [('Vs', p)])
                for qc in range(8):
                    if mla:
                        kts = [(Ks[p][0:96, kt * 128:(kt + 1) * 128], Vsl[p][:, kt, 0:65], None, [('Ks', p), ('Vs', p)])
                               for kt in range(NT)]

                        def out_fn(qs, acc, rden, keys, qc=qc, grp=grp, b=b):
                            nonlocal yi
                            ys = ystg[yi % 4]
                            ykey = ('ystg', yi % 4)
                            yi += 1
                            self.ts('dve', ys[:, 0:64], acc[:, 0:64], rden, None, ALU.mult, None, keys, [ykey])
                            tg = qc * 4 + qs
                            S.dma('pool', self.Y[b, tg * 128:(tg + 1) * 128, grp * 64:(grp + 1) * 64], ys[:, 0:64], [ykey], [('Y', b)])
                        self.attn_unit(Qs[p][0:96, qc * 512:(qc + 1) * 512], 512, kts, 64, 96 ** -0.5, [('Qs', p)], out_fn)
                    else:
                        d = grp - 8
                        for w in range(2):
                            kts = [(Ks[p][w * 64:(w + 1) * 64, kt * 128:(kt + 1) * 128], Vsl[p][:, kt, 0:129], None,
                                    [('Ks', p), ('Vs', p)]) for kt in range(NT)]

                            def out_fn(qs, acc, rden, keys, qc=qc, d=d, b=b, w=w):
                                nonlocal yi
                                if w == 0:
                                    self.ts('dve', o1[qs][:], acc[:, 0:128], rden, None, ALU.mult, None, keys, [('o1', qs)])
                                    return
                                oo = o2[qs % 2]
                                ok = ('o2', qs % 2)
                                self.ts('dve', oo[:], acc[:, 0:128], rden, None, ALU.mult, None, keys, [ok])
                                self.stt('dve', oo[:], oo[:], lam[:, 3:4], o1[qs][:], ALU.mult, ALU.add, [ok, ('o1', qs), 'lam'], [ok])
                                sk = ('oss', qs % 2)
                                c0 = qs % 2
                                self.act(oj[:], oo[:], AF.Square, [ok], ['oj', sk], accum_out=oss[:, c0:c0 + 1])
                                self.act(oss[:, 2 + c0:3 + c0], oss[:, c0:c0 + 1], AF.Sqrt, [sk, 'eps'], [sk], scale=1.0 / 128,
                                         bias=self.epsT[:, 0:1])
                                S.op('dve', lambda e, c0=c0: e.reciprocal(out=oss[:, 2 + c0:3 + c0], in_=oss[:, 2 + c0:3 + c0]), [sk], [sk])
                                ys = ystg[yi % 4]
                                ykey = ('ystg', yi % 4)
                                yi += 1
                                self.stt('dve', ys[:], oo[:], oss[:, 2 + c0:3 + c0], subg[:], ALU.mult, ALU.mult, [ok, sk, 'subg'], [ykey])
                                tg = qc * 4 + qs
                                S.dma('pool', self.Y[b, tg * 128:(tg + 1) * 128, 512 + d * 128:512 + (d + 1) * 128], ys[:], [ykey],
                                      [('Y', b)])
                            self.attn_unit(Qs[p][w * 64:(w + 1) * 64, qc * 512:(qc + 1) * 512], 512, kts, 128, 0.125, [('Qs', p)], out_fn)
        S.barrier()
        A.release(mk)

    Builder.phase_proj1 = phase_proj1
    Builder.proj_batch1 = proj_batch1
    Builder.phase_attn1 = phase_attn1


_layer1_methods()
```

```python
import math
import numpy as np
from contextlib import ExitStack
import concourse.bass as bass
import concourse.mybir as mybir
from concourse.bass_utils import run_bass_kernel_spmd

F32 = mybir.dt.float32
BF16 = mybir.dt.bfloat16
U32 = mybir.dt.uint32
AF = mybir.ActivationFunctionType
ALU = mybir.AluOpType
AX = mybir.AxisListType

NB = 2
D = 1024
SL = 4096
CT = 256
SA = 4352
NT = 34
EPS = 1e-6
NEG = -30000.0
SEM_ROT = 30000
N_CORES = 8


class Sched:
    ENGS = ('pe', 'act', 'dve', 'pool', 'sp')

    def __init__(self, nc, stack, n_lanes=28, same_engine_sync=True):
        self.nc = nc
        self.stack = stack
        self.prog = {e: [] for e in self.ENGS}
        self.cnt = {e: 0 for e in self.ENGS}
        self.esems = {e: [] for e in self.ENGS}
        self.seen = {e: {} for e in self.ENGS}
        self.res = {}
        self.lanes = []
        for i in range(n_lanes):
            s = stack.enter_context(nc.semaphore(f"lane{i}"))
            self.lanes.append([s, 0])
        self.lane_rr = 0
        self.same_engine_sync = same_engine_sync

    def _esem(self, e, n):
        k = (n - 1) // SEM_ROT
        while len(self.esems[e]) <= k:
            s = self.stack.enter_context(self.nc.semaphore(f"es_{e}_{len(self.esems[e])}"))
            self.esems[e].append(s)
        return self.esems[e][k], n - k * SEM_ROT

    def _deps(self, reads, writes):
        deps = []
        for r in reads:
            st = self.res.get(r)
            if st is not None and st['w'] is not None:
                deps.append(st['w'])
        for w in writes:
            st = self.res.get(w)
            if st is not None:
                if st['w'] is not None:
                    deps.append(st['w'])
                deps.extend(st['r'].values())
        return deps

    def _commit(self, ev, reads, writes):
        for r in reads:
            st = self.res.setdefault(r, {'w': None, 'r': {}})
            k = id(ev[1])
            if k not in st['r'] or st['r'][k][2] < ev[2]:
                st['r'][k] = ev
        for w in writes:
            self.res[w] = {'w': ev, 'r': {}}

    def _add_waits(self, e, deps):
        best = {}
        for (eng_src, sem, val) in deps:
            if eng_src == e and (e == 'pe' or not self.same_engine_sync):
                continue
            key = id(sem)
            if key not in best or best[key][1] < val:
                best[key] = (sem, val)
        for key, (sem, val) in best.items():
            if self.seen[e].get(key, 0) >= val:
                continue
            self.seen[e][key] = val
            self.prog[e].append(('wait', sem, val))

    def op(self, e, fn, reads=(), writes=()):
        deps = self._deps(reads, writes)
        self._add_waits(e, deps)
        self.cnt[e] += 1
        sem, val = self._esem(e, self.cnt[e])
        self.prog[e].append(('op', fn, sem, 1))
        ev = (e, sem, val)
        self._commit(ev, reads, writes)
        return ev

    def dma(self, q, out, in_, reads=(), writes=(), **kw):
        deps = self._deps(reads, writes)
        lane = self.lanes[self.lane_rr]
        self.lane_rr = (self.lane_rr + 1) % len(self.lanes)
        if lane[1] > 0:
            deps.append(('dma', lane[0], 16 * lane[1]))
        self._add_waits(q, deps)
        lane[1] += 1
        sem = lane[0]

        def fn(eng, out=out, in_=in_, kw=kw):
            return eng.dma_start(out=out, in_=in_, **kw)
        self.prog[q].append(('op', fn, sem, 16))
        ev = ('dma', sem, 16 * lane[1])
        self._commit(ev, reads, writes)
        return ev

    def barrier(self):
        evs = []
        for e in self.ENGS:
            if self.cnt[e] > 0:
                sem, val = self._esem(e, self.cnt[e])
                evs.append((e + '_b', sem, val))
        for lane in self.lanes:
            if lane[1] > 0:
                evs.append(('dma', lane[0], 16 * lane[1]))
        for e in self.ENGS:
            self._add_waits(e, evs)
        self.res = {}

    def emit(self):
        nc = self.nc
        engobj = {'pe': 'tensor', 'act': 'scalar', 'dve': 'vector', 'pool': 'gpsimd', 'sp': 'sync'}
        with nc.Block() as block:
            for e in self.ENGS:
                items = self.prog[e]
                if not items:
                    continue

                def body(eng, items=items):
                    for it in items:
                        if it[0] == 'wait':
                            eng.wait_ge(it[1], it[2])
                        else:
                            ins = it[1](eng)
                            ins.then_inc(it[2], it[3])
                getattr(block, engobj[e])(body)


class Arena:
    def __init__(self, nc, limit=229300):
        self.nc = nc
        self.off = 17408
        self.n = 0
        self.limit = limit

    def alloc(self, shape, dtype, name='t'):
        isz = 4 if dtype in (F32, U32) else 2
        nbytes = int(np.prod(shape[1:])) * isz
        nbytes = (nbytes + 63) // 64 * 64
        self.n += 1
        t = self.nc.alloc_sbuf_tensor_at(f"{name}_{self.n}", list(shape), dtype, offset=self.off)
        self.off += nbytes
        assert self.off <= self.limit, (name, self.off)
        return t

    def mark(self):
        return self.off

    def release(self, m):
        self.off = m


class Builder:
    def __init__(self, stop_after=None, skip_l0=False, sub=None):
        self.stop_after = stop_after
        self.skip_l0 = skip_l0
        self.sub = sub
        self.nc = nc = bass.Bass("TRN2", target_bir_lowering=False)
        self.stack = ExitStack()
        self.S = Sched(nc, self.stack)
        self.A = Arena(nc)
        di = lambda n, sh, dt=F32: nc.dram_tensor(n, list(sh), dt, kind="ExternalInput").ap()
        self.x = di("x", [NB, SL, D])
        self.ctx = di("ctx", [NB, CT, D])
        self.cc = di("cc", [3, D])
        self.ada_w = di("ada_w", [2, D, 6 * D])
        self.ada_b = di("ada_b", [2, 6 * D])
        self.norm1_g = di("norm1_g", [2, D])
        self.norm2_g = di("norm2_g", [2, D])
        self.w_out = di("w_out", [2, D, D])
        self.peer_wq = di("peer_wq", [2, D, 2048])
        self.peer_keys = di("peer_keys", [2, 2, 128, 128])
        self.peer_u = di("peer_u", [2, 16384, D])
        self.peer_v = di("peer_v", [2, 16384, D])
        self.ab_w = di("ab_w", [D, 2304])
        self.rpbT = di("rpbT", [128, 7680])
        self.maskI = di("maskI", [128, 7680])
        self.maskE = di("maskE", [128, 7680])
        self.swaL = di("swaL", [128, 128])
        self.swaU = di("swaU", [128, 128])
        self.sink = di("sink", [1, 8])
        self.cd_w = di("cd_w", [D, 1952])
        self.qn_g = di("qn_g", [256, 1])
        self.w_uq = di("w_uq", [256, 768])
        self.kvn_g = di("kvn_g", [128, 1])
        self.w_ukv = di("w_ukv", [128, 1024])
        self.dlam = di("dlam", [1, 256])
        self.subln = di("subln", [1, 128])
        self.fin_g = di("fin_g", [1, D])
        self.ropeC = di("ropeC", [128, SA])
        self.ropeS = di("ropeS", [128, SA])
        self.ropeCm = di("ropeCm", [96, SA])
        self.ropeSm = di("ropeSm", [96, SA])
        self.iota_in = di("iota_in", [128, 128])
        self.out = nc.dram_tensor("out", [NB, SL, D], F32, kind="ExternalOutput").ap()
        if stop_after is not None:
            self.dbg = nc.dram_tensor("dbg", [NB, SA, D], F32, kind="ExternalOutput").ap()
        ds = lambda n, sh, dt: nc.dram_tensor(n, list(sh), dt).ap()
        self.xs = ds("xs", [NB, SA, D], F32)
        self.mod = ds("mod", [2, 3, 6 * D], F32)
        self.QT = ds("QT", [NB, 12, 128, SA], BF16)
        self.KT = ds("KT", [NB, 12, 128, SA], BF16)
        self.V = ds("V", [NB, SA, 1040], BF16)
        self.Y = ds("Y", [NB, SA, D], BF16)
        self.UTs = ds("UTs", [128, 128, D], BF16)
        self.Vs = ds("Vs", [128, 128, D], BF16)
        self.WQs = ds("WQs", [4, 128, 8 * 512], BF16)
        self.PS = nc.alloc_psum_tensor("psall", [128, 8, 512], F32)

    def act(self, out, in_, func, r, w, **kw):
        self.S.op('act', lambda e: e.activation(out=out, in_=in_, func=func, **kw), r, w)

    def mm(self, out, lhsT, rhs, start, stop, r, w):
        self.S.op('pe', lambda e: e.matmul(out, lhsT=lhsT, rhs=rhs, start=start, stop=stop), r, w)

    def tr(self, out, in_, r, w, f32=False):
        idn = self.identf if f32 else self.ident
        self.S.op('pe', lambda e: e.transpose(out=out, in_=in_, identity=idn[:]), r, w)

    def tt(self, eng, out, in0, in1, op, r, w):
        self.S.op(eng, lambda e: e.tensor_tensor(out=out, in0=in0, in1=in1, op=op), r, w)

    def ts(self, eng, out, in0, s1, s2, op0, op1, r, w):
        if op1 is None:
            self.S.op(eng, lambda e: e.tensor_scalar(out=out, in0=in0, scalar1=s1, scalar2=None, op0=op0), r, w)
        else:
            self.S.op(eng, lambda e: e.tensor_scalar(out=out, in0=in0, scalar1=s1, scalar2=s2, op0=op0, op1=op1), r, w)

    def stt(self, eng, out, in0, scalar, in1, op0, op1, r, w):
        self.S.op(eng, lambda e: e.scalar_tensor_tensor(out=out, in0=in0, scalar=scalar, in1=in1, op0=op0, op1=op1), r, w)

    def cp(self, eng, out, in_, r, w):
        if eng == 'act':
            self.S.op('act', lambda e: e.copy(out=out, in_=in_), r, w)
        else:
            self.S.op(eng, lambda e: e.tensor_copy(out=out, in_=in_), r, w)

    def ps(self, bank, n=512):
        return self.PS[:, bank, 0:n]

    def psbf(self, bank):
        return self.PS[:, bank, :].bitcast(BF16)

    def src(self, layer, b, tg):
        if layer == 0:
            if tg < 32:
                return self.x[b, tg * 128:(tg + 1) * 128, :]
            return self.ctx[b, (tg - 32) * 128:(tg - 31) * 128, :]
        return self.xs[b, tg * 128:(tg + 1) * 128, :]

    def setup_consts(self):
        S, A = self.S, self.A
        self.ident = A.alloc([128, 128], BF16, 'ident')
        self.identf = A.alloc([128, 128], F32, 'identf')
        self.iota = A.alloc([128, 128], F32, 'iota')
        self.iotab = A.alloc([128, 128], BF16, 'iotab')
        self.epsT = A.alloc([128, 1], F32, 'eps')
        self.sinkexp = A.alloc([128, 8], F32, 'sinkexp')
        self.mL = A.alloc([128, 128], BF16, 'mL')
        self.mU = A.alloc([128, 128], BF16, 'mU')
        identf, ident = self.identf, self.ident
        S.op('pool', lambda e: e.memset(identf[:], 0.0), [], ['identf'])
        S.op('pool', lambda e: e.affine_select(out=identf[:], in_=identf[:], pattern=[[-1, 128]],
                                               compare_op=ALU.not_equal, fill=1.0, base=0, channel_multiplier=1),
             ['identf'], ['identf'])
        self.cp('dve', ident[:], identf[:], ['identf'], ['ident'])
        S.op('pool', lambda e: e.memset(self.epsT[:], EPS), [], ['eps'])
        S.dma('sp', self.iota[:], self.iota_in, [], ['iota'])
        self.cp('dve', self.iotab[:], self.iota[:], ['iota'], ['iotab'])
        S.dma('sp', self.sinkexp[:], self.sink[0:1, :].partition_broadcast(128)[:, 0, :], [], ['sinkexp'])
        self.act(self.sinkexp[:], self.sinkexp[:], AF.Exp, ['sinkexp'], ['sinkexp'])
        mk = A.mark()
        t0 = A.alloc([128, 128], F32, 'mstg')
        for (m_in, m_sb, key) in ((self.swaL, self.mL, 'mL'), (self.swaU, self.mU, 'mU')):
            S.dma('sp', t0[:, 0:128], m_in, ['t0'], ['t0'])
            self.cp('dve', m_sb[:], t0[:, 0:128], ['t0'], [key])
        S.barrier()
        A.release(mk)

    def phase_mod(self, l):
        S, A = self.S, self.A
        mk = A.mark()
        ccT = A.alloc([128, 8, 3], F32, 'ccT')
        for kc in range(8):
            S.dma('sp', ccT[:, kc, :], self.cc[:, kc * 128:(kc + 1) * 128].rearrange("r p -> p r"), [], ['ccT'],
                  allow_slow_non_contiguous=True)
        self.act(ccT[:], ccT[:], AF.Silu, ['ccT'], ['ccT'])
        modsb = A.alloc([3, 6 * D], F32, 'modsb')
        adab = A.alloc([3, 6 * D], F32, 'adab')
        S.dma('sp', adab[:], self.ada_b[l:l + 1, :].partition_broadcast(3)[:, 0, :], [], ['adab'])
        wb = [A.alloc([128, 8, 512], F32, 'modw') for _ in range(2)]
        for n in range(12):
            w = wb[n % 2]
            S.dma('sp', w[:], self.ada_w[l, :, n * 512:(n + 1) * 512].rearrange("(kc p) n -> p kc n", p=128),
                  [], [('modw', n % 2)])
            for kc in range(8):
                self.mm(self.PS[0:3, n % 2, :], ccT[:, kc, :], w[:, kc, :], kc == 0, kc == 7,
                        ['ccT', ('modw', n % 2)], [('ps', n % 2)])
            self.tt('dve', modsb[:, n * 512:(n + 1) * 512], self.PS[0:3, n % 2, :], adab[:, n * 512:(n + 1) * 512],
                    ALU.add, [('ps', n % 2), 'adab'], ['modsb'])
        S.dma('sp', self.mod[l], modsb[:], ['modsb'], ['mod'])
        S.barrier()
        A.release(mk)

    def load_row_bc(self, dst, src_row, key):
        self.S.dma('sp', dst, src_row.partition_broadcast(128)[:, 0, :], [], [key])

    def load_norm_mod(self, l, row, which, tag):
        A = self.A
        G = A.alloc([128, D], F32, 'G' + tag)
        SH = A.alloc([128, D], F32, 'SH' + tag)
        tmp = A.alloc([128, D], F32, 'ng' + tag)
        sh_off = 0 if which == 1 else 3
        ng = self.norm1_g if which == 1 else self.norm2_g
        self.load_row_bc(G[:], self.mod[l, row:row + 1, (sh_off + 1) * D:(sh_off + 2) * D], 'G' + tag)
        self.load_row_bc(SH[:], self.mod[l, row:row + 1, sh_off * D:(sh_off + 1) * D], 'SH' + tag)
        self.load_row_bc(tmp[:], ng[l:l + 1, :], 'ng' + tag)
        self.stt('dve', G[:], G[:], 1.0, tmp[:], ALU.add, ALU.mult, ['G' + tag, 'ng' + tag], ['G' + tag])
        return G, SH

    def load_gate(self, l, row, which, tag):
        A = self.A
        g = A.alloc([128, D], F32, 'gate' + tag)
        off = 2 if which == 1 else 5
        self.load_row_bc(g[:], self.mod[l, row:row + 1, off * D:(off + 1) * D], 'gate' + tag)
        return g

    def alloc_norm_bufs(self, nxt=2):
        A = self.A
        nb = {}
        nb['xt'] = [A.alloc([128, D], F32, 'xt') for _ in range(nxt)]
        nb['tmp'] = A.alloc([128, D], F32, 'ntmp')
        nb['junk'] = nb['tmp'][:].bitcast(BF16)[:, 0:D]
        nb['hb'] = [A.alloc([128, D], BF16, 'hb') for _ in range(2)]
        nb['ss'] = A.alloc([128, 4], F32, 'ss')
        nb['i'] = 0
        return nb

    def norm_tile(self, nb, src_ap, G, SH, gk, hT_dst, hT_key, psbank, xt_keep=False):
        S = self.S
        px = nb['i'] % len(nb['xt'])
        p = nb['i'] % 2
        nb['i'] += 1
        xt, hb, ss = nb['xt'][px], nb['hb'][p], nb['ss']
        S.dma('sp', xt[:], src_ap, [], [('xt', px)])
        self.act(nb['junk'], xt[:], AF.Square, [('xt', px)], ['ntmp', ('ss', p)], accum_out=ss[:, p:p + 1])
        self.act(ss[:, 2 + p:3 + p], ss[:, p:p + 1], AF.Sqrt, [('ss', p), 'eps'], [('rs', p)], scale=1.0 / D,
                 bias=self.epsT[:, 0:1])
        S.op('dve', lambda e: e.reciprocal(out=ss[:, 2 + p:3 + p], in_=ss[:, 2 + p:3 + p]), [('rs', p)], [('rs', p)])
        self.stt('dve', nb['tmp'][:], xt[:], ss[:, 2 + p:3 + p], G[:], ALU.mult, ALU.mult,
                 [('xt', px), ('rs', p)] + gk, ['ntmp'])
        self.tt('pool', hb[:], nb['tmp'][:], SH[:], ALU.add, ['ntmp'] + gk, [('hb', p)])
        pst = self.psbf(psbank)
        for kc in range(8):
            self.tr(pst[:, kc * 128:(kc + 1) * 128], hb[:, kc * 128:(kc + 1) * 128], [('hb', p), 'ident'],
                    [('ps', psbank)])
        self.cp('act', hT_dst, pst.rearrange("p (k t) -> p k t", k=8), [('ps', psbank)], [hT_key])
        return px

    def load_w_bf16(self, dst, src, ncols, key, piece=512):
        S, A = self.S, self.A
        mk = A.mark()
        stg = [A.alloc([128, 8, piece], F32, 'wstg') for _ in range(2)]
        i = 0
        for c0 in range(0, ncols, piece):
            n = min(piece, ncols - c0)
            s = stg[i % 2]
            S.dma('sp', s[:, :, 0:n], src[:, c0:c0 + n].rearrange("(kc p) n -> p kc n", p=128), [], [('wstg', i % 2)])
            self.cp('dve' if i % 2 == 0 else 'act', dst[:, :, c0:c0 + n], s[:, :, 0:n], [('wstg', i % 2)], [key])
            i += 1
        return mk

    def make_rot(self, W, src0, dst0, nblk, half, key):
        for kc in range(8):
            s = W[:, kc, src0:src0 + nblk * 2 * half].rearrange("p (b t h) -> p b t h", t=2, h=half)
            d = W[:, kc, dst0:dst0 + nblk * 2 * half].rearrange("p (b t h) -> p b t h", t=2, h=half)
            self.S.op('act', lambda e, s=s, d=d: e.mul(out=d[:, :, 0, :], in_=s[:, :, 1, :], mul=-1.0), [key], [key])
            self.cp('pool', d[:, :, 1, :], s[:, :, 0, :], [key], [key])

    def attn_setup(self):
        A = self.A
        self.E = [A.alloc([128, 512], BF16, 'E') for _ in range(4)]
        self.ei = 0
        self.sti = 0
        self.acci = 0
        self.den = A.alloc([128, 8], F32, 'den')

    def attn_unit(self, qT, nq, ktiles, dv, scale, qkeys, out_fn, sink_col=None):
        S = self.S
        nqs = nq // 128
        aset = self.acci % 2
        self.acci += 1
        accs = []
        for qs in range(nqs):
            bank = 4 + 2 * aset + qs // 2
            sub = qs % 2
            accs.append((self.PS[:, bank, sub * 256:sub * 256 + dv + 1], ('ps', bank, sub)))
        nk = len(ktiles)
        LA = 2
        live = {}

        def qk(ki):
            kT, v, bias, kkeys = ktiles[ki]
            sb = self.sti % 4
            self.sti += 1
            st = self.PS[:, sb, 0:nq]
            self.mm(st, kT, qT, True, bias is None, qkeys + kkeys, [('ps', sb)])
            if bias is not None:
                self.mm(st, self.ident[:], bias[0], False, True, ['ident'] + bias[1], [('ps', sb)])
            eb = self.ei % 4
            self.ei += 1
            E = self.E[eb]
            self.act(E[:, 0:nq], st, AF.Exp, [('ps', sb)], [('E', eb)], scale=scale)
            live[ki] = (E, eb)

        def pv(ki):
            kT, v, bias, kkeys = ktiles[ki]
            E, eb = live.pop(ki)
            for qs in range(nqs):
                self.mm(accs[qs][0], E[:, qs * 128:(qs + 1) * 128], v, ki == 0, ki == nk - 1,
                        [('E', eb)] + kkeys, [accs[qs][1]])
        for step in range(nk + LA):
            if step < nk:
                qk(step)
            if step >= LA:
                pv(step - LA)
        for qs in range(nqs):
            acc, akey = accs[qs]
            dcol = self.den[:, (aset * 4 + qs):(aset * 4 + qs) + 1]
            dkey = ('den', aset * 4 + qs)
            if sink_col is not None:
                self.tt('dve', dcol, acc[:, dv:dv + 1], self.sinkexp[:, sink_col:sink_col + 1], ALU.add,
                        [akey, 'sinkexp'], [dkey])
                S.op('dve', lambda e, dcol=dcol: e.reciprocal(out=dcol, in_=dcol), [dkey], [dkey])
            else:
                S.op('dve', lambda e, dcol=dcol, acc=acc: e.reciprocal(out=dcol, in_=acc[:, dv:dv + 1]), [akey], [dkey])
            out_fn(qs, acc, dcol, [akey, dkey])

    def phase_proj0(self, l=0):
        S, A = self.S, self.A
        mk = A.mark()
        wA = A.alloc([128, 8, 3328], BF16, 'wA')
        m2 = self.load_w_bf16(wA, self.ab_w, 2304, 'wA', piece=576)
        S.barrier()
        A.release(m2)
        self.make_rot(wA, 1536, 2304, 16, 16, 'wA')
        for kc in range(8):
            for r in range(4):
                self.cp('dve', wA[:, kc, 2816 + r * 64:2880 + r * 64], wA[:, kc, 2048 + (r // 2) * 64:2112 + (r // 2) * 64],
                        ['wA'], ['wA'])
        self.make_rot(wA, 2816, 3072, 8, 16, 'wA')
        rC = A.alloc([128, SA], F32, 'rC')
        rS = A.alloc([128, SA], F32, 'rS')
        S.dma('sp', rC[:], self.ropeC, [], ['rC'])
        S.dma('sp', rS[:], self.ropeS, [], ['rS'])
        fm = ([('Q', i, 128 * i, None) for i in range(4)] + [('K', i, 512 + 128 * i, None) for i in range(4)]
              + [('Q', 4 + i, 1536 + 128 * i, 2304 + 128 * i) for i in range(4)]
              + [('K', 4 + i, 2816 + 128 * i, 3072 + 128 * i) for i in range(2)])
        tmv = [(1024, 512, 0, 8, 64), (2176, 128, 8, 2, 64)]
        for b in range(NB):
            self.proj_batch(l, b, wA, fm, tmv, 10, rC, rS, None)
        S.barrier()
        A.release(mk)

    def proj_batch(self, l, b, wA, fm, tmv, nvh, rC, rS, extra):
        S, A = self.S, self.A
        mk = A.mark()
        Gb, SHb = self.load_norm_mod(l, b, 1, 'b')
        Gc, SHc = self.load_norm_mod(l, 2, 1, 'c')
        nb = self.alloc_norm_bufs()
        hTs = [A.alloc([128, 8, 512], BF16, 'hT') for _ in range(2)]
        stg = [A.alloc([128, 512], BF16, 'stg') for _ in range(3)]
        t1 = A.alloc([128, 512], F32, 't1')
        t2 = A.alloc([128, 512], F32, 't2')
        vdv = tmv[0][4]
        vw = sum(nh * (dvv + 1) for (_, _, _, nh, dvv) in tmv)
        vst = [A.alloc([128, vw], BF16, 'vst') for _ in range(2)]
        for v in vst:
            S.op('pool', lambda e, v=v: e.memset(v[:], 1.0), [], [('vst', 0), ('vst', 1)])
        si = 0
        pi = 0
        for ci in range(9):
            ntile = 4 if ci < 8 else 2
            n = ntile * 128
            t0 = ci * 512
            hT = hTs[ci % 2]
            hkey = ('hT', ci % 2)
            G, SH, gk = (Gb, SHb, ['Gb', 'SHb']) if ci < 8 else (Gc, SHc, ['Gc', 'SHc'])
            for ti in range(ntile):
                tg = ci * 4 + ti
                self.norm_tile(nb, self.src(l, b, tg), G, SH, gk, hT[:, :, ti * 128:(ti + 1) * 128], hkey, 6)
            for (dst, idx, col0, rot0) in fm:
                pa = pi % 2
                pi += 1
                for kc in range(8):
                    self.mm(self.PS[:, pa, 0:n], wA[:, kc, col0:col0 + 128], hT[:, kc, 0:n], kc == 0, kc == 7,
                            ['wA', hkey], [('ps', pa)])
                sg = stg[si % 3]
                skey = ('stg', si % 3)
                si += 1
                if rot0 is None:
                    self.cp('act', sg[:, 0:n], self.PS[:, pa, 0:n], [('ps', pa)], [skey])
                else:
                    for kc in range(8):
                        self.mm(self.PS[:, 2 + pa, 0:n], wA[:, kc, rot0:rot0 + 128], hT[:, kc, 0:n], kc == 0, kc == 7,
                                ['wA', hkey], [('ps', 2 + pa)])
                    self.tt('dve', t1[:, 0:n], self.PS[:, pa, 0:n], rC[:, t0:t0 + n], ALU.mult, [('ps', pa), 'rC'], ['t1'])
                    self.tt('dve', t2[:, 0:n], self.PS[:, 2 + pa, 0:n], rS[:, t0:t0 + n], ALU.mult, [('ps', 2 + pa), 'rS'], ['t2'])
                    self.tt('pool', sg[:, 0:n], t1[:, 0:n], t2[:, 0:n], ALU.add, ['t1', 't2'], [skey])
                dram = self.QT if dst == 'Q' else self.KT
                S.dma('pool', dram[b, idx, :, t0:t0 + n], sg[:, 0:n], [skey], [(dst, b, idx)])
            if extra is not None:
                extra(b, ci, t0, n, hT, hkey)
            for ti in range(ntile):
                tg = ci * 4 + ti
                vs = vst[tg % 2]
                vkey = ('vst', tg % 2)
                co = 0
                for gi, (col0, ncols, h0, nh, dvv) in enumerate(tmv):
                    bank = 4 + gi
                    for kc in range(8):
                        self.mm(self.PS[:, bank, 0:ncols], hT[:, kc, ti * 128:(ti + 1) * 128], wA[:, kc, col0:col0 + ncols],
                                kc == 0, kc == 7, ['wA', hkey], [('ps', bank)])
                    ov = vs[:, co:co + nh * (dvv + 1)].rearrange("p (h d) -> p h d", d=dvv + 1)[:, :, 0:dvv]
                    self.cp('act', ov, self.PS[:, bank, 0:ncols].rearrange("p (h d) -> p h d", d=dvv), [('ps', bank)], [vkey])
                    co += nh * (dvv + 1)
                S.dma('pool', self.V[b, tg * 128:(tg + 1) * 128, 0:vw], vs[:], [vkey], [('V', b)])
        S.barrier()
        A.release(mk)

    def phase_attn0(self):
        S, A = self.S, self.A
        mk = A.mark()
        self.TabI = A.alloc([128, 7680], BF16, 'TabI')
        self.TabE = A.alloc([128, 7680], BF16, 'TabE')
        mk2 = A.mark()
        t0 = A.alloc([128, 7680], F32, 'tabstg0')
        t1 = A.alloc([128, 7680], F32, 'tabstg1')
        S.dma('sp', t0[:], self.rpbT, [], ['t0'])
        for (msk, Tab, key) in ((self.maskI, self.TabI, 'TabI'), (self.maskE, self.TabE, 'TabE')):
            S.dma('sp', t1[:], msk, [], ['t1'])
            self.tt('dve', t1[:], t1[:], t0[:], ALU.add, ['t0', 't1'], ['t1'])
            self.ts('dve', Tab[:], t1[:], 8.0, None, ALU.mult, None, ['t1'], [key])
        S.barrier()
        A.release(mk2)
        self.attn_setup()
        Qs = [A.alloc([128, SA], BF16, 'Qs') for _ in range(2)]
        Ks = [A.alloc([128, SA], BF16, 'Ks') for _ in range(2)]
        Vsl = [A.alloc([128, NT, 130], BF16, 'Vsl') for _ in range(2)]
        ystg = [A.alloc([128, 128], BF16, 'ystg') for _ in range(2)]
        gi = 0
        yi = 0
        for b in range(NB):
            for grp in range(8):
                p = gi % 2
                gi += 1
                na = grp < 4
                c = grp if na else grp - 4
                qidx = grp
                kidx = c if na else 4 + c // 2
                nvc = 130 if na else 65
                vcol0 = (2 * c) * 65 if na else (8 + c // 2) * 65
                S.dma('sp', Qs[p][:], self.QT[b, qidx], [('Q', b, qidx)], [('Qs', p)])
                S.dma('sp', Ks[p][:], self.KT[b, kidx], [('K', b, kidx)], [('Ks', p)])
                S.dma('sp', Vsl[p][:, :, 0:nvc],
                      self.V[b, :, vcol0:vcol0 + nvc].rearrange("(t p) c -> p t c", p=128), [('V', b)], [('Vs', p)])
                for m in range(NT):
                    ys = ystg[yi % 2]
                    ykey = ('ystg', yi % 2)
                    yi += 1
                    for hh in range(2):
                        h = 2 * c + hh
                        pb = hh * 64
                        qT = Qs[p][pb:pb + 64, m * 128:(m + 1) * 128]
                        kts = []

                        def ktile(kt, bias):
                            vo = hh * 65 if na else 0
                            return (Ks[p][pb:pb + 64, kt * 128:(kt + 1) * 128], Vsl[p][:, kt, vo:vo + 65], bias,
                                    [('Ks', p), ('Vs', p)])
                        if m < 32:
                            if na:
                                if m < 2:
                                    lat, Tab, tk = range(0, 4), self.TabE, 'TabE'
                                elif m >= 30:
                                    lat, Tab, tk = range(28, 32), self.TabE, 'TabE'
                                else:
                                    lat, Tab, tk = range(m - 2, m + 3), self.TabI, 'TabI'
                                for kt in lat:
                                    j = kt - m
                                    p0 = 7 - 2 * j
                                    bias = (Tab[:, h * 960 + p0 * 64:h * 960 + p0 * 64 + 128], [tk])
                                    kts.append(ktile(kt, bias))
                            else:
                                for kt in range(max(0, m - 1), min(31, m + 1) + 1):
                                    j = kt - m
                                    bias = None if j == 0 else ((self.mL[:], ['mL']) if j < 0 else (self.mU[:], ['mU']))
                                    kts.append(ktile(kt, bias))
                        kts.append(ktile(32, None))
                        kts.append(ktile(33, None))

                        def out_fn(qs, acc, rden, keys, ys=ys, ykey=ykey, hh=hh):
                            self.ts('dve', ys[:, hh * 64:(hh + 1) * 64], acc[:, 0:64], rden, None, ALU.mult, None,
                                    keys, [ykey])
                        self.attn_unit(qT, 128, kts, 64, 0.125, [('Qs', p)], out_fn, sink_col=None if na else h)
                    S.dma('pool', self.Y[b, m * 128:(m + 1) * 128, grp * 128:(grp + 1) * 128], ys[:], [ykey], [('Y', b)])
        S.barrier()
        A.release(mk)

    def phase_out(self, l, ntiles):
        S, A = self.S, self.A
        mk = A.mark()
        wO = A.alloc([128, 8, D], BF16, 'wO')
        m2 = self.load_w_bf16(wO, self.w_out[l], D, 'wO')
        S.barrier()
        A.release(m2)
        gc = self.load_gate(l, 2, 1, 'c')
        ysb = [A.alloc([128, D], BF16, 'ysb') for _ in range(2)]
        yT = [A.alloc([128, 8, 128], BF16, 'yT') for _ in range(2)]
        xt = [A.alloc([128, D], F32, 'xo') for _ in range(2)]
        tmp = [A.alloc([128, D], F32, 'otmp') for _ in range(2)]
        i = 0
        for b in range(NB):
            m3 = A.mark()
            gb = self.load_gate(l, b, 1, 'b')
            for m in range(ntiles):
                p = i % 2
                i += 1
                g, gk = (gb, 'gateb') if m < 32 else (gc, 'gatec')
                S.dma('sp', ysb[p][:], self.Y[b, m * 128:(m + 1) * 128, :], [('Y', b)], [('ysb', p)])
                S.dma('sp', xt[p][:], self.src(l, b, m), [('xs', b, m)], [('xo', p)])
                pst = self.psbf(6 + p)
                for kc in range(8):
                    self.tr(pst[:, kc * 128:(kc + 1) * 128], ysb[p][:, kc * 128:(kc + 1) * 128], [('ysb', p), 'ident'],
                            [('ps', 6 + p)])
                self.cp('act', yT[p][:], pst.rearrange("p (k t) -> p k t", k=8), [('ps', 6 + p)], [('yT', p)])
                for half in range(2):
                    bank = 2 * p + half
                    for kc in range(8):
                        self.mm(self.PS[:, bank, :], yT[p][:, kc, :], wO[:, kc, half * 512:(half + 1) * 512], kc == 0, kc == 7,
                                [('yT', p), 'wO'], [('ps', bank)])
                    self.tt('dve', tmp[p][:, half * 512:(half + 1) * 512], self.PS[:, bank, :],
                            g[:, half * 512:(half + 1) * 512], ALU.mult, [('ps', bank), gk], [('otmp', p, half)])
                self.tt('pool', tmp[p][:], tmp[p][:], xt[p][:], ALU.add, [('otmp', p, 0), ('otmp', p, 1), ('xo', p)],
                        [('otmp', p, 0), ('otmp', p, 1)])
                S.dma('pool', self.xs[b, m * 128:(m + 1) * 128, :], tmp[p][:], [('otmp', p, 0), ('otmp', p, 1)],
                      [('xs', b, m)])
            A.release(m3)
        S.barrier()
        A.release(mk)

    def build(self):
        S = self.S
        self.setup_consts()
        if self.skip_l0:
            for b in range(NB):
                for r0 in range(0, SL, 512):
                    S.dma('sp', self.xs[b, r0:r0 + 512, :], self.x[b, r0:r0 + 512, :], [], [('xsi', b, r0)])
                S.dma('sp', self.xs[b, SL:SA, :], self.ctx[b], [], [('xsi', b, SL)])
            S.barrier()
            self.phase_mod(1)
            self.phase_proj1()
            return self.finish_dbg()
        self.phase_mod(0)
        self.phase_proj0()
        self.phase_attn0()
        self.phase_out(0, NT)
        if self.stop_after == 'attn0':
            return self.finish_dbg()
        self.phase_peer(0, NT)
        if self.stop_after == 'peer0':
            return self.finish_dbg()
        self.phase_mod(1)
        self.phase_proj1()
        if self.stop_after == 'proj1':
            return self.finish_dbg()
        self.phase_attn1()
        self.phase_out(1, 32)
        if self.stop_after == 'attn1':
            return self.finish_dbg()
        self.phase_peer(1, 32)
        S.barrier()
        S.emit()
        return self.nc

    def finish_dbg(self):
        S = self.S
        for b in range(NB):
            for r0 in range(0, SA, 544):
                S.dma('sp', self.dbg[b, r0:r0 + 544, :], self.xs[b, r0:r0 + 544, :], [], [('dbg', b, r0)])
        S.barrier()
        S.emit()
        return self.nc


def _consts():
    f = np.float32
    t = np.arange(SL)
    row, col = (t // 64).astype(f), (t % 64).astype(f)

    def table(dim_half, npart_rep):
        freq = (10000.0 ** (-np.arange(dim_half, dtype=f) / dim_half)).astype(f)
        ang_r = row[None, :] * freq[:, None]
        ang_c = col[None, :] * freq[:, None]
        C = np.concatenate([np.cos(ang_r), np.cos(ang_r), np.cos(ang_c), np.cos(ang_c)], 0)
        Sn = np.concatenate([np.sin(ang_r), np.sin(ang_r), np.sin(ang_c), np.sin(ang_c)], 0)
        C = np.concatenate([C, np.ones((C.shape[0], CT), f)], 1)
        Sn = np.concatenate([Sn, np.zeros((Sn.shape[0], CT), f)], 1)
        return C.astype(f), Sn.astype(f)
    C64, S64 = table(16, 2)
    ropeC = np.concatenate([C64, C64], 0)
    ropeS = np.concatenate([S64, S64], 0)
    C32, S32 = table(8, 1)
    ropeCm = np.concatenate([np.ones((64, SA), f), C32], 0)
    ropeSm = np.concatenate([np.zeros((64, SA), f), S32], 0)
    cq = np.arange(64)
    col_start = np.clip(cq - 8, 0, 48)
    ck = np.arange(64)
    valid = (ck[:, None] >= col_start[None, :]) & (ck[:, None] < col_start[None, :] + 16)
    maskI = np.full((2, 64, 8, 15, 64), NEG, f)
    maskE = np.full((2, 64, 8, 15, 64), NEG, f)
    for kr in range(2):
        for p in range(15):
            dr = (7 - p) if kr == 0 else (8 - p)
            if dr < -7 or dr > 7:
                continue
            mE = np.where(valid, 0.0, NEG).astype(f)
            maskE[kr, :, :, p, :] = mE[:, None, :]
            if -4 <= dr <= 3:
                maskI[kr, :, :, p, :] = mE[:, None, :]
    swaL = np.where(np.arange(128)[:, None] >= np.arange(128)[None, :], 0.0, NEG).astype(f)
    swaU = np.where(np.arange(128)[:, None] <= np.arange(128)[None, :], 0.0, NEG).astype(f)
    iota = np.tile(np.arange(128, dtype=f)[None, :], (128, 1))
    return dict(ropeC=ropeC, ropeS=ropeS, ropeCm=ropeCm, ropeSm=ropeSm, maskI=maskI.reshape(128, 7680),
                maskE=maskE.reshape(128, 7680), swaL=swaL, swaU=swaU, iota_in=iota)


def _rpb_layout(rpb):
    ck = np.arange(64)[:, None]
    cq = np.arange(64)[None, :]
    cidx = np.clip(ck - cq + 15, 0, 30)
    out = np.zeros((2, 64, 8, 15, 64), np.float32)
    for kr in range(2):
        for p in range(15):
            dr = (7 - p) if kr == 0 else (8 - p)
            if dr < -7 or dr > 7:
                continue
            out[kr, :, :, p, :] = np.transpose(rpb[:, dr + 7, :][:, cidx], (1, 0, 2))
    return np.ascontiguousarray(out.reshape(128, 7680))


_CONSTS = None


def make_in_maps(inputs, cores):
    global _CONSTS
    if _CONSTS is None:
        _CONSTS = _consts()
    f = np.float32
    g = {k: np.ascontiguousarray(np.asarray(v, dtype=f)) for k, v in inputs.items()}
    shared = dict(
        ada_w=g['ada_w'], ada_b=g['ada_b'], norm1_g=g['norm1_g'], norm2_g=g['norm2_g'], w_out=g['w_out'],
        peer_wq=g['peer_wq'], peer_keys=g['peer_keys'], peer_u=g['peer_u'], peer_v=g['peer_v'],
        ab_w=g['ab_w_in'][0], rpbT=_rpb_layout(g['na_rpb'][0]), sink=g['swa_sink'][0:1],
        cd_w=g['cd_w_in'][0], qn_g=g['mla_q_norm_g'][0].reshape(256, 1), w_uq=g['mla_w_uq'][0],
        kvn_g=g['mla_kv_norm_g'][0].reshape(128, 1), w_ukv=g['mla_w_ukv'][0], dlam=g['diff_lambda'][0].reshape(1, 256),
        subln=g['diff_subln_g'][0].reshape(1, 128), fin_g=g['final_norm_g'].reshape(1, D), **_CONSTS)
    maps = []
    for ci in cores:
        b0 = ci * NB
        m = dict(shared)
        m['x'] = g['x'][b0:b0 + NB]
        m['ctx'] = g['ctx'][b0:b0 + NB]
        m['cc'] = np.ascontiguousarray(np.concatenate([g['c'][b0:b0 + NB], g['c_ctx'][None, :]], 0))
        maps.append(m)
    return maps


def kernel(**inputs):
    nc = Builder().build()
    maps = make_in_maps(inputs, list(range(N_CORES)))
    res = run_bass_kernel_spmd(nc, maps, core_ids=list(range(N_CORES)))
    return np.concatenate([r["out"] for r in res.results], axis=0).astype(np.float32)


def _peer_methods():
    def topk16_multi(self, items):
        S = self.S
        for (src, vals, idx, wk, rk, wkey) in items:
            S.op('dve', lambda e, src=src, vals=vals: e.max(out=vals[:, 0:8], in_=src), rk, [wkey + ('v',)])
            yield
        for (src, vals, idx, wk, rk, wkey) in items:
            S.op('dve', lambda e, src=src, vals=vals, idx=idx: e.max_index(out=idx[:, 0:8], in_max=vals[:, 0:8], in_values=src),
                 rk + [wkey + ('v',)], [wkey + ('i',)])
            yield
        for (src, vals, idx, wk, rk, wkey) in items:
            S.op('dve', lambda e, src=src, vals=vals, wk=wk: e.match_replace(out=wk, in_to_replace=vals[:, 0:8], in_values=src,
                                                                             imm_value=-1e30),
                 rk + [wkey + ('v',)], [wkey + ('w',)])
            yield
        for (src, vals, idx, wk, rk, wkey) in items:
            S.op('dve', lambda e, vals=vals, wk=wk: e.max(out=vals[:, 8:16], in_=wk), [wkey + ('w',)], [wkey + ('v2',)])
            yield
        for (src, vals, idx, wk, rk, wkey) in items:
            S.op('dve', lambda e, vals=vals, idx=idx, wk=wk: e.max_index(out=idx[:, 8:16], in_max=vals[:, 8:16], in_values=wk),
                 [wkey + ('w',), wkey + ('v2',)], [wkey + ('i2',)])
            yield

    def phase_peer_prep(self, l):
        S, A = self.S, self.A
        mk = A.mark()
        ustg = [A.alloc([128, D], F32, 'ustg') for _ in range(2)]
        vstg = [A.alloc([128, D], F32, 'vstg') for _ in range(2)]
        ubf = [A.alloc([128, D], BF16, 'ubf') for _ in range(2)]
        vbf = [A.alloc([128, D], BF16, 'vbf') for _ in range(2)]
        utb = [A.alloc([128, 8, 128], BF16, 'utb') for _ in range(2)]
        U = self.peer_u[l].rearrange("(i j) d -> j i d", j=128)
        Vv = self.peer_v[l].rearrange("(i j) d -> j i d", j=128)
        for j in range(128):
            p = j % 2
            S.dma('sp', ustg[p][:], U[j], [], [('ustg', p)])
            S.dma('sp', vstg[p][:], Vv[j], [], [('vstg', p)])
            self.cp('dve', ubf[p][:], ustg[p][:], [('ustg', p)], [('ubf', p)])
            pst = self.psbf(6 + p)
            for kc in range(8):
                self.tr(pst[:, kc * 128:(kc + 1) * 128], ubf[p][:, kc * 128:(kc + 1) * 128], [('ubf', p), 'ident'], [('ps', 6 + p)])
            self.cp('act', utb[p][:], pst.rearrange("p (k t) -> p k t", k=8), [('ps', 6 + p)], [('utb', p)])
            S.dma('pool', self.UTs[j], utb[p][:].rearrange("p k t -> p (k t)"), [('utb', p)], [('UTs', j)])
            self.cp('pool', vbf[p][:], vstg[p][:], [('vstg', p)], [('vbf', p)])
            S.dma('pool', self.Vs[j], vbf[p][:], [('vbf', p)], [('Vs', j)])
        wst = [A.alloc([128, 8, 512], F32, 'wqst') for _ in range(2)]
        wbf = [A.alloc([128, 8, 512], BF16, 'wqbf') for _ in range(2)]
        for c in range(4):
            p = c % 2
            S.dma('sp', wst[p][:], self.peer_wq[l, :, c * 512:(c + 1) * 512].rearrange("(kc p) n -> p kc n", p=128), [], [('wqst', p)])
            self.cp('dve', wbf[p][:], wst[p][:], [('wqst', p)], [('wqbf', p)])
            S.dma('pool', self.WQs[c], wbf[p][:].rearrange("p k t -> p (k t)"), [('wqbf', p)], [('WQs', c)])
        S.barrier()
        A.release(mk)

    def phase_peer(self, l, ntiles):
        S, A = self.S, self.A
        self.phase_peer_prep(l)
        mk = A.mark()
        last = (l == 1)
        kT = A.alloc([128, 2, 128], BF16, 'kT')
        mk0 = A.mark()
        kst = A.alloc([128, 2, 128], F32, 'kst')
        kbf = A.alloc([128, 2, 128], BF16, 'kbf')
        for p in range(2):
            S.dma('sp', kst[:, p, :], self.peer_keys[l, p], [], ['kst'])
        self.cp('dve', kbf[:], kst[:], ['kst'], ['kbf'])
        pst = self.psbf(2)
        for p in range(2):
            self.tr(pst[:, p * 128:(p + 1) * 128], kbf[:, p, :], ['kbf', 'ident'], [('ps', 2)])
        self.cp('act', kT[:], pst[:, 0:256].rearrange("p (k t) -> p k t", k=2), [('ps', 2)], ['kT'])
        S.barrier()
        A.release(mk0)
        G2 = A.alloc([128, D], F32, 'G2')
        SH2 = A.alloc([128, D], F32, 'SH2')
        g2 = A.alloc([128, D], F32, 'g2')
        fss = A.alloc([128, 4], F32, 'fss')
        if last:
            fing = A.alloc([128, D], F32, 'fing')
            self.load_row_bc(fing[:], self.fin_g[0:1, :], 'fing')
        nb = self.alloc_norm_bufs(nxt=4)
        h2Ts = [A.alloc([128, 8, 256], BF16, 'h2T') for _ in range(2)]
        qT = A.alloc([128, 16, 256], BF16, 'qT')
        s_sb = A.alloc([128, 2048], F32, 's_sb')
        wqb = s_sb[:].bitcast(BF16).rearrange("p (k t) -> p k t", k=8)
        cand = A.alloc([128, 2048], F32, 'cand')
        ngt = cand[:, 0:D]
        sv = A.alloc([128, 256], F32, 'sv')
        si = A.alloc([128, 256], U32, 'si')
        sif = A.alloc([128, 256], F32, 'sif')
        wk = A.alloc([128, 2048], F32, 'wk')
        fv = A.alloc([128, 128], F32, 'fv')
        fp = A.alloc([128, 128], U32, 'fp')
        fpu = A.alloc([128, 128], U32, 'fpu')
        Aa = A.alloc([128, 128], F32, 'Aa')
        Bb = A.alloc([128, 128], F32, 'Bb')
        ijg = A.alloc([128, 3, 128], F32, 'ijg')
        ijgTs = [[A.alloc([128, 3, 128], BF16, 'ijgT') for _ in range(2)] for _ in range(2)]
        gz = A.alloc([128, 16], F32, 'gz')
        RT = 16
        R1 = A.alloc([128, RT, 128], BF16, 'R1')
        R2 = A.alloc([128, RT, 128], BF16, 'R2')
        Gs = A.alloc([128, 128, 256], BF16, 'Gs')
        utl = [A.alloc([128, 8, 128], BF16, 'utl') for _ in range(6)]
        vl = [A.alloc([128, D], BF16, 'vl') for _ in range(6)]
        ga = [A.alloc([128, 512], F32, 'ga') for _ in range(3)]
        wT = [A.alloc([128, 512], BF16, 'wT') for _ in range(3)]
        etmp = A.alloc([128, D], F32, 'etmp')
        iota16 = self.iota[:, 0:16]
        nchunks = ntiles // 2
        gskeys = [('Gs', a, c) for a in range(2) for c in range(128 // RT)]
        items = [(b, ch) for b in range(NB) for ch in range(nchunks)]
        state = {'row': None, 'grow': None, 'ji': 0}
        xpars = {}

        def partA(seq):
            b, ch = items[seq]
            par = seq % 2
            h2T = h2Ts[par]
            hkey = ('h2T', par)
            row = b if ch < 16 else 2
            if row != state['row']:
                state['row'] = row
                self.load_row_bc(G2[:], self.mod[l, row:row + 1, 4 * D:5 * D], 'G2')
                self.load_row_bc(SH2[:], self.mod[l, row:row + 1, 3 * D:4 * D], 'SH2')
                self.load_row_bc(ngt, self.norm2_g[l:l + 1, :], 'cand')
                self.stt('dve', G2[:], G2[:], 1.0, ngt, ALU.add, ALU.mult, ['G2', 'cand'], ['G2'])
                yield
            xp = []
            for ti in range(2):
                tg = ch * 2 + ti
                xp.append(self.norm_tile(nb, self.xs[b, tg * 128:(tg + 1) * 128, :], G2, SH2, ['G2', 'SH2'],
                                         h2T[:, :, ti * 128:(ti + 1) * 128], hkey, 3))
                yield
            xpars[seq] = xp
            for c in range(4):
                S.dma('pool', wqb.rearrange("p k t -> p (k t)"), self.WQs[c], [('WQs', c)], ['swq'])
                for q in range(4):
                    hp = c * 4 + q
                    bank = 3
                    for kc in range(8):
                        self.mm(self.PS[:, bank, 0:256], wqb[:, kc, q * 128:(q + 1) * 128], h2T[:, kc, :], kc == 0, kc == 7,
                                ['swq', hkey], [('ps', bank)])
                        if kc % 2 == 1:
                            yield
                    self.cp('act', qT[:, hp, :], self.PS[:, bank, 0:256], [('ps', bank)], ['qT'])
                    yield
            for ti in range(2):
                for g4 in range(4):
                    bank = 3
                    for q in range(4):
                        hp = g4 * 4 + q
                        self.mm(self.PS[:, bank, q * 128:(q + 1) * 128], qT[:, hp, ti * 128:(ti + 1) * 128], kT[:, hp % 2, :],
                                True, True, ['qT', 'kT'], [('ps', bank)])
                    self.cp('act', s_sb[:, g4 * 512:(g4 + 1) * 512], self.PS[:, bank, :], [('ps', bank)], ['swq'])
                    yield
                tkA = [(s_sb[:, hp * 128:(hp + 1) * 128], sv[:, hp * 16:(hp + 1) * 16], si[:, hp * 16:(hp + 1) * 16],
                        wk[:, hp * 128:(hp + 1) * 128], ['swq'], ('sv', hp)) for hp in range(16)]
                for _ in self.topk16_multi(tkA):
                    yield
                svk = [('sv', hp, x) for hp in range(16) for x in ('v', 'v2')]
                sik = [('sv', hp, x) for hp in range(16) for x in ('i', 'i2')]
                self.cp('dve', sif[:], si[:], sik, ['sif'])
                sv4 = sv[:].rearrange("p (h t k) -> p h t k", h=8, t=2)
                sif4 = sif[:].rearrange("p (h t k) -> p h t k", h=8, t=2)
                cand4 = cand[:].rearrange("p (h a b) -> p h a b", h=8, a=16)
                self.tt('dve', cand4, sv4[:, :, 0, :].unsqueeze(3).to_broadcast([128, 8, 16, 16]),
                        sv4[:, :, 1, :].unsqueeze(2).to_broadcast([128, 8, 16, 16]), ALU.add, svk, ['cand'])
                yield
                tkB = [(cand[:, h * 256:(h + 1) * 256], fv[:, h * 16:(h + 1) * 16], fp[:, h * 16:(h + 1) * 16],
                        wk[:, h * 256:(h + 1) * 256], ['cand'], ('fv', h)) for h in range(8)]
                for _ in self.topk16_multi(tkB):
                    yield
                fvk = [('fv', h, x) for h in range(8) for x in ('v', 'v2')]
                fpk = [('fv', h, x) for h in range(8) for x in ('i', 'i2')]
                S.op('dve', lambda e: e.tensor_single_scalar(out=fpu[:], in_=fp[:], scalar=4, op=ALU.logical_shift_right), fpk, ['fpu'])
                self.cp('dve', Aa[:], fpu[:], ['fpu'], ['Aa'])
                S.op('dve', lambda e: e.tensor_single_scalar(out=fpu[:], in_=fp[:], scalar=15, op=ALU.bitwise_and), fpk + ['fpu'], ['fpu'])
                self.cp('dve', Bb[:], fpu[:], ['fpu'], ['Bb'])
                yield
                fv3 = fv[:].rearrange("p (h k) -> p h k", h=8)
                g3 = ijg[:, 2, :].rearrange("p (h k) -> p h k", h=8)
                self.tt('dve', g3, fv3, fv3[:, :, 0:1].to_broadcast([128, 8, 16]), ALU.subtract, fvk, [('ijg', 2)])
                self.act(ijg[:, 2, :], ijg[:, 2, :], AF.Exp, [('ijg', 2)], [('ijg', 2)])
                io4 = iota16.unsqueeze(1).unsqueeze(1).to_broadcast([128, 8, 16, 16])
                for (sel, t_, slot, skey) in ((Aa, 0, 0, 'Aa'), (Bb, 1, 1, 'Bb')):
                    sel4 = sel[:].rearrange("p (h k) -> p h k", h=8).unsqueeze(3).to_broadcast([128, 8, 16, 16])
                    self.tt('dve', cand4, sel4, io4, ALU.is_equal, [skey, 'iota', 'cand'], ['cand'])
                    self.tt('dve', cand4, cand4, sif4[:, :, t_, :].unsqueeze(2).to_broadcast([128, 8, 16, 16]), ALU.mult,
                            ['cand', 'sif'], ['cand'])
                    S.op('dve', lambda e, slot=slot: e.tensor_reduce(
                        out=ijg[:, slot, :].rearrange("p (h k) -> p h k", h=8), in_=cand4, axis=AX.X, op=ALU.add),
                        ['cand'], [('ijg', slot)])
                    yield
                S.op('dve', lambda e: e.tensor_reduce(out=gz[:, 0:8], in_=g3, axis=AX.X, op=ALU.add), [('ijg', 2)], ['gz'])
                S.op('dve', lambda e: e.reciprocal(out=gz[:, 0:8], in_=gz[:, 0:8]), ['gz'], ['gz'])
                self.tt('dve', g3, g3, gz[:, 0:8].unsqueeze(2).to_broadcast([128, 8, 16]), ALU.mult, [('ijg', 2), 'gz'], [('ijg', 2)])
                for s3 in range(3):
                    self.tr(self.PS[:, 3, s3 * 128:(s3 + 1) * 128], ijg[:, s3, :], [('ijg', s3), 'identf'], [('ps', 3)], f32=True)
                self.cp('act', ijgTs[par][ti][:], self.PS[:, 3, 0:384].rearrange("p (s t) -> p s t", s=3), [('ps', 3)],
                        [('ijgT', par, ti)])
                yield

        def partB(seq):
            par = seq % 2
            bi = 0
            for ti in range(2):
                ijgT = ijgTs[par][ti]
                ik = ('ijgT', par, ti)
                for rq in range(128 // RT):
                    tq = rq * RT
                    iob = self.iotab[:].unsqueeze(1).to_broadcast([128, RT, 128])
                    self.tt('dve', R1[:], iob, ijgT[:, 0, tq:tq + RT].unsqueeze(2).to_broadcast([128, RT, 128]), ALU.is_equal,
                            ['iotab', ik], ['R1'])
                    self.tt('dve', R2[:], iob, ijgT[:, 1, tq:tq + RT].unsqueeze(2).to_broadcast([128, RT, 128]), ALU.is_equal,
                            ['iotab', ik], ['R2'])
                    self.tt('dve', R1[:], R1[:], ijgT[:, 2, tq:tq + RT].unsqueeze(2).to_broadcast([128, RT, 128]), ALU.mult,
                            ['R1', ik], ['R1'])
                    for t4 in range(RT // 4):
                        bank = bi % 4
                        bi += 1
                        for t_ in range(4):
                            t = t4 * 4 + t_
                            self.mm(self.PS[:, bank, t_ * 128:(t_ + 1) * 128], R1[:, t, :], R2[:, t, :], True, True, ['R1', 'R2'],
                                    [('ps', bank, 0), ('ps', bank, 1), ('ps', bank)])
                        tok0 = ti * 128 + tq + t4 * 4
                        self.cp('act' if t4 % 2 == 0 else 'dve', Gs[:, :, tok0:tok0 + 4],
                                self.PS[:, bank, :].rearrange("p (t j) -> p j t", t=4),
                                [('ps', bank, 0), ('ps', bank, 1), ('ps', bank)], [('Gs', ti, rq)])

        def jloop(seq, gen):
            par = seq % 2
            h2T = h2Ts[par]
            hkey = ('h2T', par)
            LA = 2
            base = state['ji']

            def a_pair(pp):
                pi_ = base + pp
                u = pi_ % 3
                bank = pi_ % 3
                for jj in (2 * pp, 2 * pp + 1):
                    b6 = (2 * pi_ + jj % 2) % 6
                    S.dma('sp', utl[b6][:].rearrange("p k t -> p (k t)"), self.UTs[jj], [('UTs', jj)], [('utl', b6)])
                    S.dma('pool', vl[b6][:], self.Vs[jj], [('Vs', jj)], [('vl', b6)])
                    pa = self.PS[:, bank, (jj % 2) * 256:(jj % 2 + 1) * 256]
                    for kc in range(8):
                        self.mm(pa, utl[b6][:, kc, :], h2T[:, kc, :], kc == 0, kc == 7, [('utl', b6), hkey], [('ps', bank, 0)])
                self.act(ga[u][:], self.PS[:, bank, :], AF.Gelu, [('ps', bank, 0)], [('ga', u)])
                self.tt('dve', wT[u][:], ga[u][:], Gs[:, 2 * pp:2 * pp + 2, :].rearrange("p j t -> p (j t)"), ALU.mult,
                        [('ga', u)] + gskeys, [('wT', u)])

            def f_pair(pp):
                pi_ = base + pp
                u = pi_ % 3
                for jj in (2 * pp, 2 * pp + 1):
                    b6 = (2 * pi_ + jj % 2) % 6
                    for ti in range(2):
                        for hf in range(2):
                            self.mm(self.PS[:, 4 + 2 * ti + hf, :], wT[u][:, (jj % 2) * 256 + ti * 128:(jj % 2) * 256 + (ti + 1) * 128],
                                    vl[b6][:, hf * 512:(hf + 1) * 512], jj == 0, jj == 127, [('wT', u), ('vl', b6)],
                                    [('ps', 4 + 2 * ti + hf)])
            for step in range(64 + LA):
                if step < 64:
                    a_pair(step)
                if step >= LA:
                    f_pair(step - LA)
                if gen is not None:
                    for _ in range(6):
                        if next(gen, 'done') == 'done':
                            gen = None
                            break
            state['ji'] += 64
            if gen is not None:
                for _ in gen:
                    pass

        def epilogue(seq):
            b, ch = items[seq]
            row = b if ch < 16 else 2
            if row != state['grow']:
                state['grow'] = row
                self.load_row_bc(g2[:], self.mod[l, row:row + 1, 5 * D:6 * D], 'g2')
            for ti in range(2):
                tg = ch * 2 + ti
                px = xpars[seq][ti]
                xt = nb['xt'][px]
                xk = ('xt', px)
                for hf in range(2):
                    self.tt('dve', etmp[:, hf * 512:(hf + 1) * 512], self.PS[:, 4 + 2 * ti + hf, :], g2[:, hf * 512:(hf + 1) * 512],
                            ALU.mult, [('ps', 4 + 2 * ti + hf), 'g2'], [('etmp', hf)])
                self.tt('pool', etmp[:], etmp[:], xt[:], ALU.add, [('etmp', 0), ('etmp', 1), xk], [('etmp', 0), ('etmp', 1)])
                ek = [('etmp', 0), ('etmp', 1)]
                if not last:
                    S.dma('pool', self.xs[b, tg * 128:(tg + 1) * 128, :], etmp[:], ek, [('xs', b, tg)])
                else:
                    self.act(nb['junk'], etmp[:], AF.Square, ek, ['ntmp', 'fss'], accum_out=fss[:, 0:1])
                    self.act(fss[:, 2:3], fss[:, 0:1], AF.Sqrt, ['fss', 'eps'], ['frs'], scale=1.0 / D, bias=self.epsT[:, 0:1])
                    S.op('dve', lambda e: e.reciprocal(out=fss[:, 2:3], in_=fss[:, 2:3]), ['frs'], ['frs'])
                    self.stt('dve', etmp[:], etmp[:], fss[:, 2:3], fing[:], ALU.mult, ALU.mult, ek + ['frs', 'fing'], ek)
                    S.dma('pool', self.out[b, tg * 128:(tg + 1) * 128, :], etmp[:], ek, [('out', b, tg)])

        for _ in partA(0):
            pass
        partB(0)
        for seq in range(len(items)):
            gen = partA(seq + 1) if seq + 1 < len(items) else None
            jloop(seq, gen)
            epilogue(seq)
            if seq + 1 < len(items):
                partB(seq + 1)
        S.barrier()
        A.release(mk)

    Builder.topk16_multi = topk16_multi
    Builder.phase_peer_prep = phase_peer_prep
    Builder.phase_peer = phase_peer


_peer_methods()


LAM_INIT1 = 0.8 - 0.6 * math.exp(-0.3 * 1)


def _layer1_methods():
    def phase_proj1(self, l=1):
        S, A = self.S, self.A
        mk = A.mark()
        wA = A.alloc([128, 8, 3072], BF16, 'wA')
        m2 = self.load_w_bf16(wA, self.cd_w, 1952, 'wA', piece=488)
        S.barrier()
        A.release(m2)
        self.make_rot(wA, 416, 1952, 16, 16, 'wA')
        self.make_rot(wA, 928, 2464, 16, 16, 'wA')
        self.make_rot(wA, 384, 2976, 2, 8, 'wA')
        Wg = A.alloc([128, 2, 768], BF16, 'Wg')
        Wgr = A.alloc([128, 2, 768], BF16, 'Wgr')
        Wkv = A.alloc([128, 1024], BF16, 'Wkv')
        onesf = A.alloc([128, 128], BF16, 'onesf')
        S.op('pool', lambda e: e.memset(onesf[:], 1.0), [], ['onesf'])
        m3 = A.mark()
        wst = A.alloc([128, 1024], F32, 'w1st')
        gq = A.alloc([128, 2], F32, 'gq')
        gkv = A.alloc([128, 1], F32, 'gkv')
        for c in range(2):
            S.dma('sp', gq[:, c:c + 1], self.qn_g[c * 128:(c + 1) * 128, :], [], ['gq'])
        S.dma('sp', gkv[:], self.kvn_g, [], ['gkv'])
        for c in range(2):
            S.dma('sp', wst[:, 0:768], self.w_uq[c * 128:(c + 1) * 128, :], ['w1st'], ['w1st'])
            self.ts('dve', Wg[:, c, :], wst[:, 0:768], gq[:, c:c + 1], None, ALU.mult, None, ['w1st', 'gq'], ['Wg'])
        S.op('pool', lambda e: e.memset(Wgr[:], 0.0), [], ['Wgr'])
        for c in range(2):
            s = Wg[:, c, :].rearrange("p (h f) -> p h f", f=96)[:, :, 64:96].rearrange("p h (b t e) -> p h b t e", b=2, t=2)
            d = Wgr[:, c, :].rearrange("p (h f) -> p h f", f=96)[:, :, 64:96].rearrange("p h (b t e) -> p h b t e", b=2, t=2)
            for bb in range(2):
                S.op('act', lambda e, s=s, d=d, bb=bb: e.mul(out=d[:, :, bb, 0, :], in_=s[:, :, bb, 1, :], mul=-1.0), ['Wg', 'Wgr'], ['Wgr'])
                self.cp('pool', d[:, :, bb, 1, :], s[:, :, bb, 0, :], ['Wg', 'Wgr'], ['Wgr'])
        S.dma('sp', wst[:], self.w_ukv, ['w1st'], ['w1st'])
        self.ts('dve', Wkv[:], wst[:], gkv[:, 0:1], None, ALU.mult, None, ['w1st', 'gkv'], ['Wkv'])
        S.barrier()
        A.release(m3)
        if self.sub == 'w':
            return
        for b in range(NB):
            self.proj_batch1(l, b, wA, Wg, Wgr, Wkv, onesf)
            if self.sub is not None:
                break
        S.barrier()
        A.release(mk)

    def proj_batch1(self, l, b, wA, Wg, Wgr, Wkv, onesf):
        S, A = self.S, self.A
        mk = A.mark()
        Gb, SHb = self.load_norm_mod(l, b, 1, 'b')
        Gc, SHc = self.load_norm_mod(l, 2, 1, 'c')
        nb = self.alloc_norm_bufs()
        hTs = [A.alloc([128, 8, 512], BF16, 'hT') for _ in range(2)]
        stg = [A.alloc([128, 512], BF16, 'stg') for _ in range(3)]
        t1 = A.alloc([128, 512], F32, 't1')
        t2 = A.alloc([128, 512], F32, 't2')
        rC = A.alloc([128, 512], F32, 'rC')
        rS = A.alloc([128, 512], F32, 'rS')
        rCm = A.alloc([128, 512], F32, 'rCm')
        rSm = A.alloc([128, 512], F32, 'rSm')
        rCk = A.alloc([128, 512], F32, 'rCk')
        rSk = A.alloc([128, 512], F32, 'rSk')
        cqb = A.alloc([128, 2, 512], BF16, 'cqb')
        sq = A.alloc([128, 512], BF16, 'sq')
        rq = A.alloc([128, 512], F32, 'rq')
        rkv = A.alloc([128, 512], F32, 'rkv')
        ckvf = A.alloc([128, 512], F32, 'ckvf')
        ckvn = A.alloc([128, 512], BF16, 'ckvn')
        krs = A.alloc([128, 512], BF16, 'krs')
        VW = 1036
        vst = [A.alloc([128, VW], BF16, 'vst') for _ in range(2)]
        for v in vst:
            S.op('pool', lambda e, v=v: e.memset(v[:], 1.0), [], [('vst', 0), ('vst', 1)])
        si = 0
        pi = 0
        for ci in range(9):
            ntile = 4 if ci < 8 else 2
            n = ntile * 128
            t0 = ci * 512
            hT = hTs[ci % 2]
            hkey = ('hT', ci % 2)
            G, SH, gk = (Gb, SHb, ['Gb', 'SHb']) if ci < 8 else (Gc, SHc, ['Gc', 'SHc'])
            for ti in range(ntile):
                tg = ci * 4 + ti
                self.norm_tile(nb, self.src(l, b, tg), G, SH, gk, hT[:, :, ti * 128:(ti + 1) * 128], hkey, 6)
            S.dma('sp', rC[:, 0:n], self.ropeC[:, t0:t0 + n], [], ['rC'])
            S.dma('sp', rS[:, 0:n], self.ropeS[:, t0:t0 + n], [], ['rS'])
            S.dma('sp', rCm[0:96, 0:n], self.ropeCm[:, t0:t0 + n], [], ['rCm'])
            S.dma('sp', rSm[0:96, 0:n], self.ropeSm[:, t0:t0 + n], [], ['rSm'])
            S.dma('sp', rCk[0:32, 0:n], self.ropeCm[64:96, t0:t0 + n], [], ['rCk'])
            S.dma('sp', rSk[0:32, 0:n], self.ropeSm[64:96, t0:t0 + n], [], ['rSk'])

            def proj(bank, col0, m, nn=n, hT=hT, hkey=hkey):
                for kc in range(8):
                    self.mm(self.PS[0:m, bank, 0:nn], wA[:, kc, col0:col0 + m], hT[:, kc, 0:nn], kc == 0, kc == 7,
                            ['wA', hkey], [('ps', bank)])
            if self.sub == 'c0':
                break
            for c in range(2):
                proj(c, 128 * c, 128)
                self.cp('dve', cqb[:, c, 0:n], self.PS[:, c, 0:n], [('ps', c)], [('cqb', c)])
                if self.sub == 'c0a':
                    continue
                self.cp('dve', ckvf[:, 0:n], self.PS[:, c, 0:n], [('ps', c)], ['ckvf'])
                self.act(sq[:, 0:n], ckvf[:, 0:n], AF.Square, ['ckvf'], ['sq'])
                self.mm(self.PS[:, 7, 0:n], onesf[:], sq[:, 0:n], c == 0, c == 1, ['onesf', 'sq'], [('ps', 7)])
            if self.sub != 'c0a':
                self.cp('dve', rq[:, 0:n], self.PS[:, 7, 0:n], [('ps', 7)], ['rq'])
                self.act(rq[:, 0:n], rq[:, 0:n], AF.Sqrt, ['rq', 'eps'], ['rq'], scale=1.0 / 256, bias=self.epsT[:, 0:1])
                S.op('dve', lambda e, n=n: e.reciprocal(out=rq[:, 0:n], in_=rq[:, 0:n]), ['rq'], ['rq'])
            if self.sub == 'c0a':
                break
            if self.sub == 'c0b':
                break
            proj(2, 256, 128)
            self.cp('dve', ckvf[:, 0:n], self.PS[:, 2, 0:n], [('ps', 2)], ['ckvf'])
            self.act(sq[:, 0:n], ckvf[:, 0:n], AF.Square, ['ckvf'], ['sq'])
            self.mm(self.PS[:, 7, 0:n], onesf[:], sq[:, 0:n], True, True, ['onesf', 'sq'], [('ps', 7)])
            self.cp('dve', rkv[:, 0:n], self.PS[:, 7, 0:n], [('ps', 7)], ['rkv'])
            self.act(rkv[:, 0:n], rkv[:, 0:n], AF.Sqrt, ['rkv', 'eps'], ['rkv'], scale=1.0 / 128, bias=self.epsT[:, 0:1])
            S.op('dve', lambda e, n=n: e.reciprocal(out=rkv[:, 0:n], in_=rkv[:, 0:n]), ['rkv'], ['rkv'])
            self.tt('dve', ckvn[:, 0:n], ckvf[:, 0:n], rkv[:, 0:n], ALU.mult, ['ckvf', 'rkv'], ['ckvn'])
            if self.sub == 'c1':
                break
            proj(0, 384, 32)
            proj(2, 2976, 32)
            self.tt('dve', t1[0:32, 0:n], self.PS[0:32, 0, 0:n], rCk[0:32, 0:n], ALU.mult, [('ps', 0), 'rCk'], ['t1'])
            self.tt('dve', t2[0:32, 0:n], self.PS[0:32, 2, 0:n], rSk[0:32, 0:n], ALU.mult, [('ps', 2), 'rSk'], ['t2'])
            self.tt('pool', krs[0:32, 0:n], t1[0:32, 0:n], t2[0:32, 0:n], ALU.add, ['t1', 't2'], ['krs'])
            for h in range(8):
                S.dma('pool', self.KT[b, h, 64:96, t0:t0 + n], krs[0:32, 0:n], ['krs'], [('K', b, h, 1)])
            if self.sub == 'c2':
                break
            for h in range(8):
                pa = pi % 2
                pi += 1
                for c in range(2):
                    self.mm(self.PS[0:96, pa, 0:n], Wg[:, c, h * 96:(h + 1) * 96], cqb[:, c, 0:n], c == 0, c == 1,
                            ['Wg', ('cqb', 0), ('cqb', 1)], [('ps', pa)])
                for c in range(2):
                    self.mm(self.PS[0:96, 2 + pa, 0:n], Wgr[:, c, h * 96:(h + 1) * 96], cqb[:, c, 0:n], c == 0, c == 1,
                            ['Wgr', ('cqb', 0), ('cqb', 1)], [('ps', 2 + pa)])
                sg = stg[si % 3]
                skey = ('stg', si % 3)
                si += 1
                self.tt('dve', t1[0:96, 0:n], self.PS[0:96, pa, 0:n], rCm[0:96, 0:n], ALU.mult, [('ps', pa), 'rCm'], ['t1'])
                self.tt('dve', t2[0:96, 0:n], self.PS[0:96, 2 + pa, 0:n], rSm[0:96, 0:n], ALU.mult, [('ps', 2 + pa), 'rSm'], ['t2'])
                self.tt('pool', t1[0:96, 0:n], t1[0:96, 0:n], t2[0:96, 0:n], ALU.add, ['t1', 't2'], ['t1'])
                self.tt('pool', sg[0:96, 0:n], t1[0:96, 0:n], rq[0:96, 0:n], ALU.mult, ['t1', 'rq'], [skey])
                S.dma('pool', self.QT[b, h, 0:96, t0:t0 + n], sg[0:96, 0:n], [skey], [('Q', b, h)])
                pa = pi % 2
                pi += 1
                self.mm(self.PS[0:64, pa, 0:n], Wkv[:, h * 128:h * 128 + 64], ckvn[:, 0:n], True, True, ['Wkv', 'ckvn'], [('ps', pa)])
                sg = stg[si % 3]
                skey = ('stg', si % 3)
                si += 1
                self.cp('act', sg[0:64, 0:n], self.PS[0:64, pa, 0:n], [('ps', pa)], [skey])
                S.dma('pool', self.KT[b, h, 0:64, t0:t0 + n], sg[0:64, 0:n], [skey], [('K', b, h, 0)])
            if self.sub == 'c3':
                break
            for (dst, idx, col0, rot0) in ([('Q', 8 + d, 416 + 128 * d, 1952 + 128 * d) for d in range(4)]
                                           + [('K', 8 + d, 928 + 128 * d, 2464 + 128 * d) for d in range(4)]):
                pa = pi % 2
                pi += 1
                proj(pa, col0, 128)
                proj(2 + pa, rot0, 128)
                sg = stg[si % 3]
                skey = ('stg', si % 3)
                si += 1
                self.tt('dve', t1[:, 0:n], self.PS[:, pa, 0:n], rC[:, 0:n], ALU.mult, [('ps', pa), 'rC'], ['t1'])
                self.tt('dve', t2[:, 0:n], self.PS[:, 2 + pa, 0:n], rS[:, 0:n], ALU.mult, [('ps', 2 + pa), 'rS'], ['t2'])
                self.tt('pool', sg[:, 0:n], t1[:, 0:n], t2[:, 0:n], ALU.add, ['t1', 't2'], [skey])
                dram = self.QT if dst == 'Q' else self.KT
                S.dma('pool', dram[b, idx, :, t0:t0 + n], sg[:, 0:n], [skey], [(dst, b, idx)])
            if self.sub == 'c4':
                break
            for ti in range(ntile):
                tg = ci * 4 + ti
                vs = vst[tg % 2]
                vkey = ('vst', tg % 2)
                self.mm(self.PS[:, 4, :], ckvn[:, ti * 128:(ti + 1) * 128],
                        Wkv[:].rearrange("p (h f) -> p h f", f=128)[:, :, 64:128], True, True, ['ckvn', 'Wkv'], [('ps', 4)])
                self.cp('act', vs[:, 0:520].rearrange("p (h d) -> p h d", d=65)[:, :, 0:64],
                        self.PS[:, 4, :].rearrange("p (h d) -> p h d", d=64), [('ps', 4)], [vkey])
                for kc in range(8):
                    self.mm(self.PS[:, 5, :], hT[:, kc, ti * 128:(ti + 1) * 128], wA[:, kc, 1440:1952], kc == 0, kc == 7,
                            ['wA', hkey], [('ps', 5)])
                self.cp('act', vs[:, 520:1036].rearrange("p (h d) -> p h d", d=129)[:, :, 0:128],
                        self.PS[:, 5, :].rearrange("p (h d) -> p h d", d=128), [('ps', 5)], [vkey])
                S.dma('pool', self.V[b, tg * 128:(tg + 1) * 128, 0:VW], vs[:], [vkey], [('V', b)])
            if self.sub == 'c5':
                break
        S.barrier()
        A.release(mk)

    def phase_attn1(self):
        S, A = self.S, self.A
        mk = A.mark()
        self.attn_setup()
        lam = A.alloc([128, 8], F32, 'lam')
        dl = A.alloc([128, 256], F32, 'dl')
        self.load_row_bc(dl[:], self.dlam[0:1, :], 'dl')
        dl4 = dl[:].rearrange("p (a d) -> p a d", a=4)
        self.tt('dve', dl4[:, 0, :], dl4[:, 0, :], dl4[:, 1, :], ALU.mult, ['dl'], ['dl'])
        self.tt('dve', dl4[:, 2, :], dl4[:, 2, :], dl4[:, 3, :], ALU.mult, ['dl'], ['dl'])
        S.op('dve', lambda e: e.tensor_reduce(out=lam[:, 0:1], in_=dl4[:, 0, :], axis=AX.X, op=ALU.add), ['dl'], ['lam'])
        S.op('dve', lambda e: e.tensor_reduce(out=lam[:, 1:2], in_=dl4[:, 2, :], axis=AX.X, op=ALU.add), ['dl', 'lam'], ['lam'])
        self.act(lam[:, 0:2], lam[:, 0:2], AF.Exp, ['lam'], ['lam'])
        self.tt('dve', lam[:, 2:3], lam[:, 1:2], lam[:, 0:1], ALU.subtract, ['lam'], ['lam'])
        self.ts('dve', lam[:, 3:4], lam[:, 2:3], -LAM_INIT1, None, ALU.add, None, ['lam'], ['lam'])
        subg = A.alloc([128, 128], F32, 'subg')
        self.load_row_bc(subg[:], self.subln[0:1, :], 'subg')
        self.ts('dve', subg[:], subg[:], 1.0 - LAM_INIT1, None, ALU.mult, None, ['subg'], ['subg'])
        Qs = [A.alloc([128, SL], BF16, 'Qs') for _ in range(2)]
        Ks = [A.alloc([128, SA], BF16, 'Ks') for _ in range(2)]
        Vsl = [A.alloc([128, NT, 129], BF16, 'Vsl') for _ in range(2)]
        ystg = [A.alloc([128, 128], BF16, 'ystg') for _ in range(4)]
        o1 = [A.alloc([128, 128], F32, 'o1') for _ in range(4)]
        o2 = [A.alloc([128, 128], F32, 'o2') for _ in range(2)]
        oj = A.alloc([128, 128], BF16, 'oj')
        oss = A.alloc([128, 8], F32, 'oss')
        gi = 0
        yi = 0
        for b in range(NB):
            for grp in range(12):
                p = gi % 2
                gi += 1
                mla = grp < 8
                rows = 96 if mla else 128
                dv = 64 if mla else 128
                vcol0 = grp * 65 if mla else 520 + (grp - 8) * 129
                S.dma('sp', Qs[p][0:rows, :], self.QT[b, grp, 0:rows, 0:SL], [('Q', b, grp)], [('Qs', p)])
                S.dma('sp', Ks[p][0:rows, :], self.KT[b, grp, 0:rows, :],
                      [('K', b, grp), ('K', b, grp, 0), ('K', b, grp, 1)], [('Ks', p)])
                S.dma('sp', Vsl[p][:, :, 0:dv + 1],
                      self.V[b, :, vcol0:vcol0 + dv + 1].rearrange("(t p) c -> p t c", p=128), [('V', b)], [('Vs', p)])
                for qc in range(8):
                    if mla:
                        kts = [(Ks[p][0:96, kt * 128:(kt + 1) * 128], Vsl[p][:, kt, 0:65], None, [('Ks', p), ('Vs', p)])
                               for kt in range(NT)]

                        def out_fn(qs, acc, rden, keys, qc=qc, grp=grp, b=b):
                            nonlocal yi
                            ys = ystg[yi % 4]
                            ykey = ('ystg', yi % 4)
                            yi += 1
                            self.ts('dve', ys[:, 0:64], acc[:, 0:64], rden, None, ALU.mult, None, keys, [ykey])
                            tg = qc * 4 + qs
                            S.dma('pool', self.Y[b, tg * 128:(tg + 1) * 128, grp * 64:(grp + 1) * 64], ys[:, 0:64], [ykey], [('Y', b)])
                        self.attn_unit(Qs[p][0:96, qc * 512:(qc + 1) * 512], 512, kts, 64, 96 ** -0.5, [('Qs', p)], out_fn)
                    else:
                        d = grp - 8
                        for w in range(2):
                            kts = [(Ks[p][w * 64:(w + 1) * 64, kt * 128:(kt + 1) * 128], Vsl[p][:, kt, 0:129], None,
                                    [('Ks', p), ('Vs', p)]) for kt in range(NT)]

                            def out_fn(qs, acc, rden, keys, qc=qc, d=d, b=b, w=w):
                                nonlocal yi
                                if w == 0:
                                    self.ts('dve', o1[qs][:], acc[:, 0:128], rden, None, ALU.mult, None, keys, [('o1', qs)])
                                    return
                                oo = o2[qs % 2]
                                ok = ('o2', qs % 2)
                                self.ts('dve', oo[:], acc[:, 0:128], rden, None, ALU.mult, None, keys, [ok])
                                self.stt('dve', oo[:], oo[:], lam[:, 3:4], o1[qs][:], ALU.mult, ALU.add, [ok, ('o1', qs), 'lam'], [ok])
                                sk = ('oss', qs % 2)
                                c0 = qs % 2
                                self.act(oj[:], oo[:], AF.Square, [ok], ['oj', sk], accum_out=oss[:, c0:c0 + 1])
                                self.act(oss[:, 2 + c0:3 + c0], oss[:, c0:c0 + 1], AF.Sqrt, [sk, 'eps'], [sk], scale=1.0 / 128,
                                         bias=self.epsT[:, 0:1])
                                S.op('dve', lambda e, c0=c0: e.reciprocal(out=oss[:, 2 + c0:3 + c0], in_=oss[:, 2 + c0:3 + c0]), [sk], [sk])
                                ys = ystg[yi % 4]
                                ykey = ('ystg', yi % 4)
                                yi += 1
                                self.stt('dve', ys[:], oo[:], oss[:, 2 + c0:3 + c0], subg[:], ALU.mult, ALU.mult, [ok, sk, 'subg'], [ykey])
                                tg = qc * 4 + qs
                                S.dma('pool', self.Y[b, tg * 128:(tg + 1) * 128, 512 + d * 128:512 + (d + 1) * 128], ys[:], [ykey],
                                      [('Y', b)])
                            self.attn_unit(Qs[p][w * 64:(w + 1) * 64, qc * 512:(qc + 1) * 512], 512, kts, 128, 0.125, [('Qs', p)], out_fn)
        S.barrier()
        A.release(mk)

    Builder.phase_proj1 = phase_proj1
    Builder.proj_batch1 = proj_batch1
    Builder.phase_attn1 = phase_attn1


_layer1_methods()
```

```python
import math
import numpy as np
from contextlib import ExitStack
import concourse.bass as bass
import concourse.mybir as mybir
from concourse.bass_utils import run_bass_kernel_spmd

F32 = mybir.dt.float32
BF16 = mybir.dt.bfloat16
U32 = mybir.dt.uint32
AF = mybir.ActivationFunctionType
ALU = mybir.AluOpType
AX = mybir.AxisListType

NB = 2
D = 1024
SL = 4096
CT = 256
SA = 4352
NT = 34
EPS = 1e-6
NEG = -30000.0
SEM_ROT = 30000
N_CORES = 8


class Sched:
    ENGS = ('pe', 'act', 'dve', 'pool', 'sp')

    def __init__(self, nc, stack, n_lanes=28, same_engine_sync=True):
        self.nc = nc
        self.stack = stack
        self.prog = {e: [] for e in self.ENGS}
        self.cnt = {e: 0 for e in self.ENGS}
        self.esems = {e: [] for e in self.ENGS}
        self.seen = {e: {} for e in self.ENGS}
        self.res = {}
        self.lanes = []
        for i in range(n_lanes):
            s = stack.enter_context(nc.semaphore(f"lane{i}"))
            self.lanes.append([s, 0])
        self.lane_rr = 0
        self.same_engine_sync = same_engine_sync

    def _esem(self, e, n):
        k = (n - 1) // SEM_ROT
        while len(self.esems[e]) <= k:
            s = self.stack.enter_context(self.nc.semaphore(f"es_{e}_{len(self.esems[e])}"))
            self.esems[e].append(s)
        return self.esems[e][k], n - k * SEM_ROT

    def _deps(self, reads, writes):
        deps = []
        for r in reads:
            st = self.res.get(r)
            if st is not None and st['w'] is not None:
                deps.append(st['w'])
        for w in writes:
            st = self.res.get(w)
            if st is not None:
                if st['w'] is not None:
                    deps.append(st['w'])
                deps.extend(st['r'].values())
        return deps

    def _commit(self, ev, reads, writes):
        for r in reads:
            st = self.res.setdefault(r, {'w': None, 'r': {}})
            k = id(ev[1])
            if k not in st['r'] or st['r'][k][2] < ev[2]:
                st['r'][k] = ev
        for w in writes:
            self.res[w] = {'w': ev, 'r': {}}

    def _add_waits(self, e, deps):
        best = {}
        for (eng_src, sem, val) in deps:
            if eng_src == e and (e == 'pe' or not self.same_engine_sync):
                continue
            key = id(sem)
            if key not in best or best[key][1] < val:
                best[key] = (sem, val)
        for key, (sem, val) in best.items():
            if self.seen[e].get(key, 0) >= val:
                continue
            self.seen[e][key] = val
            self.prog[e].append(('wait', sem, val))

    def op(self, e, fn, reads=(), writes=()):
        deps = self._deps(reads, writes)
        self._add_waits(e, deps)
        self.cnt[e] += 1
        sem, val = self._esem(e, self.cnt[e])
        self.prog[e].append(('op', fn, sem, 1))
        ev = (e, sem, val)
        self._commit(ev, reads, writes)
        return ev

    def dma(self, q, out, in_, reads=(), writes=(), **kw):
        deps = self._deps(reads, writes)
        lane = self.lanes[self.lane_rr]
        self.lane_rr = (self.lane_rr + 1) % len(self.lanes)
        if lane[1] > 0:
            deps.append(('dma', lane[0], 16 * lane[1]))
        self._add_waits(q, deps)
        lane[1] += 1
        sem = lane[0]

        def fn(eng, out=out, in_=in_, kw=kw):
            return eng.dma_start(out=out, in_=in_, **kw)
        self.prog[q].append(('op', fn, sem, 16))
        ev = ('dma', sem, 16 * lane[1])
        self._commit(ev, reads, writes)
        return ev

    def barrier(self):
        evs = []
        for e in self.ENGS:
            if self.cnt[e] > 0:
                sem, val = self._esem(e, self.cnt[e])
                evs.append((e + '_b', sem, val))
        for lane in self.lanes:
            if lane[1] > 0:
                evs.append(('dma', lane[0], 16 * lane[1]))
        for e in self.ENGS:
            self._add_waits(e, evs)
        self.res = {}

    def emit(self):
        nc = self.nc
        engobj = {'pe': 'tensor', 'act': 'scalar', 'dve': 'vector', 'pool': 'gpsimd', 'sp': 'sync'}
        with nc.Block() as block:
            for e in self.ENGS:
                items = self.prog[e]
                if not items:
                    continue

                def body(eng, items=items):
                    for it in items:
                        if it[0] == 'wait':
                            eng.wait_ge(it[1], it[2])
                        else:
                            ins = it[1](eng)
                            ins.then_inc(it[2], it[3])
                getattr(block, engobj[e])(body)


class Arena:
    def __init__(self, nc, limit=229300):
        self.nc = nc
        self.off = 17408
        self.n = 0
        self.limit = limit

    def alloc(self, shape, dtype, name='t'):
        isz = 4 if dtype in (F32, U32) else 2
        nbytes = int(np.prod(shape[1:])) * isz
        nbytes = (nbytes + 63) // 64 * 64
        self.n += 1
        t = self.nc.alloc_sbuf_tensor_at(f"{name}_{self.n}", list(shape), dtype, offset=self.off)
        self.off += nbytes
        assert self.off <= self.limit, (name, self.off)
        return t

    def mark(self):
        return self.off

    def release(self, m):
        self.off = m


class Builder:
    def __init__(self, stop_after=None, skip_l0=False, sub=None):
        self.stop_after = stop_after
        self.skip_l0 = skip_l0
        self.sub = sub
        self.nc = nc = bass.Bass("TRN2", target_bir_lowering=False)
        self.stack = ExitStack()
        self.S = Sched(nc, self.stack)
        self.A = Arena(nc)
        di = lambda n, sh, dt=F32: nc.dram_tensor(n, list(sh), dt, kind="ExternalInput").ap()
        self.x = di("x", [NB, SL, D])
        self.ctx = di("ctx", [NB, CT, D])
        self.cc = di("cc", [3, D])
        self.ada_w = di("ada_w", [2, D, 6 * D])
        self.ada_b = di("ada_b", [2, 6 * D])
        self.norm1_g = di("norm1_g", [2, D])
        self.norm2_g = di("norm2_g", [2, D])
        self.w_out = di("w_out", [2, D, D])
        self.peer_wq = di("peer_wq", [2, D, 2048])
        self.peer_keys = di("peer_keys", [2, 2, 128, 128])
        self.peer_u = di("peer_u", [2, 16384, D])
        self.peer_v = di("peer_v", [2, 16384, D])
        self.ab_w = di("ab_w", [D, 2304])
        self.rpbT = di("rpbT", [128, 7680])
        self.maskI = di("maskI", [128, 7680])
        self.maskE = di("maskE", [128, 7680])
        self.swaL = di("swaL", [128, 128])
        self.swaU = di("swaU", [128, 128])
        self.sink = di("sink", [1, 8])
        self.cd_w = di("cd_w", [D, 1952])
        self.qn_g = di("qn_g", [256, 1])
        self.w_uq = di("w_uq", [256, 768])
        self.kvn_g = di("kvn_g", [128, 1])
        self.w_ukv = di("w_ukv", [128, 1024])
        self.dlam = di("dlam", [1, 256])
        self.subln = di("subln", [1, 128])
        self.fin_g = di("fin_g", [1, D])
        self.ropeC = di("ropeC", [128, SA])
        self.ropeS = di("ropeS", [128, SA])
        self.ropeCm = di("ropeCm", [96, SA])
        self.ropeSm = di("ropeSm", [96, SA])
        self.iota_in = di("iota_in", [128, 128])
        self.out = nc.dram_tensor("out", [NB, SL, D], F32, kind="ExternalOutput").ap()
        if stop_after is not None:
            self.dbg = nc.dram_tensor("dbg", [NB, SA, D], F32, kind="ExternalOutput").ap()
        ds = lambda n, sh, dt: nc.dram_tensor(n, list(sh), dt).ap()
        self.xs = ds("xs", [NB, SA, D], F32)
        self.mod = ds("mod", [2, 3, 6 * D], F32)
        self.QT = ds("QT", [NB, 12, 128, SA], BF16)
        self.KT = ds("KT", [NB, 12, 128, SA], BF16)
        self.V = ds("V", [NB, SA, 1040], BF16)
        self.Y = ds("Y", [NB, SA, D], BF16)
        self.UTs = ds("UTs", [128, 128, D], BF16)
        self.Vs = ds("Vs", [128, 128, D], BF16)
        self.WQs = ds("WQs", [4, 128, 8 * 512], BF16)
        self.PS = nc.alloc_psum_tensor("psall", [128, 8, 512], F32)

    def act(self, out, in_, func, r, w, **kw):
        self.S.op('act', lambda e: e.activation(out=out, in_=in_, func=func, **kw), r, w)

    def mm(self, out, lhsT, rhs, start, stop, r, w):
        self.S.op('pe', lambda e: e.matmul(out, lhsT=lhsT, rhs=rhs, start=start, stop=stop), r, w)

    def tr(self, out, in_, r, w, f32=False):
        idn = self.identf if f32 else self.ident
        self.S.op('pe', lambda e: e.transpose(out=out, in_=in_, identity=idn[:]), r, w)

    def tt(self, eng, out, in0, in1, op, r, w):
        self.S.op(eng, lambda e: e.tensor_tensor(out=out, in0=in0, in1=in1, op=op), r, w)

    def ts(self, eng, out, in0, s1, s2, op0, op1, r, w):
        if op1 is None:
            self.S.op(eng, lambda e: e.tensor_scalar(out=out, in0=in0, scalar1=s1, scalar2=None, op0=op0), r, w)
        else:
            self.S.op(eng, lambda e: e.tensor_scalar(out=out, in0=in0, scalar1=s1, scalar2=s2, op0=op0, op1=op1), r, w)

    def stt(self, eng, out, in0, scalar, in1, op0, op1, r, w):
        self.S.op(eng, lambda e: e.scalar_tensor_tensor(out=out, in0=in0, scalar=scalar, in1=in1, op0=op0, op1=op1), r, w)

    def cp(self, eng, out, in_, r, w):
        if eng == 'act':
            self.S.op('act', lambda e: e.copy(out=out, in_=in_), r, w)
        else:
            self.S.op(eng, lambda e: e.tensor_copy(out=out, in_=in_), r, w)

    def ps(self, bank, n=512):
        return self.PS[:, bank, 0:n]

    def psbf(self, bank):
        return self.PS[:, bank, :].bitcast(BF16)

    def src(self, layer, b, tg):
        if layer == 0:
            if tg < 32:
                return self.x[b, tg * 128:(tg + 1) * 128, :]
            return self.ctx[b, (tg - 32) * 128:(tg - 31) * 128, :]
        return self.xs[b, tg * 128:(tg + 1) * 128, :]

    def setup_consts(self):
        S, A = self.S, self.A
        self.ident = A.alloc([128, 128], BF16, 'ident')
        self.identf = A.alloc([128, 128], F32, 'identf')
        self.iota = A.alloc([128, 128], F32, 'iota')
        self.iotab = A.alloc([128, 128], BF16, 'iotab')
        self.epsT = A.alloc([128, 1], F32, 'eps')
        self.sinkexp = A.alloc([128, 8], F32, 'sinkexp')
        self.mL = A.alloc([128, 128], BF16, 'mL')
        self.mU = A.alloc([128, 128], BF16, 'mU')
        identf, ident = self.identf, self.ident
        S.op('pool', lambda e: e.memset(identf[:], 0.0), [], ['identf'])
        S.op('pool', lambda e: e.affine_select(out=identf[:], in_=identf[:], pattern=[[-1, 128]],
                                               compare_op=ALU.not_equal, fill=1.0, base=0, channel_multiplier=1),
             ['identf'], ['identf'])
        self.cp('dve', ident[:], identf[:], ['identf'], ['ident'])
        S.op('pool', lambda e: e.memset(self.epsT[:], EPS), [], ['eps'])
        S.dma('sp', self.iota[:], self.iota_in, [], ['iota'])
        self.cp('dve', self.iotab[:], self.iota[:], ['iota'], ['iotab'])
        S.dma('sp', self.sinkexp[:], self.sink[0:1, :].partition_broadcast(128)[:, 0, :], [], ['sinkexp'])
        self.act(self.sinkexp[:], self.sinkexp[:], AF.Exp, ['sinkexp'], ['sinkexp'])
        mk = A.mark()
        t0 = A.alloc([128, 128], F32, 'mstg')
        for (m_in, m_sb, key) in ((self.swaL, self.mL, 'mL'), (self.swaU, self.mU, 'mU')):
            S.dma('sp', t0[:, 0:128], m_in, ['t0'], ['t0'])
            self.cp('dve', m_sb[:], t0[:, 0:128], ['t0'], [key])
        S.barrier()
        A.release(mk)

    def phase_mod(self, l):
        S, A = self.S, self.A
        mk = A.mark()
        ccT = A.alloc([128, 8, 3], F32, 'ccT')
        for kc in range(8):
            S.dma('sp', ccT[:, kc, :], self.cc[:, kc * 128:(kc + 1) * 128].rearrange("r p -> p r"), [], ['ccT'],
                  allow_slow_non_contiguous=True)
        self.act(ccT[:], ccT[:], AF.Silu, ['ccT'], ['ccT'])
        modsb = A.alloc([3, 6 * D], F32, 'modsb')
        adab = A.alloc([3, 6 * D], F32, 'adab')
        S.dma('sp', adab[:], self.ada_b[l:l + 1, :].partition_broadcast(3)[:, 0, :], [], ['adab'])
        wb = [A.alloc([128, 8, 512], F32, 'modw') for _ in range(2)]
        for n in range(12):
            w = wb[n % 2]
            S.dma('sp', w[:], self.ada_w[l, :, n * 512:(n + 1) * 512].rearrange("(kc p) n -> p kc n", p=128),
                  [], [('modw', n % 2)])
            for kc in range(8):
                self.mm(self.PS[0:3, n % 2, :], ccT[:, kc, :], w[:, kc, :], kc == 0, kc == 7,
                        ['ccT', ('modw', n % 2)], [('ps', n % 2)])
            self.tt('dve', modsb[:, n * 512:(n + 1) * 512], self.PS[0:3, n % 2, :], adab[:, n * 512:(n + 1) * 512],
                    ALU.add, [('ps', n % 2), 'adab'], ['modsb'])
        S.dma('sp', self.mod[l], modsb[:], ['modsb'], ['mod'])
        S.barrier()
        A.release(mk)

    def load_row_bc(self, dst, src_row, key):
        self.S.dma('sp', dst, src_row.partition_broadcast(128)[:, 0, :], [], [key])

    def load_norm_mod(self, l, row, which, tag):
        A = self.A
        G = A.alloc([128, D], F32, 'G' + tag)
        SH = A.alloc([128, D], F32, 'SH' + tag)
        tmp = A.alloc([128, D], F32, 'ng' + tag)
        sh_off = 0 if which == 1 else 3
        ng = self.norm1_g if which == 1 else self.norm2_g
        self.load_row_bc(G[:], self.mod[l, row:row + 1, (sh_off + 1) * D:(sh_off + 2) * D], 'G' + tag)
        self.load_row_bc(SH[:], self.mod[l, row:row + 1, sh_off * D:(sh_off + 1) * D], 'SH' + tag)
        self.load_row_bc(tmp[:], ng[l:l + 1, :], 'ng' + tag)
        self.stt('dve', G[:], G[:], 1.0, tmp[:], ALU.add, ALU.mult, ['G' + tag, 'ng' + tag], ['G' + tag])
        return G, SH

    def load_gate(self, l, row, which, tag):
        A = self.A
        g = A.alloc([128, D], F32, 'gate' + tag)
        off = 2 if which == 1 else 5
        self.load_row_bc(g[:], self.mod[l, row:row + 1, off * D:(off + 1) * D], 'gate' + tag)
        return g

    def alloc_norm_bufs(self, nxt=2):
        A = self.A
        nb = {}
        nb['xt'] = [A.alloc([128, D], F32, 'xt') for _ in range(nxt)]
        nb['tmp'] = A.alloc([128, D], F32, 'ntmp')
        nb['junk'] = nb['tmp'][:].bitcast(BF16)[:, 0:D]
        nb['hb'] = [A.alloc([128, D], BF16, 'hb') for _ in range(2)]
        nb['ss'] = A.alloc([128, 4], F32, 'ss')
        nb['i'] = 0
        return nb

    def norm_tile(self, nb, src_ap, G, SH, gk, hT_dst, hT_key, psbank, xt_keep=False):
        S = self.S
        px = nb['i'] % len(nb['xt'])
        p = nb['i'] % 2
        nb['i'] += 1
        xt, hb, ss = nb['xt'][px], nb['hb'][p], nb['ss']
        S.dma('sp', xt[:], src_ap, [], [('xt', px)])
        self.act(nb['junk'], xt[:], AF.Square, [('xt', px)], ['ntmp', ('ss', p)], accum_out=ss[:, p:p + 1])
        self.act(ss[:, 2 + p:3 + p], ss[:, p:p + 1], AF.Sqrt, [('ss', p), 'eps'], [('rs', p)], scale=1.0 / D,
                 bias=self.epsT[:, 0:1])
        S.op('dve', lambda e: e.reciprocal(out=ss[:, 2 + p:3 + p], in_=ss[:, 2 + p:3 + p]), [('rs', p)], [('rs', p)])
        self.stt('dve', nb['tmp'][:], xt[:], ss[:, 2 + p:3 + p], G[:], ALU.mult, ALU.mult,
                 [('xt', px), ('rs', p)] + gk, ['ntmp'])
        self.tt('pool', hb[:], nb['tmp'][:], SH[:], ALU.add, ['ntmp'] + gk, [('hb', p)])
        pst = self.psbf(psbank)
        for kc in range(8):
            self.tr(pst[:, kc * 128:(kc + 1) * 128], hb[:, kc * 128:(kc + 1) * 128], [('hb', p), 'ident'],
                    [('ps', psbank)])
        self.cp('act', hT_dst, pst.rearrange("p (k t) -> p k t", k=8), [('ps', psbank)], [hT_key])
        return px

    def load_w_bf16(self, dst, src, ncols, key, piece=512):
        S, A = self.S, self.A
        mk = A.mark()
        stg = [A.alloc([128, 8, piece], F32, 'wstg') for _ in range(2)]
        i = 0
        for c0 in range(0, ncols, piece):
            n = min(piece, ncols - c0)
            s = stg[i % 2]
            S.dma('sp', s[:, :, 0:n], src[:, c0:c0 + n].rearrange("(kc p) n -> p kc n", p=128), [], [('wstg', i % 2)])
            self.cp('dve' if i % 2 == 0 else 'act', dst[:, :, c0:c0 + n], s[:, :, 0:n], [('wstg', i % 2)], [key])
            i += 1
        return mk

    def make_rot(self, W, src0, dst0, nblk, half, key):
        for kc in range(8):
            s = W[:, kc, src0:src0 + nblk * 2 * half].rearrange("p (b t h) -> p b t h", t=2, h=half)
            d = W[:, kc, dst0:dst0 + nblk * 2 * half].rearrange("p (b t h) -> p b t h", t=2, h=half)
            self.S.op('act', lambda e, s=s, d=d: e.mul(out=d[:, :, 0, :], in_=s[:, :, 1, :], mul=-1.0), [key], [key])
            self.cp('pool', d[:, :, 1, :], s[:, :, 0, :], [key], [key])

    def attn_setup(self):
        A = self.A
        self.E = [A.alloc([128, 512], BF16, 'E') for _ in range(4)]
        self.ei = 0
        self.sti = 0
        self.acci = 0
        self.den = A.alloc([128, 8], F32, 'den')

    def attn_unit(self, qT, nq, ktiles, dv, scale, qkeys, out_fn, sink_col=None):
        S = self.S
        nqs = nq // 128
        aset = self.acci % 2
        self.acci += 1
        accs = []
        for qs in range(nqs):
            bank = 4 + 2 * aset + qs // 2
            sub = qs % 2
            accs.append((self.PS[:, bank, sub * 256:sub * 256 + dv + 1], ('ps', bank, sub)))
        nk = len(ktiles)
        LA = 2
        live = {}

        def qk(ki):
            kT, v, bias, kkeys = ktiles[ki]
            sb = self.sti % 4
            self.sti += 1
            st = self.PS[:, sb, 0:nq]
            self.mm(st, kT, qT, True, bias is None, qkeys + kkeys, [('ps', sb)])
            if bias is not None:
                self.mm(st, self.ident[:], bias[0], False, True, ['ident'] + bias[1], [('ps', sb)])
            eb = self.ei % 4
            self.ei += 1
            E = self.E[eb]
            self.act(E[:, 0:nq], st, AF.Exp, [('ps', sb)], [('E', eb)], scale=scale)
            live[ki] = (E, eb)

        def pv(ki):
            kT, v, bias, kkeys = ktiles[ki]
            E, eb = live.pop(ki)
            for qs in range(nqs):
                self.mm(accs[qs][0], E[:, qs * 128:(qs + 1) * 128], v, ki == 0, ki == nk - 1,
                        [('E', eb)] + kkeys, [accs[qs][1]])
        for step in range(nk + LA):
            if step < nk:
                qk(step)
            if step >= LA:
                pv(step - LA)
        for qs in range(nqs):
            acc, akey = accs[qs]
            dcol = self.den[:, (aset * 4 + qs):(aset * 4 + qs) + 1]
            dkey = ('den', aset * 4 + qs)
            if sink_col is not None:
                self.tt('dve', dcol, acc[:, dv:dv + 1], self.sinkexp[:, sink_col:sink_col + 1], ALU.add,
                        [akey, 'sinkexp'], [dkey])
                S.op('dve', lambda e, dcol=dcol: e.reciprocal(out=dcol, in_=dcol), [dkey], [dkey])
            else:
                S.op('dve', lambda e, dcol=dcol, acc=acc: e.reciprocal(out=dcol, in_=acc[:, dv:dv + 1]), [akey], [dkey])
            out_fn(qs, acc, dcol, [akey, dkey])

    def phase_proj0(self, l=0):
        S, A = self.S, self.A
        mk = A.mark()
        wA = A.alloc([128, 8, 3328], BF16, 'wA')
        m2 = self.load_w_bf16(wA, self.ab_w, 2304, 'wA', piece=576)
        S.barrier()
        A.release(m2)
        self.make_rot(wA, 1536, 2304, 16, 16, 'wA')
        for kc in range(8):
            for r in range(4):
                self.cp('dve', wA[:, kc, 2816 + r * 64:2880 + r * 64], wA[:, kc, 2048 + (r // 2) * 64:2112 + (r // 2) * 64],
                        ['wA'], ['wA'])
        self.make_rot(wA, 2816, 3072, 8, 16, 'wA')
        rC = A.alloc([128, SA], F32, 'rC')
        rS = A.alloc([128, SA], F32, 'rS')
        S.dma('sp', rC[:], self.ropeC, [], ['rC'])
        S.dma('sp', rS[:], self.ropeS, [], ['rS'])
        fm = ([('Q', i, 128 * i, None) for i in range(4)] + [('K', i, 512 + 128 * i, None) for i in range(4)]
              + [('Q', 4 + i, 1536 + 128 * i, 2304 + 128 * i) for i in range(4)]
              + [('K', 4 + i, 2816 + 128 * i, 3072 + 128 * i) for i in range(2)])
        tmv = [(1024, 512, 0, 8, 64), (2176, 128, 8, 2, 64)]
        for b in range(NB):
            self.proj_batch(l, b, wA, fm, tmv, 10, rC, rS, None)
        S.barrier()
        A.release(mk)

    def proj_batch(self, l, b, wA, fm, tmv, nvh, rC, rS, extra):
        S, A = self.S, self.A
        mk = A.mark()
        Gb, SHb = self.load_norm_mod(l, b, 1, 'b')
        Gc, SHc = self.load_norm_mod(l, 2, 1, 'c')
        nb = self.alloc_norm_bufs()
        hTs = [A.alloc([128, 8, 512], BF16, 'hT') for _ in range(2)]
        stg = [A.alloc([128, 512], BF16, 'stg') for _ in range(3)]
        t1 = A.alloc([128, 512], F32, 't1')
        t2 = A.alloc([128, 512], F32, 't2')
        vdv = tmv[0][4]
        vw = sum(nh * (dvv + 1) for (_, _, _, nh, dvv) in tmv)
        vst = [A.alloc([128, vw], BF16, 'vst') for _ in range(2)]
        for v in vst:
            S.op('pool', lambda e, v=v: e.memset(v[:], 1.0), [], [('vst', 0), ('vst', 1)])
        si = 0
        pi = 0
        for ci in range(9):
            ntile = 4 if ci < 8 else 2
            n = ntile * 128
            t0 = ci * 512
            hT = hTs[ci % 2]
            hkey = ('hT', ci % 2)
            G, SH, gk = (Gb, SHb, ['Gb', 'SHb']) if ci < 8 else (Gc, SHc, ['Gc', 'SHc'])
            for ti in range(ntile):
                tg = ci * 4 + ti
                self.norm_tile(nb, self.src(l, b, tg), G, SH, gk, hT[:, :, ti * 128:(ti + 1) * 128], hkey, 6)
            for (dst, idx, col0, rot0) in fm:
                pa = pi % 2
                pi += 1
                for kc in range(8):
                    self.mm(self.PS[:, pa, 0:n], wA[:, kc, col0:col0 + 128], hT[:, kc, 0:n], kc == 0, kc == 7,
                            ['wA', hkey], [('ps', pa)])
                sg = stg[si % 3]
                skey = ('stg', si % 3)
                si += 1
                if rot0 is None:
                    self.cp('act', sg[:, 0:n], self.PS[:, pa, 0:n], [('ps', pa)], [skey])
                else:
                    for kc in range(8):
                        self.mm(self.PS[:, 2 + pa, 0:n], wA[:, kc, rot0:rot0 + 128], hT[:, kc, 0:n], kc == 0, kc == 7,
                                ['wA', hkey], [('ps', 2 + pa)])
                    self.tt('dve', t1[:, 0:n], self.PS[:, pa, 0:n], rC[:, t0:t0 + n], ALU.mult, [('ps', pa), 'rC'], ['t1'])
                    self.tt('dve', t2[:, 0:n], self.PS[:, 2 + pa, 0:n], rS[:, t0:t0 + n], ALU.mult, [('ps', 2 + pa), 'rS'], ['t2'])
                    self.tt('pool', sg[:, 0:n], t1[:, 0:n], t2[:, 0:n], ALU.add, ['t1', 't2'], [skey])
                dram = self.QT if dst == 'Q' else self.KT
                S.dma('pool', dram[b, idx, :, t0:t0 + n], sg[:, 0:n], [skey], [(dst, b, idx)])
            if extra is not None:
                extra(b, ci, t0, n, hT, hkey)
            for ti in range(ntile):
                tg = ci * 4 + ti
                vs = vst[tg % 2]
                vkey = ('vst', tg % 2)
                co = 0
                for gi, (col0, ncols, h0, nh, dvv) in enumerate(tmv):
                    bank = 4 + gi
                    for kc in range(8):
                        self.mm(self.PS[:, bank, 0:ncols], hT[:, kc, ti * 128:(ti + 1) * 128], wA[:, kc, col0:col0 + ncols],
                                kc == 0, kc == 7, ['wA', hkey], [('ps', bank)])
                    ov = vs[:, co:co + nh * (dvv + 1)].rearrange("p (h d) -> p h d", d=dvv + 1)[:, :, 0:dvv]
                    self.cp('act', ov, self.PS[:, bank, 0:ncols].rearrange("p (h d) -> p h d", d=dvv), [('ps', bank)], [vkey])
                    co += nh * (dvv + 1)
                S.dma('pool', self.V[b, tg * 128:(tg + 1) * 128, 0:vw], vs[:], [vkey], [('V', b)])
        S.barrier()
        A.release(mk)

    def phase_attn0(self):
        S, A = self.S, self.A
        mk = A.mark()
        self.TabI = A.alloc([128, 7680], BF16, 'TabI')
        self.TabE = A.alloc([128, 7680], BF16, 'TabE')
        mk2 = A.mark()
        t0 = A.alloc([128, 7680], F32, 'tabstg0')
        t1 = A.alloc([128, 7680], F32, 'tabstg1')
        S.dma('sp', t0[:], self.rpbT, [], ['t0'])
        for (msk, Tab, key) in ((self.maskI, self.TabI, 'TabI'), (self.maskE, self.TabE, 'TabE')):
            S.dma('sp', t1[:], msk, [], ['t1'])
            self.tt('dve', t1[:], t1[:], t0[:], ALU.add, ['t0', 't1'], ['t1'])
            self.ts('dve', Tab[:], t1[:], 8.0, None, ALU.mult, None, ['t1'], [key])
        S.barrier()
        A.release(mk2)
        self.attn_setup()
        Qs = [A.alloc([128, SA], BF16, 'Qs') for _ in range(2)]
        Ks = [A.alloc([128, SA], BF16, 'Ks') for _ in range(2)]
        Vsl = [A.alloc([128, NT, 130], BF16, 'Vsl') for _ in range(2)]
        ystg = [A.alloc([128, 128], BF16, 'ystg') for _ in range(2)]
        gi = 0
        yi = 0
        for b in range(NB):
            for grp in range(8):
                p = gi % 2
                gi += 1
                na = grp < 4
                c = grp if na else grp - 4
                qidx = grp
                kidx = c if na else 4 + c // 2
                nvc = 130 if na else 65
                vcol0 = (2 * c) * 65 if na else (8 + c // 2) * 65
                S.dma('sp', Qs[p][:], self.QT[b, qidx], [('Q', b, qidx)], [('Qs', p)])
                S.dma('sp', Ks[p][:], self.KT[b, kidx], [('K', b, kidx)], [('Ks', p)])
                S.dma('sp', Vsl[p][:, :, 0:nvc],
                      self.V[b, :, vcol0:vcol0 + nvc].rearrange("(t p) c -> p t c", p=128), [('V', b)], [('Vs', p)])
                for m in range(NT):
                    ys = ystg[yi % 2]
                    ykey = ('ystg', yi % 2)
                    yi += 1
                    for hh in range(2):
                        h = 2 * c + hh
                        pb = hh * 64
                        qT = Qs[p][pb:pb + 64, m * 128:(m + 1) * 128]
                        kts = []

                        def ktile(kt, bias):
                            vo = hh * 65 if na else 0
                            return (Ks[p][pb:pb + 64, kt * 128:(kt + 1) * 128], Vsl[p][:, kt, vo:vo + 65], bias,
                                    [('Ks', p), ('Vs', p)])
                        if m < 32:
                            if na:
                                if m < 2:
                                    lat, Tab, tk = range(0, 4), self.TabE, 'TabE'
                                elif m >= 30:
                                    lat, Tab, tk = range(28, 32), self.TabE, 'TabE'
                                else:
                                    lat, Tab, tk = range(m - 2, m + 3), self.TabI, 'TabI'
                                for kt in lat:
                                    j = kt - m
                                    p0 = 7 - 2 * j
                                    bias = (Tab[:, h * 960 + p0 * 64:h * 960 + p0 * 64 + 128], [tk])
                                    kts.append(ktile(kt, bias))
                            else:
                                for kt in range(max(0, m - 1), min(31, m + 1) + 1):
                                    j = kt - m
                                    bias = None if j == 0 else ((self.mL[:], ['mL']) if j < 0 else (self.mU[:], ['mU']))
                                    kts.append(ktile(kt, bias))
                        kts.append(ktile(32, None))
                        kts.append(ktile(33, None))

                        def out_fn(qs, acc, rden, keys, ys=ys, ykey=ykey, hh=hh):
                            self.ts('dve', ys[:, hh * 64:(hh + 1) * 64], acc[:, 0:64], rden, None, ALU.mult, None,
                                    keys, [ykey])
                        self.attn_unit(qT, 128, kts, 64, 0.125, [('Qs', p)], out_fn, sink_col=None if na else h)
                    S.dma('pool', self.Y[b, m * 128:(m + 1) * 128, grp * 128:(grp + 1) * 128], ys[:], [ykey], [('Y', b)])
        S.barrier()
        A.release(mk)

    def phase_out(self, l, ntiles):
        S, A = self.S, self.A
        mk = A.mark()
        wO = A.alloc([128, 8, D], BF16, 'wO')
        m2 = self.load_w_bf16(wO, self.w_out[l], D, 'wO')
        S.barrier()
        A.release(m2)
        gc = self.load_gate(l, 2, 1, 'c')
        ysb = [A.alloc([128, D], BF16, 'ysb') for _ in range(2)]
        yT = [A.alloc([128, 8, 128], BF16, 'yT') for _ in range(2)]
        xt = [A.alloc([128, D], F32, 'xo') for _ in range(2)]
        tmp = [A.alloc([128, D], F32, 'otmp') for _ in range(2)]
        i = 0
        for b in range(NB):
            m3 = A.mark()
            gb = self.load_gate(l, b, 1, 'b')
            for m in range(ntiles):
                p = i % 2
                i += 1
                g, gk = (gb, 'gateb') if m < 32 else (gc, 'gatec')
                S.dma('sp', ysb[p][:], self.Y[b, m * 128:(m + 1) * 128, :], [('Y', b)], [('ysb', p)])
                S.dma('sp', xt[p][:], self.src(l, b, m), [('xs', b, m)], [('xo', p)])
                pst = self.psbf(6 + p)
                for kc in range(8):
                    self.tr(pst[:, kc * 128:(kc + 1) * 128], ysb[p][:, kc * 128:(kc + 1) * 128], [('ysb', p), 'ident'],
                            [('ps', 6 + p)])
                self.cp('act', yT[p][:], pst.rearrange("p (k t) -> p k t", k=8), [('ps', 6 + p)], [('yT', p)])
                for half in range(2):
                    bank = 2 * p + half
                    for kc in range(8):
                        self.mm(self.PS[:, bank, :], yT[p][:, kc, :], wO[:, kc, half * 512:(half + 1) * 512], kc == 0, kc == 7,
                                [('yT', p), 'wO'], [('ps', bank)])
                    self.tt('dve', tmp[p][:, half * 512:(half + 1) * 512], self.PS[:, bank, :],
                            g[:, half * 512:(half + 1) * 512], ALU.mult, [('ps', bank), gk], [('otmp', p, half)])
                self.tt('pool', tmp[p][:], tmp[p][:], xt[p][:], ALU.add, [('otmp', p, 0), ('otmp', p, 1), ('xo', p)],
                        [('otmp', p, 0), ('otmp', p, 1)])
                S.dma('pool', self.xs[b, m * 128:(m + 1) * 128, :], tmp[p][:], [('otmp', p, 0), ('otmp', p, 1)],
                      [('xs', b, m)])
            A.release(m3)
        S.barrier()
        A.release(mk)

    def build(self):
        S = self.S
        self.setup_consts()
        if self.skip_l0:
            for b in range(NB):
                for r0 in range(0, SL, 512):
                    S.dma('sp', self.xs[b, r0:r0 + 512, :], self.x[b, r0:r0 + 512, :], [], [('xsi', b, r0)])
                S.dma('sp', self.xs[b, SL:SA, :], self.ctx[b], [], [('xsi', b, SL)])
            S.barrier()
            self.phase_mod(1)
            self.phase_proj1()
            return self.finish_dbg()
        self.phase_mod(0)
        self.phase_proj0()
        self.phase_attn0()
        self.phase_out(0, NT)
        if self.stop_after == 'attn0':
            return self.finish_dbg()
        self.phase_peer(0, NT)
        if self.stop_after == 'peer0':
            return self.finish_dbg()
        self.phase_mod(1)
        self.phase_proj1()
        if self.stop_after == 'proj1':
            return self.finish_dbg()
        self.phase_attn1()
        self.phase_out(1, 32)
        if self.stop_after == 'attn1':
            return self.finish_dbg()
        self.phase_peer(1, 32)
        S.barrier()
        S.emit()
        return self.nc

    def finish_dbg(self):
        S = self.S
        for b in range(NB):
            for r0 in range(0, SA, 544):
                S.dma('sp', self.dbg[b, r0:r0 + 544, :], self.xs[b, r0:r0 + 544, :], [], [('dbg', b, r0)])
        S.barrier()
        S.emit()
        return self.nc


def _consts():
    f = np.float32
    t = np.arange(SL)
    row, col = (t // 64).astype(f), (t % 64).astype(f)

    def table(dim_half, npart_rep):
        freq = (10000.0 ** (-np.arange(dim_half, dtype=f) / dim_half)).astype(f)
        ang_r = row[None, :] * freq[:, None]
        ang_c = col[None, :] * freq[:, None]
        C = np.concatenate([np.cos(ang_r), np.cos(ang_r), np.cos(ang_c), np.cos(ang_c)], 0)
        Sn = np.concatenate([np.sin(ang_r), np.sin(ang_r), np.sin(ang_c), np.sin(ang_c)], 0)
        C = np.concatenate([C, np.ones((C.shape[0], CT), f)], 1)
        Sn = np.concatenate([Sn, np.zeros((Sn.shape[0], CT), f)], 1)
        return C.astype(f), Sn.astype(f)
    C64, S64 = table(16, 2)
    ropeC = np.concatenate([C64, C64], 0)
    ropeS = np.concatenate([S64, S64], 0)
    C32, S32 = table(8, 1)
    ropeCm = np.concatenate([np.ones((64, SA), f), C32], 0)
    ropeSm = np.concatenate([np.zeros((64, SA), f), S32], 0)
    cq = np.arange(64)
    col_start = np.clip(cq - 8, 0, 48)
    ck = np.arange(64)
    valid = (ck[:, None] >= col_start[None, :]) & (ck[:, None] < col_start[None, :] + 16)
    maskI = np.full((2, 64, 8, 15, 64), NEG, f)
    maskE = np.full((2, 64, 8, 15, 64), NEG, f)
    for kr in range(2):
        for p in range(15):
            dr = (7 - p) if kr == 0 else (8 - p)
            if dr < -7 or dr > 7:
                continue
            mE = np.where(valid, 0.0, NEG).astype(f)
            maskE[kr, :, :, p, :] = mE[:, None, :]
            if -4 <= dr <= 3:
                maskI[kr, :, :, p, :] = mE[:, None, :]
    swaL = np.where(np.arange(128)[:, None] >= np.arange(128)[None, :], 0.0, NEG).astype(f)
    swaU = np.where(np.arange(128)[:, None] <= np.arange(128)[None, :], 0.0, NEG).astype(f)
    iota = np.tile(np.arange(128, dtype=f)[None, :], (128, 1))
    return dict(ropeC=ropeC, ropeS=ropeS, ropeCm=ropeCm, ropeSm=ropeSm, maskI=maskI.reshape(128, 7680),
                maskE=maskE.reshape(128, 7680), swaL=swaL, swaU=swaU, iota_in=iota)


def _rpb_layout(rpb):
    ck = np.arange(64)[:, None]
    cq = np.arange(64)[None, :]
    cidx = np.clip(ck - cq + 15, 0, 30)
    out = np.zeros((2, 64, 8, 15, 64), np.float32)
    for kr in range(2):
        for p in range(15):
            dr = (7 - p) if kr == 0 else (8 - p)
            if dr < -7 or dr > 7:
                continue
            out[kr, :, :, p, :] = np.transpose(rpb[:, dr + 7, :][:, cidx], (1, 0, 2))
    return np.ascontiguousarray(out.reshape(128, 7680))


_CONSTS = None


def make_in_maps(inputs, cores):
    global _CONSTS
    if _CONSTS is None:
        _CONSTS = _consts()
    f = np.float32
    g = {k: np.ascontiguousarray(np.asarray(v, dtype=f)) for k, v in inputs.items()}
    shared = dict(
        ada_w=g['ada_w'], ada_b=g['ada_b'], norm1_g=g['norm1_g'], norm2_g=g['norm2_g'], w_out=g['w_out'],
        peer_wq=g['peer_wq'], peer_keys=g['peer_keys'], peer_u=g['peer_u'], peer_v=g['peer_v'],
        ab_w=g['ab_w_in'][0], rpbT=_rpb_layout(g['na_rpb'][0]), sink=g['swa_sink'][0:1],
        cd_w=g['cd_w_in'][0], qn_g=g['mla_q_norm_g'][0].reshape(256, 1), w_uq=g['mla_w_uq'][0],
        kvn_g=g['mla_kv_norm_g'][0].reshape(128, 1), w_ukv=g['mla_w_ukv'][0], dlam=g['diff_lambda'][0].reshape(1, 256),
        subln=g['diff_subln_g'][0].reshape(1, 128), fin_g=g['final_norm_g'].reshape(1, D), **_CONSTS)
    maps = []
    for ci in cores:
        b0 = ci * NB
        m = dict(shared)
        m['x'] = g['x'][b0:b0 + NB]
        m['ctx'] = g['ctx'][b0:b0 + NB]
        m['cc'] = np.ascontiguousarray(np.concatenate([g['c'][b0:b0 + NB], g['c_ctx'][None, :]], 0))
        maps.append(m)
    return maps


def kernel(**inputs):
    nc = Builder().build()
    maps = make_in_maps(inputs, list(range(N_CORES)))
    res = run_bass_kernel_spmd(nc, maps, core_ids=list(range(N_CORES)))
    return np.concatenate([r["out"] for r in res.results], axis=0).astype(np.float32)


def _peer_methods():
    def topk16_multi(self, items):
        S = self.S
        for (src, vals, idx, wk, rk, wkey) in items:
            S.op('dve', lambda e, src=src, vals=vals: e.max(out=vals[:, 0:8], in_=src), rk, [wkey + ('v',)])
            yield
        for (src, vals, idx, wk, rk, wkey) in items:
            S.op('dve', lambda e, src=src, vals=vals, idx=idx: e.max_index(out=idx[:, 0:8], in_max=vals[:, 0:8], in_values=src),
                 rk + [wkey + ('v',)], [wkey + ('i',)])
            yield
        for (src, vals, idx, wk, rk, wkey) in items:
            S.op('dve', lambda e, src=src, vals=vals, wk=wk: e.match_replace(out=wk, in_to_replace=vals[:, 0:8], in_values=src,
                                                                             imm_value=-1e30),
                 rk + [wkey + ('v',)], [wkey + ('w',)])
            yield
        for (src, vals, idx, wk, rk, wkey) in items:
            S.op('dve', lambda e, vals=vals, wk=wk: e.max(out=vals[:, 8:16], in_=wk), [wkey + ('w',)], [wkey + ('v2',)])
            yield
        for (src, vals, idx, wk, rk, wkey) in items:
            S.op('dve', lambda e, vals=vals, idx=idx, wk=wk: e.max_index(out=idx[:, 8:16], in_max=vals[:, 8:16], in_values=wk),
                 [wkey + ('w',), wkey + ('v2',)], [wkey + ('i2',)])
            yield

    def phase_peer_prep(self, l):
        S, A = self.S, self.A
        mk = A.mark()
        ustg = [A.alloc([128, D], F32, 'ustg') for _ in range(2)]
        vstg = [A.alloc([128, D], F32, 'vstg') for _ in range(2)]
        ubf = [A.alloc([128, D], BF16, 'ubf') for _ in range(2)]
        vbf = [A.alloc([128, D], BF16, 'vbf') for _ in range(2)]
        utb = [A.alloc([128, 8, 128], BF16, 'utb') for _ in range(2)]
        U = self.peer_u[l].rearrange("(i j) d -> j i d", j=128)
        Vv = self.peer_v[l].rearrange("(i j) d -> j i d", j=128)
        for j in range(128):
            p = j % 2
            S.dma('sp', ustg[p][:], U[j], [], [('ustg', p)])
            S.dma('sp', vstg[p][:], Vv[j], [], [('vstg', p)])
            self.cp('dve', ubf[p][:], ustg[p][:], [('ustg', p)], [('ubf', p)])
            pst = self.psbf(6 + p)
            for kc in range(8):
                self.tr(pst[:, kc * 128:(kc + 1) * 128], ubf[p][:, kc * 128:(kc + 1) * 128], [('ubf', p), 'ident'], [('ps', 6 + p)])
            self.cp('act', utb[p][:], pst.rearrange("p (k t) -> p k t", k=8), [('ps', 6 + p)], [('utb', p)])
            S.dma('pool', self.UTs[j], utb[p][:].rearrange("p k t -> p (k t)"), [('utb', p)], [('UTs', j)])
            self.cp('pool', vbf[p][:], vstg[p][:], [('vstg', p)], [('vbf', p)])
            S.dma('pool', self.Vs[j], vbf[p][:], [('vbf', p)], [('Vs', j)])
        wst = [A.alloc([128, 8, 512], F32, 'wqst') for _ in range(2)]
        wbf = [A.alloc([128, 8, 512], BF16, 'wqbf') for _ in range(2)]
        for c in range(4):
            p = c % 2
            S.dma('sp', wst[p][:], self.peer_wq[l, :, c * 512:(c + 1) * 512].rearrange("(kc p) n -> p kc n", p=128), [], [('wqst', p)])
            self.cp('dve', wbf[p][:], wst[p][:], [('wqst', p)], [('wqbf', p)])
            S.dma('pool', self.WQs[c], wbf[p][:].rearrange("p k t -> p (k t)"), [('wqbf', p)], [('WQs', c)])
        S.barrier()
        A.release(mk)

    def phase_peer(self, l, ntiles):
        S, A = self.S, self.A
        self.phase_peer_prep(l)
        mk = A.mark()
        last = (l == 1)
        kT = A.alloc([128, 2, 128], BF16, 'kT')
        mk0 = A.mark()
        kst = A.alloc([128, 2, 128], F32, 'kst')
        kbf = A.alloc([128, 2, 128], BF16, 'kbf')
        for p in range(2):
            S.dma('sp', kst[:, p, :], self.peer_keys[l, p], [], ['kst'])
        self.cp('dve', kbf[:], kst[:], ['kst'], ['kbf'])
        pst = self.psbf(2)
        for p in range(2):
            self.tr(pst[:, p * 128:(p + 1) * 128], kbf[:, p, :], ['kbf', 'ident'], [('ps', 2)])
        self.cp('act', kT[:], pst[:, 0:256].rearrange("p (k t) -> p k t", k=2), [('ps', 2)], ['kT'])
        S.barrier()
        A.release(mk0)
        G2 = A.alloc([128, D], F32, 'G2')
        SH2 = A.alloc([128, D], F32, 'SH2')
        g2 = A.alloc([128, D], F32, 'g2')
        fss = A.alloc([128, 4], F32, 'fss')
        if last:
            fing = A.alloc([128, D], F32, 'fing')
            self.load_row_bc(fing[:], self.fin_g[0:1, :], 'fing')
        nb = self.alloc_norm_bufs(nxt=4)
        h2Ts = [A.alloc([128, 8, 256], BF16, 'h2T') for _ in range(2)]
        qT = A.alloc([128, 16, 256], BF16, 'qT')
        s_sb = A.alloc([128, 2048], F32, 's_sb')
        wqb = s_sb[:].bitcast(BF16).rearrange("p (k t) -> p k t", k=8)
        cand = A.alloc([128, 2048], F32, 'cand')
        ngt = cand[:, 0:D]
        sv = A.alloc([128, 256], F32, 'sv')
        si = A.alloc([128, 256], U32, 'si')
        sif = A.alloc([128, 256], F32, 'sif')
        wk = A.alloc([128, 2048], F32, 'wk')
        fv = A.alloc([128, 128], F32, 'fv')
        fp = A.alloc([128, 128], U32, 'fp')
        fpu = A.alloc([128, 128], U32, 'fpu')
        Aa = A.alloc([128, 128], F32, 'Aa')
        Bb = A.alloc([128, 128], F32, 'Bb')
        ijg = A.alloc([128, 3, 128], F32, 'ijg')
        ijgTs = [[A.alloc([128, 3, 128], BF16, 'ijgT') for _ in range(2)] for _ in range(2)]
        gz = A.alloc([128, 16], F32, 'gz')
        RT = 16
        R1 = A.alloc([128, RT, 128], BF16, 'R1')
        R2 = A.alloc([128, RT, 128], BF16, 'R2')
        Gs = A.alloc([128, 128, 256], BF16, 'Gs')
        utl = [A.alloc([128, 8, 128], BF16, 'utl') for _ in range(6)]
        vl = [A.alloc([128, D], BF16, 'vl') for _ in range(6)]
        ga = [A.alloc([128, 512], F32, 'ga') for _ in range(3)]
        wT = [A.alloc([128, 512], BF16, 'wT') for _ in range(3)]
        etmp = A.alloc([128, D], F32, 'etmp')
        iota16 = self.iota[:, 0:16]
        nchunks = ntiles // 2
        gskeys = [('Gs', a, c) for a in range(2) for c in range(128 // RT)]
        items = [(b, ch) for b in range(NB) for ch in range(nchunks)]
        state = {'row': None, 'grow': None, 'ji': 0}
        xpars = {}

        def partA(seq):
            b, ch = items[seq]
            par = seq % 2
            h2T = h2Ts[par]
            hkey = ('h2T', par)
            row = b if ch < 16 else 2
            if row != state['row']:
                state['row'] = row
                self.load_row_bc(G2[:], self.mod[l, row:row + 1, 4 * D:5 * D], 'G2')
                self.load_row_bc(SH2[:], self.mod[l, row:row + 1, 3 * D:4 * D], 'SH2')
                self.load_row_bc(ngt, self.norm2_g[l:l + 1, :], 'cand')
                self.stt('dve', G2[:], G2[:], 1.0, ngt, ALU.add, ALU.mult, ['G2', 'cand'], ['G2'])
                yield
            xp = []
            for ti in range(2):
                tg = ch * 2 + ti
                xp.append(self.norm_tile(nb, self.xs[b, tg * 128:(tg + 1) * 128, :], G2, SH2, ['G2', 'SH2'],
                                         h2T[:, :, ti * 128:(ti + 1) * 128], hkey, 3))
                yield
            xpars[seq] = xp
            for c in range(4):
                S.dma('pool', wqb.rearrange("p k t -> p (k t)"), self.WQs[c], [('WQs', c)], ['swq'])
                for q in range(4):
                    hp = c * 4 + q
                    bank = 3
                    for kc in range(8):
                        self.mm(self.PS[:, bank, 0:256], wqb[:, kc, q * 128:(q + 1) * 128], h2T[:, kc, :], kc == 0, kc == 7,
                                ['swq', hkey], [('ps', bank)])
                        if kc % 2 == 1:
                            yield
                    self.cp('act', qT[:, hp, :], self.PS[:, bank, 0:256], [('ps', bank)], ['qT'])
                    yield
            for ti in range(2):
                for g4 in range(4):
                    bank = 3
                    for q in range(4):
                        hp = g4 * 4 + q
                        self.mm(self.PS[:, bank, q * 128:(q + 1) * 128], qT[:, hp, ti * 128:(ti + 1) * 128], kT[:, hp % 2, :],
                                True, True, ['qT', 'kT'], [('ps', bank)])
                    self.cp('act', s_sb[:, g4 * 512:(g4 + 1) * 512], self.PS[:, bank, :], [('ps', bank)], ['swq'])
                    yield
                tkA = [(s_sb[:, hp * 128:(hp + 1) * 128], sv[:, hp * 16:(hp + 1) * 16], si[:, hp * 16:(hp + 1) * 16],
                        wk[:, hp * 128:(hp + 1) * 128], ['swq'], ('sv', hp)) for hp in range(16)]
                for _ in self.topk16_multi(tkA):
                    yield
                svk = [('sv', hp, x) for hp in range(16) for x in ('v', 'v2')]
                sik = [('sv', hp, x) for hp in range(16) for x in ('i', 'i2')]
                self.cp('dve', sif[:], si[:], sik, ['sif'])
                sv4 = sv[:].rearrange("p (h t k) -> p h t k", h=8, t=2)
                sif4 = sif[:].rearrange("p (h t k) -> p h t k", h=8, t=2)
                cand4 = cand[:].rearrange("p (h a b) -> p h a b", h=8, a=16)
                self.tt('dve', cand4, sv4[:, :, 0, :].unsqueeze(3).to_broadcast([128, 8, 16, 16]),
                        sv4[:, :, 1, :].unsqueeze(2).to_broadcast([128, 8, 16, 16]), ALU.add, svk, ['cand'])
                yield
                tkB = [(cand[:, h * 256:(h + 1) * 256], fv[:, h * 16:(h + 1) * 16], fp[:, h * 16:(h + 1) * 16],
                        wk[:, h * 256:(h + 1) * 256], ['cand'], ('fv', h)) for h in range(8)]
                for _ in self.topk16_multi(tkB):
                    yield
                fvk = [('fv', h, x) for h in range(8) for x in ('v', 'v2')]
                fpk = [('fv', h, x) for h in range(8) for x in ('i', 'i2')]
                S.op('dve', lambda e: e.tensor_single_scalar(out=fpu[:], in_=fp[:], scalar=4, op=ALU.logical_shift_right), fpk, ['fpu'])
                self.cp('dve', Aa[:], fpu[:], ['fpu'], ['Aa'])
                S.op('dve', lambda e: e.tensor_single_scalar(out=fpu[:], in_=fp[:], scalar=15, op=ALU.bitwise_and), fpk + ['fpu'], ['fpu'])
                self.cp('dve', Bb[:], fpu[:], ['fpu'], ['Bb'])
                yield
                fv3 = fv[:].rearrange("p (h k) -> p h k", h=8)
                g3 = ijg[:, 2, :].rearrange("p (h k) -> p h k", h=8)
                self.tt('dve', g3, fv3, fv3[:, :, 0:1].to_broadcast([128, 8, 16]), ALU.subtract, fvk, [('ijg', 2)])
                self.act(ijg[:, 2, :], ijg[:, 2, :], AF.Exp, [('ijg', 2)], [('ijg', 2)])
                io4 = iota16.unsqueeze(1).unsqueeze(1).to_broadcast([128, 8, 16, 16])
                for (sel, t_, slot, skey) in ((Aa, 0, 0, 'Aa'), (Bb, 1, 1, 'Bb')):
                    sel4 = sel[:].rearrange("p (h k) -> p h k", h=8).unsqueeze(3).to_broadcast([128, 8, 16, 16])
                    self.tt('dve', cand4, sel4, io4, ALU.is_equal, [skey, 'iota', 'cand'], ['cand'])
                    self.tt('dve', cand4, cand4, sif4[:, :, t_, :].unsqueeze(2).to_broadcast([128, 8, 16, 16]), ALU.mult,
                            ['cand', 'sif'], ['cand'])
                    S.op('dve', lambda e, slot=slot: e.tensor_reduce(
                        out=ijg[:, slot, :].rearrange("p (h k) -> p h k", h=8), in_=cand4, axis=AX.X, op=ALU.add),
                        ['cand'], [('ijg', slot)])
                    yield
                S.op('dve', lambda e: e.tensor_reduce(out=gz[:, 0:8], in_=g3, axis=AX.X, op=ALU.add), [('ijg', 2)], ['gz'])
                S.op('dve', lambda e: e.reciprocal(out=gz[:, 0:8], in_=gz[:, 0:8]), ['gz'], ['gz'])
                self.tt('dve', g3, g3, gz[:, 0:8].unsqueeze(2).to_broadcast([128, 8, 16]), ALU.mult, [('ijg', 2), 'gz'], [('ijg', 2)])
                for s3 in range(3):
                    self.tr(self.PS[:, 3, s3 * 128:(s3 + 1) * 128], ijg[:, s3, :], [('ijg', s3), 'identf'], [('ps', 3)], f32=True)
                self.cp('act', ijgTs[par][ti][:], self.PS[:, 3, 0:384].rearrange("p (s t) -> p s t", s=3), [('ps', 3)],
                        [('ijgT', par, ti)])
                yield

        def partB(seq):
            par = seq % 2
            bi = 0
            for ti in range(2):
                ijgT = ijgTs[par][ti]
                ik = ('ijgT', par, ti)
                for rq in range(128 // RT):
                    tq = rq * RT
                    iob = self.iotab[:].unsqueeze(1).to_broadcast([128, RT, 128])
                    self.tt('dve', R1[:], iob, ijgT[:, 0, tq:tq + RT].unsqueeze(2).to_broadcast([128, RT, 128]), ALU.is_equal,
                            ['iotab', ik], ['R1'])
                    self.tt('dve', R2[:], iob, ijgT[:, 1, tq:tq + RT].unsqueeze(2).to_broadcast([128, RT, 128]), ALU.is_equal,
                            ['iotab', ik], ['R2'])
                    self.tt('dve', R1[:], R1[:], ijgT[:, 2, tq:tq + RT].unsqueeze(2).to_broadcast([128, RT, 128]), ALU.mult,
                            ['R1', ik], ['R1'])
                    for t4 in range(RT // 4):
                        bank = bi % 4
                        bi += 1
                        for t_ in range(4):
                            t = t4 * 4 + t_
                            self.mm(self.PS[:, bank, t_ * 128:(t_ + 1) * 128], R1[:, t, :], R2[:, t, :], True, True, ['R1', 'R2'],
                                    [('ps', bank, 0), ('ps', bank, 1), ('ps', bank)])
                        tok0 = ti * 128 + tq + t4 * 4
                        self.cp('act', Gs[:, :, tok0:tok0 + 4],
                                self.PS[:, bank, :].rearrange("p (t j) -> p j t", t=4),
                                [('ps', bank, 0), ('ps', bank, 1), ('ps', bank)], [('Gs', ti, rq)])

        def jloop(seq, gen):
            par = seq % 2
            h2T = h2Ts[par]
            hkey = ('h2T', par)
            LA = 2
            base = state['ji']

            def a_pair(pp):
                pi_ = base + pp
                u = pi_ % 3
                bank = pi_ % 3
                for jj in (2 * pp, 2 * pp + 1):
                    b6 = (2 * pi_ + jj % 2) % 6
                    S.dma('sp', utl[b6][:].rearrange("p k t -> p (k t)"), self.UTs[jj], [('UTs', jj)], [('utl', b6)])
                    S.dma('pool', vl[b6][:], self.Vs[jj], [('Vs', jj)], [('vl', b6)])
                    pa = self.PS[:, bank, (jj % 2) * 256:(jj % 2 + 1) * 256]
                    for kc in range(8):
                        self.mm(pa, utl[b6][:, kc, :], h2T[:, kc, :], kc == 0, kc == 7, [('utl', b6), hkey], [('ps', bank, 0)])
                self.act(ga[u][:], self.PS[:, bank, :], AF.Gelu, [('ps', bank, 0)], [('ga', u)])
                self.tt('pool', wT[u][:], ga[u][:], Gs[:, 2 * pp:2 * pp + 2, :].rearrange("p j t -> p (j t)"), ALU.mult,
                        [('ga', u)] + gskeys, [('wT', u)])

            def f_pair(pp):
                pi_ = base + pp
                u = pi_ % 3
                for jj in (2 * pp, 2 * pp + 1):
                    b6 = (2 * pi_ + jj % 2) % 6
                    for ti in range(2):
                        for hf in range(2):
                            self.mm(self.PS[:, 4 + 2 * ti + hf, :], wT[u][:, (jj % 2) * 256 + ti * 128:(jj % 2) * 256 + (ti + 1) * 128],
                                    vl[b6][:, hf * 512:(hf + 1) * 512], jj == 0, jj == 127, [('wT', u), ('vl', b6)],
                                    [('ps', 4 + 2 * ti + hf)])
            for step in range(64 + LA):
                if step < 64:
                    a_pair(step)
                if step >= LA:
                    f_pair(step - LA)
                if gen is not None:
                    for _ in range(6):
                        if next(gen, 'done') == 'done':
                            gen = None
                            break
            state['ji'] += 64
            if gen is not None:
                for _ in gen:
                    pass

        def epilogue(seq):
            b, ch = items[seq]
            row = b if ch < 16 else 2
            if row != state['grow']:
                state['grow'] = row
                self.load_row_bc(g2[:], self.mod[l, row:row + 1, 5 * D:6 * D], 'g2')
            for ti in range(2):
                tg = ch * 2 + ti
                px = xpars[seq][ti]
                xt = nb['xt'][px]
                xk = ('xt', px)
                for hf in range(2):
                    self.tt('dve', etmp[:, hf * 512:(hf + 1) * 512], self.PS[:, 4 + 2 * ti + hf, :], g2[:, hf * 512:(hf + 1) * 512],
                            ALU.mult, [('ps', 4 + 2 * ti + hf), 'g2'], [('etmp', hf)])
                self.tt('pool', etmp[:], etmp[:], xt[:], ALU.add, [('etmp', 0), ('etmp', 1), xk], [('etmp', 0), ('etmp', 1)])
                ek = [('etmp', 0), ('etmp', 1)]
                if not last:
                    S.dma('pool', self.xs[b, tg * 128:(tg + 1) * 128, :], etmp[:], ek, [('xs', b, tg)])
                else:
                    self.act(nb['junk'], etmp[:], AF.Square, ek, ['ntmp', 'fss'], accum_out=fss[:, 0:1])
                    self.act(fss[:, 2:3], fss[:, 0:1], AF.Sqrt, ['fss', 'eps'], ['frs'], scale=1.0 / D, bias=self.epsT[:, 0:1])
                    S.op('dve', lambda e: e.reciprocal(out=fss[:, 2:3], in_=fss[:, 2:3]), ['frs'], ['frs'])
                    self.stt('dve', etmp[:], etmp[:], fss[:, 2:3], fing[:], ALU.mult, ALU.mult, ek + ['frs', 'fing'], ek)
                    S.dma('pool', self.out[b, tg * 128:(tg + 1) * 128, :], etmp[:], ek, [('out', b, tg)])

        for _ in partA(0):
            pass
        partB(0)
        for seq in range(len(items)):
            gen = partA(seq + 1) if seq + 1 < len(items) else None
            jloop(seq, gen)
            epilogue(seq)
            if seq + 1 < len(items):
                partB(seq + 1)
        S.barrier()
        A.release(mk)

    Builder.topk16_multi = topk16_multi
    Builder.phase_peer_prep = phase_peer_prep
    Builder.phase_peer = phase_peer


_peer_methods()


LAM_INIT1 = 0.8 - 0.6 * math.exp(-0.3 * 1)


def _layer1_methods():
    def phase_proj1(self, l=1):
        S, A = self.S, self.A
        mk = A.mark()
        wA = A.alloc([128, 8, 3072], BF16, 'wA')
        m2 = self.load_w_bf16(wA, self.cd_w, 1952, 'wA', piece=488)
        S.barrier()
        A.release(m2)
        self.make_rot(wA, 416, 1952, 16, 16, 'wA')
        self.make_rot(wA, 928, 2464, 16, 16, 'wA')
        self.make_rot(wA, 384, 2976, 2, 8, 'wA')
        Wg = A.alloc([128, 2, 768], BF16, 'Wg')
        Wgr = A.alloc([128, 2, 768], BF16, 'Wgr')
        Wkv = A.alloc([128, 1024], BF16, 'Wkv')
        onesf = A.alloc([128, 128], BF16, 'onesf')
        S.op('pool', lambda e: e.memset(onesf[:], 1.0), [], ['onesf'])
        m3 = A.mark()
        wst = A.alloc([128, 1024], F32, 'w1st')
        gq = A.alloc([128, 2], F32, 'gq')
        gkv = A.alloc([128, 1], F32, 'gkv')
        for c in range(2):
            S.dma('sp', gq[:, c:c + 1], self.qn_g[c * 128:(c + 1) * 128, :], [], ['gq'])
        S.dma('sp', gkv[:], self.kvn_g, [], ['gkv'])
        for c in range(2):
            S.dma('sp', wst[:, 0:768], self.w_uq[c * 128:(c + 1) * 128, :], ['w1st'], ['w1st'])
            self.ts('dve', Wg[:, c, :], wst[:, 0:768], gq[:, c:c + 1], None, ALU.mult, None, ['w1st', 'gq'], ['Wg'])
        S.op('pool', lambda e: e.memset(Wgr[:], 0.0), [], ['Wgr'])
        for c in range(2):
            s = Wg[:, c, :].rearrange("p (h f) -> p h f", f=96)[:, :, 64:96].rearrange("p h (b t e) -> p h b t e", b=2, t=2)
            d = Wgr[:, c, :].rearrange("p (h f) -> p h f", f=96)[:, :, 64:96].rearrange("p h (b t e) -> p h b t e", b=2, t=2)
            for bb in range(2):
                S.op('act', lambda e, s=s, d=d, bb=bb: e.mul(out=d[:, :, bb, 0, :], in_=s[:, :, bb, 1, :], mul=-1.0), ['Wg', 'Wgr'], ['Wgr'])
                self.cp('pool', d[:, :, bb, 1, :], s[:, :, bb, 0, :], ['Wg', 'Wgr'], ['Wgr'])
        S.dma('sp', wst[:], self.w_ukv, ['w1st'], ['w1st'])
        self.ts('dve', Wkv[:], wst[:], gkv[:, 0:1], None, ALU.mult, None, ['w1st', 'gkv'], ['Wkv'])
        S.barrier()
        A.release(m3)
        if self.sub == 'w':
            return
        for b in range(NB):
            self.proj_batch1(l, b, wA, Wg, Wgr, Wkv, onesf)
            if self.sub is not None:
                break
        S.barrier()
        A.release(mk)

    def proj_batch1(self, l, b, wA, Wg, Wgr, Wkv, onesf):
        S, A = self.S, self.A
        mk = A.mark()
        Gb, SHb = self.load_norm_mod(l, b, 1, 'b')
        Gc, SHc = self.load_norm_mod(l, 2, 1, 'c')
        nb = self.alloc_norm_bufs()
        hTs = [A.alloc([128, 8, 512], BF16, 'hT') for _ in range(2)]
        stg = [A.alloc([128, 512], BF16, 'stg') for _ in range(3)]
        t1 = A.alloc([128, 512], F32, 't1')
        t2 = A.alloc([128, 512], F32, 't2')
        rC = A.alloc([128, 512], F32, 'rC')
        rS = A.alloc([128, 512], F32, 'rS')
        rCm = A.alloc([128, 512], F32, 'rCm')
        rSm = A.alloc([128, 512], F32, 'rSm')
        rCk = A.alloc([128, 512], F32, 'rCk')
        rSk = A.alloc([128, 512], F32, 'rSk')
        cqb = A.alloc([128, 2, 512], BF16, 'cqb')
        sq = A.alloc([128, 512], BF16, 'sq')
        rq = A.alloc([128, 512], F32, 'rq')
        rkv = A.alloc([128, 512], F32, 'rkv')
        ckvf = A.alloc([128, 512], F32, 'ckvf')
        ckvn = A.alloc([128, 512], BF16, 'ckvn')
        krs = A.alloc([128, 512], BF16, 'krs')
        VW = 1036
        vst = [A.alloc([128, VW], BF16, 'vst') for _ in range(2)]
        for v in vst:
            S.op('pool', lambda e, v=v: e.memset(v[:], 1.0), [], [('vst', 0), ('vst', 1)])
        si = 0
        pi = 0
        for ci in range(9):
            ntile = 4 if ci < 8 else 2
            n = ntile * 128
            t0 = ci * 512
            hT = hTs[ci % 2]
            hkey = ('hT', ci % 2)
            G, SH, gk = (Gb, SHb, ['Gb', 'SHb']) if ci < 8 else (Gc, SHc, ['Gc', 'SHc'])
            for ti in range(ntile):
                tg = ci * 4 + ti
                self.norm_tile(nb, self.src(l, b, tg), G, SH, gk, hT[:, :, ti * 128:(ti + 1) * 128], hkey, 6)
            S.dma('sp', rC[:, 0:n], self.ropeC[:, t0:t0 + n], [], ['rC'])
            S.dma('sp', rS[:, 0:n], self.ropeS[:, t0:t0 + n], [], ['rS'])
            S.dma('sp', rCm[0:96, 0:n], self.ropeCm[:, t0:t0 + n], [], ['rCm'])
            S.dma('sp', rSm[0:96, 0:n], self.ropeSm[:, t0:t0 + n], [], ['rSm'])
            S.dma('sp', rCk[0:32, 0:n], self.ropeCm[64:96, t0:t0 + n], [], ['rCk'])
            S.dma('sp', rSk[0:32, 0:n], self.ropeSm[64:96, t0:t0 + n], [], ['rSk'])

            def proj(bank, col0, m, nn=n, hT=hT, hkey=hkey):
                for kc in range(8):
                    self.mm(self.PS[0:m, bank, 0:nn], wA[:, kc, col0:col0 + m], hT[:, kc, 0:nn], kc == 0, kc == 7,
                            ['wA', hkey], [('ps', bank)])
            if self.sub == 'c0':
                break
            for c in range(2):
                proj(c, 128 * c, 128)
                self.cp('dve', cqb[:, c, 0:n], self.PS[:, c, 0:n], [('ps', c)], [('cqb', c)])
                if self.sub == 'c0a':
                    continue
                self.cp('dve', ckvf[:, 0:n], self.PS[:, c, 0:n], [('ps', c)], ['ckvf'])
                self.act(sq[:, 0:n], ckvf[:, 0:n], AF.Square, ['ckvf'], ['sq'])
                self.mm(self.PS[:, 7, 0:n], onesf[:], sq[:, 0:n], c == 0, c == 1, ['onesf', 'sq'], [('ps', 7)])
            if self.sub != 'c0a':
                self.cp('dve', rq[:, 0:n], self.PS[:, 7, 0:n], [('ps', 7)], ['rq'])
                self.act(rq[:, 0:n], rq[:, 0:n], AF.Sqrt, ['rq', 'eps'], ['rq'], scale=1.0 / 256, bias=self.epsT[:, 0:1])
                S.op('dve', lambda e, n=n: e.reciprocal(out=rq[:, 0:n], in_=rq[:, 0:n]), ['rq'], ['rq'])
            if self.sub == 'c0a':
                break
            if self.sub == 'c0b':
                break
            proj(2, 256, 128)
            self.cp('dve', ckvf[:, 0:n], self.PS[:, 2, 0:n], [('ps', 2)], ['ckvf'])
            self.act(sq[:, 0:n], ckvf[:, 0:n], AF.Square, ['ckvf'], ['sq'])
            self.mm(self.PS[:, 7, 0:n], onesf[:], sq[:, 0:n], True, True, ['onesf', 'sq'], [('ps', 7)])
            self.cp('dve', rkv[:, 0:n], self.PS[:, 7, 0:n], [('ps', 7)], ['rkv'])
            self.act(rkv[:, 0:n], rkv[:, 0:n], AF.Sqrt, ['rkv', 'eps'], ['rkv'], scale=1.0 / 128, bias=self.epsT[:, 0:1])
            S.op('dve', lambda e, n=n: e.reciprocal(out=rkv[:, 0:n], in_=rkv[:, 0:n]), ['rkv'], ['rkv'])
            self.tt('dve', ckvn[:, 0:n], ckvf[:, 0:n], rkv[:, 0:n], ALU.mult, ['ckvf', 'rkv'], ['ckvn'])
            if self.sub == 'c1':
                break
            proj(0, 384, 32)
            proj(2, 2976, 32)
            self.tt('dve', t1[0:32, 0:n], self.PS[0:32, 0, 0:n], rCk[0:32, 0:n], ALU.mult, [('ps', 0), 'rCk'], ['t1'])
            self.tt('dve', t2[0:32, 0:n], self.PS[0:32, 2, 0:n], rSk[0:32, 0:n], ALU.mult, [('ps', 2), 'rSk'], ['t2'])
            self.tt('pool', krs[0:32, 0:n], t1[0:32, 0:n], t2[0:32, 0:n], ALU.add, ['t1', 't2'], ['krs'])
            for h in range(8):
                S.dma('pool', self.KT[b, h, 64:96, t0:t0 + n], krs[0:32, 0:n], ['krs'], [('K', b, h, 1)])
            if self.sub == 'c2':
                break
            for h in range(8):
                pa = pi % 2
                pi += 1
                for c in range(2):
                    self.mm(self.PS[0:96, pa, 0:n], Wg[:, c, h * 96:(h + 1) * 96], cqb[:, c, 0:n], c == 0, c == 1,
                            ['Wg', ('cqb', 0), ('cqb', 1)], [('ps', pa)])
                for c in range(2):
                    self.mm(self.PS[0:96, 2 + pa, 0:n], Wgr[:, c, h * 96:(h + 1) * 96], cqb[:, c, 0:n], c == 0, c == 1,
                            ['Wgr', ('cqb', 0), ('cqb', 1)], [('ps', 2 + pa)])
                sg = stg[si % 3]
                skey = ('stg', si % 3)
                si += 1
                self.tt('dve', t1[0:96, 0:n], self.PS[0:96, pa, 0:n], rCm[0:96, 0:n], ALU.mult, [('ps', pa), 'rCm'], ['t1'])
                self.tt('dve', t2[0:96, 0:n], self.PS[0:96, 2 + pa, 0:n], rSm[0:96, 0:n], ALU.mult, [('ps', 2 + pa), 'rSm'], ['t2'])
                self.tt('pool', t1[0:96, 0:n], t1[0:96, 0:n], t2[0:96, 0:n], ALU.add, ['t1', 't2'], ['t1'])
                self.tt('pool', sg[0:96, 0:n], t1[0:96, 0:n], rq[0:96, 0:n], ALU.mult, ['t1', 'rq'], [skey])
                S.dma('pool', self.QT[b, h, 0:96, t0:t0 + n], sg[0:96, 0:n], [skey], [('Q', b, h)])
                pa = pi % 2
                pi += 1
                self.mm(self.PS[0:64, pa, 0:n], Wkv[:, h * 128:h * 128 + 64], ckvn[:, 0:n], True, True, ['Wkv', 'ckvn'], [('ps', pa)])
                sg = stg[si % 3]
                skey = ('stg', si % 3)
                si += 1
                self.cp('act', sg[0:64, 0:n], self.PS[0:64, pa, 0:n], [('ps', pa)], [skey])
                S.dma('pool', self.KT[b, h, 0:64, t0:t0 + n], sg[0:64, 0:n], [skey], [('K', b, h, 0)])
            if self.sub == 'c3':
                break
            for (dst, idx, col0, rot0) in ([('Q', 8 + d, 416 + 128 * d, 1952 + 128 * d) for d in range(4)]
                                           + [('K', 8 + d, 928 + 128 * d, 2464 + 128 * d) for d in range(4)]):
                pa = pi % 2
                pi += 1
                proj(pa, col0, 128)
                proj(2 + pa, rot0, 128)
                sg = stg[si % 3]
                skey = ('stg', si % 3)
                si += 1
                self.tt('dve', t1[:, 0:n], self.PS[:, pa, 0:n], rC[:, 0:n], ALU.mult, [('ps', pa), 'rC'], ['t1'])
                self.tt('dve', t2[:, 0:n], self.PS[:, 2 + pa, 0:n], rS[:, 0:n], ALU.mult, [('ps', 2 + pa), 'rS'], ['t2'])
                self.tt('pool', sg[:, 0:n], t1[:, 0:n], t2[:, 0:n], ALU.add, ['t1', 't2'], [skey])
                dram = self.QT if dst == 'Q' else self.KT
                S.dma('pool', dram[b, idx, :, t0:t0 + n], sg[:, 0:n], [skey], [(dst, b, idx)])
            if self.sub == 'c4':
                break
            for ti in range(ntile):
                tg = ci * 4 + ti
                vs = vst[tg % 2]
                vkey = ('vst', tg % 2)
                self.mm(self.PS[:, 4, :], ckvn[:, ti * 128:(ti + 1) * 128],
                        Wkv[:].rearrange("p (h f) -> p h f", f=128)[:, :, 64:128], True, True, ['ckvn', 'Wkv'], [('ps', 4)])
                self.cp('act', vs[:, 0:520].rearrange("p (h d) -> p h d", d=65)[:, :, 0:64],
                        self.PS[:, 4, :].rearrange("p (h d) -> p h d", d=64), [('ps', 4)], [vkey])
                for kc in range(8):
                    self.mm(self.PS[:, 5, :], hT[:, kc, ti * 128:(ti + 1) * 128], wA[:, kc, 1440:1952], kc == 0, kc == 7,
                            ['wA', hkey], [('ps', 5)])
                self.cp('act', vs[:, 520:1036].rearrange("p (h d) -> p h d", d=129)[:, :, 0:128],
                        self.PS[:, 5, :].rearrange("p (h d) -> p h d", d=128), [('ps', 5)], [vkey])
                S.dma('pool', self.V[b, tg * 128:(tg + 1) * 128, 0:VW], vs[:], [vkey], [('V', b)])
            if self.sub == 'c5':
                break
        S.barrier()
        A.release(mk)

    def phase_attn1(self):
        S, A = self.S, self.A
        mk = A.mark()
        self.attn_setup()
        lam = A.alloc([128, 8], F32, 'lam')
        dl = A.alloc([128, 256], F32, 'dl')
        self.load_row_bc(dl[:], self.dlam[0:1, :], 'dl')
        dl4 = dl[:].rearrange("p (a d) -> p a d", a=4)
        self.tt('dve', dl4[:, 0, :], dl4[:, 0, :], dl4[:, 1, :], ALU.mult, ['dl'], ['dl'])
        self.tt('dve', dl4[:, 2, :], dl4[:, 2, :], dl4[:, 3, :], ALU.mult, ['dl'], ['dl'])
        S.op('dve', lambda e: e.tensor_reduce(out=lam[:, 0:1], in_=dl4[:, 0, :], axis=AX.X, op=ALU.add), ['dl'], ['lam'])
        S.op('dve', lambda e: e.tensor_reduce(out=lam[:, 1:2], in_=dl4[:, 2, :], axis=AX.X, op=ALU.add), ['dl', 'lam'], ['lam'])
        self.act(lam[:, 0:2], lam[:, 0:2], AF.Exp, ['lam'], ['lam'])
        self.tt('dve', lam[:, 2:3], lam[:, 1:2], lam[:, 0:1], ALU.subtract, ['lam'], ['lam'])
        self.ts('dve', lam[:, 3:4], lam[:, 2:3], -LAM_INIT1, None, ALU.add, None, ['lam'], ['lam'])
        subg = A.alloc([128, 128], F32, 'subg')
        self.load_row_bc(subg[:], self.subln[0:1, :], 'subg')
        self.ts('dve', subg[:], subg[:], 1.0 - LAM_INIT1, None, ALU.mult, None, ['subg'], ['subg'])
        Qs = [A.alloc([128, SL], BF16, 'Qs') for _ in range(2)]
        Ks = [A.alloc([128, SA], BF16, 'Ks') for _ in range(2)]
        Vsl = [A.alloc([128, NT, 129], BF16, 'Vsl') for _ in range(2)]
        ystg = [A.alloc([128, 128], BF16, 'ystg') for _ in range(4)]
        o1 = [A.alloc([128, 128], F32, 'o1') for _ in range(4)]
        o2 = [A.alloc([128, 128], F32, 'o2') for _ in range(2)]
        oj = A.alloc([128, 128], BF16, 'oj')
        oss = A.alloc([128, 8], F32, 'oss')
        gi = 0
        yi = 0
        for b in range(NB):
            for grp in range(12):
                p = gi % 2
                gi += 1
                mla = grp < 8
                rows = 96 if mla else 128
                dv = 64 if mla else 128
                vcol0 = grp * 65 if mla else 520 + (grp - 8) * 129
                S.dma('sp', Qs[p][0:rows, :], self.QT[b, grp, 0:rows, 0:SL], [('Q', b, grp)], [('Qs', p)])
                S.dma('sp', Ks[p][0:rows, :], self.KT[b, grp, 0:rows, :],
                      [('K', b, grp), ('K', b, grp, 0), ('K', b, grp, 1)], [('Ks', p)])
                S.dma('sp', Vsl[p][:, :, 0:dv + 1],
                      self.V[b, :, vcol0:vcol0 + dv + 1].rearrange("(t p) c -> p t c", p=128), [('V', b)], [('Vs', p)])
                for qc in range(8):
                    if mla:
                        kts = [(Ks[p][0:96, kt * 128:(kt + 1) * 128], Vsl[p][:, kt, 0:65], None, [('Ks', p), ('Vs', p)])
                               for kt in range(NT)]

                        def out_fn(qs, acc, rden, keys, qc=qc, grp=grp, b=b):
                            nonlocal yi
                            ys = ystg[yi % 4]
                            ykey = ('ystg', yi % 4)
                            yi += 1
                            self.ts('dve', ys[:, 0:64], acc[:, 0:64], rden, None, ALU.mult, None, keys, [ykey])
                            tg = qc * 4 + qs
                            S.dma('pool', self.Y[b, tg * 128:(tg + 1) * 128, grp * 64:(grp + 1) * 64], ys[:, 0:64], [ykey], [('Y', b)])
                        self.attn_unit(Qs[p][0:96, qc * 512:(qc + 1) * 512], 512, kts, 64, 96 ** -0.5, [('Qs', p)], out_fn)
                    else:
                        d = grp - 8
                        for w in range(2):
                            kts = [(Ks[p][w * 64:(w + 1) * 64, kt * 128:(kt + 1) * 128], Vsl[p][:, kt, 0:129], None,
                                    [('Ks', p), ('Vs', p)]) for kt in range(NT)]

                            def out_fn(qs, acc, rden, keys, qc=qc, d=d, b=b, w=w):
                                nonlocal yi
                                if w == 0:
                                    self.ts('dve', o1[qs][:], acc[:, 0:128], rden, None, ALU.mult, None, keys, [('o1', qs)])
                                    return
                                oo = o2[qs % 2]
                                ok = ('o2', qs % 2)
                                self.ts('dve', oo[:], acc[:, 0:128], rden, None, ALU.mult, None, keys, [ok])
                                self.stt('dve', oo[:], oo[:], lam[:, 3:4], o1[qs][:], ALU.mult, ALU.add, [ok, ('o1', qs), 'lam'], [ok])
                                sk = ('oss', qs % 2)
                                c0 = qs % 2
                                self.act(oj[:], oo[:], AF.Square, [ok], ['oj', sk], accum_out=oss[:, c0:c0 + 1])
                                self.act(oss[:, 2 + c0:3 + c0], oss[:, c0:c0 + 1], AF.Sqrt, [sk, 'eps'], [sk], scale=1.0 / 128,
                                         bias=self.epsT[:, 0:1])
                                S.op('dve', lambda e, c0=c0: e.reciprocal(out=oss[:, 2 + c0:3 + c0], in_=oss[:, 2 + c0:3 + c0]), [sk], [sk])
                                ys = ystg[yi % 4]
                                ykey = ('ystg', yi % 4)
                                yi += 1
                                self.stt('dve', ys[:], oo[:], oss[:, 2 + c0:3 + c0], subg[:], ALU.mult, ALU.mult, [ok, sk, 'subg'], [ykey])
                                tg = qc * 4 + qs
                                S.dma('pool', self.Y[b, tg * 128:(tg + 1) * 128, 512 + d * 128:512 + (d + 1) * 128], ys[:], [ykey],
                                      [('Y', b)])
                            self.attn_unit(Qs[p][w * 64:(w + 1) * 64, qc * 512:(qc + 1) * 512], 512, kts, 128, 0.125, [('Qs', p)], out_fn)
        S.barrier()
        A.release(mk)

    Builder.phase_proj1 = phase_proj1
    Builder.proj_batch1 = proj_batch1
    Builder.phase_attn1 = phase_attn1


_layer1_methods()
```
